# Optimizing a Trainium2 kernel written in Bass

```python
import math, functools
import jax, jax.numpy as jnp
from jax import lax
import numpy as np

D_MODEL = 1024
BATCH = 32
SEQ = 2048
DEPTH = 4

GRID_W = 64
CTX_LEN = 256
N_MIXERS = 3
CONF_KERNEL = 31
CONF_INNER = D_MODEL
SC_KERNEL = 3
S5_GROUP = 16
S5_GROUPS = D_MODEL // S5_GROUP
S5_STATE = 64
S5_DT_MIN = 1e-3
S5_DT_MAX = 1e-1
N_GROUPS = 4
EXPERTS_PER_GROUP = 8
N_EXPERTS = N_GROUPS * EXPERTS_PER_GROUP
TOP_K_IN_GROUP = 2
D_EXPERT = D_MODEL // 2
RMS_EPS = 1e-6
LN_EPS = 1e-5

kernel_name = 'hybrid_conv_s5_hmoe_diffusion_trunk'

F32 = jnp.float32


def _rmsnorm(x, g):
    xf = x.astype(F32)
    y = xf * lax.rsqrt(jnp.mean(xf * xf, axis=-1, keepdims=True) + RMS_EPS)
    return (y * g.astype(F32)).astype(x.dtype)


def _layernorm(x, g, b):
    xf = x.astype(F32)
    mu = jnp.mean(xf, axis=-1, keepdims=True)
    var = jnp.mean(jnp.square(xf - mu), axis=-1, keepdims=True)
    y = (xf - mu) * lax.rsqrt(var + LN_EPS)
    return (y * g.astype(F32) + b.astype(F32)).astype(x.dtype)


def _dwconv_grid(x, w):
    kh, kw, ch = w.shape
    return lax.conv_general_dilated(
        x, w.astype(x.dtype)[:, :, None, :], (1, 1),
        ((kh // 2, kh // 2), (kw // 2, kw // 2)),
        dimension_numbers=('NHWC', 'HWIO', 'NHWC'), feature_group_count=ch)


def _conv_latent(z, w):
    b, s, ch = z.shape
    rows = s // GRID_W
    y = _dwconv_grid(z.reshape(b, rows, GRID_W, ch), w)
    return y.reshape(b, s, ch)


def _conv_context(z, w1d):
    b, l, ch = z.shape
    y = _dwconv_grid(z.reshape(b, l, 1, ch), w1d[:, None, :])
    return y.reshape(b, l, ch)


def _conformer_module(h, conv_fn, w_in, dw_b, ln_g, ln_b, w_out):
    val, gate = jnp.split(h @ w_in, 2, axis=-1)
    z = val * jax.nn.sigmoid(gate)
    z = conv_fn(z) + dw_b
    z = _layernorm(z, ln_g, ln_b)
    return jax.nn.silu(z) @ w_out


def _short_conv_mixer(h, conv_fn, w_in, w_out):
    gb, gc, v = jnp.split(h @ w_in, 3, axis=-1)
    return (gb * conv_fn(gc * v)) @ w_out


def _s5_discretize(a_re, a_im, log_dt, b_re, b_im):
    a_re = jnp.minimum(a_re.astype(F32), -1e-4)
    a_im = a_im.astype(F32)
    dt = jnp.exp(log_dt.astype(F32))[:, None]
    mag = jnp.exp(dt * a_re)
    ang = dt * a_im
    abar_re = mag * jnp.cos(ang)
    abar_im = mag * jnp.sin(ang)
    den = a_re * a_re + a_im * a_im
    n_re = abar_re - 1.0
    n_im = abar_im
    k_re = (n_re * a_re + n_im * a_im) / den
    k_im = (n_im * a_re - n_re * a_im) / den
    b_re = b_re.astype(F32)
    b_im = b_im.astype(F32)
    bb_re = k_re[..., None] * b_re - k_im[..., None] * b_im
    bb_im = k_re[..., None] * b_im + k_im[..., None] * b_re
    return abar_re, abar_im, bb_re, bb_im


def _complex_linear_scan(abar_re, abar_im, bu_re, bu_im, reverse):
    length = bu_re.shape[1]
    a_re = jnp.broadcast_to(abar_re, (1, length) + abar_re.shape)
    a_im = jnp.broadcast_to(abar_im, (1, length) + abar_im.shape)

    def combine(e1, e2):
        a1r, a1i, b1r, b1i = e1
        a2r, a2i, b2r, b2i = e2
        return (a2r * a1r - a2i * a1i,
                a2r * a1i + a2i * a1r,
                a2r * b1r - a2i * b1i + b2r,
                a2r * b1i + a2i * b1r + b2i)

    _, _, hr, hi = lax.associative_scan(combine, (a_re, a_im, bu_re, bu_im),
                                        reverse=reverse, axis=1)
    return hr, hi


def _s5_states(u_g, disc, h0, reverse):
    abar_re, abar_im, bb_re, bb_im = disc
    bu_re = jnp.einsum('blgc,gpc->blgp', u_g, bb_re)
    bu_im = jnp.einsum('blgc,gpc->blgp', u_g, bb_im)
    if h0 is not None:
        h0r, h0i = h0
        pos = -1 if reverse else 0
        bu_re = bu_re.at[:, pos].add(abar_re * h0r - abar_im * h0i)
        bu_im = bu_im.at[:, pos].add(abar_re * h0i + abar_im * h0r)
    return _complex_linear_scan(abar_re, abar_im, bu_re, bu_im, reverse)


def _s5_readout(hr, hi, c_re, c_im):
    return (jnp.einsum('blgp,gcp->blgc', hr, c_re.astype(F32))
            - jnp.einsum('blgp,gcp->blgc', hi, c_im.astype(F32)))


def _s5_head(y, h, d, w_glu):
    b, l, dm = h.shape
    y = y.reshape(b, l, dm) + d.astype(F32) * h.astype(F32)
    y = jax.nn.gelu(y).astype(h.dtype)
    val, gate = jnp.split(y @ w_glu, 2, axis=-1)
    return val * jax.nn.sigmoid(gate)


def _s5_mixer(h_lat, h_ctx, a_re, a_im, log_dt, b_re, b_im, c_re, c_im, d, w_glu, need_ctx_out):
    def groups(h):
        b, l, _ = h.shape
        return h.astype(F32).reshape(b, l, S5_GROUPS, S5_GROUP)
    u_lat = groups(h_lat)
    u_ctx = groups(h_ctx)
    y_lat = 0.0
    y_ctx = 0.0
    for direction, reverse in enumerate((False, True)):
        disc = _s5_discretize(a_re[direction], a_im[direction], log_dt[direction],
                              b_re[direction], b_im[direction])
        cr, ci = _s5_states(u_ctx, disc, None, reverse)
        fin = (cr[:, 0], ci[:, 0]) if reverse else (cr[:, -1], ci[:, -1])
        if need_ctx_out:
            y_ctx = y_ctx + _s5_readout(cr, ci, c_re[direction], c_im[direction])
        lr, li = _s5_states(u_lat, disc, fin, reverse)
        y_lat = y_lat + _s5_readout(lr, li, c_re[direction], c_im[direction])
    out_lat = _s5_head(y_lat, h_lat, d, w_glu)
    out_ctx = _s5_head(y_ctx, h_ctx, d, w_glu) if need_ctx_out else None
    return out_lat, out_ctx


def _hier_moe(h, wg, bg, we, be, w13, w2):
    pg = jax.nn.softmax((h @ wg + bg).astype(F32), axis=-1)
    g_w, g_idx = lax.top_k(pg, 1)
    le = (h @ we + be).astype(F32).reshape(-1, N_GROUPS, EXPERTS_PER_GROUP)
    le = jnp.take_along_axis(le, g_idx[:, :, None], axis=1)[:, 0]
    pe = jax.nn.softmax(le, axis=-1)
    top_p, top_i = lax.top_k(pe, TOP_K_IN_GROUP)
    w = g_w * top_p / jnp.sum(top_p, axis=-1, keepdims=True)
    ids = g_idx * EXPERTS_PER_GROUP + top_i
    combine = jnp.sum(jax.nn.one_hot(ids, N_EXPERTS, dtype=F32) * w[..., None], axis=1)
    combine = combine.astype(h.dtype)
    y = jnp.zeros_like(h)
    for e in range(N_EXPERTS):
        a, g = jnp.split(h @ w13[e], 2, axis=-1)
        y = y + combine[:, e:e + 1] * ((jax.nn.silu(a) * g) @ w2[e])
    return y


def setup_inputs(seed: int = 0) -> dict:
    key = jax.random.key(seed)
    it = iter(list(jax.random.split(key, 40)))
    def nrm(shape, scale):
        return scale * jax.random.normal(next(it), shape, F32)
    n_conf = len([i for i in range(DEPTH) if i % N_MIXERS == 0])
    n_sc = len([i for i in range(DEPTH) if i % N_MIXERS == 1])
    n_s5 = len([i for i in range(DEPTH) if i % N_MIXERS == 2])
    d = D_MODEL
    G, P, CG = S5_GROUPS, S5_STATE, S5_GROUP
    n_idx = jnp.arange(P, dtype=F32)
    return {
        'x': nrm((BATCH, SEQ, d), 1.0),
        'c': nrm((BATCH, d), 1.0),
        'ctx': nrm((BATCH, CTX_LEN, d), 1.0),
        'c_ctx': nrm((d,), 1.0),
        'ada_w': nrm((DEPTH, d, 6 * d), 0.5 * d ** -0.5),
        'ada_b': nrm((DEPTH, 6 * d), 0.02),
        'norm1_g': 1.0 + nrm((DEPTH, d), 0.02),
        'norm2_g': 1.0 + nrm((DEPTH, d), 0.02),
        'conf_w_in': nrm((n_conf, d, 2 * CONF_INNER), d ** -0.5),
        'conf_dw': nrm((n_conf, CONF_KERNEL, CONF_INNER), CONF_KERNEL ** -0.5),
        'conf_dw_b': nrm((n_conf, CONF_INNER), 0.02),
        'conf_ln_g': 1.0 + nrm((n_conf, CONF_INNER), 0.02),
        'conf_ln_b': nrm((n_conf, CONF_INNER), 0.02),
        'conf_w_out': nrm((n_conf, CONF_INNER, d), CONF_INNER ** -0.5),
        'sc_w_in': nrm((n_sc, d, 3 * d), d ** -0.5),
        'sc_conv': nrm((n_sc, SC_KERNEL, d), SC_KERNEL ** -0.5),
        'sc_w_out': nrm((n_sc, d, d), d ** -0.5),
        's5_a_re': -0.5 + nrm((n_s5, 2, G, P), 0.01),
        's5_a_im': math.pi * n_idx + nrm((n_s5, 2, G, P), 0.01),
        's5_log_dt': jax.random.uniform(next(it), (n_s5, 2, G), F32,
                                        math.log(S5_DT_MIN), math.log(S5_DT_MAX)),
        's5_b_re': nrm((n_s5, 2, G, P, CG), (2 * CG) ** -0.5),
        's5_b_im': nrm((n_s5, 2, G, P, CG), (2 * CG) ** -0.5),
        's5_c_re': nrm((n_s5, 2, G, CG, P), P ** -0.5),
        's5_c_im': nrm((n_s5, 2, G, CG, P), P ** -0.5),
        's5_d': nrm((n_s5, d), 1.0),
        's5_w_glu': nrm((n_s5, d, 2 * d), d ** -0.5),
        'moe_wg': nrm((DEPTH, d, N_GROUPS), d ** -0.5),
        'moe_bg': nrm((DEPTH, N_GROUPS), 0.01),
        'moe_we': nrm((DEPTH, d, N_EXPERTS), d ** -0.5),
        'moe_be': nrm((DEPTH, N_EXPERTS), 0.01),
        'moe_w13': nrm((DEPTH, N_EXPERTS, d, 2 * D_EXPERT), d ** -0.5),
        'moe_w2': nrm((DEPTH, N_EXPERTS, D_EXPERT, d), D_EXPERT ** -0.5),
        'final_g': 1.0 + nrm((d,), 0.02),
    }


def reference(x, c, ctx, c_ctx, ada_w, ada_b, norm1_g, norm2_g,
              conf_w_in, conf_dw, conf_dw_b, conf_ln_g, conf_ln_b, conf_w_out,
              sc_w_in, sc_conv, sc_w_out,
              s5_a_re, s5_a_im, s5_log_dt, s5_b_re, s5_b_im, s5_c_re, s5_c_im, s5_d, s5_w_glu,
              moe_wg, moe_bg, moe_we, moe_be, moe_w13, moe_w2, final_g):
    b, s, dm = x.shape
    lc = ctx.shape[1]
    silu_c = jax.nn.silu(c)
    silu_cc = jax.nn.silu(c_ctx)
    for i in range(DEPTH):
        kind = i % N_MIXERS
        j = i // N_MIXERS
        update_ctx = i < DEPTH - 1
        need_ctx_in = update_ctx or kind == 2
        sh1, sc1, g1, sh2, sc2, g2 = jnp.split((silu_c @ ada_w[i] + ada_b[i])[:, None, :], 6, axis=-1)
        csh1, csc1, cg1, csh2, csc2, cg2 = jnp.split(silu_cc @ ada_w[i] + ada_b[i], 6, axis=-1)

        hx = _rmsnorm(x, norm1_g[i]) * (1.0 + sc1) + sh1
        hc = _rmsnorm(ctx, norm1_g[i]) * (1.0 + csc1) + csh1 if need_ctx_in else None
        if kind == 0:
            args = (conf_w_in[j], conf_dw_b[j], conf_ln_g[j], conf_ln_b[j], conf_w_out[j])
            mx = _conformer_module(hx, functools.partial(_conv_latent, w=conf_dw[j][:, None, :]), *args)
            mc = (_conformer_module(hc, functools.partial(_conv_context, w1d=conf_dw[j]), *args)
                  if update_ctx else None)
        elif kind == 1:
            mx = _short_conv_mixer(hx, functools.partial(_conv_latent, w=sc_conv[j][None, :, :]),
                                   sc_w_in[j], sc_w_out[j])
            mc = (_short_conv_mixer(hc, functools.partial(_conv_context, w1d=sc_conv[j]),
                                    sc_w_in[j], sc_w_out[j]) if update_ctx else None)
        else:
            mx, mc = _s5_mixer(hx, hc, s5_a_re[j], s5_a_im[j], s5_log_dt[j], s5_b_re[j], s5_b_im[j],
                               s5_c_re[j], s5_c_im[j], s5_d[j], s5_w_glu[j], update_ctx)
        x = x + g1 * mx
        if update_ctx:
            ctx = ctx + cg1 * mc

        hx2 = _rmsnorm(x, norm2_g[i]) * (1.0 + sc2) + sh2
        moe_args = (moe_wg[i], moe_bg[i], moe_we[i], moe_be[i], moe_w13[i], moe_w2[i])
        if update_ctx:
            hc2 = _rmsnorm(ctx, norm2_g[i]) * (1.0 + csc2) + csh2
            tokens = jnp.concatenate([hx2.reshape(-1, dm), hc2.reshape(-1, dm)], axis=0)
            y = _hier_moe(tokens, *moe_args)
            n_lat = b * s
            x = x + g2 * y[:n_lat].reshape(b, s, dm)
            ctx = ctx + cg2 * y[n_lat:].reshape(b, lc, dm)
        else:
            x = x + g2 * _hier_moe(hx2.reshape(-1, dm), *moe_args).reshape(b, s, dm)
    return _rmsnorm(x, final_g)
```

```python
import contextlib
import numpy as np
import concourse.bass as bass
import concourse.mybir as mybir
from concourse.bass_utils import run_bass_kernel_spmd

F32 = mybir.dt.float32
BF16 = mybir.dt.bfloat16
I32 = mybir.dt.int32
AF = mybir.ActivationFunctionType
ALU = mybir.AluOpType
AX = mybir.AxisListType

D = 1024
KC = 8
SEQ = 2048
CTX = 256
NE = 32
DE = 512
TS = 256
RMS_EPS = 1e-6
LN_EPS = 1e-5
DEPTH = 4
NCORES = 8
SKIP = set()
DEBUG = False


class Buf:
    __slots__ = ("w", "wx", "r")

    def __init__(self):
        self.w = {}
        self.wx = {}
        self.r = {}


class V:
    __slots__ = ("ap", "buf")

    def __init__(self, ap, buf=None):
        self.ap = ap
        self.buf = buf if buf is not None else Buf()

    def __getitem__(self, k):
        return V(self.ap[k], self.buf)

    def re(self, pat, **kw):
        return V(self.ap.rearrange(pat, **kw), self.buf)

    def bc(self, shape):
        return V(self.ap.to_broadcast(list(shape)), self.buf)

    def un(self, axis):
        return V(self.ap.unsqueeze(axis), self.buf)

    def bitcast(self, dt):
        return V(self.ap.bitcast(dt), self.buf)

    def rev(self):
        a = list(self.ap.ap)
        s, c = a[-1]
        a[-1] = [-s, c]
        return V(bass.AP(self.ap.tensor, self.ap.offset + s * (c - 1), [list(x) for x in a]), self.buf)


def _merge(dst, src):
    for k, v in src.items():
        if dst.get(k, 0) < v:
            dst[k] = v


class Sched:
    ENG = ("pe", "act", "dve", "pool", "sp")
    NDS = {"sp": 16, "act": 6, "pool": 16}

    def __init__(self):
        self.ops = {e: [] for e in self.ENG}
        self.cnt = {e: 0 for e in self.ENG}
        self.waited = {e: {} for e in self.ENG}
        self.dnext = {e: 0 for e in self.NDS}
        self.dval = {e: [0] * n for e, n in self.NDS.items()}
        self.n_ops = 0
        self.pool_consts = set()
        self.regvals = {}

    def _emit(self, eng, fn, reads, writes, dma=False, disjoint=False, sreads=()):
        need = {}
        own = 0
        for b in reads:
            _merge(need, b.w)
        if eng != "pe":
            own = need.get(("c", eng), 0)
        for b in writes:
            _merge(need, b.r)
            _merge(need, b.wx if disjoint else b.w)
        if dma:
            j = self.dnext[eng]
            self.dnext[eng] = (j + 1) % self.NDS[eng]
            key = ("d", eng, j)
            if self.dval[eng][j] > 0:
                need[key] = max(need.get(key, 0), self.dval[eng][j])
            self.dval[eng][j] += 16
            ev = (key, self.dval[eng][j])
            inc = 16
        else:
            need.pop(("c", eng), None)
            if own > 0:
                need[("c", eng)] = own
            self.cnt[eng] += 1
            key = ("c", eng)
            ev = (key, self.cnt[eng])
            inc = 1
        wl = []
        wd = self.waited[eng]
        for k, v in need.items():
            if wd.get(k, 0) < v:
                wd[k] = v
                wl.append((k, v))
        self.ops[eng].append((wl, fn, key, inc))
        self.n_ops += 1
        for b in reads:
            if b.r.get(ev[0], 0) < ev[1]:
                b.r[ev[0]] = ev[1]
        for b in writes:
            if disjoint:
                b.w[ev[0]] = ev[1]
            else:
                b.w = {ev[0]: ev[1]}
                b.wx = {ev[0]: ev[1]}
                b.r = {}

    def I(self, eng, meth, disjoint=False, **kw):
        reads, writes, sreads, res = [], [], [], {}
        for k, v in kw.items():
            if isinstance(v, V):
                (writes if k in ("out", "accum_out") else reads).append(v.buf)
                if k in ("scalar", "scalar1", "scalar2", "scale", "bias", "initial"):
                    sreads.append(v.buf)
                res[k] = v.ap
            else:
                res[k] = v
        self._emit(eng, lambda e: getattr(e, meth)(**res), reads, writes, disjoint=disjoint, sreads=sreads)

    def dma(self, eng, out, in_, disjoint=False, **kw):
        o, i = out.ap, in_.ap
        self._emit(eng, lambda e: e.dma_start(out=o, in_=i, **kw), [in_.buf], [out.buf], dma=True, disjoint=disjoint)

    def gather(self, out, src, idx, bound=None):
        o, i, x = out.ap, src.ap, idx.ap
        if bound is None:
            fn = lambda e: e.indirect_dma_start(out=o, out_offset=None, in_=i, in_offset=bass.IndirectOffsetOnAxis(ap=x, axis=0))
        else:
            self.pool_consts.add(bound)
            fn = lambda e: e.indirect_dma_start(out=o, out_offset=None, in_=i, in_offset=bass.IndirectOffsetOnAxis(ap=x, axis=0),
                                                bounds_check=self.regvals[bound], oob_is_err=False)
        self._emit("pool", fn, [src.buf, idx.buf], [out.buf], dma=True)

    def scatter(self, out, src, idx, **kw):
        o, i, x = out.ap, src.ap, idx.ap
        self._emit("pool", lambda e: e.indirect_dma_start(out=o, out_offset=bass.IndirectOffsetOnAxis(ap=x, axis=0),
                                                          in_=i, in_offset=None, **kw),
                   [src.buf, idx.buf], [out.buf], dma=True, disjoint=True)

    def barrier(self):
        allv = {("c", e): self.cnt[e] for e in self.ENG if self.cnt[e] > 0}
        for e, vals in self.dval.items():
            for j, v in enumerate(vals):
                if v > 0:
                    allv[("d", e, j)] = v
        for eng in self.ENG:
            wl = []
            wd = self.waited[eng]
            for k, v in allv.items():
                if k == ("c", eng):
                    continue
                if wd.get(k, 0) < v:
                    wd[k] = v
                    wl.append((k, v))
            if wl:
                self.ops[eng].append((wl, None, None, 0))

    def replay(self, nc, stack):
        sems = {}
        for e in self.ENG:
            sems[("c", e)] = stack.enter_context(nc.semaphore("c_" + e))
        for e, n in self.NDS.items():
            for j in range(n):
                sems[("d", e, j)] = stack.enter_context(nc.semaphore("d_%s_%d" % (e, j)))
        block = stack.enter_context(nc.Block())
        ops = self.ops

        def run(name, e):
            for wl, fn, key, inc in ops[name]:
                for k, v in wl:
                    e.wait_ge(sems[k], v)
                if fn is not None:
                    fn(e).then_inc(sems[key], inc)

        @block.tensor
        def _(e):
            run("pe", e)

        @block.scalar
        def _(e):
            run("act", e)

        @block.vector
        def _(e):
            run("dve", e)

        @block.gpsimd
        def _(e):
            for val in sorted(self.pool_consts):
                r = e.alloc_register("bc%d" % val)
                e.reg_mov(r, val)
                self.regvals[val] = e.snap(r)
            run("pool", e)

        @block.sync
        def _(e):
            run("sp", e)


class Prog:
    def __init__(self, nb, layers, final=True):
        self.nb = nb
        self.layers = list(layers)
        self.final = final
        self.S = Sched()
        self.nc = bass.Bass("TRN2", target_bir_lowering=False)
        self.stack = contextlib.ExitStack()
        self.dbufs = {}
        self.in_names = []

    def dram_in(self, name, shape, dt=F32):
        self.in_names.append(name)
        return self.nc.dram_tensor(name, list(shape), dt, kind="ExternalInput").ap()

    def dram_tmp(self, name, shape, dt=F32):
        return self.nc.dram_tensor(name, list(shape), dt, kind="Internal").ap()

    def dv(self, ap, key):
        b = self.dbufs.get(key)
        if b is None:
            b = self.dbufs[key] = Buf()
        return V(ap, b)

    def arena_reset(self):
        self.S.barrier()
        self.aoff = self.abase

    def alloc(self, shape, dt=F32):
        n = 1
        for s in shape[1:]:
            n *= s
        words = n if dt in (F32, I32) else (n + 1) // 2
        words = (words + 7) // 8 * 8
        assert self.aoff + words <= self.awords, ("SBUF arena overflow", self.aoff, words, self.awords)
        ap = self.big[:, self.aoff:self.aoff + words]
        self.aoff += words
        if dt != F32:
            ap = ap.bitcast(dt)
        ap = ap[0:shape[0], 0:n]
        if len(shape) > 2:
            names = " ".join("d%d" % i for i in range(len(shape) - 1))
            kw = {"d%d" % i: shape[i + 1] for i in range(len(shape) - 1)}
            ap = ap.rearrange("p (%s) -> p %s" % (names, names), **kw)
        return V(ap)

    def perm(self, shape, dt=F32):
        v = self.alloc(shape, dt)
        self.abase = self.aoff
        return v

    def build(self):
        nc, S, nb = self.nc, self.S, self.nb
        st = self.stack
        self.awords = 53000
        self.big = st.enter_context(nc.sbuf_tensor("big", [128, self.awords], F32))
        self.aoff = 0
        self.abase = 0
        self.psum = [V(st.enter_context(nc.psum_tensor("ps%d" % i, [128, 512], F32))[:, :]) for i in range(8)]
        nl = len(self.layers)
        nlat, nctx = nb * SEQ, nb * CTX

        self.x_in = self.dram_in("x", [nlat, D])
        self.c_in = self.dram_in("c", [nb, D])
        self.ctx_in = self.dram_in("ctx", [nctx, D])
        self.cctx_in = self.dram_in("c_ctx", [1, D])
        self.final_g = self.dram_in("final_g", [1, D])
        self.W = {}
        for li in self.layers:
            kind = li % 3
            w = {}
            w["ada_w"] = self.dram_in("ada_w_%d" % li, [D, 6 * D])
            w["ada_b"] = self.dram_in("ada_b_%d" % li, [1, 6 * D])
            w["n1"] = self.dram_in("norm1_g_%d" % li, [1, D])
            w["n2"] = self.dram_in("norm2_g_%d" % li, [1, D])
            w["wr"] = self.dram_in("moe_wr_%d" % li, [D, 36])
            w["br"] = self.dram_in("moe_br_%d" % li, [1, 36])
            w["w13"] = self.dram_in("moe_w13_%d" % li, [NE * 128 * 4, 2048])
            w["w2"] = self.dram_in("moe_w2_%d" % li, [NE * 128 * 2, 2048])
            if kind == 0:
                w["w_in"] = self.dram_in("conf_w_in_%d" % li, [D, 2 * D])
                w["dw"] = self.dram_in("conf_dw_%d" % li, [31, D])
                w["dw_b"] = self.dram_in("conf_dw_b_%d" % li, [1, D])
                w["ln_g"] = self.dram_in("conf_ln_g_%d" % li, [1, D])
                w["ln_b"] = self.dram_in("conf_ln_b_%d" % li, [1, D])
                w["w_out"] = self.dram_in("conf_w_out_%d" % li, [D, D])
            elif kind == 1:
                w["w_in"] = self.dram_in("sc_w_in_%d" % li, [D, 3 * D])
                w["cv"] = self.dram_in("sc_conv_%d" % li, [3, D])
                w["w_out"] = self.dram_in("sc_w_out_%d" % li, [D, D])
            else:
                w["a_re"] = self.dram_in("s5_a_re_%d" % li, [2, 64, 64])
                w["a_im"] = self.dram_in("s5_a_im_%d" % li, [2, 64, 64])
                w["ldt"] = self.dram_in("s5_log_dt_%d" % li, [2, 64])
                w["b_re"] = self.dram_in("s5_b_re_%d" % li, [2, 64, 64, 16])
                w["b_im"] = self.dram_in("s5_b_im_%d" % li, [2, 64, 64, 16])
                w["c_re"] = self.dram_in("s5_c_re_%d" % li, [2, 64, 16, 64])
                w["c_im"] = self.dram_in("s5_c_im_%d" % li, [2, 64, 16, 64])
                w["d"] = self.dram_in("s5_d_%d" % li, [1, D])
                w["w_glu"] = self.dram_in("s5_w_glu_%d" % li, [D, 2 * D])
            self.W[li] = w
        self.y_out = nc.dram_tensor("y", [nlat, D], F32, kind="ExternalOutput").ap()

        self.xs = self.dram_tmp("xs", [nlat, D])
        self.cs = self.dram_tmp("cs", [nctx, D])
        self.MOD = self.dram_tmp("modv", [nl, 6, nb + 1, D])
        ntok_max = nlat + nctx
        self.nslot_max = ((2 * ntok_max + NE * (TS - 1)) + TS - 1) // TS * TS
        self.H2 = self.dram_tmp("h2", [ntok_max, D], BF16)
        self.H2S = self.dram_tmp("h2s", [self.nslot_max, D], BF16)
        self.YS = self.dram_tmp("ys", [self.nslot_max, D])
        self.HT = self.dram_tmp("ht", [D, ntok_max], BF16)
        self.YT = self.dram_tmp("yt", [D, ntok_max], BF16)

        self.identf = self.perm([128, 128])
        self.ident = self.perm([128, 128], BF16)
        self.ltri = self.perm([128, 128], BF16)
        self.ones = self.perm([128, 128], BF16)
        self.onesm = self.perm([128, 128], BF16)
        self.eps_rms = self.perm([128, 1])
        self.eps_ln = self.perm([128, 1])
        self.iota_p = self.perm([128, 1])
        self.one_c = self.perm([128, 1])
        tmpf = self.alloc([128, 128])
        S.I("pool", "iota", out=self.identf, pattern=[[1, 128]], base=0, channel_multiplier=-1,
            allow_small_or_imprecise_dtypes=True)
        S.I("dve", "tensor_single_scalar", out=tmpf, in_=self.identf, scalar=0.0, op=ALU.is_gt)
        S.I("dve", "tensor_copy", out=self.ltri, in_=tmpf)
        S.I("dve", "tensor_single_scalar", out=self.identf, in_=self.identf, scalar=0.0, op=ALU.is_equal)
        S.I("dve", "tensor_copy", out=self.ident, in_=self.identf)
        self._memset(self.ones, 1.0)
        self._memset(self.onesm, 1.0 / 1024.0)
        self._memset(self.eps_rms, RMS_EPS)
        self._memset(self.eps_ln, LN_EPS)
        self._memset(self.one_c, 1.0)
        S.I("pool", "iota", out=self.iota_p, pattern=[[0, 1]], base=0, channel_multiplier=1,
            allow_small_or_imprecise_dtypes=True)
        self.aoff = self.abase

        self.arena_reset()
        z = self.alloc([128, 8 * D], BF16)
        self._memset(z, 0.0)
        rows = self.nslot_max
        r0 = 0
        while r0 < rows:
            n = min(1024, rows - r0)
            assert n % 128 == 0
            S.dma("sp", self.dv(self.H2S[r0:r0 + n, :].rearrange("(p a) d -> p (a d)", p=128), "h2s"),
                  z[:, 0:(n // 128) * D], disjoint=True)
            r0 += n

        self.prologue()
        first = True
        for idx, li in enumerate(self.layers):
            kind = li % 3
            upd = li < DEPTH - 1
            need_ctx = upd or kind == 2
            if "mixer" in SKIP:
                self.copy_x(first, upd)
            elif kind == 0:
                self.conformer(idx, li, first, upd)
            elif kind == 1:
                self.shortconv(idx, li, first, upd)
            else:
                self.s5(idx, li, first, upd)
            first = False
            if "moe" not in SKIP:
                self.moe(idx, li, upd)
        if self.final:
            self.final_norm()
        S.barrier()
        S.replay(nc, st)
        st.close()
        return nc

    def copy_x(self, first, upd):
        self.arena_reset()
        t = [self.alloc([128, D]) for _ in range(4)]
        i = 0
        for lat in ((True, False) if upd else (True,)):
            src, skey = self.xsrc(first, lat)
            dst, dkey = self.xdst(lat)
            n = self.nb * (SEQ if lat else CTX)
            for r0 in range(0, n, 128):
                self.S.dma("sp", t[i % 4], self.dv(src[r0:r0 + 128, :], (skey, r0)))
                self.S.dma("sp", self.dv(dst[r0:r0 + 128, :], (dkey, r0)), t[i % 4])
                i += 1

    def dbg(self, name, v, dt=F32):
        if not DEBUG:
            return
        shape = list(v.ap.shape)
        o = self.nc.dram_tensor("dbg_" + name, shape, dt, kind="ExternalOutput").ap()
        self.S.dma("sp", self.dv(o, "dbg_" + name), v)

    def _memset(self, v, val):
        ap = v.ap
        self.S._emit("dve", lambda e: e.memset(ap, val), [], [v.buf])

    def xsrc(self, first, lat):
        if lat:
            return (self.x_in if first else self.xs), ("xin" if first else "xs")
        return (self.ctx_in if first else self.cs), ("cin" if first else "cs")

    def xdst(self, lat):
        return (self.xs, "xs") if lat else (self.cs, "cs")

    def load_rows_bc(self, dst, src_row_ap, key, nparts=128, eng="sp"):
        n = src_row_ap.shape[-1]
        self.S.dma(eng, dst, self.dv(src_row_ap.to_broadcast([nparts, n]), key))

    def load_featT(self, dst, row_ap, key):
        self.S.dma("sp", dst, self.dv(row_ap.rearrange("o (k p) -> p (o k)", p=128), key), allow_slow_non_contiguous=True)

    def prologue(self):
        S, nb, nc = self.S, self.nb, self.nc
        self.arena_reset()
        ns = nb + 1
        cT = self.alloc([128, KC, ns])
        for b in range(nb):
            self.load_featT(cT[:, :, b], self.c_in[b:b + 1, :], "c_in")
        self.load_featT(cT[:, :, nb], self.cctx_in, "cctx_in")
        S.I("act", "activation", out=cT, in_=cT, func=AF.Silu)
        mrow = self.alloc([ns, 6 * D])
        abrow = self.alloc([ns, 6 * D])
        n1 = self.alloc([ns, D])
        n2 = self.alloc([ns, D])
        orow = self.alloc([ns, 6, D])
        awt = [self.alloc([128, KC, 512]) for _ in range(2)]
        for idx, li in enumerate(self.layers):
            w = self.W[li]
            self.load_rows_bc(abrow, w["ada_b"], "ada_b%d" % li, nparts=ns)
            self.load_rows_bc(n1, w["n1"], "n1_%d" % li, nparts=ns)
            self.load_rows_bc(n2, w["n2"], "n2_%d" % li, nparts=ns)
            awv = w["ada_w"].rearrange("(k p) n -> p k n", p=128)
            for cg in range(12):
                t = awt[cg % 2]
                S.dma("sp" if cg % 2 == 0 else "act", t, self.dv(awv[:, :, cg * 512:(cg + 1) * 512], "ada_w%d" % li))
                ps = self.psum[cg % 2]
                for k in range(KC):
                    S.I("pe", "matmul", out=ps[0:ns, :], lhsT=cT[:, k, :], rhs=t[:, k, :], start=(k == 0), stop=(k == KC - 1))
                S.I("dve", "tensor_tensor", out=mrow[:, cg * 512:(cg + 1) * 512], in0=ps[0:ns, :],
                    in1=abrow[:, cg * 512:(cg + 1) * 512], op=ALU.add)
            S.I("dve", "scalar_tensor_tensor", out=orow[:, 0, :], in0=mrow[:, D:2 * D], scalar=1.0, in1=n1, op0=ALU.add, op1=ALU.mult)
            S.I("dve", "tensor_copy", out=orow[:, 1, :], in_=mrow[:, 0:D])
            S.I("dve", "tensor_copy", out=orow[:, 2, :], in_=mrow[:, 2 * D:3 * D])
            S.I("dve", "scalar_tensor_tensor", out=orow[:, 3, :], in0=mrow[:, 4 * D:5 * D], scalar=1.0, in1=n2, op0=ALU.add, op1=ALU.mult)
            S.I("dve", "tensor_copy", out=orow[:, 4, :], in_=mrow[:, 3 * D:4 * D])
            S.I("dve", "tensor_copy", out=orow[:, 5, :], in_=mrow[:, 5 * D:6 * D])
            S.dma("sp", self.dv(self.MOD[idx].rearrange("k s d -> s k d"), "mod%d" % idx), orow)

    def mod_row(self, idx, kind, s):
        return self.dv(self.MOD[idx, kind, s:s + 1, :], "mod%d" % idx)

    def load_modT(self, idx):
        ns = self.nb + 1
        sT = self.alloc([128, KC, ns])
        bT = self.alloc([128, KC, ns])
        for s in range(ns):
            self.load_featT(sT[:, :, s], self.MOD[idx, 0, s:s + 1, :], "mod%d" % idx)
            self.load_featT(bT[:, :, s], self.MOD[idx, 1, s:s + 1, :], "mod%d" % idx)
        return sT, bT

    def norm_bufs(self):
        nbuf = {}
        nbuf["xt"] = [self.alloc([128, D]) for _ in range(2)]
        nbuf["sq"] = self.alloc([128, D])
        nbuf["x16"] = [self.alloc([128, D], BF16) for _ in range(2)]
        nbuf["ss"] = [self.alloc([128, 1]) for _ in range(2)]
        nbuf["rs"] = [self.alloc([128, 1]) for _ in range(2)]
        nbuf["tmp"] = self.alloc([128, KC, 128])
        nbuf["i"] = 0
        return nbuf

    def rms_tile(self, nbuf, src_v):
        S = self.S
        i = nbuf["i"] % 2
        nbuf["i"] += 1
        xt, ss, rs = nbuf["xt"][i], nbuf["ss"][i], nbuf["rs"][i]
        S.dma("sp", xt, src_v)
        S.I("act", "activation", out=nbuf["sq"], in_=xt, func=AF.Square)
        S.I("dve", "reduce_sum", out=ss, in_=nbuf["sq"], axis=AX.X)
        S.I("act", "activation", out=rs, in_=ss, func=AF.Sqrt, scale=1.0 / D, bias=self.eps_rms)
        S.I("dve", "reciprocal", out=rs, in_=rs)
        return xt, rs, i

    def norm_transpose(self, nbuf, src_v, sT, bT, s, dst):
        S = self.S
        xt, rs, i = self.rms_tile(nbuf, src_v)
        x16 = nbuf["x16"][i]
        S.I("act", "activation", out=x16, in_=xt, func=AF.Identity, scale=rs)
        pt = self.psum[0].bitcast(BF16)
        for k in range(KC):
            S.I("pe", "transpose", out=pt[:, k * 128:(k + 1) * 128], in_=x16[:, k * 128:(k + 1) * 128], identity=self.ident)
        tmp = nbuf["tmp"]
        S.I("dve", "tensor_tensor", out=tmp, in0=pt.re("p (k t) -> p k t", k=KC), in1=sT[:, :, s:s + 1].bc([128, KC, 128]), op=ALU.mult)
        S.I("dve", "tensor_tensor", out=dst, in0=tmp, in1=bT[:, :, s:s + 1].bc([128, KC, 128]), op=ALU.add)

    def seqs(self, with_ctx):
        out = [("lat", b) for b in range(self.nb)]
        if with_ctx:
            out.append(("ctx", self.nb))
        return out

    def load_w_bf16(self, dst, w_ap, key, ncols):
        wv = w_ap.rearrange("(k p) n -> p k n", p=128)
        for k in range(KC):
            for c0 in range(0, ncols, 2048):
                c1 = min(ncols, c0 + 2048)
                self.S.dma("pool", dst[:, k, c0:c1], self.dv(wv[:, k, c0:c1], key), disjoint=True)

    def residual_out(self, ps_halves, g_bc, src_v, dst_v, obuf, xbuf):
        S = self.S
        S.dma("sp", xbuf, src_v)
        for h in range(2):
            S.I("dve", "tensor_tensor", out=obuf[:, h * 512:(h + 1) * 512], in0=ps_halves[h], in1=g_bc[:, h * 512:(h + 1) * 512], op=ALU.mult)
        S.I("pool", "tensor_tensor", out=obuf, in0=obuf, in1=xbuf, op=ALU.add)
        S.dma("sp", dst_v, obuf)

    def conformer(self, idx, li, first, upd):
        S, nb, w = self.S, self.nb, self.W[li]
        self.arena_reset()
        sT, bT = self.load_modT(idx)
        w_in = self.alloc([128, KC, 2 * D], BF16)
        w_out = self.alloc([128, KC, D], BF16)
        self.load_w_bf16(w_in, w["w_in"], "cw_in%d" % li, 2 * D)
        self.load_w_bf16(w_out, w["w_out"], "cw_out%d" % li, D)
        dwT = self.alloc([128, KC, 31])
        for k in range(KC):
            S.dma("sp", dwT[:, k, :], self.dv(w["dw"][:, k * 128:(k + 1) * 128].rearrange("t p -> p t"), "dw%d" % li), allow_slow_non_contiguous=True)
        dwb = self.alloc([128, KC])
        lng = self.alloc([128, KC])
        lnb = self.alloc([128, KC])
        self.load_featT(dwb, w["dw_b"], "dwb%d" % li)
        self.load_featT(lng, w["ln_g"], "lng%d" % li)
        self.load_featT(lnb, w["ln_b"], "lnb%d" % li)
        nbuf = self.norm_bufs()
        g1 = [self.alloc([128, D]) for _ in range(2)]
        hT = [self.alloc([128, KC, 512], BF16) for _ in range(2)]
        zbuf = self.alloc([128, KC, SEQ], BF16)
        sg = [self.alloc([128, 512]) for _ in range(2)]
        dwd = [self.alloc([128, 31, 128], BF16) for _ in range(2)]
        vall = self.alloc([128, KC, SEQ], BF16)
        vsq = [self.alloc([128, 512], BF16) for _ in range(2)]
        s16 = self.alloc([128, KC, 512], BF16)
        msq = self.alloc([128, 512])
        mean = self.alloc([128, 512])
        rstd = self.alloc([128, 512])
        nmr = self.alloc([128, 512])
        t1 = [self.alloc([128, 512]) for _ in range(2)]
        obuf = [self.alloc([128, D])] * 2
        P = self.psum
        gi = 0
        di = 0
        ci2 = 0
        for si, (sk, s) in enumerate(self.seqs(upd)):
            lat = sk == "lat"
            ntok = SEQ if lat else nb * CTX
            src, skey = self.xsrc(first, lat)
            dst, dkey = self.xdst(lat)
            base = s * SEQ if lat else 0
            GS = min(512, ntok)
            ng = ntok // GS
            gb = g1[si % 2]
            self.load_rows_bc(gb, self.MOD[idx, 2, s:s + 1, :], "mod%d" % idx)
            for g in range(ng):
                h = hT[gi % 2]
                gi += 1
                for j in range(GS // 128):
                    r0 = base + g * GS + j * 128
                    self.norm_transpose(nbuf, self.dv(src[r0:r0 + 128, :], (skey, r0)), sT, bT, s, h[:, :, j * 128:(j + 1) * 128])
                for m in range(KC):
                    pv, pg = P[1 + m % 2], P[3 + m % 2]
                    for k in range(KC):
                        S.I("pe", "matmul", out=pv[:, 0:GS], lhsT=w_in[:, k, m * 128:(m + 1) * 128], rhs=h[:, k, 0:GS], start=(k == 0), stop=(k == KC - 1))
                    for k in range(KC):
                        S.I("pe", "matmul", out=pg[:, 0:GS], lhsT=w_in[:, k, D + m * 128:D + (m + 1) * 128], rhs=h[:, k, 0:GS], start=(k == 0), stop=(k == KC - 1))
                    sgt = sg[m % 2]
                    S.I("act", "activation", out=sgt[:, 0:GS], in_=pg[:, 0:GS], func=AF.Sigmoid)
                    S.I("dve", "tensor_tensor", out=zbuf[:, m, g * GS:(g + 1) * GS], in0=pv[:, 0:GS], in1=sgt[:, 0:GS], op=ALU.mult)
            order = [15] + [k for k in range(31) if k != 15]
            for m in range(KC):
                dd = dwd[di % 2]
                di += 1
                for k in range(31):
                    S.I("dve", "tensor_single_scalar", out=dd[:, k, :], in_=self.ident, scalar=dwT[:, m, k:k + 1], op=ALU.mult)
                for g in range(ng):
                    g0 = g * GS
                    pc = P[1 + ci2 % 2]
                    ci2 += 1
                    for n_, k in enumerate(order):
                        d = k - 15
                        if lat:
                            dt_ = d * 64
                            lo, hi = max(g0, -dt_), min(g0 + GS, SEQ - dt_)
                            if lo >= hi:
                                continue
                            S.I("pe", "matmul", out=pc[:, lo - g0:hi - g0], lhsT=dd[:, k, :], rhs=zbuf[:, m, lo + dt_:hi + dt_],
                                start=(n_ == 0), stop=(n_ == 30), skip_group_check=True)
                        else:
                            lo, hi = max(0, -d), min(CTX, CTX - d)
                            o = pc[:, 0:GS].re("p (s t) -> p s t", t=CTX)[:, :, lo:hi]
                            r = zbuf[:, m, g0:g0 + GS].re("p (s t) -> p s t", t=CTX)[:, :, lo + d:hi + d]
                            S.I("pe", "matmul", out=o, lhsT=dd[:, k, :], rhs=r, start=(n_ == 0), stop=(n_ == 30), skip_group_check=True)
                    S.I("act", "activation", out=vall[:, m, g0:g0 + GS], in_=pc[:, 0:GS], func=AF.Identity, bias=dwb[:, m:m + 1])
            for g in range(ng):
                g0 = g * GS
                v16 = vall[:, :, g0:g0 + GS]
                for m in range(KC):
                    vq = vsq[m % 2]
                    S.I("act", "activation", out=vq[:, 0:GS], in_=v16[:, m, :], func=AF.Square)
                    S.I("pe", "matmul", out=P[5][:, 0:GS], lhsT=self.onesm, rhs=v16[:, m, :], start=(m == 0), stop=(m == KC - 1))
                    S.I("pe", "matmul", out=P[6][:, 0:GS], lhsT=self.onesm, rhs=vq[:, 0:GS], start=(m == 0), stop=(m == KC - 1))
                S.I("act", "activation", out=mean[:, 0:GS], in_=P[5][:, 0:GS], func=AF.Identity)
                S.I("act", "activation", out=msq[:, 0:GS], in_=P[5][:, 0:GS], func=AF.Square)
                S.I("dve", "tensor_tensor", out=rstd[:, 0:GS], in0=P[6][:, 0:GS], in1=msq[:, 0:GS], op=ALU.subtract)
                S.I("act", "activation", out=rstd[:, 0:GS], in_=rstd[:, 0:GS], func=AF.Sqrt, bias=self.eps_ln)
                S.I("dve", "reciprocal", out=rstd[:, 0:GS], in_=rstd[:, 0:GS])
                S.I("dve", "scalar_tensor_tensor", out=nmr[:, 0:GS], in0=mean[:, 0:GS], scalar=-1.0, in1=rstd[:, 0:GS], op0=ALU.mult, op1=ALU.mult)
                for m in range(KC):
                    tt = t1[m % 2]
                    S.I("dve", "tensor_tensor", out=tt[:, 0:GS], in0=v16[:, m, :], in1=rstd[:, 0:GS], op=ALU.mult)
                    S.I("pool", "tensor_tensor", out=tt[:, 0:GS], in0=tt[:, 0:GS], in1=nmr[:, 0:GS], op=ALU.add)
                    S.I("act", "activation", out=s16[:, m, 0:GS], in_=tt[:, 0:GS], func=AF.Silu, scale=lng[:, m:m + 1], bias=lnb[:, m:m + 1])
                for j in range(GS // 128):
                    r0 = base + g0 + j * 128
                    for h_ in range(2):
                        for k in range(KC):
                            S.I("pe", "matmul", out=P[3 + h_], lhsT=s16[:, k, j * 128:(j + 1) * 128], rhs=w_out[:, k, h_ * 512:(h_ + 1) * 512],
                                start=(k == 0), stop=(k == KC - 1))
                    ob = obuf[j % 2]
                    self.residual_out([P[3], P[4]], gb, self.dv(src[r0:r0 + 128, :], (skey, r0)), self.dv(dst[r0:r0 + 128, :], (dkey, r0)),
                                      ob, nbuf["xt"][j % 2])

    def shortconv(self, idx, li, first, upd):
        S, nb, w = self.S, self.nb, self.W[li]
        self.arena_reset()
        sT, bT = self.load_modT(idx)
        w_in = self.alloc([128, KC, 3 * D], BF16)
        w_out = self.alloc([128, KC, D], BF16)
        self.load_w_bf16(w_in, w["w_in"], "sw_in%d" % li, 3 * D)
        self.load_w_bf16(w_out, w["w_out"], "sw_out%d" % li, D)
        cvT = self.alloc([128, KC, 3])
        for k in range(KC):
            S.dma("sp", cvT[:, k, :], self.dv(w["cv"][:, k * 128:(k + 1) * 128].rearrange("t p -> p t"), "cv%d" % li), allow_slow_non_contiguous=True)
        nbuf = self.norm_bufs()
        g1 = [self.alloc([128, D]) for _ in range(2)]
        hT = [self.alloc([128, KC, 512], BF16) for _ in range(2)]
        gcs = [self.alloc([128, 512]) for _ in range(2)]
        q = [self.alloc([128, 512]) for _ in range(2)]
        cc = [self.alloc([128, 512]) for _ in range(2)]
        p16 = self.alloc([128, KC, 512], BF16)
        obuf = [self.alloc([128, D]) for _ in range(2)]
        P = self.psum
        gi = 0
        for si, (sk, s) in enumerate(self.seqs(upd)):
            lat = sk == "lat"
            ntok = SEQ if lat else nb * CTX
            src, skey = self.xsrc(first, lat)
            dst, dkey = self.xdst(lat)
            base = s * SEQ if lat else 0
            GS = min(512, ntok)
            ng = ntok // GS
            RL = 64 if lat else CTX
            gb = g1[si % 2]
            self.load_rows_bc(gb, self.MOD[idx, 2, s:s + 1, :], "mod%d" % idx)
            for g in range(ng):
                g0 = g * GS
                h = hT[gi % 2]
                gi += 1
                for j in range(GS // 128):
                    r0 = base + g0 + j * 128
                    self.norm_transpose(nbuf, self.dv(src[r0:r0 + 128, :], (skey, r0)), sT, bT, s, h[:, :, j * 128:(j + 1) * 128])
                for m in range(KC):
                    pb, pc_, pv = P[1], P[2 + m % 2], P[4 + m % 2]
                    for (pp, off) in ((pc_, D), (pv, 2 * D)):
                        for k in range(KC):
                            S.I("pe", "matmul", out=pp[:, 0:GS], lhsT=w_in[:, k, off + m * 128:off + (m + 1) * 128], rhs=h[:, k, 0:GS],
                                start=(k == 0), stop=(k == KC - 1))
                    gct, qt, ct = gcs[m % 2], q[m % 2], cc[m % 2]
                    S.I("act", "activation", out=gct[:, 0:GS], in_=pc_[:, 0:GS], func=AF.Identity)
                    S.I("dve", "tensor_tensor", out=qt[:, 0:GS], in0=pv[:, 0:GS], in1=gct[:, 0:GS], op=ALU.mult)
                    S.I("act", "activation", out=ct[:, 0:GS], in_=qt[:, 0:GS], func=AF.Identity, scale=cvT[:, m, 1:2])
                    q3 = qt[:, 0:GS].re("p (r c) -> p r c", c=RL)
                    c3 = ct[:, 0:GS].re("p (r c) -> p r c", c=RL)
                    S.I("dve", "scalar_tensor_tensor", out=c3[:, :, 1:RL], in0=q3[:, :, 0:RL - 1], scalar=cvT[:, m, 0:1], in1=c3[:, :, 1:RL],
                        op0=ALU.mult, op1=ALU.add)
                    S.I("dve", "scalar_tensor_tensor", out=c3[:, :, 0:RL - 1], in0=q3[:, :, 1:RL], scalar=cvT[:, m, 2:3], in1=c3[:, :, 0:RL - 1],
                        op0=ALU.mult, op1=ALU.add)
                    for k in range(KC):
                        S.I("pe", "matmul", out=pb[:, 0:GS], lhsT=w_in[:, k, m * 128:(m + 1) * 128], rhs=h[:, k, 0:GS], start=(k == 0), stop=(k == KC - 1))
                    S.I("dve", "tensor_tensor", out=p16[:, m, 0:GS], in0=pb[:, 0:GS], in1=ct[:, 0:GS], op=ALU.mult)
                for j in range(GS // 128):
                    r0 = base + g0 + j * 128
                    for h_ in range(2):
                        for k in range(KC):
                            S.I("pe", "matmul", out=P[6 + h_], lhsT=p16[:, k, j * 128:(j + 1) * 128], rhs=w_out[:, k, h_ * 512:(h_ + 1) * 512],
                                start=(k == 0), stop=(k == KC - 1))
                    self.residual_out([P[6], P[7]], gb, self.dv(src[r0:r0 + 128, :], (skey, r0)), self.dv(dst[r0:r0 + 128, :], (dkey, r0)),
                                      obuf[j % 2], nbuf["xt"][j % 2])

    def s5(self, idx, li, first, upd):
        import math
        S, nb, w, P = self.S, self.nb, self.W[li], self.psum
        NTK = CTX + SEQ
        HTv = self.HT.rearrange("(k p) t -> p k t", p=128)
        YTv = self.YT.rearrange("(k p) t -> p k t", p=128)
        self.arena_reset()
        sT, bT = self.load_modT(idx)
        nbuf = self.norm_bufs()
        ht = [self.alloc([128, KC, 128], BF16) for _ in range(3)]
        i = 0
        for b in range(nb):
            for lat, n_t in ((False, CTX // 128), (True, SEQ // 128)):
                src, skey = self.xsrc(first, lat)
                for j in range(n_t):
                    r0 = (b * SEQ if lat else b * CTX) + j * 128
                    col = b * NTK + (CTX if lat else 0) + j * 128
                    t = ht[i % 3]
                    i += 1
                    self.norm_transpose(nbuf, self.dv(src[r0:r0 + 128, :], (skey, r0)), sT, bT, (b if lat else nb), t)
                    S.dma("sp", self.dv(HTv[:, :, col:col + 128], "ht"), t, disjoint=True)
        self.arena_reset()
        ND = 64
        f2 = lambda v: v.re("p d g -> p (d g)")
        are, aim, ldt = (self.alloc([128, 2, 32]) for _ in range(3))
        for d in range(2):
            S.dma("sp", are[:, d, :], self.dv(w["a_re"][d].rearrange("(G g) p -> (g p) G", g=2), "s5a"), allow_slow_non_contiguous=True)
            S.dma("sp", aim[:, d, :], self.dv(w["a_im"][d].rearrange("(G g) p -> (g p) G", g=2), "s5a"), allow_slow_non_contiguous=True)
            for g2 in range(2):
                srcv = w["ldt"][d:d + 1, :].rearrange("o (G g) -> o g G", g=2)[:, g2, :]
                S.dma("sp", ldt[g2 * 64:(g2 + 1) * 64, d, :], self.dv(srcv.to_broadcast([64, 32]), "s5a"), allow_slow_non_contiguous=True)
        names = ("dt", "mag", "ang", "c", "s", "ta", "tb", "den", "nre", "kre", "kim", "abr", "abi")
        T_ = {n: self.alloc([128, ND]) for n in names}
        hpi = self.alloc([128, 1])
        self._memset(hpi, math.pi / 2)
        A, B_ = f2(are), f2(aim)
        S.I("dve", "tensor_single_scalar", out=A, in_=A, scalar=-1e-4, op=ALU.min)
        S.I("act", "activation", out=T_["dt"], in_=f2(ldt), func=AF.Exp)
        S.I("dve", "tensor_tensor", out=T_["ta"], in0=T_["dt"], in1=A, op=ALU.mult)
        S.I("act", "activation", out=T_["mag"], in_=T_["ta"], func=AF.Exp)
        S.I("dve", "tensor_tensor", out=T_["ang"], in0=T_["dt"], in1=B_, op=ALU.mult)
        S.I("act", "activation", out=T_["s"], in_=T_["ang"], func=AF.Sin, scale=1.0 / 16)
        S.I("act", "activation", out=T_["c"], in_=T_["ang"], func=AF.Sin, scale=1.0 / 16, bias=hpi)

        def csquare(c, s, ta, tb):
            S.I("dve", "tensor_tensor", out=ta, in0=c, in1=c, op=ALU.mult)
            S.I("dve", "tensor_tensor", out=tb, in0=s, in1=s, op=ALU.mult)
            S.I("dve", "scalar_tensor_tensor", out=s, in0=c, scalar=2.0, in1=s, op0=ALU.mult, op1=ALU.mult)
            S.I("dve", "tensor_tensor", out=c, in0=ta, in1=tb, op=ALU.subtract)
        for _ in range(4):
            csquare(T_["c"], T_["s"], T_["ta"], T_["tb"])
        NP2 = 12
        Er = self.alloc([128, NP2, ND])
        Ei = self.alloc([128, NP2, ND])
        S.I("dve", "tensor_copy", out=Er[:, 0, :], in_=T_["c"])
        S.I("dve", "tensor_copy", out=Ei[:, 0, :], in_=T_["s"])
        for j in range(1, NP2):
            S.I("dve", "tensor_copy", out=Er[:, j, :], in_=Er[:, j - 1, :])
            S.I("dve", "tensor_copy", out=Ei[:, j, :], in_=Ei[:, j - 1, :])
            csquare(Er[:, j, :], Ei[:, j, :], T_["ta"], T_["tb"])
        S.I("dve", "tensor_tensor", out=T_["abr"], in0=T_["mag"], in1=T_["c"], op=ALU.mult)
        S.I("dve", "tensor_tensor", out=T_["abi"], in0=T_["mag"], in1=T_["s"], op=ALU.mult)
        S.I("dve", "tensor_tensor", out=T_["ta"], in0=A, in1=A, op=ALU.mult)
        S.I("dve", "tensor_tensor", out=T_["tb"], in0=B_, in1=B_, op=ALU.mult)
        S.I("dve", "tensor_tensor", out=T_["den"], in0=T_["ta"], in1=T_["tb"], op=ALU.add)
        S.I("dve", "reciprocal", out=T_["den"], in_=T_["den"])
        S.I("dve", "tensor_single_scalar", out=T_["nre"], in_=T_["abr"], scalar=-1.0, op=ALU.add)
        S.I("dve", "tensor_tensor", out=T_["ta"], in0=T_["nre"], in1=A, op=ALU.mult)
        S.I("dve", "tensor_tensor", out=T_["tb"], in0=T_["abi"], in1=B_, op=ALU.mult)
        S.I("dve", "tensor_tensor", out=T_["kre"], in0=T_["ta"], in1=T_["tb"], op=ALU.add)
        S.I("dve", "tensor_tensor", out=T_["kre"], in0=T_["kre"], in1=T_["den"], op=ALU.mult)
        S.I("dve", "tensor_tensor", out=T_["ta"], in0=T_["abi"], in1=A, op=ALU.mult)
        S.I("dve", "tensor_tensor", out=T_["tb"], in0=T_["nre"], in1=B_, op=ALU.mult)
        S.I("dve", "tensor_tensor", out=T_["kim"], in0=T_["ta"], in1=T_["tb"], op=ALU.subtract)
        S.I("dve", "tensor_tensor", out=T_["kim"], in0=T_["kim"], in1=T_["den"], op=ALU.mult)
        mag = T_["mag"]
        lB = self.alloc([32, ND, 2, 128], BF16)
        lC = self.alloc([128, ND, 2, 32], BF16)
        dvec = self.alloc([128, KC])
        self.load_featT(dvec, w["d"], "s5d")
        keep = self.aoff
        bre = self.alloc([128, 2, 32, 16])
        bim = self.alloc([128, 2, 32, 16])
        for d in range(2):
            S.dma("sp", bre[:, d], self.dv(w["b_re"][d].rearrange("(G g) p c -> (g p) G c", g=2), "s5b"))
            S.dma("sp", bim[:, d], self.dv(w["b_im"][d].rearrange("(G g) p c -> (g p) G c", g=2), "s5b"))
        bbr = self.alloc([128, ND, 16])
        bbi = self.alloc([128, ND, 16])
        tq = self.alloc([128, ND, 16])
        brf, bif = bre.re("p d g c -> p (d g) c"), bim.re("p d g c -> p (d g) c")
        kr3 = T_["kre"].un(2).bc([128, ND, 16])
        ki3 = T_["kim"].un(2).bc([128, ND, 16])
        S.I("dve", "tensor_tensor", out=bbr, in0=brf, in1=kr3, op=ALU.mult)
        S.I("dve", "tensor_tensor", out=tq, in0=bif, in1=ki3, op=ALU.mult)
        S.I("dve", "tensor_tensor", out=bbr, in0=bbr, in1=tq, op=ALU.subtract)
        S.I("dve", "tensor_tensor", out=bbi, in0=bif, in1=kr3, op=ALU.mult)
        S.I("dve", "tensor_tensor", out=tq, in0=brf, in1=ki3, op=ALU.mult)
        S.I("dve", "tensor_tensor", out=bbi, in0=bbi, in1=tq, op=ALU.add)
        bblk = [self.alloc([128, 2, 32], BF16) for _ in range(2)]
        for t in bblk:
            self._memset(t, 0.0)
        for dg in range(ND):
            t = bblk[dg % 2]
            for c_, bb in ((0, bbr), (1, bbi)):
                S.I("dve", "tensor_copy", out=t[0:64, c_, 0:16], in_=bb[0:64, dg, :])
                S.I("dve", "tensor_copy", out=t[64:128, c_, 16:32], in_=bb[64:128, dg, :])
            pt = P[dg % 2].bitcast(BF16)
            for c_ in range(2):
                S.I("pe", "transpose", out=pt[0:32, c_ * 128:(c_ + 1) * 128], in_=t[:, c_, :], identity=self.ident)
            S.I("act", "activation", out=lB[:, dg, :, :], in_=pt[0:32, 0:256].re("p (c q) -> p c q", c=2), func=AF.Identity)
        cnat = [self.alloc([32, 32, 128]) for _ in range(2)]
        ci = 0
        for d in range(2):
            for c_, nm in ((0, "c_re"), (1, "c_im")):
                t = cnat[ci % 2]
                ci += 1
                self._memset(t, 0.0)
                for g2 in range(2):
                    srcv = w[nm][d].rearrange("(G g) c p -> g c G p", g=2)[g2]
                    S.dma("sp", t[16 * g2:16 * g2 + 16, :, 64 * g2:64 * g2 + 64], self.dv(srcv, "s5c"), disjoint=True)
                for G in range(32):
                    pp = P[2 + G % 2]
                    S.I("pe", "transpose", out=pp[:, 0:32], in_=t[:, G, :], identity=self.identf[0:32, 0:32])
                    S.I("act", "activation", out=lC[:, d * 32 + G, c_, :], in_=pp[:, 0:32], func=AF.Identity, scale=(1.0 if c_ == 0 else -1.0))
        S.barrier()
        self.aoff = keep
        cosT = self.alloc([128, NTK])
        sinT = self.alloc([128, NTK])
        ttmp = [self.alloc([128, 1024]) for _ in range(2)]
        lCp = [self.alloc([128, 2, 128], BF16) for _ in range(2)]
        for t in lCp:
            self._memset(t, 0.0)
        u = [self.alloc([32, NTK], BF16) for _ in range(2)]
        bus = [[self.alloc([128, 512]) for _ in range(2)] for _ in range(2)]
        tm = [[self.alloc([128, 512]) for _ in range(4)] for _ in range(2)]
        Wr = [self.alloc([128, 512]) for _ in range(2)]
        Wi = [self.alloc([128, 512]) for _ in range(2)]
        Gr = [self.alloc([128, 512]) for _ in range(2)]
        Gi = [self.alloc([128, 512]) for _ in range(2)]
        to = tm
        Hr = [self.alloc([128, 512], BF16) for _ in range(2)]
        Hi = [self.alloc([128, 512], BF16) for _ in range(2)]
        yacc = [self.alloc([128, NTK]) for _ in range(nb)]
        hch = [self.alloc([128, NTK], BF16) for _ in range(2)]
        gt = [tm[0][0:3], tm[1][0:3]]
        y16 = [self.alloc([128, 512], BF16) for _ in range(2)]
        fw = [(0, CTX)] + [(CTX + 512 * i_, 512) for i_ in range(SEQ // 512)]
        rv = [(0, CTX)] + [(CTX + 512 * i_, 512) for i_ in reversed(range(SEQ // 512))]
        pcount = 0
        ui = 0
        for m in range(KC):
            for jj in range(4):
                G = 4 * m + jj
                for d in range(2):
                    dg = d * 32 + G
                    first_acc = (jj == 0 and d == 0)
                    self._memset(cosT[:, 0:1], 1.0)
                    self._memset(sinT[:, 0:1], 0.0)
                    n_have = 1
                    j = 0
                    while n_have < NTK:
                        n_new = min(n_have, NTK - n_have)
                        er, ei = Er[:, j, dg:dg + 1], Ei[:, j, dg:dg + 1]
                        ta, tb = ttmp[0][:, 0:n_new], ttmp[1][:, 0:n_new]
                        S.I("dve", "tensor_single_scalar", out=ta, in_=sinT[:, 0:n_new], scalar=ei, op=ALU.mult)
                        S.I("dve", "tensor_single_scalar", out=tb, in_=sinT[:, 0:n_new], scalar=er, op=ALU.mult)
                        S.I("dve", "scalar_tensor_tensor", out=sinT[:, n_have:n_have + n_new], in0=cosT[:, 0:n_new], scalar=ei, in1=tb, op0=ALU.mult, op1=ALU.add)
                        S.I("dve", "scalar_tensor_tensor", out=cosT[:, n_have:n_have + n_new], in0=cosT[:, 0:n_new], scalar=er, in1=ta, op0=ALU.mult, op1=ALU.subtract)
                        n_have += n_new
                        j += 1
                    lc = lCp[dg % 2]
                    S.I("dve", "tensor_copy", out=lc[:, :, 32 * jj:32 * jj + 32], in_=lC[:, dg, :, :])
                    if jj > 0:
                        pass
                    mg = mag[:, dg:dg + 1]
                    for b in range(nb):
                        ut = u[ui % 2]
                        ui += 1
                        S.dma("sp", ut, self.dv(self.HT[32 * G:32 * G + 32, b * NTK:(b + 1) * NTK], "ht"))
                        prev = None
                        n0 = 0
                        for (c0, L) in (fw if d == 0 else rv):
                            pi_ = pcount % 2
                            pcount += 1
                            pr, pim = P[0 + pi_], P[2 + pi_]
                            S.I("pe", "matmul", out=pr[:, 0:L], lhsT=lB[:, dg, 0, :], rhs=ut[:, c0:c0 + L], start=True, stop=True)
                            S.I("pe", "matmul", out=pim[:, 0:L], lhsT=lB[:, dg, 1, :], rhs=ut[:, c0:c0 + L], start=True, stop=True)
                            br_, bi_ = bus[pi_][0][:, 0:L], bus[pi_][1][:, 0:L]
                            S.I("act", "activation", out=br_, in_=pr[:, 0:L], func=AF.Identity)
                            S.I("act", "activation", out=bi_, in_=pim[:, 0:L], func=AF.Identity)
                            cs_, sn_ = cosT[:, n0:n0 + L], sinT[:, n0:n0 + L]
                            if d == 1:
                                cs_, sn_ = cs_.rev(), sn_.rev()
                            t1, t2, t3, t4 = (x_[:, 0:L] for x_ in tm[pi_])
                            wr_, wi_ = Wr[pi_][:, 0:L], Wi[pi_][:, 0:L]
                            S.I("dve", "tensor_tensor", out=t1, in0=br_, in1=cs_, op=ALU.mult)
                            S.I("dve", "tensor_tensor", out=t2, in0=bi_, in1=sn_, op=ALU.mult)
                            S.I("dve", "tensor_tensor", out=wr_, in0=t1, in1=t2, op=ALU.add)
                            S.I("pool", "tensor_tensor", out=t3, in0=bi_, in1=cs_, op=ALU.mult)
                            S.I("pool", "tensor_tensor", out=t4, in0=br_, in1=sn_, op=ALU.mult)
                            S.I("pool", "tensor_tensor", out=wi_, in0=t3, in1=t4, op=ALU.subtract)
                            gr_, gi_ = Gr[pi_][:, 0:L], Gi[pi_][:, 0:L]
                            if prev is None:
                                ir, ii_ = 0.0, 0.0
                            else:
                                pgr, pgi, pL = prev
                                ir = pgr[:, pL - 1:pL] if d == 0 else pgr[:, 0:1]
                                ii_ = pgi[:, pL - 1:pL] if d == 0 else pgi[:, 0:1]
                            mb = mg.bc([128, L])
                            if d == 0:
                                S.I("dve", "tensor_tensor_scan", out=gr_, data0=mb, data1=wr_, initial=ir, op0=ALU.mult, op1=ALU.add)
                                S.I("dve", "tensor_tensor_scan", out=gi_, data0=mb, data1=wi_, initial=ii_, op0=ALU.mult, op1=ALU.add)
                            else:
                                S.I("dve", "tensor_tensor_scan", out=gr_.rev(), data0=mb, data1=wr_.rev(), initial=ir, op0=ALU.mult, op1=ALU.add)
                                S.I("dve", "tensor_tensor_scan", out=gi_.rev(), data0=mb, data1=wi_.rev(), initial=ii_, op0=ALU.mult, op1=ALU.add)
                            prev = (Gr[pi_], Gi[pi_], L)
                            o1, o2, o3, o4 = (x_[:, 0:L] for x_ in to[pi_])
                            hr_, hi_ = Hr[pi_][:, 0:L], Hi[pi_][:, 0:L]
                            S.I("dve", "tensor_tensor", out=o1, in0=gr_, in1=cs_, op=ALU.mult)
                            S.I("dve", "tensor_tensor", out=o2, in0=gi_, in1=sn_, op=ALU.mult)
                            S.I("dve", "tensor_tensor", out=hr_, in0=o1, in1=o2, op=ALU.subtract)
                            S.I("pool", "tensor_tensor", out=o3, in0=gr_, in1=sn_, op=ALU.mult)
                            S.I("pool", "tensor_tensor", out=o4, in0=gi_, in1=cs_, op=ALU.mult)
                            S.I("pool", "tensor_tensor", out=hi_, in0=o3, in1=o4, op=ALU.add)
                            py = P[4 + pi_]
                            S.I("pe", "matmul", out=py[:, 0:L], lhsT=lc[:, 0, :], rhs=hr_, start=True, stop=False)
                            S.I("pe", "matmul", out=py[:, 0:L], lhsT=lc[:, 1, :], rhs=hi_, start=False, stop=True)
                            ya = yacc[b][:, c0:c0 + L]
                            if first_acc:
                                S.I("act", "activation", out=ya, in_=py[:, 0:L], func=AF.Identity)
                            else:
                                S.I("dve", "tensor_tensor", out=ya, in0=py[:, 0:L], in1=ya, op=ALU.add)
                            n0 += L
                    self.S._emit("dve", (lambda apx: (lambda e: e.memset(apx, 0.0)))(lc[:, :, 32 * jj:32 * jj + 32].ap), [], [lc.buf])
            for b in range(nb):
                hc = hch[b % 2]
                S.dma("sp", hc, self.dv(self.HT[128 * m:128 * m + 128, b * NTK:(b + 1) * NTK], "ht"))
                for pi2, (c0, L) in enumerate(fw):
                    tt, sq, sg_ = (x_[:, 0:L] for x_ in gt[pi2 % 2])
                    yo_ = y16[pi2 % 2][:, 0:L]
                    S.I("dve", "scalar_tensor_tensor", out=tt, in0=hc[:, c0:c0 + L], scalar=dvec[:, m:m + 1], in1=yacc[b][:, c0:c0 + L], op0=ALU.mult, op1=ALU.add)
                    S.I("act", "activation", out=sq, in_=tt, func=AF.Square)
                    S.I("act", "activation", out=sq, in_=sq, func=AF.Identity, scale=0.044715, bias=self.one_c)
                    S.I("pool", "tensor_tensor", out=sq, in0=sq, in1=tt, op=ALU.mult)
                    S.I("act", "activation", out=sg_, in_=sq, func=AF.Sigmoid, scale=1.5957691216057308)
                    S.I("pool", "tensor_tensor", out=yo_, in0=tt, in1=sg_, op=ALU.mult)
                    col = b * NTK + c0
                    S.dma("sp", self.dv(YTv[:, m, col:col + L], "yt"), yo_, disjoint=True)
        self.arena_reset()
        wg = self.alloc([128, KC, 2 * D], BF16)
        self.load_w_bf16(wg, w["w_glu"], "s5wg%d" % li, 2 * D)
        g1 = [self.alloc([128, D]) for _ in range(2)]
        yt = [self.alloc([128, KC, 128], BF16) for _ in range(2)]
        sgb = [self.alloc([128, D]) for _ in range(2)]
        obuf = [self.alloc([128, D]) for _ in range(2)]
        xb = [self.alloc([128, D]) for _ in range(2)]
        ti = 0
        for b in range(nb):
            self.load_rows_bc(g1[0], self.MOD[idx, 2, b:b + 1, :], "mod%d" % idx)
            if upd:
                self.load_rows_bc(g1[1], self.MOD[idx, 2, nb:nb + 1, :], "mod%d" % idx)
            for lat, n_t in (((False, CTX // 128),) if upd else ()) + ((True, SEQ // 128),):
                src, skey = self.xsrc(first, lat)
                dst, dkey = self.xdst(lat)
                gb = g1[0] if lat else g1[1]
                for j in range(n_t):
                    r0 = (b * SEQ if lat else b * CTX) + j * 128
                    col = b * NTK + (CTX if lat else 0) + j * 128
                    y_ = yt[ti % 2]
                    S.dma("sp", y_, self.dv(YTv[:, :, col:col + 128], "yt"))
                    for cb in range(4):
                        pp = P[cb]
                        for k in range(KC):
                            S.I("pe", "matmul", out=pp, lhsT=y_[:, k, :], rhs=wg[:, k, cb * 512:(cb + 1) * 512], start=(k == 0), stop=(k == KC - 1))
                    sg_, ob, xx = sgb[ti % 2], obuf[ti % 2], xb[ti % 2]
                    ti += 1
                    S.dma("sp", xx, self.dv(src[r0:r0 + 128, :], (skey, r0)))
                    for h_ in range(2):
                        S.I("act", "activation", out=sg_[:, h_ * 512:(h_ + 1) * 512], in_=P[2 + h_], func=AF.Sigmoid)
                        S.I("dve", "tensor_tensor", out=ob[:, h_ * 512:(h_ + 1) * 512], in0=P[h_], in1=sg_[:, h_ * 512:(h_ + 1) * 512], op=ALU.mult)
                    S.I("pool", "tensor_tensor", out=ob, in0=ob, in1=gb, op=ALU.mult)
                    S.I("dve", "tensor_tensor", out=ob, in0=ob, in1=xx, op=ALU.add)
                    S.dma("sp", self.dv(dst[r0:r0 + 128, :], (dkey, r0)), ob)

    def moe(self, idx, li, upd):
        S, nb, w, nc = self.S, self.nb, self.W[li], self.nc
        self.arena_reset()
        P = self.psum
        tiles = []
        for b in range(nb):
            for j in range(SEQ // 128):
                r0 = b * SEQ + j * 128
                tiles.append((self.xs, "xs", r0, b))
        if upd:
            for j in range(nb * CTX // 128):
                tiles.append((self.cs, "cs", j * 128, nb))
        NT = len(tiles)
        ntok = NT * 128
        nslot = ((2 * ntok + NE * (TS - 1)) + TS - 1) // TS * TS
        NST = nslot // TS

        oh1 = self.alloc([128, NT, NE])
        oh2 = self.alloc([128, NT, NE])
        L1 = self.alloc([128, NT])
        L2 = self.alloc([128, NT])
        W1 = self.alloc([128, NT])
        W2 = self.alloc([128, NT])
        run = self.alloc([128, NE])
        sl1 = self.alloc([128, NT], I32)
        sl2 = self.alloc([128, NT], I32)
        widx = self.alloc([128, 6, NST], I32)
        keep = self.aoff

        wr = self.alloc([128, KC, 36])
        S.dma("sp", wr, self.dv(w["wr"].rearrange("(k p) n -> p k n", p=128), "wr%d" % li))
        brb = self.alloc([128, 36])
        self.load_rows_bc(brb, w["br"], "br%d" % li)
        sc2 = [self.alloc([128, D]) for _ in range(2)]
        sh2 = [self.alloc([128, D]) for _ in range(2)]
        nbuf = self.norm_bufs()
        h2 = [self.alloc([128, D]) for _ in range(2)]
        h16 = [self.alloc([128, D], BF16) for _ in range(2)]
        h2T = [self.alloc([128, KC, 128]) for _ in range(2)]
        lg = self.alloc([128, 36])
        sm = {n: self.alloc([128, 8]) for n in ("gmx", "ohg", "ex", "le", "le2", "o1", "o2", "t8")}
        sc1_ = {n: self.alloc([128, 1]) for n in ("gsum", "gw", "m1", "m2", "dm", "den")}
        sel = self.alloc([128, NE])
        sel16 = self.alloc([128, NE], BF16)
        pf = self.alloc([128, NE])
        tmp32 = self.alloc([128, NE])
        self._memset(run, 0.0)
        cur_s = None
        for ti, (src, skey, r0, s) in enumerate(tiles):
            if s != cur_s:
                cur_s = s
                cb = s % 2
                self.load_rows_bc(sc2[cb], self.MOD[idx, 3, s:s + 1, :], "mod%d" % idx)
                self.load_rows_bc(sh2[cb], self.MOD[idx, 4, s:s + 1, :], "mod%d" % idx, eng="act")
            xt, rs, i = self.rms_tile(nbuf, self.dv(src[r0:r0 + 128, :], (skey, r0)))
            hh, hb, hT_ = h2[ti % 2], h16[ti % 2], h2T[ti % 2]
            S.I("dve", "scalar_tensor_tensor", out=hh, in0=xt, scalar=rs, in1=sc2[cb], op0=ALU.mult, op1=ALU.mult)
            S.I("pool", "tensor_tensor", out=hh, in0=hh, in1=sh2[cb], op=ALU.add)
            S.I("act", "activation", out=hb, in_=hh, func=AF.Identity)
            S.dma("sp", self.dv(self.H2[ti * 128:(ti + 1) * 128, :], ("h2", ti)), hb)
            for k in range(KC):
                pt = P[1 + (k // 4) % 2]
                S.I("pe", "transpose", out=pt[:, (k % 4) * 128:(k % 4 + 1) * 128], in_=hh[:, k * 128:(k + 1) * 128], identity=self.identf)
                if k % 4 == 3:
                    S.I("act", "activation", out=hT_[:, k - 3:k + 1, :], in_=pt.re("p (k t) -> p k t", k=4), func=AF.Identity)
            for k in range(KC):
                S.I("pe", "matmul", out=P[3][:, 0:36], lhsT=hT_[:, k, :], rhs=wr[:, k, :], start=(k == 0), stop=(k == KC - 1))
            S.I("dve", "tensor_tensor", out=lg, in0=P[3][:, 0:36], in1=brb, op=ALU.add)
            gmx, ohg, ex, le, le2, o1, o2, t8 = (sm[n] for n in ("gmx", "ohg", "ex", "le", "le2", "o1", "o2", "t8"))
            S.I("dve", "reduce_max", out=gmx[:, 0:1], in_=lg[:, 0:4], axis=AX.X)
            S.I("dve", "tensor_single_scalar", out=ohg[:, 0:4], in_=lg[:, 0:4], scalar=gmx[:, 0:1], op=ALU.is_equal)
            S.I("dve", "tensor_single_scalar", out=ex[:, 0:4], in_=lg[:, 0:4], scalar=gmx[:, 0:1], op=ALU.subtract)
            S.I("act", "activation", out=ex[:, 0:4], in_=ex[:, 0:4], func=AF.Exp)
            S.I("dve", "reduce_sum", out=sc1_["gsum"], in_=ex[:, 0:4], axis=AX.X)
            S.I("dve", "reciprocal", out=sc1_["gw"], in_=sc1_["gsum"])
            S.I("dve", "tensor_single_scalar", out=le, in_=lg[:, 4:12], scalar=ohg[:, 0:1], op=ALU.mult)
            for g in range(1, 4):
                S.I("dve", "scalar_tensor_tensor", out=le, in0=lg[:, 4 + 8 * g:12 + 8 * g], scalar=ohg[:, g:g + 1], in1=le, op0=ALU.mult, op1=ALU.add)
            S.I("dve", "reduce_max", out=sc1_["m1"], in_=le, axis=AX.X)
            S.I("dve", "tensor_single_scalar", out=o1, in_=le, scalar=sc1_["m1"], op=ALU.is_equal)
            S.I("dve", "scalar_tensor_tensor", out=le2, in0=o1, scalar=-1e30, in1=le, op0=ALU.mult, op1=ALU.add)
            S.I("dve", "reduce_max", out=sc1_["m2"], in_=le2, axis=AX.X)
            S.I("dve", "tensor_single_scalar", out=o2, in_=le2, scalar=sc1_["m2"], op=ALU.is_equal)
            S.I("dve", "tensor_tensor", out=sc1_["dm"], in0=sc1_["m2"], in1=sc1_["m1"], op=ALU.subtract)
            S.I("act", "activation", out=sc1_["dm"], in_=sc1_["dm"], func=AF.Exp)
            S.I("dve", "tensor_single_scalar", out=sc1_["den"], in_=sc1_["dm"], scalar=1.0, op=ALU.add)
            S.I("dve", "reciprocal", out=sc1_["den"], in_=sc1_["den"])
            S.I("dve", "tensor_tensor", out=W1[:, ti:ti + 1], in0=sc1_["gw"], in1=sc1_["den"], op=ALU.mult)
            S.I("dve", "tensor_tensor", out=W2[:, ti:ti + 1], in0=sc1_["gw"], in1=W1[:, ti:ti + 1], op=ALU.subtract)
            o1g = oh1[:, ti, :].re("p (g e) -> p g e", g=4)
            o2g = oh2[:, ti, :].re("p (g e) -> p g e", g=4)
            S.I("dve", "tensor_tensor", out=o1g, in0=ohg[:, 0:4].un(2).bc([128, 4, 8]), in1=o1.un(1).bc([128, 4, 8]), op=ALU.mult)
            S.I("dve", "tensor_tensor", out=o2g, in0=ohg[:, 0:4].un(2).bc([128, 4, 8]), in1=o2.un(1).bc([128, 4, 8]), op=ALU.mult)
            S.I("dve", "tensor_tensor", out=sel, in0=oh1[:, ti, :], in1=oh2[:, ti, :], op=ALU.add)
            S.I("dve", "tensor_copy", out=sel16, in_=sel)
            S.I("pe", "matmul", out=P[4][:, 0:NE], lhsT=self.ltri, rhs=sel16, start=True, stop=True)
            S.I("pe", "matmul", out=P[5][:, 0:NE], lhsT=self.ones, rhs=sel16, start=True, stop=True)
            S.I("dve", "tensor_tensor", out=pf, in0=P[4][:, 0:NE], in1=run, op=ALU.add)
            S.I("dve", "tensor_tensor", out=run, in0=P[5][:, 0:NE], in1=run, op=ALU.add)
            S.I("dve", "tensor_tensor", out=tmp32, in0=pf, in1=oh1[:, ti, :], op=ALU.mult)
            S.I("dve", "reduce_sum", out=L1[:, ti:ti + 1], in_=tmp32, axis=AX.X)
            S.I("dve", "tensor_tensor", out=tmp32, in0=pf, in1=oh2[:, ti, :], op=ALU.mult)
            S.I("dve", "reduce_sum", out=L2[:, ti:ti + 1], in_=tmp32, axis=AX.X)

        self.dbg("lg", lg); self.dbg("hh", h2[(NT - 1) % 2]); self.dbg("hT", h2T[(NT - 1) % 2]); self.dbg("sm_ohg", sm["ohg"]); self.dbg("sm_le", sm["le"])
        self.dbg("sm_o1", sm["o1"]); self.dbg("sm_o2", sm["o2"]); self.dbg("sm_le2", sm["le2"]); self.dbg("m1", sc1_["m1"]); self.dbg("m2", sc1_["m2"]); self.dbg("oh1A", oh1); self.dbg("oh2A", oh2); self.dbg("sel", sel); self.dbg("pf", pf); self.dbg("runA", run); self.dbg("wr", wr)
        if 'moeB' in SKIP:
            return
        self.S.barrier()
        self.aoff = keep
        cnti = self.alloc([128, NE], I32)
        pad = self.alloc([128, NE])
        incl = self.alloc([128, NE])
        basee = self.alloc([128, NE])
        onesf = self.alloc([128, NE])
        big3 = self.alloc([128, NT, NE])
        sf = self.alloc([128, NT])
        sgrid = self.alloc([128, NST])
        cmp3 = self.alloc([128, NST, NE])
        ef = self.alloc([128, NST])
        S.I("dve", "tensor_copy", out=cnti, in_=run)
        S.I("dve", "tensor_single_scalar", out=cnti, in_=cnti, scalar=TS - 1, op=ALU.add)
        sh = TS.bit_length() - 1
        S.I("dve", "tensor_scalar", out=cnti, in0=cnti, scalar1=sh, scalar2=sh, op0=ALU.arith_shift_right, op1=ALU.logical_shift_left)
        S.I("dve", "tensor_copy", out=pad, in_=cnti)
        self._memset(onesf, 1.0)
        S.I("dve", "tensor_tensor_scan", out=incl, data0=onesf, data1=pad, initial=0.0, op0=ALU.mult, op1=ALU.add)
        S.I("dve", "tensor_tensor", out=basee, in0=incl, in1=pad, op=ALU.subtract)
        for (oh, L, sl) in ((oh1, L1, sl1), (oh2, L2, sl2)):
            S.I("dve", "tensor_tensor", out=big3, in0=oh, in1=basee.un(1).bc([128, NT, NE]), op=ALU.mult)
            S.I("dve", "reduce_sum", out=sf, in_=big3, axis=AX.X)
            S.I("dve", "tensor_tensor", out=sf, in0=sf, in1=L, op=ALU.add)
            S.I("dve", "tensor_copy", out=sl, in_=sf)
        S.I("pool", "iota", out=sgrid, pattern=[[TS, NST]], base=0, channel_multiplier=0, allow_small_or_imprecise_dtypes=True)
        S.I("dve", "tensor_tensor", out=cmp3, in0=incl.un(1).bc([128, NST, NE]), in1=sgrid.un(2).bc([128, NST, NE]), op=ALU.is_le)
        S.I("dve", "reduce_sum", out=ef, in_=cmp3, axis=AX.X)
        S.I("dve", "tensor_single_scalar", out=ef, in_=ef, scalar=float(NE - 1), op=ALU.min)
        same = self.alloc([128, NST])
        self._memset(same, 0.0)
        S.I("dve", "tensor_tensor", out=same[:, 2:NST], in0=ef[:, 2:NST], in1=ef[:, 0:NST - 2], op=ALU.is_equal)
        S.I("dve", "tensor_single_scalar", out=same, in_=same, scalar=float(1 << 20), op=ALU.mult)
        S.I("dve", "tensor_single_scalar", out=ef, in_=ef, scalar=128.0, op=ALU.mult)
        S.I("dve", "tensor_single_scalar", out=ef, in_=ef, scalar=self.iota_p, op=ALU.add)
        ef2 = self.alloc([128, NST])
        for a in range(4):
            S.I("dve", "tensor_scalar", out=ef2, in0=ef, scalar1=4.0, scalar2=float(a), op0=ALU.mult, op1=ALU.add)
            S.I("dve", "tensor_tensor", out=ef2, in0=ef2, in1=same, op=ALU.add)
            S.I("dve", "tensor_copy", out=widx[:, a, :], in_=ef2)
        for a in range(2):
            S.I("dve", "tensor_scalar", out=ef2, in0=ef, scalar1=2.0, scalar2=float(a), op0=ALU.mult, op1=ALU.add)
            S.I("dve", "tensor_tensor", out=ef2, in0=ef2, in1=same, op=ALU.add)
            S.I("dve", "tensor_copy", out=widx[:, 4 + a, :], in_=ef2)
        self.dbg("W1", W1); self.dbg("W2", W2); self.dbg("L1", L1); self.dbg("L2", L2); self.dbg("run", run)
        self.dbg("sl1", sl1, I32); self.dbg("sl2", sl2, I32); self.dbg("widx", widx, I32); self.dbg("oh1", oh1); self.dbg("oh2", oh2)
        self.dbg("incl", incl); self.dbg("basee", basee)
        hl = [self.alloc([128, D], BF16) for _ in range(4)]
        h2s_v = self.dv(self.H2S[0:nslot, :], "h2s")
        for ti in range(NT):
            t = hl[ti % 4]
            S.dma("sp", t, self.dv(self.H2[ti * 128:(ti + 1) * 128, :], ("h2", ti)))
            S.scatter(h2s_v, t, sl1[:, ti:ti + 1])
            S.scatter(h2s_v, t, sl2[:, ti:ti + 1])

        if 'moeC' in SKIP:
            return
        self.S.barrier()
        self.aoff = keep
        w13t = [self.alloc([128, KC * D], BF16) for _ in range(2)]
        w2t = [self.alloc([128, 4 * D], BF16) for _ in range(2)]
        hs = [self.alloc([128, 2, D], BF16) for _ in range(2)]
        hT = [self.alloc([128, KC, TS], BF16) for _ in range(2)]
        sa = [self.alloc([128, TS]) for _ in range(2)]
        u16 = [self.alloc([128, 4, TS], BF16) for _ in range(2)]
        yo = [self.alloc([128, D]) for _ in range(2)]
        w13v = self.dv(w["w13"], "w13_%d" % li)
        w2v = self.dv(w["w2"], "w2_%d" % li)
        yi = 0
        for s_ in range(NST):
            wa, wb, hsl, hTt, ut = w13t[s_ % 2], w2t[s_ % 2], hs[s_ % 2], hT[s_ % 2], u16[s_ % 2]
            for a in range(4):
                S.gather(wa[:, a * 2048:(a + 1) * 2048], w13v, widx[:, a, s_:s_ + 1], bound=NE * 128 * 4 - 1)
            for a in range(2):
                S.gather(wb[:, a * 2048:(a + 1) * 2048], w2v, widx[:, 4 + a, s_:s_ + 1], bound=NE * 128 * 2 - 1)
            S.dma("sp", hsl, self.dv(self.H2S[s_ * TS:(s_ + 1) * TS, :].rearrange("(a p) d -> p a d", p=128), "h2s"))
            for a in range(2):
                pt = P[a].bitcast(BF16)
                for k in range(KC):
                    S.I("pe", "transpose", out=pt[:, k * 128:(k + 1) * 128], in_=hsl[:, a, k * 128:(k + 1) * 128], identity=self.ident)
                S.I("act" if a == 0 else "dve", "activation" if a == 0 else "tensor_copy", out=hTt[:, :, a * 128:(a + 1) * 128],
                    in_=pt.re("p (k t) -> p k t", k=KC), **({"func": AF.Copy} if a == 0 else {}))
            for m in range(4):
                pa, pg = P[2 + m % 2], P[4 + m % 2]
                for (pp, off) in ((pa, 0), (pg, DE)):
                    for k in range(KC):
                        c0 = k * D + off + m * 128
                        S.I("pe", "matmul", out=pp[:, 0:TS], lhsT=wa[:, c0:c0 + 128], rhs=hTt[:, k, :], start=(k == 0), stop=(k == KC - 1))
                sat = sa[m % 2]
                S.I("act", "activation", out=sat, in_=pa[:, 0:TS], func=AF.Silu)
                S.I("dve", "tensor_tensor", out=ut[:, m, :], in0=pg[:, 0:TS], in1=sat, op=ALU.mult)
            for a in range(2):
                yt = yo[yi % 2]
                yi += 1
                for h_ in range(2):
                    pp = P[6 + h_]
                    for k in range(4):
                        S.I("pe", "matmul", out=pp, lhsT=ut[:, k, a * 128:(a + 1) * 128], rhs=wb[:, k * D + h_ * 512:k * D + (h_ + 1) * 512],
                            start=(k == 0), stop=(k == 3))
                    S.I("act" if h_ == 0 else "dve", "activation" if h_ == 0 else "tensor_copy", out=yt[:, h_ * 512:(h_ + 1) * 512], in_=pp,
                        **({"func": AF.Copy} if h_ == 0 else {}))
                r0 = s_ * TS + a * 128
                S.dma("sp", self.dv(self.YS[r0:r0 + 128, :], "ys"), yt, disjoint=True)

        if 'moeD' in SKIP:
            return
        self.S.barrier()
        self.aoff = keep
        g2 = [self.alloc([128, D]) for _ in range(2)]
        y1 = [self.alloc([128, D]) for _ in range(2)]
        y2 = [self.alloc([128, D]) for _ in range(2)]
        xt2 = [self.alloc([128, D]) for _ in range(2)]
        acc = [self.alloc([128, D]) for _ in range(2)]
        ysv = self.dv(self.YS[0:nslot, :], "ys")
        cur_s = None
        for ti, (src, skey, r0, s) in enumerate(tiles):
            if s != cur_s:
                cur_s = s
                cb = s % 2
                self.load_rows_bc(g2[cb], self.MOD[idx, 5, s:s + 1, :], "mod%d" % idx)
            a1, a2, xx, ac = y1[ti % 2], y2[ti % 2], xt2[ti % 2], acc[ti % 2]
            S.gather(a1, ysv, sl1[:, ti:ti + 1])
            S.gather(a2, ysv, sl2[:, ti:ti + 1])
            S.dma("sp", xx, self.dv(src[r0:r0 + 128, :], (skey, r0)))
            S.I("act", "activation", out=ac, in_=a1, func=AF.Identity, scale=W1[:, ti:ti + 1])
            S.I("dve", "scalar_tensor_tensor", out=ac, in0=a2, scalar=W2[:, ti:ti + 1], in1=ac, op0=ALU.mult, op1=ALU.add)
            S.I("pool", "tensor_tensor", out=ac, in0=ac, in1=g2[cb], op=ALU.mult)
            S.I("dve", "tensor_tensor", out=ac, in0=ac, in1=xx, op=ALU.add)
            S.dma("sp", self.dv(src[r0:r0 + 128, :], (skey, r0)), ac)

    def final_norm(self):
        S, nb = self.S, self.nb
        self.arena_reset()
        nbuf = self.norm_bufs()
        fg = self.alloc([128, D])
        self.load_rows_bc(fg, self.final_g, "final_g")
        ob = [self.alloc([128, D]) for _ in range(2)]
        for ti in range(nb * SEQ // 128):
            r0 = ti * 128
            xt, rs, i = self.rms_tile(nbuf, self.dv(self.xs[r0:r0 + 128, :], ("xs", r0)))
            o = ob[ti % 2]
            S.I("dve", "scalar_tensor_tensor", out=o, in0=xt, scalar=rs, in1=fg, op0=ALU.mult, op1=ALU.mult)
            S.dma("sp", self.dv(self.y_out[r0:r0 + 128, :], ("y", r0)), o)


def prep_weights(inp, layers):
    out = {}
    f = lambda a: np.ascontiguousarray(a, dtype=np.float32)
    out["c_ctx"] = f(inp["c_ctx"]).reshape(1, D)
    out["final_g"] = f(inp["final_g"]).reshape(1, D)
    for li in layers:
        kind, j = li % 3, li // 3
        out["ada_w_%d" % li] = f(inp["ada_w"][li])
        out["ada_b_%d" % li] = f(inp["ada_b"][li]).reshape(1, -1)
        out["norm1_g_%d" % li] = f(inp["norm1_g"][li]).reshape(1, D)
        out["norm2_g_%d" % li] = f(inp["norm2_g"][li]).reshape(1, D)
        out["moe_wr_%d" % li] = f(np.concatenate([inp["moe_wg"][li], inp["moe_we"][li]], axis=1))
        out["moe_br_%d" % li] = f(np.concatenate([inp["moe_bg"][li], inp["moe_be"][li]], axis=0)).reshape(1, 36)
        w13 = np.asarray(inp["moe_w13"][li]).reshape(NE, KC, 128, D).transpose(0, 2, 1, 3).reshape(NE * 128 * 4, 2048)
        out["moe_w13_%d" % li] = f(w13)
        w2 = np.asarray(inp["moe_w2"][li]).reshape(NE, 4, 128, D).transpose(0, 2, 1, 3).reshape(NE * 128 * 2, 2048)
        out["moe_w2_%d" % li] = f(w2)
        if kind == 0:
            out["conf_w_in_%d" % li] = f(inp["conf_w_in"][j])
            out["conf_dw_%d" % li] = f(inp["conf_dw"][j])
            out["conf_dw_b_%d" % li] = f(inp["conf_dw_b"][j]).reshape(1, D)
            out["conf_ln_g_%d" % li] = f(inp["conf_ln_g"][j]).reshape(1, D)
            out["conf_ln_b_%d" % li] = f(inp["conf_ln_b"][j]).reshape(1, D)
            out["conf_w_out_%d" % li] = f(inp["conf_w_out"][j])
        elif kind == 1:
            out["sc_w_in_%d" % li] = f(inp["sc_w_in"][j])
            out["sc_conv_%d" % li] = f(inp["sc_conv"][j])
            out["sc_w_out_%d" % li] = f(inp["sc_w_out"][j])
        else:
            for n in ("a_re", "a_im", "b_re", "b_im", "c_re", "c_im"):
                out["s5_%s_%d" % (n, li)] = f(inp["s5_" + n][j])
            out["s5_log_dt_%d" % li] = f(inp["s5_log_dt"][j])
            out["s5_d_%d" % li] = f(inp["s5_d"][j]).reshape(1, D)
            out["s5_w_glu_%d" % li] = f(inp["s5_w_glu"][j])
    return out


def run(inp, nb, ncores, layers, final=True, trace=False):
    prog = Prog(nb, layers, final)
    nc = prog.build()
    shared = prep_weights(inp, layers)
    x = np.asarray(inp["x"], dtype=np.float32)
    c = np.asarray(inp["c"], dtype=np.float32)
    ctx = np.asarray(inp["ctx"], dtype=np.float32)
    in_maps = []
    for k in range(ncores):
        m = dict(shared)
        m["x"] = np.ascontiguousarray(x[k * nb:(k + 1) * nb]).reshape(nb * SEQ, D)
        m["c"] = np.ascontiguousarray(c[k * nb:(k + 1) * nb])
        m["ctx"] = np.ascontiguousarray(ctx[k * nb:(k + 1) * nb]).reshape(nb * CTX, D)
        in_maps.append({n: m[n] for n in prog.in_names})
    res = run_bass_kernel_spmd(nc, in_maps, core_ids=list(range(ncores)), **({"trace": True} if trace else {}))
    y = np.concatenate([r["y"].reshape(nb, SEQ, D) for r in res.results], axis=0)
    return y, res


def kernel(**inputs):
    y, _ = run(inputs, nb=4, ncores=NCORES, layers=list(range(DEPTH)), final=True)
    return y.astype(np.float32)
```

```python
import contextlib
import numpy as np
import concourse.bass as bass
import concourse.mybir as mybir
from concourse.bass_utils import run_bass_kernel_spmd

F32 = mybir.dt.float32
BF16 = mybir.dt.bfloat16
I32 = mybir.dt.int32
AF = mybir.ActivationFunctionType
ALU = mybir.AluOpType
AX = mybir.AxisListType

D = 1024
KC = 8
SEQ = 2048
CTX = 256
NE = 32
DE = 512
TS = 256
RMS_EPS = 1e-6
LN_EPS = 1e-5
DEPTH = 4
NCORES = 8
SKIP = set()
DEBUG = False


class Buf:
    __slots__ = ("w", "wx", "r")

    def __init__(self):
        self.w = {}
        self.wx = {}
        self.r = {}


class V:
    __slots__ = ("ap", "buf")

    def __init__(self, ap, buf=None):
        self.ap = ap
        self.buf = buf if buf is not None else Buf()

    def __getitem__(self, k):
        return V(self.ap[k], self.buf)

    def re(self, pat, **kw):
        return V(self.ap.rearrange(pat, **kw), self.buf)

    def bc(self, shape):
        return V(self.ap.to_broadcast(list(shape)), self.buf)

    def un(self, axis):
        return V(self.ap.unsqueeze(axis), self.buf)

    def bitcast(self, dt):
        return V(self.ap.bitcast(dt), self.buf)

    def rev(self):
        a = list(self.ap.ap)
        s, c = a[-1]
        a[-1] = [-s, c]
        return V(bass.AP(self.ap.tensor, self.ap.offset + s * (c - 1), [list(x) for x in a]), self.buf)


def _merge(dst, src):
    for k, v in src.items():
        if dst.get(k, 0) < v:
            dst[k] = v


class Sched:
    ENG = ("pe", "act", "dve", "pool", "sp")
    NDS = {"sp": 16, "act": 6, "pool": 16}

    def __init__(self):
        self.ops = {e: [] for e in self.ENG}
        self.cnt = {e: 0 for e in self.ENG}
        self.waited = {e: {} for e in self.ENG}
        self.dnext = {e: 0 for e in self.NDS}
        self.dval = {e: [0] * n for e, n in self.NDS.items()}
        self.n_ops = 0
        self.pool_consts = set()
        self.regvals = {}

    def _emit(self, eng, fn, reads, writes, dma=False, disjoint=False, sreads=()):
        need = {}
        own = 0
        for b in reads:
            _merge(need, b.w)
        if eng != "pe":
            own = need.get(("c", eng), 0)
        for b in writes:
            _merge(need, b.r)
            _merge(need, b.wx if disjoint else b.w)
        if dma:
            j = self.dnext[eng]
            self.dnext[eng] = (j + 1) % self.NDS[eng]
            key = ("d", eng, j)
            if self.dval[eng][j] > 0:
                need[key] = max(need.get(key, 0), self.dval[eng][j])
            self.dval[eng][j] += 16
            ev = (key, self.dval[eng][j])
            inc = 16
        else:
            need.pop(("c", eng), None)
            if own > 0:
                need[("c", eng)] = own
            self.cnt[eng] += 1
            key = ("c", eng)
            ev = (key, self.cnt[eng])
            inc = 1
        wl = []
        wd = self.waited[eng]
        for k, v in need.items():
            if wd.get(k, 0) < v:
                wd[k] = v
                wl.append((k, v))
        self.ops[eng].append((wl, fn, key, inc))
        self.n_ops += 1
        for b in reads:
            if b.r.get(ev[0], 0) < ev[1]:
                b.r[ev[0]] = ev[1]
        for b in writes:
            if disjoint:
                b.w[ev[0]] = ev[1]
            else:
                b.w = {ev[0]: ev[1]}
                b.wx = {ev[0]: ev[1]}
                b.r = {}

    def I(self, eng, meth, disjoint=False, **kw):
        reads, writes, sreads, res = [], [], [], {}
        for k, v in kw.items():
            if isinstance(v, V):
                (writes if k in ("out", "accum_out") else reads).append(v.buf)
                if k in ("scalar", "scalar1", "scalar2", "scale", "bias", "initial"):
                    sreads.append(v.buf)
                res[k] = v.ap
            else:
                res[k] = v
        self._emit(eng, lambda e: getattr(e, meth)(**res), reads, writes, disjoint=disjoint, sreads=sreads)

    def dma(self, eng, out, in_, disjoint=False, **kw):
        o, i = out.ap, in_.ap
        self._emit(eng, lambda e: e.dma_start(out=o, in_=i, **kw), [in_.buf], [out.buf], dma=True, disjoint=disjoint)

    def gather(self, out, src, idx, bound=None):
        o, i, x = out.ap, src.ap, idx.ap
        if bound is None:
            fn = lambda e: e.indirect_dma_start(out=o, out_offset=None, in_=i, in_offset=bass.IndirectOffsetOnAxis(ap=x, axis=0))
        else:
            self.pool_consts.add(bound)
            fn = lambda e: e.indirect_dma_start(out=o, out_offset=None, in_=i, in_offset=bass.IndirectOffsetOnAxis(ap=x, axis=0),
                                                bounds_check=self.regvals[bound], oob_is_err=False)
        self._emit("pool", fn, [src.buf, idx.buf], [out.buf], dma=True)

    def scatter(self, out, src, idx, **kw):
        o, i, x = out.ap, src.ap, idx.ap
        self._emit("pool", lambda e: e.indirect_dma_start(out=o, out_offset=bass.IndirectOffsetOnAxis(ap=x, axis=0),
                                                          in_=i, in_offset=None, **kw),
                   [src.buf, idx.buf], [out.buf], dma=True, disjoint=True)

    def barrier(self):
        allv = {("c", e): self.cnt[e] for e in self.ENG if self.cnt[e] > 0}
        for e, vals in self.dval.items():
            for j, v in enumerate(vals):
                if v > 0:
                    allv[("d", e, j)] = v
        for eng in self.ENG:
            wl = []
            wd = self.waited[eng]
            for k, v in allv.items():
                if k == ("c", eng):
                    continue
                if wd.get(k, 0) < v:
                    wd[k] = v
                    wl.append((k, v))
            if wl:
                self.ops[eng].append((wl, None, None, 0))

    def replay(self, nc, stack):
        sems = {}
        for e in self.ENG:
            sems[("c", e)] = stack.enter_context(nc.semaphore("c_" + e))
        for e, n in self.NDS.items():
            for j in range(n):
                sems[("d", e, j)] = stack.enter_context(nc.semaphore("d_%s_%d" % (e, j)))
        block = stack.enter_context(nc.Block())
        ops = self.ops

        def run(name, e):
            for wl, fn, key, inc in ops[name]:
                for k, v in wl:
                    e.wait_ge(sems[k], v)
                if fn is not None:
                    fn(e).then_inc(sems[key], inc)

        @block.tensor
        def _(e):
            run("pe", e)

        @block.scalar
        def _(e):
            run("act", e)

        @block.vector
        def _(e):
            run("dve", e)

        @block.gpsimd
        def _(e):
            for val in sorted(self.pool_consts):
                r = e.alloc_register("bc%d" % val)
                e.reg_mov(r, val)
                self.regvals[val] = e.snap(r)
            run("pool", e)

        @block.sync
        def _(e):
            run("sp", e)


class Prog:
    def __init__(self, nb, layers, final=True):
        self.nb = nb
        self.layers = list(layers)
        self.final = final
        self.S = Sched()
        self.nc = bass.Bass("TRN2", target_bir_lowering=False)
        self.stack = contextlib.ExitStack()
        self.dbufs = {}
        self.in_names = []

    def dram_in(self, name, shape, dt=F32):
        self.in_names.append(name)
        return self.nc.dram_tensor(name, list(shape), dt, kind="ExternalInput").ap()

    def dram_tmp(self, name, shape, dt=F32):
        return self.nc.dram_tensor(name, list(shape), dt, kind="Internal").ap()

    def dv(self, ap, key):
        b = self.dbufs.get(key)
        if b is None:
            b = self.dbufs[key] = Buf()
        return V(ap, b)

    def arena_reset(self):
        self.S.barrier()
        self.aoff = self.abase

    def alloc(self, shape, dt=F32):
        n = 1
        for s in shape[1:]:
            n *= s
        words = n if dt in (F32, I32) else (n + 1) // 2
        words = (words + 7) // 8 * 8
        assert self.aoff + words <= self.awords, ("SBUF arena overflow", self.aoff, words, self.awords)
        ap = self.big[:, self.aoff:self.aoff + words]
        self.aoff += words
        if dt != F32:
            ap = ap.bitcast(dt)
        ap = ap[0:shape[0], 0:n]
        if len(shape) > 2:
            names = " ".join("d%d" % i for i in range(len(shape) - 1))
            kw = {"d%d" % i: shape[i + 1] for i in range(len(shape) - 1)}
            ap = ap.rearrange("p (%s) -> p %s" % (names, names), **kw)
        return V(ap)

    def perm(self, shape, dt=F32):
        v = self.alloc(shape, dt)
        self.abase = self.aoff
        return v

    def build(self):
        nc, S, nb = self.nc, self.S, self.nb
        st = self.stack
        self.awords = 53000
        self.big = st.enter_context(nc.sbuf_tensor("big", [128, self.awords], F32))
        self.aoff = 0
        self.abase = 0
        self.psum = [V(st.enter_context(nc.psum_tensor("ps%d" % i, [128, 512], F32))[:, :]) for i in range(8)]
        nl = len(self.layers)
        nlat, nctx = nb * SEQ, nb * CTX

        self.x_in = self.dram_in("x", [nlat, D])
        self.c_in = self.dram_in("c", [nb, D])
        self.ctx_in = self.dram_in("ctx", [nctx, D])
        self.cctx_in = self.dram_in("c_ctx", [1, D])
        self.final_g = self.dram_in("final_g", [1, D])
        self.W = {}
        for li in self.layers:
            kind = li % 3
            w = {}
            w["ada_w"] = self.dram_in("ada_w_%d" % li, [D, 6 * D])
            w["ada_b"] = self.dram_in("ada_b_%d" % li, [1, 6 * D])
            w["n1"] = self.dram_in("norm1_g_%d" % li, [1, D])
            w["n2"] = self.dram_in("norm2_g_%d" % li, [1, D])
            w["wr"] = self.dram_in("moe_wr_%d" % li, [D, 36])
            w["br"] = self.dram_in("moe_br_%d" % li, [1, 36])
            w["w13"] = self.dram_in("moe_w13_%d" % li, [NE * 128 * 4, 2048])
            w["w2"] = self.dram_in("moe_w2_%d" % li, [NE * 128 * 2, 2048])
            if kind == 0:
                w["w_in"] = self.dram_in("conf_w_in_%d" % li, [D, 2 * D])
                w["dw"] = self.dram_in("conf_dw_%d" % li, [31, D])
                w["dw_b"] = self.dram_in("conf_dw_b_%d" % li, [1, D])
                w["ln_g"] = self.dram_in("conf_ln_g_%d" % li, [1, D])
                w["ln_b"] = self.dram_in("conf_ln_b_%d" % li, [1, D])
                w["w_out"] = self.dram_in("conf_w_out_%d" % li, [D, D])
            elif kind == 1:
                w["w_in"] = self.dram_in("sc_w_in_%d" % li, [D, 3 * D])
                w["cv"] = self.dram_in("sc_conv_%d" % li, [3, D])
                w["w_out"] = self.dram_in("sc_w_out_%d" % li, [D, D])
            else:
                w["a_re"] = self.dram_in("s5_a_re_%d" % li, [2, 64, 64])
                w["a_im"] = self.dram_in("s5_a_im_%d" % li, [2, 64, 64])
                w["ldt"] = self.dram_in("s5_log_dt_%d" % li, [2, 64])
                w["b_re"] = self.dram_in("s5_b_re_%d" % li, [2, 64, 64, 16])
                w["b_im"] = self.dram_in("s5_b_im_%d" % li, [2, 64, 64, 16])
                w["c_re"] = self.dram_in("s5_c_re_%d" % li, [2, 64, 16, 64])
                w["c_im"] = self.dram_in("s5_c_im_%d" % li, [2, 64, 16, 64])
                w["d"] = self.dram_in("s5_d_%d" % li, [1, D])
                w["w_glu"] = self.dram_in("s5_w_glu_%d" % li, [D, 2 * D])
            self.W[li] = w
        self.y_out = nc.dram_tensor("y", [nlat, D], F32, kind="ExternalOutput").ap()

        self.xs = self.dram_tmp("xs", [nlat, D])
        self.cs = self.dram_tmp("cs", [nctx, D])
        self.MOD = self.dram_tmp("modv", [nl, 6, nb + 1, D])
        ntok_max = nlat + nctx
        self.nslot_max = ((2 * ntok_max + NE * (TS - 1)) + TS - 1) // TS * TS
        self.H2 = self.dram_tmp("h2", [ntok_max, D], BF16)
        self.H2S = self.dram_tmp("h2s", [self.nslot_max, D], BF16)
        self.YS = self.dram_tmp("ys", [self.nslot_max, D])
        self.HT = self.dram_tmp("ht", [D, ntok_max], BF16)
        self.W13B = self.dram_tmp("w13b", [NE * 128 * 4, 2048], BF16)
        self.W2B = self.dram_tmp("w2b", [NE * 128 * 2, 2048], BF16)
        self.YT = self.dram_tmp("yt", [D, ntok_max], BF16)

        self.identf = self.perm([128, 128])
        self.ident = self.perm([128, 128], BF16)
        self.ltri = self.perm([128, 128], BF16)
        self.ones = self.perm([128, 128], BF16)
        self.onesm = self.perm([128, 128], BF16)
        self.eps_rms = self.perm([128, 1])
        self.eps_ln = self.perm([128, 1])
        self.iota_p = self.perm([128, 1])
        self.one_c = self.perm([128, 1])
        tmpf = self.alloc([128, 128])
        S.I("pool", "iota", out=self.identf, pattern=[[1, 128]], base=0, channel_multiplier=-1,
            allow_small_or_imprecise_dtypes=True)
        S.I("dve", "tensor_single_scalar", out=tmpf, in_=self.identf, scalar=0.0, op=ALU.is_gt)
        S.I("dve", "tensor_copy", out=self.ltri, in_=tmpf)
        S.I("dve", "tensor_single_scalar", out=self.identf, in_=self.identf, scalar=0.0, op=ALU.is_equal)
        S.I("dve", "tensor_copy", out=self.ident, in_=self.identf)
        self._memset(self.ones, 1.0)
        self._memset(self.onesm, 1.0 / 1024.0)
        self._memset(self.eps_rms, RMS_EPS)
        self._memset(self.eps_ln, LN_EPS)
        self._memset(self.one_c, 1.0)
        S.I("pool", "iota", out=self.iota_p, pattern=[[0, 1]], base=0, channel_multiplier=1,
            allow_small_or_imprecise_dtypes=True)
        self.aoff = self.abase

        self.arena_reset()
        z = self.alloc([128, 8 * D], BF16)
        self._memset(z, 0.0)
        rows = self.nslot_max
        r0 = 0
        while r0 < rows:
            n = min(1024, rows - r0)
            assert n % 128 == 0
            S.dma("sp", self.dv(self.H2S[r0:r0 + n, :].rearrange("(p a) d -> p (a d)", p=128), "h2s"),
                  z[:, 0:(n // 128) * D], disjoint=True)
            r0 += n

        self.prologue()
        first = True
        for idx, li in enumerate(self.layers):
            kind = li % 3
            upd = li < DEPTH - 1
            need_ctx = upd or kind == 2
            if "moe" not in SKIP:
                self.precast(li)
            if "mixer" in SKIP:
                self.copy_x(first, upd)
            elif kind == 0:
                self.conformer(idx, li, first, upd)
            elif kind == 1:
                self.shortconv(idx, li, first, upd)
            else:
                self.s5(idx, li, first, upd)
            first = False
            if "moe" not in SKIP:
                self.moe(idx, li, upd)
        if self.final:
            self.final_norm()
        S.barrier()
        S.replay(nc, st)
        st.close()
        return nc

    def precast(self, li):
        w = self.W[li]
        for (src, dst, key, nrows) in ((w["w13"], self.W13B, "w13b", NE * 128 * 4), (w["w2"], self.W2B, "w2b", NE * 128 * 2)):
            for r0 in range(0, nrows, 1024):
                sv = src[r0:r0 + 1024, :].rearrange("(p a) n -> p a n", p=128)
                dv_ = dst[r0:r0 + 1024, :].rearrange("(p a) n -> p a n", p=128)
                self.S.dma("pool", self.dv(dv_, key), self.dv(sv, "w_in_%s_%d" % (key, li)), disjoint=True)

    def copy_x(self, first, upd):
        self.arena_reset()
        t = [self.alloc([128, D]) for _ in range(4)]
        i = 0
        for lat in ((True, False) if upd else (True,)):
            src, skey = self.xsrc(first, lat)
            dst, dkey = self.xdst(lat)
            n = self.nb * (SEQ if lat else CTX)
            for r0 in range(0, n, 128):
                self.S.dma("sp", t[i % 4], self.dv(src[r0:r0 + 128, :], (skey, r0)))
                self.S.dma("sp", self.dv(dst[r0:r0 + 128, :], (dkey, r0)), t[i % 4])
                i += 1

    def dbg(self, name, v, dt=F32):
        if not DEBUG:
            return
        shape = list(v.ap.shape)
        o = self.nc.dram_tensor("dbg_" + name, shape, dt, kind="ExternalOutput").ap()
        self.S.dma("sp", self.dv(o, "dbg_" + name), v)

    def _memset(self, v, val):
        ap = v.ap
        self.S._emit("dve", lambda e: e.memset(ap, val), [], [v.buf])

    def xsrc(self, first, lat):
        if lat:
            return (self.x_in if first else self.xs), ("xin" if first else "xs")
        return (self.ctx_in if first else self.cs), ("cin" if first else "cs")

    def xdst(self, lat):
        return (self.xs, "xs") if lat else (self.cs, "cs")

    def load_rows_bc(self, dst, src_row_ap, key, nparts=128, eng="sp"):
        n = src_row_ap.shape[-1]
        self.S.dma(eng, dst, self.dv(src_row_ap.to_broadcast([nparts, n]), key))

    def load_featT(self, dst, row_ap, key):
        self.S.dma("sp", dst, self.dv(row_ap.rearrange("o (k p) -> p (o k)", p=128), key), allow_slow_non_contiguous=True)

    def prologue(self):
        S, nb, nc = self.S, self.nb, self.nc
        self.arena_reset()
        ns = nb + 1
        cT = self.alloc([128, KC, ns])
        for b in range(nb):
            self.load_featT(cT[:, :, b], self.c_in[b:b + 1, :], "c_in")
        self.load_featT(cT[:, :, nb], self.cctx_in, "cctx_in")
        S.I("act", "activation", out=cT, in_=cT, func=AF.Silu)
        mrow = self.alloc([ns, 6 * D])
        abrow = self.alloc([ns, 6 * D])
        n1 = self.alloc([ns, D])
        n2 = self.alloc([ns, D])
        orow = self.alloc([ns, 6, D])
        awt = [self.alloc([128, KC, 512]) for _ in range(2)]
        for idx, li in enumerate(self.layers):
            w = self.W[li]
            self.load_rows_bc(abrow, w["ada_b"], "ada_b%d" % li, nparts=ns)
            self.load_rows_bc(n1, w["n1"], "n1_%d" % li, nparts=ns)
            self.load_rows_bc(n2, w["n2"], "n2_%d" % li, nparts=ns)
            awv = w["ada_w"].rearrange("(k p) n -> p k n", p=128)
            for cg in range(12):
                t = awt[cg % 2]
                S.dma("sp" if cg % 2 == 0 else "act", t, self.dv(awv[:, :, cg * 512:(cg + 1) * 512], "ada_w%d" % li))
                ps = self.psum[cg % 2]
                for k in range(KC):
                    S.I("pe", "matmul", out=ps[0:ns, :], lhsT=cT[:, k, :], rhs=t[:, k, :], start=(k == 0), stop=(k == KC - 1))
                S.I("dve", "tensor_tensor", out=mrow[:, cg * 512:(cg + 1) * 512], in0=ps[0:ns, :],
                    in1=abrow[:, cg * 512:(cg + 1) * 512], op=ALU.add)
            S.I("dve", "scalar_tensor_tensor", out=orow[:, 0, :], in0=mrow[:, D:2 * D], scalar=1.0, in1=n1, op0=ALU.add, op1=ALU.mult)
            S.I("dve", "tensor_copy", out=orow[:, 1, :], in_=mrow[:, 0:D])
            S.I("dve", "tensor_copy", out=orow[:, 2, :], in_=mrow[:, 2 * D:3 * D])
            S.I("dve", "scalar_tensor_tensor", out=orow[:, 3, :], in0=mrow[:, 4 * D:5 * D], scalar=1.0, in1=n2, op0=ALU.add, op1=ALU.mult)
            S.I("dve", "tensor_copy", out=orow[:, 4, :], in_=mrow[:, 3 * D:4 * D])
            S.I("dve", "tensor_copy", out=orow[:, 5, :], in_=mrow[:, 5 * D:6 * D])
            S.dma("sp", self.dv(self.MOD[idx].rearrange("k s d -> s k d"), "mod%d" % idx), orow)

    def mod_row(self, idx, kind, s):
        return self.dv(self.MOD[idx, kind, s:s + 1, :], "mod%d" % idx)

    def load_modT(self, idx):
        ns = self.nb + 1
        sT = self.alloc([128, KC, ns])
        bT = self.alloc([128, KC, ns])
        for s in range(ns):
            self.load_featT(sT[:, :, s], self.MOD[idx, 0, s:s + 1, :], "mod%d" % idx)
            self.load_featT(bT[:, :, s], self.MOD[idx, 1, s:s + 1, :], "mod%d" % idx)
        return sT, bT

    def norm_bufs(self):
        nbuf = {}
        nbuf["xt"] = [self.alloc([128, D]) for _ in range(2)]
        nbuf["sq"] = self.alloc([128, D])
        nbuf["x16"] = [self.alloc([128, D], BF16) for _ in range(2)]
        nbuf["ss"] = [self.alloc([128, 1]) for _ in range(2)]
        nbuf["rs"] = [self.alloc([128, 1]) for _ in range(2)]
        nbuf["tmp"] = self.alloc([128, KC, 128])
        nbuf["i"] = 0
        return nbuf

    def rms_tile(self, nbuf, src_v):
        S = self.S
        i = nbuf["i"] % 2
        nbuf["i"] += 1
        xt, ss, rs = nbuf["xt"][i], nbuf["ss"][i], nbuf["rs"][i]
        S.dma("sp", xt, src_v)
        S.I("act", "activation", out=nbuf["sq"], in_=xt, func=AF.Square)
        S.I("dve", "reduce_sum", out=ss, in_=nbuf["sq"], axis=AX.X)
        S.I("act", "activation", out=rs, in_=ss, func=AF.Sqrt, scale=1.0 / D, bias=self.eps_rms)
        S.I("dve", "reciprocal", out=rs, in_=rs)
        return xt, rs, i

    def norm_transpose(self, nbuf, src_v, sT, bT, s, dst):
        S = self.S
        xt, rs, i = self.rms_tile(nbuf, src_v)
        x16 = nbuf["x16"][i]
        S.I("act", "activation", out=x16, in_=xt, func=AF.Identity, scale=rs)
        pt = self.psum[0].bitcast(BF16)
        for k in range(KC):
            S.I("pe", "transpose", out=pt[:, k * 128:(k + 1) * 128], in_=x16[:, k * 128:(k + 1) * 128], identity=self.ident)
        tmp = nbuf["tmp"]
        S.I("dve", "tensor_tensor", out=tmp, in0=pt.re("p (k t) -> p k t", k=KC), in1=sT[:, :, s:s + 1].bc([128, KC, 128]), op=ALU.mult)
        S.I("dve", "tensor_tensor", out=dst, in0=tmp, in1=bT[:, :, s:s + 1].bc([128, KC, 128]), op=ALU.add)

    def seqs(self, with_ctx):
        out = [("lat", b) for b in range(self.nb)]
        if with_ctx:
            out.append(("ctx", self.nb))
        return out

    def load_w_bf16(self, dst, w_ap, key, ncols):
        wv = w_ap.rearrange("(k p) n -> p k n", p=128)
        for k in range(KC):
            for c0 in range(0, ncols, 2048):
                c1 = min(ncols, c0 + 2048)
                self.S.dma("pool", dst[:, k, c0:c1], self.dv(wv[:, k, c0:c1], key), disjoint=True)

    def residual_out(self, ps_halves, g_bc, src_v, dst_v, obuf, xbuf):
        S = self.S
        S.dma("sp", xbuf, src_v)
        for h in range(2):
            S.I("dve", "tensor_tensor", out=obuf[:, h * 512:(h + 1) * 512], in0=ps_halves[h], in1=g_bc[:, h * 512:(h + 1) * 512], op=ALU.mult)
        S.I("pool", "tensor_tensor", out=obuf, in0=obuf, in1=xbuf, op=ALU.add)
        S.dma("sp", dst_v, obuf)

    def conformer(self, idx, li, first, upd):
        S, nb, w = self.S, self.nb, self.W[li]
        self.arena_reset()
        sT, bT = self.load_modT(idx)
        w_in = self.alloc([128, KC, 2 * D], BF16)
        w_out = self.alloc([128, KC, D], BF16)
        self.load_w_bf16(w_in, w["w_in"], "cw_in%d" % li, 2 * D)
        self.load_w_bf16(w_out, w["w_out"], "cw_out%d" % li, D)
        dwT = self.alloc([128, KC, 31])
        for k in range(KC):
            S.dma("sp", dwT[:, k, :], self.dv(w["dw"][:, k * 128:(k + 1) * 128].rearrange("t p -> p t"), "dw%d" % li), allow_slow_non_contiguous=True)
        dwb = self.alloc([128, KC])
        lng = self.alloc([128, KC])
        lnb = self.alloc([128, KC])
        self.load_featT(dwb, w["dw_b"], "dwb%d" % li)
        self.load_featT(lng, w["ln_g"], "lng%d" % li)
        self.load_featT(lnb, w["ln_b"], "lnb%d" % li)
        nbuf = self.norm_bufs()
        g1 = [self.alloc([128, D]) for _ in range(2)]
        hT = [self.alloc([128, KC, 512], BF16) for _ in range(2)]
        zbuf = self.alloc([128, KC, SEQ], BF16)
        sg = [self.alloc([128, 512]) for _ in range(2)]
        dwd = [self.alloc([128, 31, 128], BF16) for _ in range(2)]
        vall = self.alloc([128, KC, SEQ], BF16)
        vsq = [self.alloc([128, 512], BF16) for _ in range(2)]
        s16 = self.alloc([128, KC, 512], BF16)
        msq = self.alloc([128, 512])
        mean = self.alloc([128, 512])
        rstd = self.alloc([128, 512])
        nmr = self.alloc([128, 512])
        t1 = [self.alloc([128, 512]) for _ in range(2)]
        obuf = [self.alloc([128, D])] * 2
        P = self.psum
        gi = 0
        di = 0
        ci2 = 0
        for si, (sk, s) in enumerate(self.seqs(upd)):
            lat = sk == "lat"
            ntok = SEQ if lat else nb * CTX
            src, skey = self.xsrc(first, lat)
            dst, dkey = self.xdst(lat)
            base = s * SEQ if lat else 0
            GS = min(512, ntok)
            ng = ntok // GS
            gb = g1[si % 2]
            self.load_rows_bc(gb, self.MOD[idx, 2, s:s + 1, :], "mod%d" % idx)
            for g in range(ng):
                h = hT[gi % 2]
                gi += 1
                for j in range(GS // 128):
                    r0 = base + g * GS + j * 128
                    self.norm_transpose(nbuf, self.dv(src[r0:r0 + 128, :], (skey, r0)), sT, bT, s, h[:, :, j * 128:(j + 1) * 128])
                for m in range(KC):
                    pv, pg = P[1 + m % 2], P[3 + m % 2]
                    for k in range(KC):
                        S.I("pe", "matmul", out=pv[:, 0:GS], lhsT=w_in[:, k, m * 128:(m + 1) * 128], rhs=h[:, k, 0:GS], start=(k == 0), stop=(k == KC - 1))
                    for k in range(KC):
                        S.I("pe", "matmul", out=pg[:, 0:GS], lhsT=w_in[:, k, D + m * 128:D + (m + 1) * 128], rhs=h[:, k, 0:GS], start=(k == 0), stop=(k == KC - 1))
                    sgt = sg[m % 2]
                    S.I("act", "activation", out=sgt[:, 0:GS], in_=pg[:, 0:GS], func=AF.Sigmoid)
                    S.I("dve", "tensor_tensor", out=zbuf[:, m, g * GS:(g + 1) * GS], in0=pv[:, 0:GS], in1=sgt[:, 0:GS], op=ALU.mult)
            order = [15] + [k for k in range(31) if k != 15]
            for m in range(KC):
                dd = dwd[di % 2]
                di += 1
                for k in range(31):
                    S.I("dve", "tensor_single_scalar", out=dd[:, k, :], in_=self.ident, scalar=dwT[:, m, k:k + 1], op=ALU.mult)
                for g in range(ng):
                    g0 = g * GS
                    pc = P[1 + ci2 % 2]
                    ci2 += 1
                    for n_, k in enumerate(order):
                        d = k - 15
                        if lat:
                            dt_ = d * 64
                            lo, hi = max(g0, -dt_), min(g0 + GS, SEQ - dt_)
                            if lo >= hi:
                                continue
                            S.I("pe", "matmul", out=pc[:, lo - g0:hi - g0], lhsT=dd[:, k, :], rhs=zbuf[:, m, lo + dt_:hi + dt_],
                                start=(n_ == 0), stop=(n_ == 30), skip_group_check=True)
                        else:
                            lo, hi = max(0, -d), min(CTX, CTX - d)
                            o = pc[:, 0:GS].re("p (s t) -> p s t", t=CTX)[:, :, lo:hi]
                            r = zbuf[:, m, g0:g0 + GS].re("p (s t) -> p s t", t=CTX)[:, :, lo + d:hi + d]
                            S.I("pe", "matmul", out=o, lhsT=dd[:, k, :], rhs=r, start=(n_ == 0), stop=(n_ == 30), skip_group_check=True)
                    S.I("act", "activation", out=vall[:, m, g0:g0 + GS], in_=pc[:, 0:GS], func=AF.Identity, bias=dwb[:, m:m + 1])
            for g in range(ng):
                g0 = g * GS
                v16 = vall[:, :, g0:g0 + GS]
                for m in range(KC):
                    vq = vsq[m % 2]
                    S.I("act", "activation", out=vq[:, 0:GS], in_=v16[:, m, :], func=AF.Square)
                    S.I("pe", "matmul", out=P[5][:, 0:GS], lhsT=self.onesm, rhs=v16[:, m, :], start=(m == 0), stop=(m == KC - 1))
                    S.I("pe", "matmul", out=P[6][:, 0:GS], lhsT=self.onesm, rhs=vq[:, 0:GS], start=(m == 0), stop=(m == KC - 1))
                S.I("act", "activation", out=mean[:, 0:GS], in_=P[5][:, 0:GS], func=AF.Identity)
                S.I("act", "activation", out=msq[:, 0:GS], in_=P[5][:, 0:GS], func=AF.Square)
                S.I("dve", "tensor_tensor", out=rstd[:, 0:GS], in0=P[6][:, 0:GS], in1=msq[:, 0:GS], op=ALU.subtract)
                S.I("act", "activation", out=rstd[:, 0:GS], in_=rstd[:, 0:GS], func=AF.Sqrt, bias=self.eps_ln)
                S.I("dve", "reciprocal", out=rstd[:, 0:GS], in_=rstd[:, 0:GS])
                S.I("dve", "scalar_tensor_tensor", out=nmr[:, 0:GS], in0=mean[:, 0:GS], scalar=-1.0, in1=rstd[:, 0:GS], op0=ALU.mult, op1=ALU.mult)
                for m in range(KC):
                    tt = t1[m % 2]
                    S.I("dve", "tensor_tensor", out=tt[:, 0:GS], in0=v16[:, m, :], in1=rstd[:, 0:GS], op=ALU.mult)
                    S.I("pool", "tensor_tensor", out=tt[:, 0:GS], in0=tt[:, 0:GS], in1=nmr[:, 0:GS], op=ALU.add)
                    S.I("act", "activation", out=s16[:, m, 0:GS], in_=tt[:, 0:GS], func=AF.Silu, scale=lng[:, m:m + 1], bias=lnb[:, m:m + 1])
                for j in range(GS // 128):
                    r0 = base + g0 + j * 128
                    for h_ in range(2):
                        for k in range(KC):
                            S.I("pe", "matmul", out=P[3 + h_], lhsT=s16[:, k, j * 128:(j + 1) * 128], rhs=w_out[:, k, h_ * 512:(h_ + 1) * 512],
                                start=(k == 0), stop=(k == KC - 1))
                    ob = obuf[j % 2]
                    self.residual_out([P[3], P[4]], gb, self.dv(src[r0:r0 + 128, :], (skey, r0)), self.dv(dst[r0:r0 + 128, :], (dkey, r0)),
                                      ob, nbuf["xt"][j % 2])

    def shortconv(self, idx, li, first, upd):
        S, nb, w = self.S, self.nb, self.W[li]
        self.arena_reset()
        sT, bT = self.load_modT(idx)
        w_in = self.alloc([128, KC, 3 * D], BF16)
        w_out = self.alloc([128, KC, D], BF16)
        self.load_w_bf16(w_in, w["w_in"], "sw_in%d" % li, 3 * D)
        self.load_w_bf16(w_out, w["w_out"], "sw_out%d" % li, D)
        cvT = self.alloc([128, KC, 3])
        for k in range(KC):
            S.dma("sp", cvT[:, k, :], self.dv(w["cv"][:, k * 128:(k + 1) * 128].rearrange("t p -> p t"), "cv%d" % li), allow_slow_non_contiguous=True)
        nbuf = self.norm_bufs()
        g1 = [self.alloc([128, D]) for _ in range(2)]
        hT = [self.alloc([128, KC, 512], BF16) for _ in range(2)]
        gcs = [self.alloc([128, 512]) for _ in range(2)]
        q = [self.alloc([128, 512]) for _ in range(2)]
        cc = [self.alloc([128, 512]) for _ in range(2)]
        p16 = self.alloc([128, KC, 512], BF16)
        obuf = [self.alloc([128, D]) for _ in range(2)]
        P = self.psum
        gi = 0
        for si, (sk, s) in enumerate(self.seqs(upd)):
            lat = sk == "lat"
            ntok = SEQ if lat else nb * CTX
            src, skey = self.xsrc(first, lat)
            dst, dkey = self.xdst(lat)
            base = s * SEQ if lat else 0
            GS = min(512, ntok)
            ng = ntok // GS
            RL = 64 if lat else CTX
            gb = g1[si % 2]
            self.load_rows_bc(gb, self.MOD[idx, 2, s:s + 1, :], "mod%d" % idx)
            for g in range(ng):
                g0 = g * GS
                h = hT[gi % 2]
                gi += 1
                for j in range(GS // 128):
                    r0 = base + g0 + j * 128
                    self.norm_transpose(nbuf, self.dv(src[r0:r0 + 128, :], (skey, r0)), sT, bT, s, h[:, :, j * 128:(j + 1) * 128])
                for m in range(KC):
                    pb, pc_, pv = P[1], P[2 + m % 2], P[4 + m % 2]
                    for (pp, off) in ((pc_, D), (pv, 2 * D)):
                        for k in range(KC):
                            S.I("pe", "matmul", out=pp[:, 0:GS], lhsT=w_in[:, k, off + m * 128:off + (m + 1) * 128], rhs=h[:, k, 0:GS],
                                start=(k == 0), stop=(k == KC - 1))
                    gct, qt, ct = gcs[m % 2], q[m % 2], cc[m % 2]
                    S.I("act", "activation", out=gct[:, 0:GS], in_=pc_[:, 0:GS], func=AF.Identity)
                    S.I("dve", "tensor_tensor", out=qt[:, 0:GS], in0=pv[:, 0:GS], in1=gct[:, 0:GS], op=ALU.mult)
                    S.I("act", "activation", out=ct[:, 0:GS], in_=qt[:, 0:GS], func=AF.Identity, scale=cvT[:, m, 1:2])
                    q3 = qt[:, 0:GS].re("p (r c) -> p r c", c=RL)
                    c3 = ct[:, 0:GS].re("p (r c) -> p r c", c=RL)
                    S.I("dve", "scalar_tensor_tensor", out=c3[:, :, 1:RL], in0=q3[:, :, 0:RL - 1], scalar=cvT[:, m, 0:1], in1=c3[:, :, 1:RL],
                        op0=ALU.mult, op1=ALU.add)
                    S.I("dve", "scalar_tensor_tensor", out=c3[:, :, 0:RL - 1], in0=q3[:, :, 1:RL], scalar=cvT[:, m, 2:3], in1=c3[:, :, 0:RL - 1],
                        op0=ALU.mult, op1=ALU.add)
                    for k in range(KC):
                        S.I("pe", "matmul", out=pb[:, 0:GS], lhsT=w_in[:, k, m * 128:(m + 1) * 128], rhs=h[:, k, 0:GS], start=(k == 0), stop=(k == KC - 1))
                    S.I("dve", "tensor_tensor", out=p16[:, m, 0:GS], in0=pb[:, 0:GS], in1=ct[:, 0:GS], op=ALU.mult)
                for j in range(GS // 128):
                    r0 = base + g0 + j * 128
                    for h_ in range(2):
                        for k in range(KC):
                            S.I("pe", "matmul", out=P[6 + h_], lhsT=p16[:, k, j * 128:(j + 1) * 128], rhs=w_out[:, k, h_ * 512:(h_ + 1) * 512],
                                start=(k == 0), stop=(k == KC - 1))
                    self.residual_out([P[6], P[7]], gb, self.dv(src[r0:r0 + 128, :], (skey, r0)), self.dv(dst[r0:r0 + 128, :], (dkey, r0)),
                                      obuf[j % 2], nbuf["xt"][j % 2])

    def s5(self, idx, li, first, upd):
        import math
        S, nb, w, P = self.S, self.nb, self.W[li], self.psum
        NTK = CTX + SEQ
        HTv = self.HT.rearrange("(k p) t -> p k t", p=128)
        YTv = self.YT.rearrange("(k p) t -> p k t", p=128)
        self.arena_reset()
        sT, bT = self.load_modT(idx)
        nbuf = self.norm_bufs()
        ht = [self.alloc([128, KC, 128], BF16) for _ in range(3)]
        i = 0
        for b in range(nb):
            for lat, n_t in ((False, CTX // 128), (True, SEQ // 128)):
                src, skey = self.xsrc(first, lat)
                for j in range(n_t):
                    r0 = (b * SEQ if lat else b * CTX) + j * 128
                    col = b * NTK + (CTX if lat else 0) + j * 128
                    t = ht[i % 3]
                    i += 1
                    self.norm_transpose(nbuf, self.dv(src[r0:r0 + 128, :], (skey, r0)), sT, bT, (b if lat else nb), t)
                    S.dma("sp", self.dv(HTv[:, :, col:col + 128], "ht"), t, disjoint=True)
        self.arena_reset()
        ND = 64
        f2 = lambda v: v.re("p d g -> p (d g)")
        are, aim, ldt = (self.alloc([128, 2, 32]) for _ in range(3))
        for d in range(2):
            S.dma("sp", are[:, d, :], self.dv(w["a_re"][d].rearrange("(G g) p -> (g p) G", g=2), "s5a"), allow_slow_non_contiguous=True)
            S.dma("sp", aim[:, d, :], self.dv(w["a_im"][d].rearrange("(G g) p -> (g p) G", g=2), "s5a"), allow_slow_non_contiguous=True)
            for g2 in range(2):
                srcv = w["ldt"][d:d + 1, :].rearrange("o (G g) -> o g G", g=2)[:, g2, :]
                S.dma("sp", ldt[g2 * 64:(g2 + 1) * 64, d, :], self.dv(srcv.to_broadcast([64, 32]), "s5a"), allow_slow_non_contiguous=True)
        names = ("dt", "mag", "ang", "c", "s", "ta", "tb", "den", "nre", "kre", "kim", "abr", "abi")
        T_ = {n: self.alloc([128, ND]) for n in names}
        hpi = self.alloc([128, 1])
        self._memset(hpi, math.pi / 2)
        A, B_ = f2(are), f2(aim)
        S.I("dve", "tensor_single_scalar", out=A, in_=A, scalar=-1e-4, op=ALU.min)
        S.I("act", "activation", out=T_["dt"], in_=f2(ldt), func=AF.Exp)
        S.I("dve", "tensor_tensor", out=T_["ta"], in0=T_["dt"], in1=A, op=ALU.mult)
        S.I("act", "activation", out=T_["mag"], in_=T_["ta"], func=AF.Exp)
        S.I("dve", "tensor_tensor", out=T_["ang"], in0=T_["dt"], in1=B_, op=ALU.mult)
        S.I("act", "activation", out=T_["s"], in_=T_["ang"], func=AF.Sin, scale=1.0 / 16)
        S.I("act", "activation", out=T_["c"], in_=T_["ang"], func=AF.Sin, scale=1.0 / 16, bias=hpi)

        def csquare(c, s, ta, tb):
            S.I("dve", "tensor_tensor", out=ta, in0=c, in1=c, op=ALU.mult)
            S.I("dve", "tensor_tensor", out=tb, in0=s, in1=s, op=ALU.mult)
            S.I("dve", "scalar_tensor_tensor", out=s, in0=c, scalar=2.0, in1=s, op0=ALU.mult, op1=ALU.mult)
            S.I("dve", "tensor_tensor", out=c, in0=ta, in1=tb, op=ALU.subtract)
        for _ in range(4):
            csquare(T_["c"], T_["s"], T_["ta"], T_["tb"])
        NP2 = 12
        Er = self.alloc([128, NP2, ND])
        Ei = self.alloc([128, NP2, ND])
        S.I("dve", "tensor_copy", out=Er[:, 0, :], in_=T_["c"])
        S.I("dve", "tensor_copy", out=Ei[:, 0, :], in_=T_["s"])
        for j in range(1, NP2):
            S.I("dve", "tensor_copy", out=Er[:, j, :], in_=Er[:, j - 1, :])
            S.I("dve", "tensor_copy", out=Ei[:, j, :], in_=Ei[:, j - 1, :])
            csquare(Er[:, j, :], Ei[:, j, :], T_["ta"], T_["tb"])
        S.I("dve", "tensor_tensor", out=T_["abr"], in0=T_["mag"], in1=T_["c"], op=ALU.mult)
        S.I("dve", "tensor_tensor", out=T_["abi"], in0=T_["mag"], in1=T_["s"], op=ALU.mult)
        S.I("dve", "tensor_tensor", out=T_["ta"], in0=A, in1=A, op=ALU.mult)
        S.I("dve", "tensor_tensor", out=T_["tb"], in0=B_, in1=B_, op=ALU.mult)
        S.I("dve", "tensor_tensor", out=T_["den"], in0=T_["ta"], in1=T_["tb"], op=ALU.add)
        S.I("dve", "reciprocal", out=T_["den"], in_=T_["den"])
        S.I("dve", "tensor_single_scalar", out=T_["nre"], in_=T_["abr"], scalar=-1.0, op=ALU.add)
        S.I("dve", "tensor_tensor", out=T_["ta"], in0=T_["nre"], in1=A, op=ALU.mult)
        S.I("dve", "tensor_tensor", out=T_["tb"], in0=T_["abi"], in1=B_, op=ALU.mult)
        S.I("dve", "tensor_tensor", out=T_["kre"], in0=T_["ta"], in1=T_["tb"], op=ALU.add)
        S.I("dve", "tensor_tensor", out=T_["kre"], in0=T_["kre"], in1=T_["den"], op=ALU.mult)
        S.I("dve", "tensor_tensor", out=T_["ta"], in0=T_["abi"], in1=A, op=ALU.mult)
        S.I("dve", "tensor_tensor", out=T_["tb"], in0=T_["nre"], in1=B_, op=ALU.mult)
        S.I("dve", "tensor_tensor", out=T_["kim"], in0=T_["ta"], in1=T_["tb"], op=ALU.subtract)
        S.I("dve", "tensor_tensor", out=T_["kim"], in0=T_["kim"], in1=T_["den"], op=ALU.mult)
        mag = T_["mag"]
        lB = self.alloc([32, ND, 2, 128], BF16)
        lC = self.alloc([128, ND, 2, 32], BF16)
        dvec = self.alloc([128, KC])
        self.load_featT(dvec, w["d"], "s5d")
        keep = self.aoff
        bre = self.alloc([128, 2, 32, 16])
        bim = self.alloc([128, 2, 32, 16])
        for d in range(2):
            S.dma("sp", bre[:, d], self.dv(w["b_re"][d].rearrange("(G g) p c -> (g p) G c", g=2), "s5b"))
            S.dma("sp", bim[:, d], self.dv(w["b_im"][d].rearrange("(G g) p c -> (g p) G c", g=2), "s5b"))
        bbr = self.alloc([128, ND, 16])
        bbi = self.alloc([128, ND, 16])
        tq = self.alloc([128, ND, 16])
        brf, bif = bre.re("p d g c -> p (d g) c"), bim.re("p d g c -> p (d g) c")
        kr3 = T_["kre"].un(2).bc([128, ND, 16])
        ki3 = T_["kim"].un(2).bc([128, ND, 16])
        S.I("dve", "tensor_tensor", out=bbr, in0=brf, in1=kr3, op=ALU.mult)
        S.I("dve", "tensor_tensor", out=tq, in0=bif, in1=ki3, op=ALU.mult)
        S.I("dve", "tensor_tensor", out=bbr, in0=bbr, in1=tq, op=ALU.subtract)
        S.I("dve", "tensor_tensor", out=bbi, in0=bif, in1=kr3, op=ALU.mult)
        S.I("dve", "tensor_tensor", out=tq, in0=brf, in1=ki3, op=ALU.mult)
        S.I("dve", "tensor_tensor", out=bbi, in0=bbi, in1=tq, op=ALU.add)
        bblk = [self.alloc([128, 2, 32], BF16) for _ in range(2)]
        for t in bblk:
            self._memset(t, 0.0)
        for dg in range(ND):
            t = bblk[dg % 2]
            for c_, bb in ((0, bbr), (1, bbi)):
                S.I("dve", "tensor_copy", out=t[0:64, c_, 0:16], in_=bb[0:64, dg, :])
                S.I("dve", "tensor_copy", out=t[64:128, c_, 16:32], in_=bb[64:128, dg, :])
            pt = P[dg % 2].bitcast(BF16)
            for c_ in range(2):
                S.I("pe", "transpose", out=pt[0:32, c_ * 128:(c_ + 1) * 128], in_=t[:, c_, :], identity=self.ident)
            S.I("act", "activation", out=lB[:, dg, :, :], in_=pt[0:32, 0:256].re("p (c q) -> p c q", c=2), func=AF.Identity)
        cnat = [self.alloc([32, 32, 128]) for _ in range(2)]
        ci = 0
        for d in range(2):
            for c_, nm in ((0, "c_re"), (1, "c_im")):
                t = cnat[ci % 2]
                ci += 1
                self._memset(t, 0.0)
                for g2 in range(2):
                    srcv = w[nm][d].rearrange("(G g) c p -> g c G p", g=2)[g2]
                    S.dma("sp", t[16 * g2:16 * g2 + 16, :, 64 * g2:64 * g2 + 64], self.dv(srcv, "s5c"), disjoint=True)
                for G in range(32):
                    pp = P[2 + G % 2]
                    S.I("pe", "transpose", out=pp[:, 0:32], in_=t[:, G, :], identity=self.identf[0:32, 0:32])
                    S.I("act", "activation", out=lC[:, d * 32 + G, c_, :], in_=pp[:, 0:32], func=AF.Identity, scale=(1.0 if c_ == 0 else -1.0))
        S.barrier()
        self.aoff = keep
        cosT = self.alloc([128, NTK])
        sinT = self.alloc([128, NTK])
        ttmp = [self.alloc([128, 1024]) for _ in range(2)]
        lCp = [self.alloc([128, 2, 128], BF16) for _ in range(2)]
        for t in lCp:
            self._memset(t, 0.0)
        u = [self.alloc([32, NTK], BF16) for _ in range(2)]
        bus = [[self.alloc([128, 512]) for _ in range(2)] for _ in range(2)]
        tm = [[self.alloc([128, 512]) for _ in range(4)] for _ in range(2)]
        Wr = [self.alloc([128, 512]) for _ in range(2)]
        Wi = [self.alloc([128, 512]) for _ in range(2)]
        Gr = [self.alloc([128, 512]) for _ in range(2)]
        Gi = [self.alloc([128, 512]) for _ in range(2)]
        to = tm
        Hr = [self.alloc([128, 512], BF16) for _ in range(2)]
        Hi = [self.alloc([128, 512], BF16) for _ in range(2)]
        yacc = [self.alloc([128, NTK]) for _ in range(nb)]
        hch = [self.alloc([128, NTK], BF16) for _ in range(2)]
        gt = [tm[0][0:3], tm[1][0:3]]
        y16 = [self.alloc([128, 512], BF16) for _ in range(2)]
        fw = [(0, CTX)] + [(CTX + 512 * i_, 512) for i_ in range(SEQ // 512)]
        rv = [(0, CTX)] + [(CTX + 512 * i_, 512) for i_ in reversed(range(SEQ // 512))]
        pcount = 0
        ui = 0
        for m in range(KC):
            for jj in range(4):
                G = 4 * m + jj
                for d in range(2):
                    dg = d * 32 + G
                    first_acc = (jj == 0 and d == 0)
                    self._memset(cosT[:, 0:1], 1.0)
                    self._memset(sinT[:, 0:1], 0.0)
                    n_have = 1
                    j = 0
                    while n_have < NTK:
                        n_new = min(n_have, NTK - n_have)
                        er, ei = Er[:, j, dg:dg + 1], Ei[:, j, dg:dg + 1]
                        ta, tb = ttmp[0][:, 0:n_new], ttmp[1][:, 0:n_new]
                        S.I("dve", "tensor_single_scalar", out=ta, in_=sinT[:, 0:n_new], scalar=ei, op=ALU.mult)
                        S.I("dve", "tensor_single_scalar", out=tb, in_=sinT[:, 0:n_new], scalar=er, op=ALU.mult)
                        S.I("dve", "scalar_tensor_tensor", out=sinT[:, n_have:n_have + n_new], in0=cosT[:, 0:n_new], scalar=ei, in1=tb, op0=ALU.mult, op1=ALU.add)
                        S.I("dve", "scalar_tensor_tensor", out=cosT[:, n_have:n_have + n_new], in0=cosT[:, 0:n_new], scalar=er, in1=ta, op0=ALU.mult, op1=ALU.subtract)
                        n_have += n_new
                        j += 1
                    lc = lCp[dg % 2]
                    S.I("dve", "tensor_copy", out=lc[:, :, 32 * jj:32 * jj + 32], in_=lC[:, dg, :, :])
                    if jj > 0:
                        pass
                    mg = mag[:, dg:dg + 1]
                    for b in range(nb):
                        ut = u[ui % 2]
                        ui += 1
                        S.dma("sp", ut, self.dv(self.HT[32 * G:32 * G + 32, b * NTK:(b + 1) * NTK], "ht"))
                        prev = None
                        n0 = 0
                        for (c0, L) in (fw if d == 0 else rv):
                            pi_ = pcount % 2
                            pcount += 1
                            pr, pim = P[0 + pi_], P[2 + pi_]
                            S.I("pe", "matmul", out=pr[:, 0:L], lhsT=lB[:, dg, 0, :], rhs=ut[:, c0:c0 + L], start=True, stop=True)
                            S.I("pe", "matmul", out=pim[:, 0:L], lhsT=lB[:, dg, 1, :], rhs=ut[:, c0:c0 + L], start=True, stop=True)
                            br_, bi_ = bus[pi_][0][:, 0:L], bus[pi_][1][:, 0:L]
                            S.I("act", "activation", out=br_, in_=pr[:, 0:L], func=AF.Identity)
                            S.I("act", "activation", out=bi_, in_=pim[:, 0:L], func=AF.Identity)
                            cs_, sn_ = cosT[:, n0:n0 + L], sinT[:, n0:n0 + L]
                            if d == 1:
                                cs_, sn_ = cs_.rev(), sn_.rev()
                            t1, t2, t3, t4 = (x_[:, 0:L] for x_ in tm[pi_])
                            wr_, wi_ = Wr[pi_][:, 0:L], Wi[pi_][:, 0:L]
                            S.I("dve", "tensor_tensor", out=t1, in0=br_, in1=cs_, op=ALU.mult)
                            S.I("dve", "tensor_tensor", out=t2, in0=bi_, in1=sn_, op=ALU.mult)
                            S.I("dve", "tensor_tensor", out=wr_, in0=t1, in1=t2, op=ALU.add)
                            S.I("pool", "tensor_tensor", out=t3, in0=bi_, in1=cs_, op=ALU.mult)
                            S.I("pool", "tensor_tensor", out=t4, in0=br_, in1=sn_, op=ALU.mult)
                            S.I("pool", "tensor_tensor", out=wi_, in0=t3, in1=t4, op=ALU.subtract)
                            gr_, gi_ = Gr[pi_][:, 0:L], Gi[pi_][:, 0:L]
                            if prev is None:
                                ir, ii_ = 0.0, 0.0
                            else:
                                pgr, pgi, pL = prev
                                ir = pgr[:, pL - 1:pL] if d == 0 else pgr[:, 0:1]
                                ii_ = pgi[:, pL - 1:pL] if d == 0 else pgi[:, 0:1]
                            mb = mg.bc([128, L])
                            if d == 0:
                                S.I("dve", "tensor_tensor_scan", out=gr_, data0=mb, data1=wr_, initial=ir, op0=ALU.mult, op1=ALU.add)
                                S.I("dve", "tensor_tensor_scan", out=gi_, data0=mb, data1=wi_, initial=ii_, op0=ALU.mult, op1=ALU.add)
                            else:
                                S.I("dve", "tensor_tensor_scan", out=gr_.rev(), data0=mb, data1=wr_.rev(), initial=ir, op0=ALU.mult, op1=ALU.add)
                                S.I("dve", "tensor_tensor_scan", out=gi_.rev(), data0=mb, data1=wi_.rev(), initial=ii_, op0=ALU.mult, op1=ALU.add)
                            prev = (Gr[pi_], Gi[pi_], L)
                            o1, o2, o3, o4 = (x_[:, 0:L] for x_ in to[pi_])
                            hr_, hi_ = Hr[pi_][:, 0:L], Hi[pi_][:, 0:L]
                            S.I("dve", "tensor_tensor", out=o1, in0=gr_, in1=cs_, op=ALU.mult)
                            S.I("dve", "tensor_tensor", out=o2, in0=gi_, in1=sn_, op=ALU.mult)
                            S.I("dve", "tensor_tensor", out=hr_, in0=o1, in1=o2, op=ALU.subtract)
                            S.I("pool", "tensor_tensor", out=o3, in0=gr_, in1=sn_, op=ALU.mult)
                            S.I("pool", "tensor_tensor", out=o4, in0=gi_, in1=cs_, op=ALU.mult)
                            S.I("pool", "tensor_tensor", out=hi_, in0=o3, in1=o4, op=ALU.add)
                            py = P[4 + pi_]
                            S.I("pe", "matmul", out=py[:, 0:L], lhsT=lc[:, 0, :], rhs=hr_, start=True, stop=False)
                            S.I("pe", "matmul", out=py[:, 0:L], lhsT=lc[:, 1, :], rhs=hi_, start=False, stop=True)
                            ya = yacc[b][:, c0:c0 + L]
                            if first_acc:
                                S.I("act", "activation", out=ya, in_=py[:, 0:L], func=AF.Identity)
                            else:
                                S.I("dve", "tensor_tensor", out=ya, in0=py[:, 0:L], in1=ya, op=ALU.add)
                            n0 += L
                    self.S._emit("dve", (lambda apx: (lambda e: e.memset(apx, 0.0)))(lc[:, :, 32 * jj:32 * jj + 32].ap), [], [lc.buf])
            for b in range(nb):
                hc = hch[b % 2]
                S.dma("sp", hc, self.dv(self.HT[128 * m:128 * m + 128, b * NTK:(b + 1) * NTK], "ht"))
                for pi2, (c0, L) in enumerate(fw):
                    tt, sq, sg_ = (x_[:, 0:L] for x_ in gt[pi2 % 2])
                    yo_ = y16[pi2 % 2][:, 0:L]
                    S.I("dve", "scalar_tensor_tensor", out=tt, in0=hc[:, c0:c0 + L], scalar=dvec[:, m:m + 1], in1=yacc[b][:, c0:c0 + L], op0=ALU.mult, op1=ALU.add)
                    S.I("act", "activation", out=sq, in_=tt, func=AF.Square)
                    S.I("act", "activation", out=sq, in_=sq, func=AF.Identity, scale=0.044715, bias=self.one_c)
                    S.I("pool", "tensor_tensor", out=sq, in0=sq, in1=tt, op=ALU.mult)
                    S.I("act", "activation", out=sg_, in_=sq, func=AF.Sigmoid, scale=1.5957691216057308)
                    S.I("pool", "tensor_tensor", out=yo_, in0=tt, in1=sg_, op=ALU.mult)
                    col = b * NTK + c0
                    S.dma("sp", self.dv(YTv[:, m, col:col + L], "yt"), yo_, disjoint=True)
        self.arena_reset()
        wg = self.alloc([128, KC, 2 * D], BF16)
        self.load_w_bf16(wg, w["w_glu"], "s5wg%d" % li, 2 * D)
        g1 = [self.alloc([128, D]) for _ in range(2)]
        yt = [self.alloc([128, KC, 128], BF16) for _ in range(2)]
        sgb = [self.alloc([128, D]) for _ in range(2)]
        obuf = [self.alloc([128, D]) for _ in range(2)]
        xb = [self.alloc([128, D]) for _ in range(2)]
        ti = 0
        for b in range(nb):
            self.load_rows_bc(g1[0], self.MOD[idx, 2, b:b + 1, :], "mod%d" % idx)
            if upd:
                self.load_rows_bc(g1[1], self.MOD[idx, 2, nb:nb + 1, :], "mod%d" % idx)
            for lat, n_t in (((False, CTX // 128),) if upd else ()) + ((True, SEQ // 128),):
                src, skey = self.xsrc(first, lat)
                dst, dkey = self.xdst(lat)
                gb = g1[0] if lat else g1[1]
                for j in range(n_t):
                    r0 = (b * SEQ if lat else b * CTX) + j * 128
                    col = b * NTK + (CTX if lat else 0) + j * 128
                    y_ = yt[ti % 2]
                    S.dma("sp", y_, self.dv(YTv[:, :, col:col + 128], "yt"))
                    for cb in range(4):
                        pp = P[cb]
                        for k in range(KC):
                            S.I("pe", "matmul", out=pp, lhsT=y_[:, k, :], rhs=wg[:, k, cb * 512:(cb + 1) * 512], start=(k == 0), stop=(k == KC - 1))
                    sg_, ob, xx = sgb[ti % 2], obuf[ti % 2], xb[ti % 2]
                    ti += 1
                    S.dma("sp", xx, self.dv(src[r0:r0 + 128, :], (skey, r0)))
                    for h_ in range(2):
                        S.I("act", "activation", out=sg_[:, h_ * 512:(h_ + 1) * 512], in_=P[2 + h_], func=AF.Sigmoid)
                        S.I("dve", "tensor_tensor", out=ob[:, h_ * 512:(h_ + 1) * 512], in0=P[h_], in1=sg_[:, h_ * 512:(h_ + 1) * 512], op=ALU.mult)
                    S.I("pool", "tensor_tensor", out=ob, in0=ob, in1=gb, op=ALU.mult)
                    S.I("dve", "tensor_tensor", out=ob, in0=ob, in1=xx, op=ALU.add)
                    S.dma("sp", self.dv(dst[r0:r0 + 128, :], (dkey, r0)), ob)

    def moe(self, idx, li, upd):
        S, nb, w, nc = self.S, self.nb, self.W[li], self.nc
        self.arena_reset()
        P = self.psum
        tiles = []
        for b in range(nb):
            for j in range(SEQ // 128):
                r0 = b * SEQ + j * 128
                tiles.append((self.xs, "xs", r0, b))
        if upd:
            for j in range(nb * CTX // 128):
                tiles.append((self.cs, "cs", j * 128, nb))
        NT = len(tiles)
        ntok = NT * 128
        nslot = ((2 * ntok + NE * (TS - 1)) + TS - 1) // TS * TS
        NST = nslot // TS

        oh1 = self.alloc([128, NT, NE])
        oh2 = self.alloc([128, NT, NE])
        L1 = self.alloc([128, NT])
        L2 = self.alloc([128, NT])
        W1 = self.alloc([128, NT])
        W2 = self.alloc([128, NT])
        run = self.alloc([128, NE])
        sl1 = self.alloc([128, NT], I32)
        sl2 = self.alloc([128, NT], I32)
        widx = self.alloc([128, NST], I32)
        keep = self.aoff

        wr = self.alloc([128, KC, 36])
        S.dma("sp", wr, self.dv(w["wr"].rearrange("(k p) n -> p k n", p=128), "wr%d" % li))
        brb = self.alloc([128, 36])
        self.load_rows_bc(brb, w["br"], "br%d" % li)
        sc2 = [self.alloc([128, D]) for _ in range(2)]
        sh2 = [self.alloc([128, D]) for _ in range(2)]
        nbuf = self.norm_bufs()
        h2 = [self.alloc([128, D]) for _ in range(2)]
        h16 = [self.alloc([128, D], BF16) for _ in range(2)]
        h2T = [self.alloc([128, KC, 128]) for _ in range(2)]
        GB = 4
        lg = self.alloc([128, GB, 36])
        gmx, gsum, gw, m1, m2, dm, den = (self.alloc([128, GB]) for _ in range(7))
        ohg = self.alloc([128, GB, 4])
        ex = self.alloc([128, GB, 4])
        t48 = self.alloc([128, GB, 4, 8])
        le, le2, o1, o2 = (self.alloc([128, GB, 8]) for _ in range(4))
        sel = self.alloc([128, GB, NE])
        sel16 = self.alloc([128, GB, NE], BF16)
        pf = self.alloc([128, GB, NE])
        tmp32 = self.alloc([128, GB, NE])
        self._memset(run, 0.0)
        cur_s = None
        for t0 in range(0, NT, GB):
            n = min(GB, NT - t0)
            for j in range(n):
                ti = t0 + j
                src, skey, r0, s = tiles[ti]
                if s != cur_s:
                    cur_s = s
                    cb = s % 2
                    self.load_rows_bc(sc2[cb], self.MOD[idx, 3, s:s + 1, :], "mod%d" % idx)
                    self.load_rows_bc(sh2[cb], self.MOD[idx, 4, s:s + 1, :], "mod%d" % idx, eng="act")
                xt, rs, i = self.rms_tile(nbuf, self.dv(src[r0:r0 + 128, :], (skey, r0)))
                hh, hb, hT_ = h2[ti % 2], h16[ti % 2], h2T[ti % 2]
                S.I("dve", "scalar_tensor_tensor", out=hh, in0=xt, scalar=rs, in1=sc2[cb], op0=ALU.mult, op1=ALU.mult)
                S.I("pool", "tensor_tensor", out=hh, in0=hh, in1=sh2[cb], op=ALU.add)
                S.I("act", "activation", out=hb, in_=hh, func=AF.Identity)
                S.dma("sp", self.dv(self.H2[ti * 128:(ti + 1) * 128, :], ("h2", ti)), hb)
                for k in range(KC):
                    pt = P[1 + (k // 4) % 2]
                    S.I("pe", "transpose", out=pt[:, (k % 4) * 128:(k % 4 + 1) * 128], in_=hh[:, k * 128:(k + 1) * 128], identity=self.identf)
                    if k % 4 == 3:
                        S.I("act", "activation", out=hT_[:, k - 3:k + 1, :], in_=pt.re("p (k t) -> p k t", k=4), func=AF.Identity)
                for k in range(KC):
                    S.I("pe", "matmul", out=P[3][:, j * 36:(j + 1) * 36], lhsT=hT_[:, k, :], rhs=wr[:, k, :], start=(k == 0), stop=(k == KC - 1),
                        skip_group_check=True)
            N_ = slice(0, n)
            S.I("dve", "tensor_tensor", out=lg[:, N_, :], in0=P[3][:, 0:n * 36].re("p (t c) -> p t c", c=36), in1=brb.un(1).bc([128, n, 36]), op=ALU.add)
            lgg = lg[:, N_, 0:4]
            S.I("dve", "reduce_max", out=gmx[:, N_], in_=lgg, axis=AX.X)
            S.I("dve", "tensor_tensor", out=ohg[:, N_, :], in0=lgg, in1=gmx[:, N_].un(2).bc([128, n, 4]), op=ALU.is_equal)
            S.I("dve", "tensor_tensor", out=ex[:, N_, :], in0=lgg, in1=gmx[:, N_].un(2).bc([128, n, 4]), op=ALU.subtract)
            S.I("act", "activation", out=ex[:, N_, :], in_=ex[:, N_, :], func=AF.Exp)
            S.I("dve", "reduce_sum", out=gsum[:, N_], in_=ex[:, N_, :], axis=AX.X)
            S.I("dve", "reciprocal", out=gw[:, N_], in_=gsum[:, N_])
            S.I("dve", "tensor_tensor", out=t48[:, N_], in0=lg[:, N_, 4:36].re("p t (g e) -> p t g e", g=4),
                in1=ohg[:, N_, :].un(3).bc([128, n, 4, 8]), op=ALU.mult)
            S.I("dve", "reduce_sum", out=le[:, N_, :], in_=t48[:, N_].re("p t g e -> p t e g"), axis=AX.X)
            S.I("dve", "reduce_max", out=m1[:, N_], in_=le[:, N_, :], axis=AX.X)
            S.I("dve", "tensor_tensor", out=o1[:, N_, :], in0=le[:, N_, :], in1=m1[:, N_].un(2).bc([128, n, 8]), op=ALU.is_equal)
            S.I("dve", "scalar_tensor_tensor", out=le2[:, N_, :], in0=o1[:, N_, :], scalar=-1e30, in1=le[:, N_, :], op0=ALU.mult, op1=ALU.add)
            S.I("dve", "reduce_max", out=m2[:, N_], in_=le2[:, N_, :], axis=AX.X)
            S.I("dve", "tensor_tensor", out=o2[:, N_, :], in0=le2[:, N_, :], in1=m2[:, N_].un(2).bc([128, n, 8]), op=ALU.is_equal)
            S.I("dve", "tensor_tensor", out=dm[:, N_], in0=m2[:, N_], in1=m1[:, N_], op=ALU.subtract)
            S.I("act", "activation", out=dm[:, N_], in_=dm[:, N_], func=AF.Exp)
            S.I("dve", "tensor_single_scalar", out=den[:, N_], in_=dm[:, N_], scalar=1.0, op=ALU.add)
            S.I("dve", "reciprocal", out=den[:, N_], in_=den[:, N_])
            S.I("dve", "tensor_tensor", out=W1[:, t0:t0 + n], in0=gw[:, N_], in1=den[:, N_], op=ALU.mult)
            S.I("dve", "tensor_tensor", out=W2[:, t0:t0 + n], in0=gw[:, N_], in1=W1[:, t0:t0 + n], op=ALU.subtract)
            o1g = oh1[:, t0:t0 + n, :].re("p t (g e) -> p t g e", g=4)
            o2g = oh2[:, t0:t0 + n, :].re("p t (g e) -> p t g e", g=4)
            S.I("dve", "tensor_tensor", out=o1g, in0=ohg[:, N_, :].un(3).bc([128, n, 4, 8]), in1=o1[:, N_, :].un(2).bc([128, n, 4, 8]), op=ALU.mult)
            S.I("dve", "tensor_tensor", out=o2g, in0=ohg[:, N_, :].un(3).bc([128, n, 4, 8]), in1=o2[:, N_, :].un(2).bc([128, n, 4, 8]), op=ALU.mult)
            S.I("dve", "tensor_tensor", out=sel[:, N_, :], in0=oh1[:, t0:t0 + n, :], in1=oh2[:, t0:t0 + n, :], op=ALU.add)
            S.I("dve", "tensor_copy", out=sel16[:, N_, :], in_=sel[:, N_, :])
            for j in range(n):
                S.I("pe", "matmul", out=P[4][:, j * NE:(j + 1) * NE], lhsT=self.ltri, rhs=sel16[:, j, :], start=True, stop=True, skip_group_check=True)
                S.I("pe", "matmul", out=P[5][:, j * NE:(j + 1) * NE], lhsT=self.ones, rhs=sel16[:, j, :], start=True, stop=True, skip_group_check=True)
            for j in range(n):
                S.I("dve", "tensor_tensor", out=pf[:, j, :], in0=P[4][:, j * NE:(j + 1) * NE], in1=run, op=ALU.add)
                S.I("dve", "tensor_tensor", out=run, in0=P[5][:, j * NE:(j + 1) * NE], in1=run, op=ALU.add)
            S.I("dve", "tensor_tensor", out=tmp32[:, N_, :], in0=pf[:, N_, :], in1=oh1[:, t0:t0 + n, :], op=ALU.mult)
            S.I("dve", "reduce_sum", out=L1[:, t0:t0 + n], in_=tmp32[:, N_, :], axis=AX.X)
            S.I("dve", "tensor_tensor", out=tmp32[:, N_, :], in0=pf[:, N_, :], in1=oh2[:, t0:t0 + n, :], op=ALU.mult)
            S.I("dve", "reduce_sum", out=L2[:, t0:t0 + n], in_=tmp32[:, N_, :], axis=AX.X)

        if 'moeB' in SKIP:
            return
        self.S.barrier()
        self.aoff = keep
        cnti = self.alloc([128, NE], I32)
        pad = self.alloc([128, NE])
        incl = self.alloc([128, NE])
        basee = self.alloc([128, NE])
        onesf = self.alloc([128, NE])
        big3 = self.alloc([128, NT, NE])
        sf = self.alloc([128, NT])
        sgrid = self.alloc([128, NST])
        cmp3 = self.alloc([128, NST, NE])
        ef = self.alloc([128, NST])
        S.I("dve", "tensor_copy", out=cnti, in_=run)
        S.I("dve", "tensor_single_scalar", out=cnti, in_=cnti, scalar=TS - 1, op=ALU.add)
        sh = TS.bit_length() - 1
        S.I("dve", "tensor_scalar", out=cnti, in0=cnti, scalar1=sh, scalar2=sh, op0=ALU.arith_shift_right, op1=ALU.logical_shift_left)
        S.I("dve", "tensor_copy", out=pad, in_=cnti)
        self._memset(onesf, 1.0)
        S.I("dve", "tensor_tensor_scan", out=incl, data0=onesf, data1=pad, initial=0.0, op0=ALU.mult, op1=ALU.add)
        S.I("dve", "tensor_tensor", out=basee, in0=incl, in1=pad, op=ALU.subtract)
        for (oh, L, sl) in ((oh1, L1, sl1), (oh2, L2, sl2)):
            S.I("dve", "tensor_tensor", out=big3, in0=oh, in1=basee.un(1).bc([128, NT, NE]), op=ALU.mult)
            S.I("dve", "reduce_sum", out=sf, in_=big3, axis=AX.X)
            S.I("dve", "tensor_tensor", out=sf, in0=sf, in1=L, op=ALU.add)
            S.I("dve", "tensor_copy", out=sl, in_=sf)
        S.I("pool", "iota", out=sgrid, pattern=[[TS, NST]], base=0, channel_multiplier=0, allow_small_or_imprecise_dtypes=True)
        S.I("dve", "tensor_tensor", out=cmp3, in0=incl.un(1).bc([128, NST, NE]), in1=sgrid.un(2).bc([128, NST, NE]), op=ALU.is_le)
        S.I("dve", "reduce_sum", out=ef, in_=cmp3, axis=AX.X)
        S.I("dve", "tensor_single_scalar", out=ef, in_=ef, scalar=float(NE - 1), op=ALU.min)
        same = self.alloc([128, NST])
        self._memset(same, 0.0)
        S.I("dve", "tensor_tensor", out=same[:, 2:NST], in0=ef[:, 2:NST], in1=ef[:, 0:NST - 2], op=ALU.is_equal)
        S.I("dve", "tensor_single_scalar", out=same, in_=same, scalar=float(1 << 20), op=ALU.mult)
        S.I("dve", "tensor_single_scalar", out=ef, in_=ef, scalar=128.0, op=ALU.mult)
        S.I("dve", "tensor_single_scalar", out=ef, in_=ef, scalar=self.iota_p, op=ALU.add)
        S.I("dve", "tensor_tensor", out=ef, in0=ef, in1=same, op=ALU.add)
        S.I("dve", "tensor_copy", out=widx, in_=ef)
        self.dbg("W1", W1); self.dbg("W2", W2); self.dbg("L1", L1); self.dbg("L2", L2); self.dbg("run", run)
        self.dbg("sl1", sl1, I32); self.dbg("sl2", sl2, I32); self.dbg("widx", widx, I32); self.dbg("oh1", oh1); self.dbg("oh2", oh2)
        self.dbg("incl", incl); self.dbg("basee", basee)
        hl = [self.alloc([128, D], BF16) for _ in range(4)]
        h2s_v = self.dv(self.H2S[0:nslot, :], "h2s")
        for ti in range(NT):
            t = hl[ti % 4]
            S.dma("sp", t, self.dv(self.H2[ti * 128:(ti + 1) * 128, :], ("h2", ti)))
            S.scatter(h2s_v, t, sl1[:, ti:ti + 1])
            S.scatter(h2s_v, t, sl2[:, ti:ti + 1])

        if 'moeC' in SKIP:
            return
        self.S.barrier()
        self.aoff = keep
        w13t = [self.alloc([128, KC * D], BF16) for _ in range(2)]
        w2t = [self.alloc([128, 4 * D], BF16) for _ in range(2)]
        hs = [self.alloc([128, 2, D], BF16) for _ in range(2)]
        hT = [self.alloc([128, KC, TS], BF16) for _ in range(2)]
        sa = [self.alloc([128, TS]) for _ in range(2)]
        u16 = [self.alloc([128, 4, TS], BF16) for _ in range(2)]
        yo = [self.alloc([128, D]) for _ in range(2)]
        w13v = self.dv(self.W13B.rearrange("(r a) n -> r (a n)", a=4), "w13b")
        w2v = self.dv(self.W2B.rearrange("(r a) n -> r (a n)", a=2), "w2b")
        yi = 0
        for s_ in range(NST):
            wa, wb, hsl, hTt, ut = w13t[s_ % 2], w2t[s_ % 2], hs[s_ % 2], hT[s_ % 2], u16[s_ % 2]
            S.gather(wa, w13v, widx[:, s_:s_ + 1], bound=NE * 128 - 1)
            S.gather(wb, w2v, widx[:, s_:s_ + 1], bound=NE * 128 - 1)
            S.dma("sp", hsl, self.dv(self.H2S[s_ * TS:(s_ + 1) * TS, :].rearrange("(a p) d -> p a d", p=128), "h2s"))
            for a in range(2):
                pt = P[a].bitcast(BF16)
                for k in range(KC):
                    S.I("pe", "transpose", out=pt[:, k * 128:(k + 1) * 128], in_=hsl[:, a, k * 128:(k + 1) * 128], identity=self.ident)
                S.I("act" if a == 0 else "dve", "activation" if a == 0 else "tensor_copy", out=hTt[:, :, a * 128:(a + 1) * 128],
                    in_=pt.re("p (k t) -> p k t", k=KC), **({"func": AF.Copy} if a == 0 else {}))
            for m in range(4):
                pa, pg = P[2 + m % 2], P[4 + m % 2]
                for (pp, off) in ((pa, 0), (pg, DE)):
                    for k in range(KC):
                        c0 = k * D + off + m * 128
                        S.I("pe", "matmul", out=pp[:, 0:TS], lhsT=wa[:, c0:c0 + 128], rhs=hTt[:, k, :], start=(k == 0), stop=(k == KC - 1))
                sat = sa[m % 2]
                S.I("act", "activation", out=sat, in_=pa[:, 0:TS], func=AF.Silu)
                S.I("dve", "tensor_tensor", out=ut[:, m, :], in0=pg[:, 0:TS], in1=sat, op=ALU.mult)
            for a in range(2):
                yt = yo[yi % 2]
                yi += 1
                for h_ in range(2):
                    pp = P[6 + h_]
                    for k in range(4):
                        S.I("pe", "matmul", out=pp, lhsT=ut[:, k, a * 128:(a + 1) * 128], rhs=wb[:, k * D + h_ * 512:k * D + (h_ + 1) * 512],
                            start=(k == 0), stop=(k == 3))
                    S.I("act" if h_ == 0 else "dve", "activation" if h_ == 0 else "tensor_copy", out=yt[:, h_ * 512:(h_ + 1) * 512], in_=pp,
                        **({"func": AF.Copy} if h_ == 0 else {}))
                r0 = s_ * TS + a * 128
                S.dma("sp", self.dv(self.YS[r0:r0 + 128, :], "ys"), yt, disjoint=True)

        if 'moeD' in SKIP:
            return
        self.S.barrier()
        self.aoff = keep
        g2 = [self.alloc([128, D]) for _ in range(2)]
        y1 = [self.alloc([128, D]) for _ in range(4)]
        y2 = [self.alloc([128, D]) for _ in range(4)]
        xt2 = [self.alloc([128, D]) for _ in range(4)]
        acc = [self.alloc([128, D]) for _ in range(4)]
        ysv = self.dv(self.YS[0:nslot, :], "ys")
        cur_s = None
        for ti, (src, skey, r0, s) in enumerate(tiles):
            if s != cur_s:
                cur_s = s
                cb = s % 2
                self.load_rows_bc(g2[cb], self.MOD[idx, 5, s:s + 1, :], "mod%d" % idx)
            a1, a2, xx, ac = y1[ti % 4], y2[ti % 4], xt2[ti % 4], acc[ti % 4]
            S.gather(a1, ysv, sl1[:, ti:ti + 1])
            S.gather(a2, ysv, sl2[:, ti:ti + 1])
            S.dma("sp", xx, self.dv(src[r0:r0 + 128, :], (skey, r0)))
            S.I("act", "activation", out=ac, in_=a1, func=AF.Identity, scale=W1[:, ti:ti + 1])
            S.I("dve", "scalar_tensor_tensor", out=ac, in0=a2, scalar=W2[:, ti:ti + 1], in1=ac, op0=ALU.mult, op1=ALU.add)
            S.I("pool", "tensor_tensor", out=ac, in0=ac, in1=g2[cb], op=ALU.mult)
            S.I("dve", "tensor_tensor", out=ac, in0=ac, in1=xx, op=ALU.add)
            S.dma("sp", self.dv(src[r0:r0 + 128, :], (skey, r0)), ac)

    def final_norm(self):
        S, nb = self.S, self.nb
        self.arena_reset()
        nbuf = self.norm_bufs()
        fg = self.alloc([128, D])
        self.load_rows_bc(fg, self.final_g, "final_g")
        ob = [self.alloc([128, D]) for _ in range(2)]
        for ti in range(nb * SEQ // 128):
            r0 = ti * 128
            xt, rs, i = self.rms_tile(nbuf, self.dv(self.xs[r0:r0 + 128, :], ("xs", r0)))
            o = ob[ti % 2]
            S.I("dve", "scalar_tensor_tensor", out=o, in0=xt, scalar=rs, in1=fg, op0=ALU.mult, op1=ALU.mult)
            S.dma("sp", self.dv(self.y_out[r0:r0 + 128, :], ("y", r0)), o)


def prep_weights(inp, layers):
    out = {}
    f = lambda a: np.ascontiguousarray(a, dtype=np.float32)
    out["c_ctx"] = f(inp["c_ctx"]).reshape(1, D)
    out["final_g"] = f(inp["final_g"]).reshape(1, D)
    for li in layers:
        kind, j = li % 3, li // 3
        out["ada_w_%d" % li] = f(inp["ada_w"][li])
        out["ada_b_%d" % li] = f(inp["ada_b"][li]).reshape(1, -1)
        out["norm1_g_%d" % li] = f(inp["norm1_g"][li]).reshape(1, D)
        out["norm2_g_%d" % li] = f(inp["norm2_g"][li]).reshape(1, D)
        out["moe_wr_%d" % li] = f(np.concatenate([inp["moe_wg"][li], inp["moe_we"][li]], axis=1))
        out["moe_br_%d" % li] = f(np.concatenate([inp["moe_bg"][li], inp["moe_be"][li]], axis=0)).reshape(1, 36)
        w13 = np.asarray(inp["moe_w13"][li]).reshape(NE, KC, 128, D).transpose(0, 2, 1, 3).reshape(NE * 128 * 4, 2048)
        out["moe_w13_%d" % li] = f(w13)
        w2 = np.asarray(inp["moe_w2"][li]).reshape(NE, 4, 128, D).transpose(0, 2, 1, 3).reshape(NE * 128 * 2, 2048)
        out["moe_w2_%d" % li] = f(w2)
        if kind == 0:
            out["conf_w_in_%d" % li] = f(inp["conf_w_in"][j])
            out["conf_dw_%d" % li] = f(inp["conf_dw"][j])
            out["conf_dw_b_%d" % li] = f(inp["conf_dw_b"][j]).reshape(1, D)
            out["conf_ln_g_%d" % li] = f(inp["conf_ln_g"][j]).reshape(1, D)
            out["conf_ln_b_%d" % li] = f(inp["conf_ln_b"][j]).reshape(1, D)
            out["conf_w_out_%d" % li] = f(inp["conf_w_out"][j])
        elif kind == 1:
            out["sc_w_in_%d" % li] = f(inp["sc_w_in"][j])
            out["sc_conv_%d" % li] = f(inp["sc_conv"][j])
            out["sc_w_out_%d" % li] = f(inp["sc_w_out"][j])
        else:
            for n in ("a_re", "a_im", "b_re", "b_im", "c_re", "c_im"):
                out["s5_%s_%d" % (n, li)] = f(inp["s5_" + n][j])
            out["s5_log_dt_%d" % li] = f(inp["s5_log_dt"][j])
            out["s5_d_%d" % li] = f(inp["s5_d"][j]).reshape(1, D)
            out["s5_w_glu_%d" % li] = f(inp["s5_w_glu"][j])
    return out


def run(inp, nb, ncores, layers, final=True, trace=False):
    prog = Prog(nb, layers, final)
    nc = prog.build()
    shared = prep_weights(inp, layers)
    x = np.asarray(inp["x"], dtype=np.float32)
    c = np.asarray(inp["c"], dtype=np.float32)
    ctx = np.asarray(inp["ctx"], dtype=np.float32)
    in_maps = []
    for k in range(ncores):
        m = dict(shared)
        m["x"] = np.ascontiguousarray(x[k * nb:(k + 1) * nb]).reshape(nb * SEQ, D)
        m["c"] = np.ascontiguousarray(c[k * nb:(k + 1) * nb])
        m["ctx"] = np.ascontiguousarray(ctx[k * nb:(k + 1) * nb]).reshape(nb * CTX, D)
        in_maps.append({n: m[n] for n in prog.in_names})
    res = run_bass_kernel_spmd(nc, in_maps, core_ids=list(range(ncores)), **({"trace": True} if trace else {}))
    y = np.concatenate([r["y"].reshape(nb, SEQ, D) for r in res.results], axis=0)
    return y, res


def kernel(**inputs):
    y, _ = run(inputs, nb=4, ncores=NCORES, layers=list(range(DEPTH)), final=True)
    return y.astype(np.float32)
```

```python
import contextlib
import numpy as np
import concourse.bass as bass
import concourse.mybir as mybir
from concourse.bass_utils import run_bass_kernel_spmd

F32 = mybir.dt.float32
BF16 = mybir.dt.bfloat16
I32 = mybir.dt.int32
AF = mybir.ActivationFunctionType
ALU = mybir.AluOpType
AX = mybir.AxisListType

D = 1024
KC = 8
SEQ = 2048
CTX = 256
NE = 32
DE = 512
TS = 256
RMS_EPS = 1e-6
LN_EPS = 1e-5
DEPTH = 4
NCORES = 8
SKIP = set()
DEBUG = False


class Buf:
    __slots__ = ("w", "wx", "r")

    def __init__(self):
        self.w = {}
        self.wx = {}
        self.r = {}


class V:
    __slots__ = ("ap", "buf")

    def __init__(self, ap, buf=None):
        self.ap = ap
        self.buf = buf if buf is not None else Buf()

    def __getitem__(self, k):
        return V(self.ap[k], self.buf)

    def re(self, pat, **kw):
        return V(self.ap.rearrange(pat, **kw), self.buf)

    def bc(self, shape):
        return V(self.ap.to_broadcast(list(shape)), self.buf)

    def un(self, axis):
        return V(self.ap.unsqueeze(axis), self.buf)

    def bitcast(self, dt):
        return V(self.ap.bitcast(dt), self.buf)

    def rev(self):
        a = list(self.ap.ap)
        s, c = a[-1]
        a[-1] = [-s, c]
        return V(bass.AP(self.ap.tensor, self.ap.offset + s * (c - 1), [list(x) for x in a]), self.buf)


def _merge(dst, src):
    for k, v in src.items():
        if dst.get(k, 0) < v:
            dst[k] = v


class Sched:
    ENG = ("pe", "act", "dve", "pool", "sp")
    NDS = {"sp": 16, "act": 16, "pool": 16}

    def __init__(self):
        self.ops = {e: [] for e in self.ENG}
        self.cnt = {e: 0 for e in self.ENG}
        self.waited = {e: {} for e in self.ENG}
        self.dnext = {e: 0 for e in self.NDS}
        self.dval = {e: [0] * n for e, n in self.NDS.items()}
        self.n_ops = 0
        self.pool_consts = set()
        self.regvals = {}

    def _emit(self, eng, fn, reads, writes, dma=False, disjoint=False, sreads=()):
        need = {}
        own = 0
        for b in reads:
            _merge(need, b.w)
        if eng != "pe":
            own = need.get(("c", eng), 0)
        for b in writes:
            _merge(need, b.r)
            _merge(need, b.wx if disjoint else b.w)
        if dma:
            j = self.dnext[eng]
            self.dnext[eng] = (j + 1) % self.NDS[eng]
            key = ("d", eng, j)
            if self.dval[eng][j] > 0:
                need[key] = max(need.get(key, 0), self.dval[eng][j])
            self.dval[eng][j] += 16
            ev = (key, self.dval[eng][j])
            inc = 16
        else:
            need.pop(("c", eng), None)
            if own > 0:
                need[("c", eng)] = own
            self.cnt[eng] += 1
            key = ("c", eng)
            ev = (key, self.cnt[eng])
            inc = 1
        wl = []
        wd = self.waited[eng]
        for k, v in need.items():
            if wd.get(k, 0) < v:
                wd[k] = v
                wl.append((k, v))
        self.ops[eng].append((wl, fn, key, inc))
        self.n_ops += 1
        for b in reads:
            if b.r.get(ev[0], 0) < ev[1]:
                b.r[ev[0]] = ev[1]
        for b in writes:
            if disjoint:
                b.w[ev[0]] = ev[1]
            else:
                b.w = {ev[0]: ev[1]}
                b.wx = {ev[0]: ev[1]}
                b.r = {}

    def I(self, eng, meth, disjoint=False, **kw):
        reads, writes, sreads, res = [], [], [], {}
        for k, v in kw.items():
            if isinstance(v, V):
                (writes if k in ("out", "accum_out") else reads).append(v.buf)
                if k in ("scalar", "scalar1", "scalar2", "scale", "bias", "initial"):
                    sreads.append(v.buf)
                res[k] = v.ap
            else:
                res[k] = v
        self._emit(eng, lambda e: getattr(e, meth)(**res), reads, writes, disjoint=disjoint, sreads=sreads)

    def dma(self, eng, out, in_, disjoint=False, **kw):
        o, i = out.ap, in_.ap
        if eng == "sp" and type(o.tensor).__name__ != "DRamTensorHandle":
            eng = "act"
        self._emit(eng, lambda e: e.dma_start(out=o, in_=i, **kw), [in_.buf], [out.buf], dma=True, disjoint=disjoint)

    def gather(self, out, src, idx, bound=None):
        o, i, x = out.ap, src.ap, idx.ap
        if bound is None:
            fn = lambda e: e.indirect_dma_start(out=o, out_offset=None, in_=i, in_offset=bass.IndirectOffsetOnAxis(ap=x, axis=0))
        else:
            self.pool_consts.add(bound)
            fn = lambda e: e.indirect_dma_start(out=o, out_offset=None, in_=i, in_offset=bass.IndirectOffsetOnAxis(ap=x, axis=0),
                                                bounds_check=self.regvals[bound], oob_is_err=False)
        self._emit("pool", fn, [src.buf, idx.buf], [out.buf], dma=True)

    def scatter(self, out, src, idx, **kw):
        o, i, x = out.ap, src.ap, idx.ap
        self._emit("pool", lambda e: e.indirect_dma_start(out=o, out_offset=bass.IndirectOffsetOnAxis(ap=x, axis=0),
                                                          in_=i, in_offset=None, **kw),
                   [src.buf, idx.buf], [out.buf], dma=True, disjoint=True)

    def barrier(self):
        allv = {("c", e): self.cnt[e] for e in self.ENG if self.cnt[e] > 0}
        for e, vals in self.dval.items():
            for j, v in enumerate(vals):
                if v > 0:
                    allv[("d", e, j)] = v
        for eng in self.ENG:
            wl = []
            wd = self.waited[eng]
            for k, v in allv.items():
                if k == ("c", eng):
                    continue
                if wd.get(k, 0) < v:
                    wd[k] = v
                    wl.append((k, v))
            if wl:
                self.ops[eng].append((wl, None, None, 0))

    def replay(self, nc, stack):
        sems = {}
        for e in self.ENG:
            sems[("c", e)] = stack.enter_context(nc.semaphore("c_" + e))
        for e, n in self.NDS.items():
            for j in range(n):
                sems[("d", e, j)] = stack.enter_context(nc.semaphore("d_%s_%d" % (e, j)))
        block = stack.enter_context(nc.Block())
        ops = self.ops

        def run(name, e):
            for wl, fn, key, inc in ops[name]:
                for k, v in wl:
                    e.wait_ge(sems[k], v)
                if fn is not None:
                    fn(e).then_inc(sems[key], inc)

        @block.tensor
        def _(e):
            run("pe", e)

        @block.scalar
        def _(e):
            run("act", e)

        @block.vector
        def _(e):
            run("dve", e)

        @block.gpsimd
        def _(e):
            for val in sorted(self.pool_consts):
                r = e.alloc_register("bc%d" % val)
                e.reg_mov(r, val)
                self.regvals[val] = e.snap(r)
            run("pool", e)

        @block.sync
        def _(e):
            run("sp", e)


class Prog:
    def __init__(self, nb, layers, final=True):
        self.nb = nb
        self.layers = list(layers)
        self.final = final
        self.S = Sched()
        self.nc = bass.Bass("TRN2", target_bir_lowering=False)
        self.stack = contextlib.ExitStack()
        self.dbufs = {}
        self.in_names = []

    def dram_in(self, name, shape, dt=F32):
        self.in_names.append(name)
        return self.nc.dram_tensor(name, list(shape), dt, kind="ExternalInput").ap()

    def dram_tmp(self, name, shape, dt=F32):
        return self.nc.dram_tensor(name, list(shape), dt, kind="Internal").ap()

    def dv(self, ap, key):
        b = self.dbufs.get(key)
        if b is None:
            b = self.dbufs[key] = Buf()
        return V(ap, b)

    def arena_reset(self):
        self.S.barrier()
        self.aoff = self.abase

    def alloc(self, shape, dt=F32):
        n = 1
        for s in shape[1:]:
            n *= s
        words = n if dt in (F32, I32) else (n + 1) // 2
        words = (words + 7) // 8 * 8
        assert self.aoff + words <= self.awords, ("SBUF arena overflow", self.aoff, words, self.awords)
        ap = self.big[:, self.aoff:self.aoff + words]
        self.aoff += words
        if dt != F32:
            ap = ap.bitcast(dt)
        ap = ap[0:shape[0], 0:n]
        if len(shape) > 2:
            names = " ".join("d%d" % i for i in range(len(shape) - 1))
            kw = {"d%d" % i: shape[i + 1] for i in range(len(shape) - 1)}
            ap = ap.rearrange("p (%s) -> p %s" % (names, names), **kw)
        return V(ap)

    def perm(self, shape, dt=F32):
        v = self.alloc(shape, dt)
        self.abase = self.aoff
        return v

    def build(self):
        nc, S, nb = self.nc, self.S, self.nb
        st = self.stack
        self.awords = 53000
        self.big = st.enter_context(nc.sbuf_tensor("big", [128, self.awords], F32))
        self.aoff = 0
        self.abase = 0
        self.psum = [V(st.enter_context(nc.psum_tensor("ps%d" % i, [128, 512], F32))[:, :]) for i in range(8)]
        nl = len(self.layers)
        nlat, nctx = nb * SEQ, nb * CTX

        self.x_in = self.dram_in("x", [nlat, D])
        self.c_in = self.dram_in("c", [nb, D])
        self.ctx_in = self.dram_in("ctx", [nctx, D])
        self.cctx_in = self.dram_in("c_ctx", [1, D])
        self.final_g = self.dram_in("final_g", [1, D])
        self.W = {}
        for li in self.layers:
            kind = li % 3
            w = {}
            w["ada_w"] = self.dram_in("ada_w_%d" % li, [D, 6 * D])
            w["ada_b"] = self.dram_in("ada_b_%d" % li, [1, 6 * D])
            w["n1"] = self.dram_in("norm1_g_%d" % li, [1, D])
            w["n2"] = self.dram_in("norm2_g_%d" % li, [1, D])
            w["wr"] = self.dram_in("moe_wr_%d" % li, [D, 36])
            w["br"] = self.dram_in("moe_br_%d" % li, [1, 36])
            w["w13"] = self.dram_in("moe_w13_%d" % li, [NE * 128 * 4, 2048])
            w["w2"] = self.dram_in("moe_w2_%d" % li, [NE * 128 * 2, 2048])
            if kind == 0:
                w["w_in"] = self.dram_in("conf_w_in_%d" % li, [D, 2 * D])
                w["dw"] = self.dram_in("conf_dw_%d" % li, [31, D])
                w["dw_b"] = self.dram_in("conf_dw_b_%d" % li, [1, D])
                w["ln_g"] = self.dram_in("conf_ln_g_%d" % li, [1, D])
                w["ln_b"] = self.dram_in("conf_ln_b_%d" % li, [1, D])
                w["w_out"] = self.dram_in("conf_w_out_%d" % li, [D, D])
            elif kind == 1:
                w["w_in"] = self.dram_in("sc_w_in_%d" % li, [D, 3 * D])
                w["cv"] = self.dram_in("sc_conv_%d" % li, [3, D])
                w["w_out"] = self.dram_in("sc_w_out_%d" % li, [D, D])
            else:
                w["a_re"] = self.dram_in("s5_a_re_%d" % li, [2, 64, 64])
                w["a_im"] = self.dram_in("s5_a_im_%d" % li, [2, 64, 64])
                w["ldt"] = self.dram_in("s5_log_dt_%d" % li, [2, 64])
                w["b_re"] = self.dram_in("s5_b_re_%d" % li, [2, 64, 64, 16])
                w["b_im"] = self.dram_in("s5_b_im_%d" % li, [2, 64, 64, 16])
                w["c_re"] = self.dram_in("s5_c_re_%d" % li, [2, 64, 16, 64])
                w["c_im"] = self.dram_in("s5_c_im_%d" % li, [2, 64, 16, 64])
                w["d"] = self.dram_in("s5_d_%d" % li, [1, D])
                w["w_glu"] = self.dram_in("s5_w_glu_%d" % li, [D, 2 * D])
            self.W[li] = w
        self.y_out = nc.dram_tensor("y", [nlat, D], F32, kind="ExternalOutput").ap()

        self.xs = self.dram_tmp("xs", [nlat, D])
        self.cs = self.dram_tmp("cs", [nctx, D])
        self.MOD = self.dram_tmp("modv", [nl, 6, nb + 1, D])
        ntok_max = nlat + nctx
        self.nslot_max = ((2 * ntok_max + NE * (TS - 1)) + TS - 1) // TS * TS
        self.H2 = self.dram_tmp("h2", [ntok_max, D], BF16)
        self.H2S = self.dram_tmp("h2s", [self.nslot_max, D], BF16)
        self.YS = self.dram_tmp("ys", [self.nslot_max, D])
        self.HT = self.dram_tmp("ht", [D, ntok_max], BF16)
        self.W13B = self.dram_tmp("w13b", [NE * 128 * 4, 2048], BF16)
        self.W2B = self.dram_tmp("w2b", [NE * 128 * 2, 2048], BF16)
        self.YT = self.dram_tmp("yt", [D, ntok_max], BF16)

        self.identf = self.perm([128, 128])
        self.ident = self.perm([128, 128], BF16)
        self.ltri = self.perm([128, 128], BF16)
        self.ones = self.perm([128, 128], BF16)
        self.onesm = self.perm([128, 128], BF16)
        self.eps_rms = self.perm([128, 1])
        self.eps_ln = self.perm([128, 1])
        self.iota_p = self.perm([128, 1])
        self.one_c = self.perm([128, 1])
        tmpf = self.alloc([128, 128])
        S.I("pool", "iota", out=self.identf, pattern=[[1, 128]], base=0, channel_multiplier=-1,
            allow_small_or_imprecise_dtypes=True)
        S.I("dve", "tensor_single_scalar", out=tmpf, in_=self.identf, scalar=0.0, op=ALU.is_gt)
        S.I("dve", "tensor_copy", out=self.ltri, in_=tmpf)
        S.I("dve", "tensor_single_scalar", out=self.identf, in_=self.identf, scalar=0.0, op=ALU.is_equal)
        S.I("dve", "tensor_copy", out=self.ident, in_=self.identf)
        self._memset(self.ones, 1.0)
        self._memset(self.onesm, 1.0 / 1024.0)
        self._memset(self.eps_rms, RMS_EPS)
        self._memset(self.eps_ln, LN_EPS)
        self._memset(self.one_c, 1.0)
        S.I("pool", "iota", out=self.iota_p, pattern=[[0, 1]], base=0, channel_multiplier=1,
            allow_small_or_imprecise_dtypes=True)
        self.aoff = self.abase

        self.arena_reset()
        z = self.alloc([128, 8 * D], BF16)
        self._memset(z, 0.0)
        rows = self.nslot_max
        r0 = 0
        while r0 < rows:
            n = min(1024, rows - r0)
            assert n % 128 == 0
            S.dma("sp", self.dv(self.H2S[r0:r0 + n, :].rearrange("(p a) d -> p (a d)", p=128), "h2s"),
                  z[:, 0:(n // 128) * D], disjoint=True)
            r0 += n

        self.prologue()
        first = True
        for idx, li in enumerate(self.layers):
            kind = li % 3
            upd = li < DEPTH - 1
            need_ctx = upd or kind == 2
            if "moe" not in SKIP:
                self.precast(li)
            if "mixer" in SKIP:
                self.copy_x(first, upd)
            elif kind == 0:
                self.conformer(idx, li, first, upd)
            elif kind == 1:
                self.shortconv(idx, li, first, upd)
            else:
                self.s5(idx, li, first, upd)
            first = False
            if "moe" not in SKIP:
                self.moe(idx, li, upd)
        if self.final:
            self.final_norm()
        S.barrier()
        S.replay(nc, st)
        st.close()
        return nc

    def precast(self, li):
        w = self.W[li]
        for (src, dst, key, nrows) in ((w["w13"], self.W13B, "w13b", NE * 128 * 4), (w["w2"], self.W2B, "w2b", NE * 128 * 2)):
            for r0 in range(0, nrows, 1024):
                sv = src[r0:r0 + 1024, :].rearrange("(p a) n -> p a n", p=128)
                dv_ = dst[r0:r0 + 1024, :].rearrange("(p a) n -> p a n", p=128)
                self.S.dma("pool", self.dv(dv_, key), self.dv(sv, "w_in_%s_%d" % (key, li)), disjoint=True)

    def copy_x(self, first, upd):
        self.arena_reset()
        t = [self.alloc([128, D]) for _ in range(4)]
        i = 0
        for lat in ((True, False) if upd else (True,)):
            src, skey = self.xsrc(first, lat)
            dst, dkey = self.xdst(lat)
            n = self.nb * (SEQ if lat else CTX)
            for r0 in range(0, n, 128):
                self.S.dma("sp", t[i % 4], self.dv(src[r0:r0 + 128, :], (skey, r0)))
                self.S.dma("sp", self.dv(dst[r0:r0 + 128, :], (dkey, r0)), t[i % 4])
                i += 1

    def dbg(self, name, v, dt=F32):
        if not DEBUG:
            return
        shape = list(v.ap.shape)
        o = self.nc.dram_tensor("dbg_" + name, shape, dt, kind="ExternalOutput").ap()
        self.S.dma("sp", self.dv(o, "dbg_" + name), v)

    def _memset(self, v, val):
        ap = v.ap
        self.S._emit("dve", lambda e: e.memset(ap, val), [], [v.buf])

    def xsrc(self, first, lat):
        if lat:
            return (self.x_in if first else self.xs), ("xin" if first else "xs")
        return (self.ctx_in if first else self.cs), ("cin" if first else "cs")

    def xdst(self, lat):
        return (self.xs, "xs") if lat else (self.cs, "cs")

    def load_rows_bc(self, dst, src_row_ap, key, nparts=128, eng="sp"):
        n = src_row_ap.shape[-1]
        self.S.dma(eng, dst, self.dv(src_row_ap.to_broadcast([nparts, n]), key))

    def load_featT(self, dst, row_ap, key):
        self.S.dma("sp", dst, self.dv(row_ap.rearrange("o (k p) -> p (o k)", p=128), key), allow_slow_non_contiguous=True)

    def prologue(self):
        S, nb, nc = self.S, self.nb, self.nc
        self.arena_reset()
        ns = nb + 1
        cT = self.alloc([128, KC, ns])
        for b in range(nb):
            self.load_featT(cT[:, :, b], self.c_in[b:b + 1, :], "c_in")
        self.load_featT(cT[:, :, nb], self.cctx_in, "cctx_in")
        S.I("act", "activation", out=cT, in_=cT, func=AF.Silu)
        mrow = self.alloc([ns, 6 * D])
        abrow = self.alloc([ns, 6 * D])
        n1 = self.alloc([ns, D])
        n2 = self.alloc([ns, D])
        orow = self.alloc([ns, 6, D])
        awt = [self.alloc([128, KC, 512]) for _ in range(2)]
        for idx, li in enumerate(self.layers):
            w = self.W[li]
            self.load_rows_bc(abrow, w["ada_b"], "ada_b%d" % li, nparts=ns)
            self.load_rows_bc(n1, w["n1"], "n1_%d" % li, nparts=ns)
            self.load_rows_bc(n2, w["n2"], "n2_%d" % li, nparts=ns)
            awv = w["ada_w"].rearrange("(k p) n -> p k n", p=128)
            for cg in range(12):
                t = awt[cg % 2]
                S.dma("sp" if cg % 2 == 0 else "act", t, self.dv(awv[:, :, cg * 512:(cg + 1) * 512], "ada_w%d" % li))
                ps = self.psum[cg % 2]
                for k in range(KC):
                    S.I("pe", "matmul", out=ps[0:ns, :], lhsT=cT[:, k, :], rhs=t[:, k, :], start=(k == 0), stop=(k == KC - 1))
                S.I("dve", "tensor_tensor", out=mrow[:, cg * 512:(cg + 1) * 512], in0=ps[0:ns, :],
                    in1=abrow[:, cg * 512:(cg + 1) * 512], op=ALU.add)
            S.I("dve", "scalar_tensor_tensor", out=orow[:, 0, :], in0=mrow[:, D:2 * D], scalar=1.0, in1=n1, op0=ALU.add, op1=ALU.mult)
            S.I("dve", "tensor_copy", out=orow[:, 1, :], in_=mrow[:, 0:D])
            S.I("dve", "tensor_copy", out=orow[:, 2, :], in_=mrow[:, 2 * D:3 * D])
            S.I("dve", "scalar_tensor_tensor", out=orow[:, 3, :], in0=mrow[:, 4 * D:5 * D], scalar=1.0, in1=n2, op0=ALU.add, op1=ALU.mult)
            S.I("dve", "tensor_copy", out=orow[:, 4, :], in_=mrow[:, 3 * D:4 * D])
            S.I("dve", "tensor_copy", out=orow[:, 5, :], in_=mrow[:, 5 * D:6 * D])
            S.dma("sp", self.dv(self.MOD[idx].rearrange("k s d -> s k d"), "mod%d" % idx), orow)

    def mod_row(self, idx, kind, s):
        return self.dv(self.MOD[idx, kind, s:s + 1, :], "mod%d" % idx)

    def load_modT(self, idx):
        ns = self.nb + 1
        sT = self.alloc([128, KC, ns])
        bT = self.alloc([128, KC, ns])
        for s in range(ns):
            self.load_featT(sT[:, :, s], self.MOD[idx, 0, s:s + 1, :], "mod%d" % idx)
            self.load_featT(bT[:, :, s], self.MOD[idx, 1, s:s + 1, :], "mod%d" % idx)
        return sT, bT

    def norm_bufs(self, depth=2):
        nbuf = {"depth": depth}
        nbuf["xt"] = [self.alloc([128, D]) for _ in range(depth)]
        nbuf["sq"] = self.alloc([128, D])
        nbuf["x16"] = [self.alloc([128, D], BF16) for _ in range(2)]
        nbuf["ss"] = [self.alloc([128, 1]) for _ in range(depth)]
        nbuf["rs"] = [self.alloc([128, 1]) for _ in range(depth)]
        nbuf["tmp"] = self.alloc([128, KC, 128])
        nbuf["i"] = 0
        return nbuf

    def rms_tile(self, nbuf, src_v):
        S = self.S
        i = nbuf["i"] % nbuf["depth"]
        nbuf["i"] += 1
        xt, ss, rs = nbuf["xt"][i], nbuf["ss"][i], nbuf["rs"][i]
        S.dma("sp", xt, src_v)
        S.I("act", "activation", out=nbuf["sq"], in_=xt, func=AF.Square)
        S.I("dve", "reduce_sum", out=ss, in_=nbuf["sq"], axis=AX.X)
        S.I("act", "activation", out=rs, in_=ss, func=AF.Sqrt, scale=1.0 / D, bias=self.eps_rms)
        S.I("dve", "reciprocal", out=rs, in_=rs)
        return xt, rs, i

    def norm_transpose(self, nbuf, src_v, sT, bT, s, dst):
        S = self.S
        xt, rs, i = self.rms_tile(nbuf, src_v)
        x16 = nbuf["x16"][i % 2]
        S.I("act", "activation", out=x16, in_=xt, func=AF.Identity, scale=rs)
        pt = self.psum[0].bitcast(BF16)
        for k in range(KC):
            S.I("pe", "transpose", out=pt[:, k * 128:(k + 1) * 128], in_=x16[:, k * 128:(k + 1) * 128], identity=self.ident)
        tmp = nbuf["tmp"]
        S.I("dve", "tensor_tensor", out=tmp, in0=pt.re("p (k t) -> p k t", k=KC), in1=sT[:, :, s:s + 1].bc([128, KC, 128]), op=ALU.mult)
        S.I("dve", "tensor_tensor", out=dst, in0=tmp, in1=bT[:, :, s:s + 1].bc([128, KC, 128]), op=ALU.add)

    def seqs(self, with_ctx):
        out = [("lat", b) for b in range(self.nb)]
        if with_ctx:
            out.append(("ctx", self.nb))
        return out

    def load_w_bf16(self, dst, w_ap, key, ncols):
        wv = w_ap.rearrange("(k p) n -> p k n", p=128)
        for k in range(KC):
            for c0 in range(0, ncols, 2048):
                c1 = min(ncols, c0 + 2048)
                self.S.dma("pool", dst[:, k, c0:c1], self.dv(wv[:, k, c0:c1], key), disjoint=True)

    def residual_out(self, ps_halves, g_bc, src_v, dst_v, obuf, xbuf):
        S = self.S
        S.dma("sp", xbuf, src_v)
        for h in range(2):
            S.I("dve", "tensor_tensor", out=obuf[:, h * 512:(h + 1) * 512], in0=ps_halves[h], in1=g_bc[:, h * 512:(h + 1) * 512], op=ALU.mult)
        S.I("pool", "tensor_tensor", out=obuf, in0=obuf, in1=xbuf, op=ALU.add)
        S.dma("sp", dst_v, obuf)

    def conformer(self, idx, li, first, upd):
        S, nb, w = self.S, self.nb, self.W[li]
        self.arena_reset()
        sT, bT = self.load_modT(idx)
        w_in = self.alloc([128, KC, 2 * D], BF16)
        w_out = self.alloc([128, KC, D], BF16)
        self.load_w_bf16(w_in, w["w_in"], "cw_in%d" % li, 2 * D)
        self.load_w_bf16(w_out, w["w_out"], "cw_out%d" % li, D)
        dwT = self.alloc([128, KC, 31])
        for k in range(KC):
            S.dma("sp", dwT[:, k, :], self.dv(w["dw"][:, k * 128:(k + 1) * 128].rearrange("t p -> p t"), "dw%d" % li), allow_slow_non_contiguous=True)
        dwb = self.alloc([128, KC])
        lng = self.alloc([128, KC])
        lnb = self.alloc([128, KC])
        self.load_featT(dwb, w["dw_b"], "dwb%d" % li)
        self.load_featT(lng, w["ln_g"], "lng%d" % li)
        self.load_featT(lnb, w["ln_b"], "lnb%d" % li)
        nbuf = self.norm_bufs()
        g1 = [self.alloc([128, D]) for _ in range(2)]
        hT = [self.alloc([128, KC, 512], BF16) for _ in range(2)]
        zbuf = self.alloc([128, KC, SEQ], BF16)
        sg = [self.alloc([128, 512]) for _ in range(2)]
        dwd = [self.alloc([128, 31, 128], BF16) for _ in range(2)]
        vall = self.alloc([128, KC, SEQ], BF16)
        vsq = [self.alloc([128, 512], BF16) for _ in range(2)]
        s16 = self.alloc([128, KC, 512], BF16)
        msq = self.alloc([128, 512])
        mean = self.alloc([128, 512])
        rstd = self.alloc([128, 512])
        nmr = self.alloc([128, 512])
        t1 = [self.alloc([128, 512]) for _ in range(2)]
        obuf = [self.alloc([128, D])] * 2
        P = self.psum
        gi = 0
        di = 0
        ci2 = 0
        for si, (sk, s) in enumerate(self.seqs(upd)):
            lat = sk == "lat"
            ntok = SEQ if lat else nb * CTX
            src, skey = self.xsrc(first, lat)
            dst, dkey = self.xdst(lat)
            base = s * SEQ if lat else 0
            GS = min(512, ntok)
            ng = ntok // GS
            gb = g1[si % 2]
            self.load_rows_bc(gb, self.MOD[idx, 2, s:s + 1, :], "mod%d" % idx)
            for g in range(ng):
                h = hT[gi % 2]
                gi += 1
                for j in range(GS // 128):
                    r0 = base + g * GS + j * 128
                    self.norm_transpose(nbuf, self.dv(src[r0:r0 + 128, :], (skey, r0)), sT, bT, s, h[:, :, j * 128:(j + 1) * 128])
                for m in range(KC):
                    pv, pg = P[1 + m % 2], P[3 + m % 2]
                    for k in range(KC):
                        S.I("pe", "matmul", out=pv[:, 0:GS], lhsT=w_in[:, k, m * 128:(m + 1) * 128], rhs=h[:, k, 0:GS], start=(k == 0), stop=(k == KC - 1))
                    for k in range(KC):
                        S.I("pe", "matmul", out=pg[:, 0:GS], lhsT=w_in[:, k, D + m * 128:D + (m + 1) * 128], rhs=h[:, k, 0:GS], start=(k == 0), stop=(k == KC - 1))
                    sgt = sg[m % 2]
                    S.I("act", "activation", out=sgt[:, 0:GS], in_=pg[:, 0:GS], func=AF.Sigmoid)
                    S.I("dve", "tensor_tensor", out=zbuf[:, m, g * GS:(g + 1) * GS], in0=pv[:, 0:GS], in1=sgt[:, 0:GS], op=ALU.mult)
            order = [15] + [k for k in range(31) if k != 15]
            for m in range(KC):
                dd = dwd[di % 2]
                di += 1
                for k in range(31):
                    S.I("dve", "tensor_single_scalar", out=dd[:, k, :], in_=self.ident, scalar=dwT[:, m, k:k + 1], op=ALU.mult)
                for g in range(ng):
                    g0 = g * GS
                    pc = P[1 + ci2 % 2]
                    ci2 += 1
                    for n_, k in enumerate(order):
                        d = k - 15
                        if lat:
                            dt_ = d * 64
                            lo, hi = max(g0, -dt_), min(g0 + GS, SEQ - dt_)
                            if lo >= hi:
                                continue
                            S.I("pe", "matmul", out=pc[:, lo - g0:hi - g0], lhsT=dd[:, k, :], rhs=zbuf[:, m, lo + dt_:hi + dt_],
                                start=(n_ == 0), stop=(n_ == 30), skip_group_check=True)
                        else:
                            lo, hi = max(0, -d), min(CTX, CTX - d)
                            o = pc[:, 0:GS].re("p (s t) -> p s t", t=CTX)[:, :, lo:hi]
                            r = zbuf[:, m, g0:g0 + GS].re("p (s t) -> p s t", t=CTX)[:, :, lo + d:hi + d]
                            S.I("pe", "matmul", out=o, lhsT=dd[:, k, :], rhs=r, start=(n_ == 0), stop=(n_ == 30), skip_group_check=True)
                    S.I("act", "activation", out=vall[:, m, g0:g0 + GS], in_=pc[:, 0:GS], func=AF.Identity, bias=dwb[:, m:m + 1])
            for g in range(ng):
                g0 = g * GS
                v16 = vall[:, :, g0:g0 + GS]
                for m in range(KC):
                    vq = vsq[m % 2]
                    S.I("act", "activation", out=vq[:, 0:GS], in_=v16[:, m, :], func=AF.Square)
                    S.I("pe", "matmul", out=P[5][:, 0:GS], lhsT=self.onesm, rhs=v16[:, m, :], start=(m == 0), stop=(m == KC - 1))
                    S.I("pe", "matmul", out=P[6][:, 0:GS], lhsT=self.onesm, rhs=vq[:, 0:GS], start=(m == 0), stop=(m == KC - 1))
                S.I("act", "activation", out=mean[:, 0:GS], in_=P[5][:, 0:GS], func=AF.Identity)
                S.I("act", "activation", out=msq[:, 0:GS], in_=P[5][:, 0:GS], func=AF.Square)
                S.I("dve", "tensor_tensor", out=rstd[:, 0:GS], in0=P[6][:, 0:GS], in1=msq[:, 0:GS], op=ALU.subtract)
                S.I("act", "activation", out=rstd[:, 0:GS], in_=rstd[:, 0:GS], func=AF.Sqrt, bias=self.eps_ln)
                S.I("dve", "reciprocal", out=rstd[:, 0:GS], in_=rstd[:, 0:GS])
                S.I("dve", "scalar_tensor_tensor", out=nmr[:, 0:GS], in0=mean[:, 0:GS], scalar=-1.0, in1=rstd[:, 0:GS], op0=ALU.mult, op1=ALU.mult)
                for m in range(KC):
                    tt = t1[m % 2]
                    S.I("dve", "tensor_tensor", out=tt[:, 0:GS], in0=v16[:, m, :], in1=rstd[:, 0:GS], op=ALU.mult)
                    S.I("pool", "tensor_tensor", out=tt[:, 0:GS], in0=tt[:, 0:GS], in1=nmr[:, 0:GS], op=ALU.add)
                    S.I("act", "activation", out=s16[:, m, 0:GS], in_=tt[:, 0:GS], func=AF.Silu, scale=lng[:, m:m + 1], bias=lnb[:, m:m + 1])
                for j in range(GS // 128):
                    r0 = base + g0 + j * 128
                    for h_ in range(2):
                        for k in range(KC):
                            S.I("pe", "matmul", out=P[3 + h_], lhsT=s16[:, k, j * 128:(j + 1) * 128], rhs=w_out[:, k, h_ * 512:(h_ + 1) * 512],
                                start=(k == 0), stop=(k == KC - 1))
                    ob = obuf[j % 2]
                    self.residual_out([P[3], P[4]], gb, self.dv(src[r0:r0 + 128, :], (skey, r0)), self.dv(dst[r0:r0 + 128, :], (dkey, r0)),
                                      ob, nbuf["xt"][j % 2])

    def shortconv(self, idx, li, first, upd):
        S, nb, w = self.S, self.nb, self.W[li]
        self.arena_reset()
        sT, bT = self.load_modT(idx)
        w_in = self.alloc([128, KC, 3 * D], BF16)
        w_out = self.alloc([128, KC, D], BF16)
        self.load_w_bf16(w_in, w["w_in"], "sw_in%d" % li, 3 * D)
        self.load_w_bf16(w_out, w["w_out"], "sw_out%d" % li, D)
        cvT = self.alloc([128, KC, 3])
        for k in range(KC):
            S.dma("sp", cvT[:, k, :], self.dv(w["cv"][:, k * 128:(k + 1) * 128].rearrange("t p -> p t"), "cv%d" % li), allow_slow_non_contiguous=True)
        nbuf = self.norm_bufs()
        g1 = [self.alloc([128, D]) for _ in range(2)]
        hT = [self.alloc([128, KC, 512], BF16) for _ in range(2)]
        gcs = [self.alloc([128, 512]) for _ in range(2)]
        q = [self.alloc([128, 512]) for _ in range(2)]
        cc = [self.alloc([128, 512]) for _ in range(2)]
        p16 = self.alloc([128, KC, 512], BF16)
        obuf = [self.alloc([128, D]) for _ in range(2)]
        P = self.psum
        gi = 0
        for si, (sk, s) in enumerate(self.seqs(upd)):
            lat = sk == "lat"
            ntok = SEQ if lat else nb * CTX
            src, skey = self.xsrc(first, lat)
            dst, dkey = self.xdst(lat)
            base = s * SEQ if lat else 0
            GS = min(512, ntok)
            ng = ntok // GS
            RL = 64 if lat else CTX
            gb = g1[si % 2]
            self.load_rows_bc(gb, self.MOD[idx, 2, s:s + 1, :], "mod%d" % idx)
            for g in range(ng):
                g0 = g * GS
                h = hT[gi % 2]
                gi += 1
                for j in range(GS // 128):
                    r0 = base + g0 + j * 128
                    self.norm_transpose(nbuf, self.dv(src[r0:r0 + 128, :], (skey, r0)), sT, bT, s, h[:, :, j * 128:(j + 1) * 128])
                for m in range(KC):
                    pb, pc_, pv = P[1], P[2 + m % 2], P[4 + m % 2]
                    for (pp, off) in ((pc_, D), (pv, 2 * D)):
                        for k in range(KC):
                            S.I("pe", "matmul", out=pp[:, 0:GS], lhsT=w_in[:, k, off + m * 128:off + (m + 1) * 128], rhs=h[:, k, 0:GS],
                                start=(k == 0), stop=(k == KC - 1))
                    gct, qt, ct = gcs[m % 2], q[m % 2], cc[m % 2]
                    S.I("act", "activation", out=gct[:, 0:GS], in_=pc_[:, 0:GS], func=AF.Identity)
                    S.I("dve", "tensor_tensor", out=qt[:, 0:GS], in0=pv[:, 0:GS], in1=gct[:, 0:GS], op=ALU.mult)
                    S.I("act", "activation", out=ct[:, 0:GS], in_=qt[:, 0:GS], func=AF.Identity, scale=cvT[:, m, 1:2])
                    q3 = qt[:, 0:GS].re("p (r c) -> p r c", c=RL)
                    c3 = ct[:, 0:GS].re("p (r c) -> p r c", c=RL)
                    S.I("dve", "scalar_tensor_tensor", out=c3[:, :, 1:RL], in0=q3[:, :, 0:RL - 1], scalar=cvT[:, m, 0:1], in1=c3[:, :, 1:RL],
                        op0=ALU.mult, op1=ALU.add)
                    S.I("dve", "scalar_tensor_tensor", out=c3[:, :, 0:RL - 1], in0=q3[:, :, 1:RL], scalar=cvT[:, m, 2:3], in1=c3[:, :, 0:RL - 1],
                        op0=ALU.mult, op1=ALU.add)
                    for k in range(KC):
                        S.I("pe", "matmul", out=pb[:, 0:GS], lhsT=w_in[:, k, m * 128:(m + 1) * 128], rhs=h[:, k, 0:GS], start=(k == 0), stop=(k == KC - 1))
                    S.I("dve", "tensor_tensor", out=p16[:, m, 0:GS], in0=pb[:, 0:GS], in1=ct[:, 0:GS], op=ALU.mult)
                for j in range(GS // 128):
                    r0 = base + g0 + j * 128
                    for h_ in range(2):
                        for k in range(KC):
                            S.I("pe", "matmul", out=P[6 + h_], lhsT=p16[:, k, j * 128:(j + 1) * 128], rhs=w_out[:, k, h_ * 512:(h_ + 1) * 512],
                                start=(k == 0), stop=(k == KC - 1))
                    self.residual_out([P[6], P[7]], gb, self.dv(src[r0:r0 + 128, :], (skey, r0)), self.dv(dst[r0:r0 + 128, :], (dkey, r0)),
                                      obuf[j % 2], nbuf["xt"][j % 2])

    def s5(self, idx, li, first, upd):
        import math
        S, nb, w, P = self.S, self.nb, self.W[li], self.psum
        NTK = CTX + SEQ
        HTv = self.HT.rearrange("(k p) t -> p k t", p=128)
        YTv = self.YT.rearrange("(k p) t -> p k t", p=128)
        self.arena_reset()
        sT, bT = self.load_modT(idx)
        nbuf = self.norm_bufs()
        ht = [self.alloc([128, KC, 128], BF16) for _ in range(3)]
        i = 0
        for b in range(nb):
            for lat, n_t in ((False, CTX // 128), (True, SEQ // 128)):
                src, skey = self.xsrc(first, lat)
                for j in range(n_t):
                    r0 = (b * SEQ if lat else b * CTX) + j * 128
                    col = b * NTK + (CTX if lat else 0) + j * 128
                    t = ht[i % 3]
                    i += 1
                    self.norm_transpose(nbuf, self.dv(src[r0:r0 + 128, :], (skey, r0)), sT, bT, (b if lat else nb), t)
                    S.dma("sp", self.dv(HTv[:, :, col:col + 128], "ht"), t, disjoint=True)
        self.arena_reset()
        ND = 64
        f2 = lambda v: v.re("p d g -> p (d g)")
        are, aim, ldt = (self.alloc([128, 2, 32]) for _ in range(3))
        for d in range(2):
            S.dma("sp", are[:, d, :], self.dv(w["a_re"][d].rearrange("(G g) p -> (g p) G", g=2), "s5a"), allow_slow_non_contiguous=True)
            S.dma("sp", aim[:, d, :], self.dv(w["a_im"][d].rearrange("(G g) p -> (g p) G", g=2), "s5a"), allow_slow_non_contiguous=True)
            for g2 in range(2):
                srcv = w["ldt"][d:d + 1, :].rearrange("o (G g) -> o g G", g=2)[:, g2, :]
                S.dma("sp", ldt[g2 * 64:(g2 + 1) * 64, d, :], self.dv(srcv.to_broadcast([64, 32]), "s5a"), allow_slow_non_contiguous=True)
        names = ("dt", "mag", "ang", "c", "s", "ta", "tb", "den", "nre", "kre", "kim", "abr", "abi")
        T_ = {n: self.alloc([128, ND]) for n in names}
        hpi = self.alloc([128, 1])
        self._memset(hpi, math.pi / 2)
        A, B_ = f2(are), f2(aim)
        S.I("dve", "tensor_single_scalar", out=A, in_=A, scalar=-1e-4, op=ALU.min)
        S.I("act", "activation", out=T_["dt"], in_=f2(ldt), func=AF.Exp)
        S.I("dve", "tensor_tensor", out=T_["ta"], in0=T_["dt"], in1=A, op=ALU.mult)
        S.I("act", "activation", out=T_["mag"], in_=T_["ta"], func=AF.Exp)
        S.I("dve", "tensor_tensor", out=T_["ang"], in0=T_["dt"], in1=B_, op=ALU.mult)
        S.I("act", "activation", out=T_["s"], in_=T_["ang"], func=AF.Sin, scale=1.0 / 16)
        S.I("act", "activation", out=T_["c"], in_=T_["ang"], func=AF.Sin, scale=1.0 / 16, bias=hpi)

        def csquare(c, s, ta, tb):
            S.I("dve", "tensor_tensor", out=ta, in0=c, in1=c, op=ALU.mult)
            S.I("dve", "tensor_tensor", out=tb, in0=s, in1=s, op=ALU.mult)
            S.I("dve", "scalar_tensor_tensor", out=s, in0=c, scalar=2.0, in1=s, op0=ALU.mult, op1=ALU.mult)
            S.I("dve", "tensor_tensor", out=c, in0=ta, in1=tb, op=ALU.subtract)
        for _ in range(4):
            csquare(T_["c"], T_["s"], T_["ta"], T_["tb"])
        NP2 = 12
        Er = self.alloc([128, NP2, ND])
        Ei = self.alloc([128, NP2, ND])
        S.I("dve", "tensor_copy", out=Er[:, 0, :], in_=T_["c"])
        S.I("dve", "tensor_copy", out=Ei[:, 0, :], in_=T_["s"])
        for j in range(1, NP2):
            S.I("dve", "tensor_copy", out=Er[:, j, :], in_=Er[:, j - 1, :])
            S.I("dve", "tensor_copy", out=Ei[:, j, :], in_=Ei[:, j - 1, :])
            csquare(Er[:, j, :], Ei[:, j, :], T_["ta"], T_["tb"])
        S.I("dve", "tensor_tensor", out=T_["abr"], in0=T_["mag"], in1=T_["c"], op=ALU.mult)
        S.I("dve", "tensor_tensor", out=T_["abi"], in0=T_["mag"], in1=T_["s"], op=ALU.mult)
        S.I("dve", "tensor_tensor", out=T_["ta"], in0=A, in1=A, op=ALU.mult)
        S.I("dve", "tensor_tensor", out=T_["tb"], in0=B_, in1=B_, op=ALU.mult)
        S.I("dve", "tensor_tensor", out=T_["den"], in0=T_["ta"], in1=T_["tb"], op=ALU.add)
        S.I("dve", "reciprocal", out=T_["den"], in_=T_["den"])
        S.I("dve", "tensor_single_scalar", out=T_["nre"], in_=T_["abr"], scalar=-1.0, op=ALU.add)
        S.I("dve", "tensor_tensor", out=T_["ta"], in0=T_["nre"], in1=A, op=ALU.mult)
        S.I("dve", "tensor_tensor", out=T_["tb"], in0=T_["abi"], in1=B_, op=ALU.mult)
        S.I("dve", "tensor_tensor", out=T_["kre"], in0=T_["ta"], in1=T_["tb"], op=ALU.add)
        S.I("dve", "tensor_tensor", out=T_["kre"], in0=T_["kre"], in1=T_["den"], op=ALU.mult)
        S.I("dve", "tensor_tensor", out=T_["ta"], in0=T_["abi"], in1=A, op=ALU.mult)
        S.I("dve", "tensor_tensor", out=T_["tb"], in0=T_["nre"], in1=B_, op=ALU.mult)
        S.I("dve", "tensor_tensor", out=T_["kim"], in0=T_["ta"], in1=T_["tb"], op=ALU.subtract)
        S.I("dve", "tensor_tensor", out=T_["kim"], in0=T_["kim"], in1=T_["den"], op=ALU.mult)
        mag = T_["mag"]
        lB = self.alloc([32, ND, 2, 128], BF16)
        lC = self.alloc([128, ND, 2, 32], BF16)
        dvec = self.alloc([128, KC])
        self.load_featT(dvec, w["d"], "s5d")
        keep = self.aoff
        bre = self.alloc([128, 2, 32, 16])
        bim = self.alloc([128, 2, 32, 16])
        for d in range(2):
            S.dma("sp", bre[:, d], self.dv(w["b_re"][d].rearrange("(G g) p c -> (g p) G c", g=2), "s5b"))
            S.dma("sp", bim[:, d], self.dv(w["b_im"][d].rearrange("(G g) p c -> (g p) G c", g=2), "s5b"))
        bbr = self.alloc([128, ND, 16])
        bbi = self.alloc([128, ND, 16])
        tq = self.alloc([128, ND, 16])
        brf, bif = bre.re("p d g c -> p (d g) c"), bim.re("p d g c -> p (d g) c")
        kr3 = T_["kre"].un(2).bc([128, ND, 16])
        ki3 = T_["kim"].un(2).bc([128, ND, 16])
        S.I("dve", "tensor_tensor", out=bbr, in0=brf, in1=kr3, op=ALU.mult)
        S.I("dve", "tensor_tensor", out=tq, in0=bif, in1=ki3, op=ALU.mult)
        S.I("dve", "tensor_tensor", out=bbr, in0=bbr, in1=tq, op=ALU.subtract)
        S.I("dve", "tensor_tensor", out=bbi, in0=bif, in1=kr3, op=ALU.mult)
        S.I("dve", "tensor_tensor", out=tq, in0=brf, in1=ki3, op=ALU.mult)
        S.I("dve", "tensor_tensor", out=bbi, in0=bbi, in1=tq, op=ALU.add)
        bblk = [self.alloc([128, 2, 32], BF16) for _ in range(2)]
        for t in bblk:
            self._memset(t, 0.0)
        for dg in range(ND):
            t = bblk[dg % 2]
            for c_, bb in ((0, bbr), (1, bbi)):
                S.I("dve", "tensor_copy", out=t[0:64, c_, 0:16], in_=bb[0:64, dg, :])
                S.I("dve", "tensor_copy", out=t[64:128, c_, 16:32], in_=bb[64:128, dg, :])
            pt = P[dg % 2].bitcast(BF16)
            for c_ in range(2):
                S.I("pe", "transpose", out=pt[0:32, c_ * 128:(c_ + 1) * 128], in_=t[:, c_, :], identity=self.ident)
            S.I("act", "activation", out=lB[:, dg, :, :], in_=pt[0:32, 0:256].re("p (c q) -> p c q", c=2), func=AF.Identity)
        cnat = [self.alloc([32, 32, 128]) for _ in range(2)]
        ci = 0
        for d in range(2):
            for c_, nm in ((0, "c_re"), (1, "c_im")):
                t = cnat[ci % 2]
                ci += 1
                self._memset(t, 0.0)
                for g2 in range(2):
                    srcv = w[nm][d].rearrange("(G g) c p -> g c G p", g=2)[g2]
                    S.dma("sp", t[16 * g2:16 * g2 + 16, :, 64 * g2:64 * g2 + 64], self.dv(srcv, "s5c"), disjoint=True)
                for G in range(32):
                    pp = P[2 + G % 2]
                    S.I("pe", "transpose", out=pp[:, 0:32], in_=t[:, G, :], identity=self.identf[0:32, 0:32])
                    S.I("act", "activation", out=lC[:, d * 32 + G, c_, :], in_=pp[:, 0:32], func=AF.Identity, scale=(1.0 if c_ == 0 else -1.0))
        S.barrier()
        self.aoff = keep
        cosT = self.alloc([128, NTK])
        sinT = self.alloc([128, NTK])
        ttmp = [self.alloc([128, 1024]) for _ in range(2)]
        lCp = [self.alloc([128, 2, 128], BF16) for _ in range(2)]
        for t in lCp:
            self._memset(t, 0.0)
        u = [self.alloc([32, NTK], BF16) for _ in range(2)]
        bus = [[self.alloc([128, 512]) for _ in range(2)] for _ in range(2)]
        tm = [[self.alloc([128, 512]) for _ in range(4)] for _ in range(2)]
        Wr = [self.alloc([128, 512]) for _ in range(2)]
        Wi = [self.alloc([128, 512]) for _ in range(2)]
        Gr = [self.alloc([128, 512]) for _ in range(2)]
        Gi = [self.alloc([128, 512]) for _ in range(2)]
        to = tm
        Hr = [self.alloc([128, 512], BF16) for _ in range(2)]
        Hi = [self.alloc([128, 512], BF16) for _ in range(2)]
        yacc = [self.alloc([128, NTK]) for _ in range(nb)]
        hch = [self.alloc([128, NTK], BF16) for _ in range(2)]
        gt = [tm[0][0:3], tm[1][0:3]]
        y16 = [self.alloc([128, 512], BF16) for _ in range(2)]
        fw = [(0, CTX)] + [(CTX + 512 * i_, 512) for i_ in range(SEQ // 512)]
        rv = [(0, CTX)] + [(CTX + 512 * i_, 512) for i_ in reversed(range(SEQ // 512))]
        pcount = 0
        ui = 0
        for m in range(KC):
            for jj in range(4):
                G = 4 * m + jj
                for d in range(2):
                    dg = d * 32 + G
                    first_acc = (jj == 0 and d == 0)
                    self._memset(cosT[:, 0:1], 1.0)
                    self._memset(sinT[:, 0:1], 0.0)
                    n_have = 1
                    j = 0
                    while n_have < NTK:
                        n_new = min(n_have, NTK - n_have)
                        er, ei = Er[:, j, dg:dg + 1], Ei[:, j, dg:dg + 1]
                        ta, tb = ttmp[0][:, 0:n_new], ttmp[1][:, 0:n_new]
                        S.I("dve", "tensor_single_scalar", out=ta, in_=sinT[:, 0:n_new], scalar=ei, op=ALU.mult)
                        S.I("dve", "tensor_single_scalar", out=tb, in_=sinT[:, 0:n_new], scalar=er, op=ALU.mult)
                        S.I("dve", "scalar_tensor_tensor", out=sinT[:, n_have:n_have + n_new], in0=cosT[:, 0:n_new], scalar=ei, in1=tb, op0=ALU.mult, op1=ALU.add)
                        S.I("dve", "scalar_tensor_tensor", out=cosT[:, n_have:n_have + n_new], in0=cosT[:, 0:n_new], scalar=er, in1=ta, op0=ALU.mult, op1=ALU.subtract)
                        n_have += n_new
                        j += 1
                    lc = lCp[dg % 2]
                    S.I("dve", "tensor_copy", out=lc[:, :, 32 * jj:32 * jj + 32], in_=lC[:, dg, :, :])
                    if jj > 0:
                        pass
                    mg = mag[:, dg:dg + 1]
                    for b in range(nb):
                        ut = u[ui % 2]
                        ui += 1
                        S.dma("sp", ut, self.dv(self.HT[32 * G:32 * G + 32, b * NTK:(b + 1) * NTK], "ht"))
                        prev = None
                        n0 = 0
                        for (c0, L) in (fw if d == 0 else rv):
                            pi_ = pcount % 2
                            pcount += 1
                            pr, pim = P[0 + pi_], P[2 + pi_]
                            S.I("pe", "matmul", out=pr[:, 0:L], lhsT=lB[:, dg, 0, :], rhs=ut[:, c0:c0 + L], start=True, stop=True)
                            S.I("pe", "matmul", out=pim[:, 0:L], lhsT=lB[:, dg, 1, :], rhs=ut[:, c0:c0 + L], start=True, stop=True)
                            br_, bi_ = bus[pi_][0][:, 0:L], bus[pi_][1][:, 0:L]
                            S.I("act", "activation", out=br_, in_=pr[:, 0:L], func=AF.Identity)
                            S.I("act", "activation", out=bi_, in_=pim[:, 0:L], func=AF.Identity)
                            cs_, sn_ = cosT[:, n0:n0 + L], sinT[:, n0:n0 + L]
                            if d == 1:
                                cs_, sn_ = cs_.rev(), sn_.rev()
                            t1, t2, t3, t4 = (x_[:, 0:L] for x_ in tm[pi_])
                            wr_, wi_ = Wr[pi_][:, 0:L], Wi[pi_][:, 0:L]
                            S.I("dve", "tensor_tensor", out=t1, in0=br_, in1=cs_, op=ALU.mult)
                            S.I("dve", "tensor_tensor", out=t2, in0=bi_, in1=sn_, op=ALU.mult)
                            S.I("dve", "tensor_tensor", out=wr_, in0=t1, in1=t2, op=ALU.add)
                            S.I("pool", "tensor_tensor", out=t3, in0=bi_, in1=cs_, op=ALU.mult)
                            S.I("pool", "tensor_tensor", out=t4, in0=br_, in1=sn_, op=ALU.mult)
                            S.I("pool", "tensor_tensor", out=wi_, in0=t3, in1=t4, op=ALU.subtract)
                            gr_, gi_ = Gr[pi_][:, 0:L], Gi[pi_][:, 0:L]
                            if prev is None:
                                ir, ii_ = 0.0, 0.0
                            else:
                                pgr, pgi, pL = prev
                                ir = pgr[:, pL - 1:pL] if d == 0 else pgr[:, 0:1]
                                ii_ = pgi[:, pL - 1:pL] if d == 0 else pgi[:, 0:1]
                            mb = mg.bc([128, L])
                            if d == 0:
                                S.I("dve", "tensor_tensor_scan", out=gr_, data0=mb, data1=wr_, initial=ir, op0=ALU.mult, op1=ALU.add)
                                S.I("dve", "tensor_tensor_scan", out=gi_, data0=mb, data1=wi_, initial=ii_, op0=ALU.mult, op1=ALU.add)
                            else:
                                S.I("dve", "tensor_tensor_scan", out=gr_.rev(), data0=mb, data1=wr_.rev(), initial=ir, op0=ALU.mult, op1=ALU.add)
                                S.I("dve", "tensor_tensor_scan", out=gi_.rev(), data0=mb, data1=wi_.rev(), initial=ii_, op0=ALU.mult, op1=ALU.add)
                            prev = (Gr[pi_], Gi[pi_], L)
                            o1, o2, o3, o4 = (x_[:, 0:L] for x_ in to[pi_])
                            hr_, hi_ = Hr[pi_][:, 0:L], Hi[pi_][:, 0:L]
                            S.I("dve", "tensor_tensor", out=o1, in0=gr_, in1=cs_, op=ALU.mult)
                            S.I("dve", "tensor_tensor", out=o2, in0=gi_, in1=sn_, op=ALU.mult)
                            S.I("dve", "tensor_tensor", out=hr_, in0=o1, in1=o2, op=ALU.subtract)
                            S.I("pool", "tensor_tensor", out=o3, in0=gr_, in1=sn_, op=ALU.mult)
                            S.I("pool", "tensor_tensor", out=o4, in0=gi_, in1=cs_, op=ALU.mult)
                            S.I("pool", "tensor_tensor", out=hi_, in0=o3, in1=o4, op=ALU.add)
                            py = P[4 + pi_]
                            S.I("pe", "matmul", out=py[:, 0:L], lhsT=lc[:, 0, :], rhs=hr_, start=True, stop=False)
                            S.I("pe", "matmul", out=py[:, 0:L], lhsT=lc[:, 1, :], rhs=hi_, start=False, stop=True)
                            ya = yacc[b][:, c0:c0 + L]
                            if first_acc:
                                S.I("act", "activation", out=ya, in_=py[:, 0:L], func=AF.Identity)
                            else:
                                S.I("dve", "tensor_tensor", out=ya, in0=py[:, 0:L], in1=ya, op=ALU.add)
                            n0 += L
                    self.S._emit("dve", (lambda apx: (lambda e: e.memset(apx, 0.0)))(lc[:, :, 32 * jj:32 * jj + 32].ap), [], [lc.buf])
            for b in range(nb):
                hc = hch[b % 2]
                S.dma("sp", hc, self.dv(self.HT[128 * m:128 * m + 128, b * NTK:(b + 1) * NTK], "ht"))
                for pi2, (c0, L) in enumerate(fw):
                    tt, sq, sg_ = (x_[:, 0:L] for x_ in gt[pi2 % 2])
                    yo_ = y16[pi2 % 2][:, 0:L]
                    S.I("dve", "scalar_tensor_tensor", out=tt, in0=hc[:, c0:c0 + L], scalar=dvec[:, m:m + 1], in1=yacc[b][:, c0:c0 + L], op0=ALU.mult, op1=ALU.add)
                    S.I("act", "activation", out=sq, in_=tt, func=AF.Square)
                    S.I("act", "activation", out=sq, in_=sq, func=AF.Identity, scale=0.044715, bias=self.one_c)
                    S.I("pool", "tensor_tensor", out=sq, in0=sq, in1=tt, op=ALU.mult)
                    S.I("act", "activation", out=sg_, in_=sq, func=AF.Sigmoid, scale=1.5957691216057308)
                    S.I("pool", "tensor_tensor", out=yo_, in0=tt, in1=sg_, op=ALU.mult)
                    col = b * NTK + c0
                    S.dma("sp", self.dv(YTv[:, m, col:col + L], "yt"), yo_, disjoint=True)
        self.arena_reset()
        wg = self.alloc([128, KC, 2 * D], BF16)
        self.load_w_bf16(wg, w["w_glu"], "s5wg%d" % li, 2 * D)
        g1 = [self.alloc([128, D]) for _ in range(2)]
        yt = [self.alloc([128, KC, 128], BF16) for _ in range(2)]
        sgb = [self.alloc([128, D]) for _ in range(2)]
        obuf = [self.alloc([128, D]) for _ in range(2)]
        xb = [self.alloc([128, D]) for _ in range(2)]
        ti = 0
        for b in range(nb):
            self.load_rows_bc(g1[0], self.MOD[idx, 2, b:b + 1, :], "mod%d" % idx)
            if upd:
                self.load_rows_bc(g1[1], self.MOD[idx, 2, nb:nb + 1, :], "mod%d" % idx)
            for lat, n_t in (((False, CTX // 128),) if upd else ()) + ((True, SEQ // 128),):
                src, skey = self.xsrc(first, lat)
                dst, dkey = self.xdst(lat)
                gb = g1[0] if lat else g1[1]
                for j in range(n_t):
                    r0 = (b * SEQ if lat else b * CTX) + j * 128
                    col = b * NTK + (CTX if lat else 0) + j * 128
                    y_ = yt[ti % 2]
                    S.dma("sp", y_, self.dv(YTv[:, :, col:col + 128], "yt"))
                    for cb in range(4):
                        pp = P[cb]
                        for k in range(KC):
                            S.I("pe", "matmul", out=pp, lhsT=y_[:, k, :], rhs=wg[:, k, cb * 512:(cb + 1) * 512], start=(k == 0), stop=(k == KC - 1))
                    sg_, ob, xx = sgb[ti % 2], obuf[ti % 2], xb[ti % 2]
                    ti += 1
                    S.dma("sp", xx, self.dv(src[r0:r0 + 128, :], (skey, r0)))
                    for h_ in range(2):
                        S.I("act", "activation", out=sg_[:, h_ * 512:(h_ + 1) * 512], in_=P[2 + h_], func=AF.Sigmoid)
                        S.I("dve", "tensor_tensor", out=ob[:, h_ * 512:(h_ + 1) * 512], in0=P[h_], in1=sg_[:, h_ * 512:(h_ + 1) * 512], op=ALU.mult)
                    S.I("pool", "tensor_tensor", out=ob, in0=ob, in1=gb, op=ALU.mult)
                    S.I("dve", "tensor_tensor", out=ob, in0=ob, in1=xx, op=ALU.add)
                    S.dma("sp", self.dv(dst[r0:r0 + 128, :], (dkey, r0)), ob)

    def moe(self, idx, li, upd):
        S, nb, w, nc = self.S, self.nb, self.W[li], self.nc
        self.arena_reset()
        P = self.psum
        tiles = []
        for b in range(nb):
            for j in range(SEQ // 128):
                r0 = b * SEQ + j * 128
                tiles.append((self.xs, "xs", r0, b))
        if upd:
            for j in range(nb * CTX // 128):
                tiles.append((self.cs, "cs", j * 128, nb))
        NT = len(tiles)
        ntok = NT * 128
        nslot = ((2 * ntok + NE * (TS - 1)) + TS - 1) // TS * TS
        NST = nslot // TS

        oh1 = self.alloc([128, NT, NE])
        oh2 = self.alloc([128, NT, NE])
        L1 = self.alloc([128, NT])
        L2 = self.alloc([128, NT])
        W1 = self.alloc([128, NT])
        W2 = self.alloc([128, NT])
        run = self.alloc([128, NE])
        sl1 = self.alloc([128, NT], I32)
        sl2 = self.alloc([128, NT], I32)
        widx = self.alloc([128, NST], I32)
        keep = self.aoff

        wr = self.alloc([128, KC, 36])
        S.dma("sp", wr, self.dv(w["wr"].rearrange("(k p) n -> p k n", p=128), "wr%d" % li))
        brb = self.alloc([128, 36])
        self.load_rows_bc(brb, w["br"], "br%d" % li)
        sc2 = [self.alloc([128, D]) for _ in range(2)]
        sh2 = [self.alloc([128, D]) for _ in range(2)]
        nbuf = self.norm_bufs(4)
        h2 = [self.alloc([128, D]) for _ in range(4)]
        h16 = [self.alloc([128, D], BF16) for _ in range(4)]
        h2T = [self.alloc([128, KC, 128]) for _ in range(4)]
        GB = 4
        lg = self.alloc([128, GB, 36])
        gmx, gsum, gw, m1, m2, dm, den = (self.alloc([128, GB]) for _ in range(7))
        ohg = self.alloc([128, GB, 4])
        ex = self.alloc([128, GB, 4])
        t48 = self.alloc([128, GB, 4, 8])
        le, le2, o1, o2 = (self.alloc([128, GB, 8]) for _ in range(4))
        sel = self.alloc([128, GB, NE])
        sel16 = self.alloc([128, GB, NE], BF16)
        pf = self.alloc([128, GB, NE])
        tmp32 = self.alloc([128, GB, NE])
        self._memset(run, 0.0)
        cur_s = None
        for t0 in range(0, NT, GB):
            n = min(GB, NT - t0)
            for j in range(n):
                ti = t0 + j
                src, skey, r0, s = tiles[ti]
                if s != cur_s:
                    cur_s = s
                    cb = s % 2
                    self.load_rows_bc(sc2[cb], self.MOD[idx, 3, s:s + 1, :], "mod%d" % idx)
                    self.load_rows_bc(sh2[cb], self.MOD[idx, 4, s:s + 1, :], "mod%d" % idx, eng="act")
                xt, rs, i = self.rms_tile(nbuf, self.dv(src[r0:r0 + 128, :], (skey, r0)))
                hh, hb, hT_ = h2[ti % 4], h16[ti % 4], h2T[ti % 4]
                S.I("dve", "scalar_tensor_tensor", out=hh, in0=xt, scalar=rs, in1=sc2[cb], op0=ALU.mult, op1=ALU.mult)
                S.I("pool", "tensor_tensor", out=hh, in0=hh, in1=sh2[cb], op=ALU.add)
                S.I("act", "activation", out=hb, in_=hh, func=AF.Identity)
                S.dma("sp", self.dv(self.H2[ti * 128:(ti + 1) * 128, :], ("h2", ti)), hb)
                for k in range(KC):
                    pt = P[1 + (k // 4) % 2]
                    S.I("pe", "transpose", out=pt[:, (k % 4) * 128:(k % 4 + 1) * 128], in_=hh[:, k * 128:(k + 1) * 128], identity=self.identf)
                    if k % 4 == 3:
                        S.I("act", "activation", out=hT_[:, k - 3:k + 1, :], in_=pt.re("p (k t) -> p k t", k=4), func=AF.Identity)
                for k in range(KC):
                    S.I("pe", "matmul", out=P[3][:, j * 36:(j + 1) * 36], lhsT=hT_[:, k, :], rhs=wr[:, k, :], start=(k == 0), stop=(k == KC - 1),
                        skip_group_check=True)
            N_ = slice(0, n)
            S.I("dve", "tensor_tensor", out=lg[:, N_, :], in0=P[3][:, 0:n * 36].re("p (t c) -> p t c", c=36), in1=brb.un(1).bc([128, n, 36]), op=ALU.add)
            lgg = lg[:, N_, 0:4]
            S.I("dve", "reduce_max", out=gmx[:, N_], in_=lgg, axis=AX.X)
            S.I("dve", "tensor_tensor", out=ohg[:, N_, :], in0=lgg, in1=gmx[:, N_].un(2).bc([128, n, 4]), op=ALU.is_equal)
            S.I("dve", "tensor_tensor", out=ex[:, N_, :], in0=lgg, in1=gmx[:, N_].un(2).bc([128, n, 4]), op=ALU.subtract)
            S.I("act", "activation", out=ex[:, N_, :], in_=ex[:, N_, :], func=AF.Exp)
            S.I("dve", "reduce_sum", out=gsum[:, N_], in_=ex[:, N_, :], axis=AX.X)
            S.I("dve", "reciprocal", out=gw[:, N_], in_=gsum[:, N_])
            S.I("dve", "tensor_tensor", out=t48[:, N_], in0=lg[:, N_, 4:36].re("p t (g e) -> p t g e", g=4),
                in1=ohg[:, N_, :].un(3).bc([128, n, 4, 8]), op=ALU.mult)
            S.I("dve", "reduce_sum", out=le[:, N_, :], in_=t48[:, N_].re("p t g e -> p t e g"), axis=AX.X)
            S.I("dve", "reduce_max", out=m1[:, N_], in_=le[:, N_, :], axis=AX.X)
            S.I("dve", "tensor_tensor", out=o1[:, N_, :], in0=le[:, N_, :], in1=m1[:, N_].un(2).bc([128, n, 8]), op=ALU.is_equal)
            S.I("dve", "scalar_tensor_tensor", out=le2[:, N_, :], in0=o1[:, N_, :], scalar=-1e30, in1=le[:, N_, :], op0=ALU.mult, op1=ALU.add)
            S.I("dve", "reduce_max", out=m2[:, N_], in_=le2[:, N_, :], axis=AX.X)
            S.I("dve", "tensor_tensor", out=o2[:, N_, :], in0=le2[:, N_, :], in1=m2[:, N_].un(2).bc([128, n, 8]), op=ALU.is_equal)
            S.I("dve", "tensor_tensor", out=dm[:, N_], in0=m2[:, N_], in1=m1[:, N_], op=ALU.subtract)
            S.I("act", "activation", out=dm[:, N_], in_=dm[:, N_], func=AF.Exp)
            S.I("dve", "tensor_single_scalar", out=den[:, N_], in_=dm[:, N_], scalar=1.0, op=ALU.add)
            S.I("dve", "reciprocal", out=den[:, N_], in_=den[:, N_])
            S.I("dve", "tensor_tensor", out=W1[:, t0:t0 + n], in0=gw[:, N_], in1=den[:, N_], op=ALU.mult)
            S.I("dve", "tensor_tensor", out=W2[:, t0:t0 + n], in0=gw[:, N_], in1=W1[:, t0:t0 + n], op=ALU.subtract)
            o1g = oh1[:, t0:t0 + n, :].re("p t (g e) -> p t g e", g=4)
            o2g = oh2[:, t0:t0 + n, :].re("p t (g e) -> p t g e", g=4)
            S.I("dve", "tensor_tensor", out=o1g, in0=ohg[:, N_, :].un(3).bc([128, n, 4, 8]), in1=o1[:, N_, :].un(2).bc([128, n, 4, 8]), op=ALU.mult)
            S.I("dve", "tensor_tensor", out=o2g, in0=ohg[:, N_, :].un(3).bc([128, n, 4, 8]), in1=o2[:, N_, :].un(2).bc([128, n, 4, 8]), op=ALU.mult)
            S.I("dve", "tensor_tensor", out=sel[:, N_, :], in0=oh1[:, t0:t0 + n, :], in1=oh2[:, t0:t0 + n, :], op=ALU.add)
            S.I("dve", "tensor_copy", out=sel16[:, N_, :], in_=sel[:, N_, :])
            for j in range(n):
                S.I("pe", "matmul", out=P[4][:, j * NE:(j + 1) * NE], lhsT=self.ltri, rhs=sel16[:, j, :], start=True, stop=True, skip_group_check=True)
                S.I("pe", "matmul", out=P[5][:, j * NE:(j + 1) * NE], lhsT=self.ones, rhs=sel16[:, j, :], start=True, stop=True, skip_group_check=True)
            for j in range(n):
                S.I("dve", "tensor_tensor", out=pf[:, j, :], in0=P[4][:, j * NE:(j + 1) * NE], in1=run, op=ALU.add)
                S.I("dve", "tensor_tensor", out=run, in0=P[5][:, j * NE:(j + 1) * NE], in1=run, op=ALU.add)
            S.I("dve", "tensor_tensor", out=tmp32[:, N_, :], in0=pf[:, N_, :], in1=oh1[:, t0:t0 + n, :], op=ALU.mult)
            S.I("dve", "reduce_sum", out=L1[:, t0:t0 + n], in_=tmp32[:, N_, :], axis=AX.X)
            S.I("dve", "tensor_tensor", out=tmp32[:, N_, :], in0=pf[:, N_, :], in1=oh2[:, t0:t0 + n, :], op=ALU.mult)
            S.I("dve", "reduce_sum", out=L2[:, t0:t0 + n], in_=tmp32[:, N_, :], axis=AX.X)

        if 'moeB' in SKIP:
            return
        self.S.barrier()
        self.aoff = keep
        cnti = self.alloc([128, NE], I32)
        pad = self.alloc([128, NE])
        incl = self.alloc([128, NE])
        basee = self.alloc([128, NE])
        onesf = self.alloc([128, NE])
        big3 = self.alloc([128, NT, NE])
        sf = self.alloc([128, NT])
        sgrid = self.alloc([128, NST])
        cmp3 = self.alloc([128, NST, NE])
        ef = self.alloc([128, NST])
        S.I("dve", "tensor_copy", out=cnti, in_=run)
        S.I("dve", "tensor_single_scalar", out=cnti, in_=cnti, scalar=TS - 1, op=ALU.add)
        sh = TS.bit_length() - 1
        S.I("dve", "tensor_scalar", out=cnti, in0=cnti, scalar1=sh, scalar2=sh, op0=ALU.arith_shift_right, op1=ALU.logical_shift_left)
        S.I("dve", "tensor_copy", out=pad, in_=cnti)
        self._memset(onesf, 1.0)
        S.I("dve", "tensor_tensor_scan", out=incl, data0=onesf, data1=pad, initial=0.0, op0=ALU.mult, op1=ALU.add)
        S.I("dve", "tensor_tensor", out=basee, in0=incl, in1=pad, op=ALU.subtract)
        for (oh, L, sl) in ((oh1, L1, sl1), (oh2, L2, sl2)):
            S.I("dve", "tensor_tensor", out=big3, in0=oh, in1=basee.un(1).bc([128, NT, NE]), op=ALU.mult)
            S.I("dve", "reduce_sum", out=sf, in_=big3, axis=AX.X)
            S.I("dve", "tensor_tensor", out=sf, in0=sf, in1=L, op=ALU.add)
            S.I("dve", "tensor_copy", out=sl, in_=sf)
        S.I("pool", "iota", out=sgrid, pattern=[[TS, NST]], base=0, channel_multiplier=0, allow_small_or_imprecise_dtypes=True)
        S.I("dve", "tensor_tensor", out=cmp3, in0=incl.un(1).bc([128, NST, NE]), in1=sgrid.un(2).bc([128, NST, NE]), op=ALU.is_le)
        S.I("dve", "reduce_sum", out=ef, in_=cmp3, axis=AX.X)
        S.I("dve", "tensor_single_scalar", out=ef, in_=ef, scalar=float(NE - 1), op=ALU.min)
        same = self.alloc([128, NST])
        self._memset(same, 0.0)
        S.I("dve", "tensor_tensor", out=same[:, 2:NST], in0=ef[:, 2:NST], in1=ef[:, 0:NST - 2], op=ALU.is_equal)
        S.I("dve", "tensor_single_scalar", out=same, in_=same, scalar=float(1 << 20), op=ALU.mult)
        S.I("dve", "tensor_single_scalar", out=ef, in_=ef, scalar=128.0, op=ALU.mult)
        S.I("dve", "tensor_single_scalar", out=ef, in_=ef, scalar=self.iota_p, op=ALU.add)
        S.I("dve", "tensor_tensor", out=ef, in0=ef, in1=same, op=ALU.add)
        S.I("dve", "tensor_copy", out=widx, in_=ef)
        self.dbg("W1", W1); self.dbg("W2", W2); self.dbg("L1", L1); self.dbg("L2", L2); self.dbg("run", run)
        self.dbg("sl1", sl1, I32); self.dbg("sl2", sl2, I32); self.dbg("widx", widx, I32); self.dbg("oh1", oh1); self.dbg("oh2", oh2)
        self.dbg("incl", incl); self.dbg("basee", basee)
        hl = [self.alloc([128, D], BF16) for _ in range(4)]
        h2s_v = self.dv(self.H2S[0:nslot, :], "h2s")
        for ti in range(NT):
            t = hl[ti % 4]
            S.dma("sp", t, self.dv(self.H2[ti * 128:(ti + 1) * 128, :], ("h2", ti)))
            S.scatter(h2s_v, t, sl1[:, ti:ti + 1])
            S.scatter(h2s_v, t, sl2[:, ti:ti + 1])

        if 'moeC' in SKIP:
            return
        self.S.barrier()
        self.aoff = keep
        w13t = [self.alloc([128, KC * D], BF16) for _ in range(2)]
        w2t = [self.alloc([128, 4 * D], BF16) for _ in range(2)]
        hs = [self.alloc([128, 2, D], BF16) for _ in range(2)]
        hT = [self.alloc([128, KC, TS], BF16) for _ in range(2)]
        sa = [self.alloc([128, TS]) for _ in range(2)]
        u16 = [self.alloc([128, 4, TS], BF16) for _ in range(2)]
        yo = [self.alloc([128, D]) for _ in range(2)]
        w13v = self.dv(self.W13B.rearrange("(r a) n -> r (a n)", a=4), "w13b")
        w2v = self.dv(self.W2B.rearrange("(r a) n -> r (a n)", a=2), "w2b")
        yi = 0
        for s_ in range(NST):
            wa, wb, hsl, hTt, ut = w13t[s_ % 2], w2t[s_ % 2], hs[s_ % 2], hT[s_ % 2], u16[s_ % 2]
            S.gather(wa, w13v, widx[:, s_:s_ + 1], bound=NE * 128 - 1)
            S.gather(wb, w2v, widx[:, s_:s_ + 1], bound=NE * 128 - 1)
            S.dma("sp", hsl, self.dv(self.H2S[s_ * TS:(s_ + 1) * TS, :].rearrange("(a p) d -> p a d", p=128), "h2s"))
            for a in range(2):
                pt = P[a].bitcast(BF16)
                for k in range(KC):
                    S.I("pe", "transpose", out=pt[:, k * 128:(k + 1) * 128], in_=hsl[:, a, k * 128:(k + 1) * 128], identity=self.ident)
                S.I("act" if a == 0 else "dve", "activation" if a == 0 else "tensor_copy", out=hTt[:, :, a * 128:(a + 1) * 128],
                    in_=pt.re("p (k t) -> p k t", k=KC), **({"func": AF.Copy} if a == 0 else {}))
            for m in range(4):
                pa, pg = P[2 + m % 2], P[4 + m % 2]
                for (pp, off) in ((pa, 0), (pg, DE)):
                    for k in range(KC):
                        c0 = k * D + off + m * 128
                        S.I("pe", "matmul", out=pp[:, 0:TS], lhsT=wa[:, c0:c0 + 128], rhs=hTt[:, k, :], start=(k == 0), stop=(k == KC - 1))
                sat = sa[m % 2]
                S.I("act", "activation", out=sat, in_=pa[:, 0:TS], func=AF.Silu)
                S.I("dve", "tensor_tensor", out=ut[:, m, :], in0=pg[:, 0:TS], in1=sat, op=ALU.mult)
            for a in range(2):
                yt = yo[yi % 2]
                yi += 1
                for h_ in range(2):
                    pp = P[6 + h_]
                    for k in range(4):
                        S.I("pe", "matmul", out=pp, lhsT=ut[:, k, a * 128:(a + 1) * 128], rhs=wb[:, k * D + h_ * 512:k * D + (h_ + 1) * 512],
                            start=(k == 0), stop=(k == 3))
                    S.I("act" if h_ == 0 else "dve", "activation" if h_ == 0 else "tensor_copy", out=yt[:, h_ * 512:(h_ + 1) * 512], in_=pp,
                        **({"func": AF.Copy} if h_ == 0 else {}))
                r0 = s_ * TS + a * 128
                S.dma("sp", self.dv(self.YS[r0:r0 + 128, :], "ys"), yt, disjoint=True)

        if 'moeD' in SKIP:
            return
        self.S.barrier()
        self.aoff = keep
        g2 = [self.alloc([128, D]) for _ in range(2)]
        y1 = [self.alloc([128, D]) for _ in range(4)]
        y2 = [self.alloc([128, D]) for _ in range(4)]
        xt2 = [self.alloc([128, D]) for _ in range(4)]
        acc = [self.alloc([128, D]) for _ in range(4)]
        ysv = self.dv(self.YS[0:nslot, :], "ys")
        cur_s = None
        for ti, (src, skey, r0, s) in enumerate(tiles):
            if s != cur_s:
                cur_s = s
                cb = s % 2
                self.load_rows_bc(g2[cb], self.MOD[idx, 5, s:s + 1, :], "mod%d" % idx)
            a1, a2, xx, ac = y1[ti % 4], y2[ti % 4], xt2[ti % 4], acc[ti % 4]
            S.gather(a1, ysv, sl1[:, ti:ti + 1])
            S.gather(a2, ysv, sl2[:, ti:ti + 1])
            S.dma("sp", xx, self.dv(src[r0:r0 + 128, :], (skey, r0)))
            S.I("act", "activation", out=ac, in_=a1, func=AF.Identity, scale=W1[:, ti:ti + 1])
            S.I("dve", "scalar_tensor_tensor", out=ac, in0=a2, scalar=W2[:, ti:ti + 1], in1=ac, op0=ALU.mult, op1=ALU.add)
            S.I("pool", "tensor_tensor", out=ac, in0=ac, in1=g2[cb], op=ALU.mult)
            S.I("dve", "tensor_tensor", out=ac, in0=ac, in1=xx, op=ALU.add)
            S.dma("sp", self.dv(src[r0:r0 + 128, :], (skey, r0)), ac)

    def final_norm(self):
        S, nb = self.S, self.nb
        self.arena_reset()
        nbuf = self.norm_bufs(4)
        fg = self.alloc([128, D])
        self.load_rows_bc(fg, self.final_g, "final_g")
        ob = [self.alloc([128, D]) for _ in range(4)]
        for ti in range(nb * SEQ // 128):
            r0 = ti * 128
            xt, rs, i = self.rms_tile(nbuf, self.dv(self.xs[r0:r0 + 128, :], ("xs", r0)))
            o = ob[ti % 4]
            S.I("dve", "scalar_tensor_tensor", out=o, in0=xt, scalar=rs, in1=fg, op0=ALU.mult, op1=ALU.mult)
            S.dma("sp", self.dv(self.y_out[r0:r0 + 128, :], ("y", r0)), o)


def prep_weights(inp, layers):
    out = {}
    f = lambda a: np.ascontiguousarray(a, dtype=np.float32)
    out["c_ctx"] = f(inp["c_ctx"]).reshape(1, D)
    out["final_g"] = f(inp["final_g"]).reshape(1, D)
    for li in layers:
        kind, j = li % 3, li // 3
        out["ada_w_%d" % li] = f(inp["ada_w"][li])
        out["ada_b_%d" % li] = f(inp["ada_b"][li]).reshape(1, -1)
        out["norm1_g_%d" % li] = f(inp["norm1_g"][li]).reshape(1, D)
        out["norm2_g_%d" % li] = f(inp["norm2_g"][li]).reshape(1, D)
        out["moe_wr_%d" % li] = f(np.concatenate([inp["moe_wg"][li], inp["moe_we"][li]], axis=1))
        out["moe_br_%d" % li] = f(np.concatenate([inp["moe_bg"][li], inp["moe_be"][li]], axis=0)).reshape(1, 36)
        w13 = np.asarray(inp["moe_w13"][li]).reshape(NE, KC, 128, D).transpose(0, 2, 1, 3).reshape(NE * 128 * 4, 2048)
        out["moe_w13_%d" % li] = f(w13)
        w2 = np.asarray(inp["moe_w2"][li]).reshape(NE, 4, 128, D).transpose(0, 2, 1, 3).reshape(NE * 128 * 2, 2048)
        out["moe_w2_%d" % li] = f(w2)
        if kind == 0:
            out["conf_w_in_%d" % li] = f(inp["conf_w_in"][j])
            out["conf_dw_%d" % li] = f(inp["conf_dw"][j])
            out["conf_dw_b_%d" % li] = f(inp["conf_dw_b"][j]).reshape(1, D)
            out["conf_ln_g_%d" % li] = f(inp["conf_ln_g"][j]).reshape(1, D)
            out["conf_ln_b_%d" % li] = f(inp["conf_ln_b"][j]).reshape(1, D)
            out["conf_w_out_%d" % li] = f(inp["conf_w_out"][j])
        elif kind == 1:
            out["sc_w_in_%d" % li] = f(inp["sc_w_in"][j])
            out["sc_conv_%d" % li] = f(inp["sc_conv"][j])
            out["sc_w_out_%d" % li] = f(inp["sc_w_out"][j])
        else:
            for n in ("a_re", "a_im", "b_re", "b_im", "c_re", "c_im"):
                out["s5_%s_%d" % (n, li)] = f(inp["s5_" + n][j])
            out["s5_log_dt_%d" % li] = f(inp["s5_log_dt"][j])
            out["s5_d_%d" % li] = f(inp["s5_d"][j]).reshape(1, D)
            out["s5_w_glu_%d" % li] = f(inp["s5_w_glu"][j])
    return out


def run(inp, nb, ncores, layers, final=True, trace=False):
    prog = Prog(nb, layers, final)
    nc = prog.build()
    shared = prep_weights(inp, layers)
    x = np.asarray(inp["x"], dtype=np.float32)
    c = np.asarray(inp["c"], dtype=np.float32)
    ctx = np.asarray(inp["ctx"], dtype=np.float32)
    in_maps = []
    for k in range(ncores):
        m = dict(shared)
        m["x"] = np.ascontiguousarray(x[k * nb:(k + 1) * nb]).reshape(nb * SEQ, D)
        m["c"] = np.ascontiguousarray(c[k * nb:(k + 1) * nb])
        m["ctx"] = np.ascontiguousarray(ctx[k * nb:(k + 1) * nb]).reshape(nb * CTX, D)
        in_maps.append({n: m[n] for n in prog.in_names})
    res = run_bass_kernel_spmd(nc, in_maps, core_ids=list(range(ncores)), **({"trace": True} if trace else {}))
    y = np.concatenate([r["y"].reshape(nb, SEQ, D) for r in res.results], axis=0)
    return y, res


def kernel(**inputs):
    y, _ = run(inputs, nb=4, ncores=NCORES, layers=list(range(DEPTH)), final=True)
    return y.astype(np.float32)
```

```python
import contextlib
import numpy as np
import concourse.bass as bass
import concourse.mybir as mybir
from concourse.bass_utils import run_bass_kernel_spmd

F32 = mybir.dt.float32
BF16 = mybir.dt.bfloat16
I32 = mybir.dt.int32
AF = mybir.ActivationFunctionType
ALU = mybir.AluOpType
AX = mybir.AxisListType

D = 1024
KC = 8
SEQ = 2048
CTX = 256
NE = 32
DE = 512
TS = 256
RMS_EPS = 1e-6
LN_EPS = 1e-5
DEPTH = 4
NCORES = 8
SKIP = set()
DEBUG = False


class Buf:
    __slots__ = ("w", "wx", "r")

    def __init__(self):
        self.w = {}
        self.wx = {}
        self.r = {}


class V:
    __slots__ = ("ap", "buf")

    def __init__(self, ap, buf=None):
        self.ap = ap
        self.buf = buf if buf is not None else Buf()

    def __getitem__(self, k):
        return V(self.ap[k], self.buf)

    def re(self, pat, **kw):
        return V(self.ap.rearrange(pat, **kw), self.buf)

    def bc(self, shape):
        return V(self.ap.to_broadcast(list(shape)), self.buf)

    def un(self, axis):
        return V(self.ap.unsqueeze(axis), self.buf)

    def bitcast(self, dt):
        return V(self.ap.bitcast(dt), self.buf)

    def rev(self):
        a = list(self.ap.ap)
        s, c = a[-1]
        a[-1] = [-s, c]
        return V(bass.AP(self.ap.tensor, self.ap.offset + s * (c - 1), [list(x) for x in a]), self.buf)


def _merge(dst, src):
    for k, v in src.items():
        if dst.get(k, 0) < v:
            dst[k] = v


class Sched:
    ENG = ("pe", "act", "dve", "pool", "sp")
    NDS = {"sp": 16, "act": 16, "pool": 16}

    def __init__(self):
        self.ops = {e: [] for e in self.ENG}
        self.cnt = {e: 0 for e in self.ENG}
        self.waited = {e: {} for e in self.ENG}
        self.dnext = {e: 0 for e in self.NDS}
        self.dval = {e: [0] * n for e, n in self.NDS.items()}
        self.n_ops = 0
        self.pool_consts = set()
        self.regvals = {}

    def _emit(self, eng, fn, reads, writes, dma=False, disjoint=False, sreads=()):
        need = {}
        own = 0
        for b in reads:
            _merge(need, b.w)
        if eng != "pe":
            own = need.get(("c", eng), 0)
        for b in writes:
            _merge(need, b.r)
            _merge(need, b.wx if disjoint else b.w)
        if dma:
            j = self.dnext[eng]
            self.dnext[eng] = (j + 1) % self.NDS[eng]
            key = ("d", eng, j)
            if self.dval[eng][j] > 0:
                need[key] = max(need.get(key, 0), self.dval[eng][j])
            self.dval[eng][j] += 16
            ev = (key, self.dval[eng][j])
            inc = 16
        else:
            need.pop(("c", eng), None)
            if own > 0:
                need[("c", eng)] = own
            self.cnt[eng] += 1
            key = ("c", eng)
            ev = (key, self.cnt[eng])
            inc = 1
        wl = []
        wd = self.waited[eng]
        for k, v in need.items():
            if wd.get(k, 0) < v:
                wd[k] = v
                wl.append((k, v))
        self.ops[eng].append((wl, fn, key, inc))
        self.n_ops += 1
        for b in reads:
            if b.r.get(ev[0], 0) < ev[1]:
                b.r[ev[0]] = ev[1]
        for b in writes:
            if disjoint:
                b.w[ev[0]] = ev[1]
            else:
                b.w = {ev[0]: ev[1]}
                b.wx = {ev[0]: ev[1]}
                b.r = {}

    def I(self, eng, meth, disjoint=False, **kw):
        reads, writes, sreads, res = [], [], [], {}
        for k, v in kw.items():
            if isinstance(v, V):
                (writes if k in ("out", "accum_out") else reads).append(v.buf)
                if k in ("scalar", "scalar1", "scalar2", "scale", "bias", "initial"):
                    sreads.append(v.buf)
                res[k] = v.ap
            else:
                res[k] = v
        self._emit(eng, lambda e: getattr(e, meth)(**res), reads, writes, disjoint=disjoint, sreads=sreads)

    def dma(self, eng, out, in_, disjoint=False, **kw):
        o, i = out.ap, in_.ap
        if eng == "sp" and type(o.tensor).__name__ != "DRamTensorHandle":
            eng = "act"
        self._emit(eng, lambda e: e.dma_start(out=o, in_=i, **kw), [in_.buf], [out.buf], dma=True, disjoint=disjoint)

    def gather(self, out, src, idx, bound=None):
        o, i, x = out.ap, src.ap, idx.ap
        if bound is None:
            fn = lambda e: e.indirect_dma_start(out=o, out_offset=None, in_=i, in_offset=bass.IndirectOffsetOnAxis(ap=x, axis=0))
        else:
            self.pool_consts.add(bound)
            fn = lambda e: e.indirect_dma_start(out=o, out_offset=None, in_=i, in_offset=bass.IndirectOffsetOnAxis(ap=x, axis=0),
                                                bounds_check=self.regvals[bound], oob_is_err=False)
        self._emit("pool", fn, [src.buf, idx.buf], [out.buf], dma=True)

    def scatter(self, out, src, idx, **kw):
        o, i, x = out.ap, src.ap, idx.ap
        self._emit("pool", lambda e: e.indirect_dma_start(out=o, out_offset=bass.IndirectOffsetOnAxis(ap=x, axis=0),
                                                          in_=i, in_offset=None, **kw),
                   [src.buf, idx.buf], [out.buf], dma=True, disjoint=True)

    def barrier(self):
        allv = {("c", e): self.cnt[e] for e in self.ENG if self.cnt[e] > 0}
        for e, vals in self.dval.items():
            for j, v in enumerate(vals):
                if v > 0:
                    allv[("d", e, j)] = v
        for eng in self.ENG:
            wl = []
            wd = self.waited[eng]
            for k, v in allv.items():
                if k == ("c", eng):
                    continue
                if wd.get(k, 0) < v:
                    wd[k] = v
                    wl.append((k, v))
            if wl:
                self.ops[eng].append((wl, None, None, 0))

    def replay(self, nc, stack):
        sems = {}
        for e in self.ENG:
            sems[("c", e)] = stack.enter_context(nc.semaphore("c_" + e))
        for e, n in self.NDS.items():
            for j in range(n):
                sems[("d", e, j)] = stack.enter_context(nc.semaphore("d_%s_%d" % (e, j)))
        block = stack.enter_context(nc.Block())
        ops = self.ops

        def run(name, e):
            for wl, fn, key, inc in ops[name]:
                for k, v in wl:
                    e.wait_ge(sems[k], v)
                if fn is not None:
                    fn(e).then_inc(sems[key], inc)

        @block.tensor
        def _(e):
            run("pe", e)

        @block.scalar
        def _(e):
            run("act", e)

        @block.vector
        def _(e):
            run("dve", e)

        @block.gpsimd
        def _(e):
            for val in sorted(self.pool_consts):
                r = e.alloc_register("bc%d" % val)
                e.reg_mov(r, val)
                self.regvals[val] = e.snap(r)
            run("pool", e)

        @block.sync
        def _(e):
            run("sp", e)


class Prog:
    def __init__(self, nb, layers, final=True):
        self.nb = nb
        self.layers = list(layers)
        self.final = final
        self.S = Sched()
        self.nc = bass.Bass("TRN2", target_bir_lowering=False)
        self.stack = contextlib.ExitStack()
        self.dbufs = {}
        self.in_names = []

    def dram_in(self, name, shape, dt=F32):
        self.in_names.append(name)
        return self.nc.dram_tensor(name, list(shape), dt, kind="ExternalInput").ap()

    def dram_tmp(self, name, shape, dt=F32):
        return self.nc.dram_tensor(name, list(shape), dt, kind="Internal").ap()

    def dv(self, ap, key):
        b = self.dbufs.get(key)
        if b is None:
            b = self.dbufs[key] = Buf()
        return V(ap, b)

    def arena_reset(self):
        self.S.barrier()
        self.aoff = self.abase

    def alloc(self, shape, dt=F32):
        n = 1
        for s in shape[1:]:
            n *= s
        words = n if dt in (F32, I32) else (n + 1) // 2
        words = (words + 7) // 8 * 8
        assert self.aoff + words <= self.awords, ("SBUF arena overflow", self.aoff, words, self.awords)
        ap = self.big[:, self.aoff:self.aoff + words]
        self.aoff += words
        if dt != F32:
            ap = ap.bitcast(dt)
        ap = ap[0:shape[0], 0:n]
        if len(shape) > 2:
            names = " ".join("d%d" % i for i in range(len(shape) - 1))
            kw = {"d%d" % i: shape[i + 1] for i in range(len(shape) - 1)}
            ap = ap.rearrange("p (%s) -> p %s" % (names, names), **kw)
        return V(ap)

    def perm(self, shape, dt=F32):
        v = self.alloc(shape, dt)
        self.abase = self.aoff
        return v

    def build(self):
        nc, S, nb = self.nc, self.S, self.nb
        st = self.stack
        self.awords = 53000
        self.big = st.enter_context(nc.sbuf_tensor("big", [128, self.awords], F32))
        self.aoff = 0
        self.abase = 0
        self.psum = [V(st.enter_context(nc.psum_tensor("ps%d" % i, [128, 512], F32))[:, :]) for i in range(8)]
        nl = len(self.layers)
        nlat, nctx = nb * SEQ, nb * CTX

        self.x_in = self.dram_in("x", [nlat, D])
        self.c_in = self.dram_in("c", [nb, D])
        self.ctx_in = self.dram_in("ctx", [nctx, D])
        self.cctx_in = self.dram_in("c_ctx", [1, D])
        self.final_g = self.dram_in("final_g", [1, D])
        self.W = {}
        for li in self.layers:
            kind = li % 3
            w = {}
            w["ada_w"] = self.dram_in("ada_w_%d" % li, [D, 6 * D])
            w["ada_b"] = self.dram_in("ada_b_%d" % li, [1, 6 * D])
            w["n1"] = self.dram_in("norm1_g_%d" % li, [1, D])
            w["n2"] = self.dram_in("norm2_g_%d" % li, [1, D])
            w["wr"] = self.dram_in("moe_wr_%d" % li, [D, 36])
            w["br"] = self.dram_in("moe_br_%d" % li, [1, 36])
            w["w13"] = self.dram_in("moe_w13_%d" % li, [NE * 128 * 4, 2048])
            w["w2"] = self.dram_in("moe_w2_%d" % li, [NE * 128 * 2, 2048])
            if kind == 0:
                w["w_in"] = self.dram_in("conf_w_in_%d" % li, [D, 2 * D])
                w["dw"] = self.dram_in("conf_dw_%d" % li, [31, D])
                w["dw_b"] = self.dram_in("conf_dw_b_%d" % li, [1, D])
                w["ln_g"] = self.dram_in("conf_ln_g_%d" % li, [1, D])
                w["ln_b"] = self.dram_in("conf_ln_b_%d" % li, [1, D])
                w["w_out"] = self.dram_in("conf_w_out_%d" % li, [D, D])
            elif kind == 1:
                w["w_in"] = self.dram_in("sc_w_in_%d" % li, [D, 3 * D])
                w["cv"] = self.dram_in("sc_conv_%d" % li, [3, D])
                w["w_out"] = self.dram_in("sc_w_out_%d" % li, [D, D])
            else:
                w["a_re"] = self.dram_in("s5_a_re_%d" % li, [2, 64, 64])
                w["a_im"] = self.dram_in("s5_a_im_%d" % li, [2, 64, 64])
                w["ldt"] = self.dram_in("s5_log_dt_%d" % li, [2, 64])
                w["b_re"] = self.dram_in("s5_b_re_%d" % li, [2, 64, 64, 16])
                w["b_im"] = self.dram_in("s5_b_im_%d" % li, [2, 64, 64, 16])
                w["c_re"] = self.dram_in("s5_c_re_%d" % li, [2, 64, 16, 64])
                w["c_im"] = self.dram_in("s5_c_im_%d" % li, [2, 64, 16, 64])
                w["d"] = self.dram_in("s5_d_%d" % li, [1, D])
                w["w_glu"] = self.dram_in("s5_w_glu_%d" % li, [D, 2 * D])
            self.W[li] = w
        self.y_out = nc.dram_tensor("y", [nlat, D], F32, kind="ExternalOutput").ap()

        self.xs = self.dram_tmp("xs", [nlat, D])
        self.cs = self.dram_tmp("cs", [nctx, D])
        self.MOD = self.dram_tmp("modv", [nl, 6, nb + 1, D])
        ntok_max = nlat + nctx
        self.nslot_max = ((2 * ntok_max + NE * (TS - 1)) + TS - 1) // TS * TS
        self.H2 = self.dram_tmp("h2", [ntok_max, D], BF16)
        self.H2S = self.dram_tmp("h2s", [self.nslot_max, D], BF16)
        self.YS = self.dram_tmp("ys", [self.nslot_max, D])
        self.HT = self.dram_tmp("ht", [D, ntok_max], BF16)
        self.W13B = self.dram_tmp("w13b", [NE * 128 * 4, 2048], BF16)
        self.W2B = self.dram_tmp("w2b", [NE * 128 * 2, 2048], BF16)
        self.YT = self.dram_tmp("yt", [D, ntok_max], BF16)

        self.identf = self.perm([128, 128])
        self.ident = self.perm([128, 128], BF16)
        self.ltri = self.perm([128, 128], BF16)
        self.ones = self.perm([128, 128], BF16)
        self.onesm = self.perm([128, 128], BF16)
        self.eps_rms = self.perm([128, 1])
        self.eps_ln = self.perm([128, 1])
        self.iota_p = self.perm([128, 1])
        self.one_c = self.perm([128, 1])
        tmpf = self.alloc([128, 128])
        S.I("pool", "iota", out=self.identf, pattern=[[1, 128]], base=0, channel_multiplier=-1,
            allow_small_or_imprecise_dtypes=True)
        S.I("dve", "tensor_single_scalar", out=tmpf, in_=self.identf, scalar=0.0, op=ALU.is_gt)
        S.I("dve", "tensor_copy", out=self.ltri, in_=tmpf)
        S.I("dve", "tensor_single_scalar", out=self.identf, in_=self.identf, scalar=0.0, op=ALU.is_equal)
        S.I("dve", "tensor_copy", out=self.ident, in_=self.identf)
        self._memset(self.ones, 1.0)
        self._memset(self.onesm, 1.0 / 1024.0)
        self._memset(self.eps_rms, RMS_EPS)
        self._memset(self.eps_ln, LN_EPS)
        self._memset(self.one_c, 1.0)
        S.I("pool", "iota", out=self.iota_p, pattern=[[0, 1]], base=0, channel_multiplier=1,
            allow_small_or_imprecise_dtypes=True)
        self.aoff = self.abase

        self.arena_reset()
        z = self.alloc([128, 8 * D], BF16)
        self._memset(z, 0.0)
        rows = self.nslot_max
        r0 = 0
        while r0 < rows:
            n = min(1024, rows - r0)
            assert n % 128 == 0
            S.dma("sp", self.dv(self.H2S[r0:r0 + n, :].rearrange("(p a) d -> p (a d)", p=128), "h2s"),
                  z[:, 0:(n // 128) * D], disjoint=True)
            r0 += n

        self.prologue()
        first = True
        for idx, li in enumerate(self.layers):
            kind = li % 3
            upd = li < DEPTH - 1
            need_ctx = upd or kind == 2
            if "moe" not in SKIP:
                self.precast(li)
            if "mixer" in SKIP:
                self.copy_x(first, upd)
            elif kind == 0:
                self.conformer(idx, li, first, upd)
            elif kind == 1:
                self.shortconv(idx, li, first, upd)
            else:
                self.s5(idx, li, first, upd)
            first = False
            if "moe" not in SKIP:
                self.moe(idx, li, upd)
        if self.final:
            self.final_norm()
        S.barrier()
        S.replay(nc, st)
        st.close()
        return nc

    def precast(self, li):
        w = self.W[li]
        for (src, dst, key, nrows) in ((w["w13"], self.W13B, "w13b", NE * 128 * 4), (w["w2"], self.W2B, "w2b", NE * 128 * 2)):
            for r0 in range(0, nrows, 1024):
                sv = src[r0:r0 + 1024, :].rearrange("(p a) n -> p a n", p=128)
                dv_ = dst[r0:r0 + 1024, :].rearrange("(p a) n -> p a n", p=128)
                self.S.dma("pool", self.dv(dv_, key), self.dv(sv, "w_in_%s_%d" % (key, li)), disjoint=True)

    def copy_x(self, first, upd):
        self.arena_reset()
        t = [self.alloc([128, D]) for _ in range(4)]
        i = 0
        for lat in ((True, False) if upd else (True,)):
            src, skey = self.xsrc(first, lat)
            dst, dkey = self.xdst(lat)
            n = self.nb * (SEQ if lat else CTX)
            for r0 in range(0, n, 128):
                self.S.dma("sp", t[i % 4], self.dv(src[r0:r0 + 128, :], (skey, r0)))
                self.S.dma("sp", self.dv(dst[r0:r0 + 128, :], (dkey, r0)), t[i % 4])
                i += 1

    def dbg(self, name, v, dt=F32):
        if not DEBUG:
            return
        shape = list(v.ap.shape)
        o = self.nc.dram_tensor("dbg_" + name, shape, dt, kind="ExternalOutput").ap()
        self.S.dma("sp", self.dv(o, "dbg_" + name), v)

    def _memset(self, v, val):
        ap = v.ap
        self.S._emit("dve", lambda e: e.memset(ap, val), [], [v.buf])

    def xsrc(self, first, lat):
        if lat:
            return (self.x_in if first else self.xs), ("xin" if first else "xs")
        return (self.ctx_in if first else self.cs), ("cin" if first else "cs")

    def xdst(self, lat):
        return (self.xs, "xs") if lat else (self.cs, "cs")

    def load_rows_bc(self, dst, src_row_ap, key, nparts=128, eng="sp"):
        n = src_row_ap.shape[-1]
        self.S.dma(eng, dst, self.dv(src_row_ap.to_broadcast([nparts, n]), key))

    def load_featT(self, dst, row_ap, key):
        self.S.dma("sp", dst, self.dv(row_ap.rearrange("o (k p) -> p (o k)", p=128), key), allow_slow_non_contiguous=True)

    def prologue(self):
        S, nb, nc = self.S, self.nb, self.nc
        self.arena_reset()
        ns = nb + 1
        cT = self.alloc([128, KC, ns])
        for b in range(nb):
            self.load_featT(cT[:, :, b], self.c_in[b:b + 1, :], "c_in")
        self.load_featT(cT[:, :, nb], self.cctx_in, "cctx_in")
        S.I("act", "activation", out=cT, in_=cT, func=AF.Silu)
        mrow = self.alloc([ns, 6 * D])
        abrow = self.alloc([ns, 6 * D])
        n1 = self.alloc([ns, D])
        n2 = self.alloc([ns, D])
        orow = self.alloc([ns, 6, D])
        awt = [self.alloc([128, KC, 512]) for _ in range(2)]
        for idx, li in enumerate(self.layers):
            w = self.W[li]
            self.load_rows_bc(abrow, w["ada_b"], "ada_b%d" % li, nparts=ns)
            self.load_rows_bc(n1, w["n1"], "n1_%d" % li, nparts=ns)
            self.load_rows_bc(n2, w["n2"], "n2_%d" % li, nparts=ns)
            awv = w["ada_w"].rearrange("(k p) n -> p k n", p=128)
            for cg in range(12):
                t = awt[cg % 2]
                S.dma("sp" if cg % 2 == 0 else "act", t, self.dv(awv[:, :, cg * 512:(cg + 1) * 512], "ada_w%d" % li))
                ps = self.psum[cg % 2]
                for k in range(KC):
                    S.I("pe", "matmul", out=ps[0:ns, :], lhsT=cT[:, k, :], rhs=t[:, k, :], start=(k == 0), stop=(k == KC - 1))
                S.I("dve", "tensor_tensor", out=mrow[:, cg * 512:(cg + 1) * 512], in0=ps[0:ns, :],
                    in1=abrow[:, cg * 512:(cg + 1) * 512], op=ALU.add)
            S.I("dve", "scalar_tensor_tensor", out=orow[:, 0, :], in0=mrow[:, D:2 * D], scalar=1.0, in1=n1, op0=ALU.add, op1=ALU.mult)
            S.I("dve", "tensor_copy", out=orow[:, 1, :], in_=mrow[:, 0:D])
            S.I("dve", "tensor_copy", out=orow[:, 2, :], in_=mrow[:, 2 * D:3 * D])
            S.I("dve", "scalar_tensor_tensor", out=orow[:, 3, :], in0=mrow[:, 4 * D:5 * D], scalar=1.0, in1=n2, op0=ALU.add, op1=ALU.mult)
            S.I("dve", "tensor_copy", out=orow[:, 4, :], in_=mrow[:, 3 * D:4 * D])
            S.I("dve", "tensor_copy", out=orow[:, 5, :], in_=mrow[:, 5 * D:6 * D])
            S.dma("sp", self.dv(self.MOD[idx].rearrange("k s d -> s k d"), "mod%d" % idx), orow)

    def mod_row(self, idx, kind, s):
        return self.dv(self.MOD[idx, kind, s:s + 1, :], "mod%d" % idx)

    def load_modT(self, idx):
        ns = self.nb + 1
        sT = self.alloc([128, KC, ns])
        bT = self.alloc([128, KC, ns])
        for s in range(ns):
            self.load_featT(sT[:, :, s], self.MOD[idx, 0, s:s + 1, :], "mod%d" % idx)
            self.load_featT(bT[:, :, s], self.MOD[idx, 1, s:s + 1, :], "mod%d" % idx)
        return sT, bT

    def norm_bufs(self, depth=2):
        nbuf = {"depth": depth}
        nbuf["xt"] = [self.alloc([128, D]) for _ in range(depth)]
        nbuf["sq"] = self.alloc([128, D])
        nbuf["x16"] = [self.alloc([128, D], BF16) for _ in range(2)]
        nbuf["ss"] = [self.alloc([128, 1]) for _ in range(depth)]
        nbuf["rs"] = [self.alloc([128, 1]) for _ in range(depth)]
        nbuf["tmp"] = self.alloc([128, KC, 128])
        nbuf["i"] = 0
        return nbuf

    def rms_tile(self, nbuf, src_v):
        S = self.S
        i = nbuf["i"] % nbuf["depth"]
        nbuf["i"] += 1
        xt, ss, rs = nbuf["xt"][i], nbuf["ss"][i], nbuf["rs"][i]
        S.dma("sp", xt, src_v)
        S.I("act", "activation", out=nbuf["sq"], in_=xt, func=AF.Square)
        S.I("dve", "reduce_sum", out=ss, in_=nbuf["sq"], axis=AX.X)
        S.I("act", "activation", out=rs, in_=ss, func=AF.Sqrt, scale=1.0 / D, bias=self.eps_rms)
        S.I("dve", "reciprocal", out=rs, in_=rs)
        return xt, rs, i

    def norm_transpose(self, nbuf, src_v, sT, bT, s, dst):
        S = self.S
        xt, rs, i = self.rms_tile(nbuf, src_v)
        x16 = nbuf["x16"][i % 2]
        S.I("act", "activation", out=x16, in_=xt, func=AF.Identity, scale=rs)
        pt = self.psum[0].bitcast(BF16)
        for k in range(KC):
            S.I("pe", "transpose", out=pt[:, k * 128:(k + 1) * 128], in_=x16[:, k * 128:(k + 1) * 128], identity=self.ident)
        tmp = nbuf["tmp"]
        S.I("dve", "tensor_tensor", out=tmp, in0=pt.re("p (k t) -> p k t", k=KC), in1=sT[:, :, s:s + 1].bc([128, KC, 128]), op=ALU.mult)
        S.I("dve", "tensor_tensor", out=dst, in0=tmp, in1=bT[:, :, s:s + 1].bc([128, KC, 128]), op=ALU.add)

    def seqs(self, with_ctx):
        out = [("lat", b) for b in range(self.nb)]
        if with_ctx:
            out.append(("ctx", self.nb))
        return out

    def load_w_bf16(self, dst, w_ap, key, ncols):
        wv = w_ap.rearrange("(k p) n -> p k n", p=128)
        for k in range(KC):
            for c0 in range(0, ncols, 2048):
                c1 = min(ncols, c0 + 2048)
                self.S.dma("pool", dst[:, k, c0:c1], self.dv(wv[:, k, c0:c1], key), disjoint=True)

    def residual_out(self, ps_halves, g_bc, src_v, dst_v, obuf, xbuf):
        S = self.S
        S.dma("sp", xbuf, src_v)
        for h in range(2):
            S.I("dve", "tensor_tensor", out=obuf[:, h * 512:(h + 1) * 512], in0=ps_halves[h], in1=g_bc[:, h * 512:(h + 1) * 512], op=ALU.mult)
        S.I("pool", "tensor_tensor", out=obuf, in0=obuf, in1=xbuf, op=ALU.add)
        S.dma("sp", dst_v, obuf)

    def conformer(self, idx, li, first, upd):
        S, nb, w = self.S, self.nb, self.W[li]
        self.arena_reset()
        sT, bT = self.load_modT(idx)
        w_in = self.alloc([128, KC, 2 * D], BF16)
        w_out = self.alloc([128, KC, D], BF16)
        self.load_w_bf16(w_in, w["w_in"], "cw_in%d" % li, 2 * D)
        self.load_w_bf16(w_out, w["w_out"], "cw_out%d" % li, D)
        dwT = self.alloc([128, KC, 31])
        for k in range(KC):
            S.dma("sp", dwT[:, k, :], self.dv(w["dw"][:, k * 128:(k + 1) * 128].rearrange("t p -> p t"), "dw%d" % li), allow_slow_non_contiguous=True)
        dwb = self.alloc([128, KC])
        lng = self.alloc([128, KC])
        lnb = self.alloc([128, KC])
        self.load_featT(dwb, w["dw_b"], "dwb%d" % li)
        self.load_featT(lng, w["ln_g"], "lng%d" % li)
        self.load_featT(lnb, w["ln_b"], "lnb%d" % li)
        nbuf = self.norm_bufs()
        g1 = [self.alloc([128, D]) for _ in range(2)]
        hT = [self.alloc([128, KC, 512], BF16) for _ in range(2)]
        zbuf = self.alloc([128, KC, SEQ], BF16)
        sg = [self.alloc([128, 512]) for _ in range(2)]
        dwd = [self.alloc([128, 31, 128], BF16) for _ in range(2)]
        vall = self.alloc([128, KC, SEQ], BF16)
        vsq = [self.alloc([128, 512], BF16) for _ in range(2)]
        s16 = self.alloc([128, KC, 512], BF16)
        msq = self.alloc([128, 512])
        mean = self.alloc([128, 512])
        rstd = self.alloc([128, 512])
        nmr = self.alloc([128, 512])
        t1 = [self.alloc([128, 512]) for _ in range(2)]
        obuf = [self.alloc([128, D])] * 2
        P = self.psum
        gi = 0
        di = 0
        ci2 = 0
        for si, (sk, s) in enumerate(self.seqs(upd)):
            lat = sk == "lat"
            ntok = SEQ if lat else nb * CTX
            src, skey = self.xsrc(first, lat)
            dst, dkey = self.xdst(lat)
            base = s * SEQ if lat else 0
            GS = min(512, ntok)
            ng = ntok // GS
            gb = g1[si % 2]
            self.load_rows_bc(gb, self.MOD[idx, 2, s:s + 1, :], "mod%d" % idx)
            for g in range(ng):
                h = hT[gi % 2]
                gi += 1
                for j in range(GS // 128):
                    r0 = base + g * GS + j * 128
                    self.norm_transpose(nbuf, self.dv(src[r0:r0 + 128, :], (skey, r0)), sT, bT, s, h[:, :, j * 128:(j + 1) * 128])
                for m in range(KC):
                    pv, pg = P[1 + m % 2], P[3 + m % 2]
                    for k in range(KC):
                        S.I("pe", "matmul", out=pv[:, 0:GS], lhsT=w_in[:, k, m * 128:(m + 1) * 128], rhs=h[:, k, 0:GS], start=(k == 0), stop=(k == KC - 1))
                    for k in range(KC):
                        S.I("pe", "matmul", out=pg[:, 0:GS], lhsT=w_in[:, k, D + m * 128:D + (m + 1) * 128], rhs=h[:, k, 0:GS], start=(k == 0), stop=(k == KC - 1))
                    sgt = sg[m % 2]
                    S.I("act", "activation", out=sgt[:, 0:GS], in_=pg[:, 0:GS], func=AF.Sigmoid)
                    S.I("dve", "tensor_tensor", out=zbuf[:, m, g * GS:(g + 1) * GS], in0=pv[:, 0:GS], in1=sgt[:, 0:GS], op=ALU.mult)
            order = [15] + [k for k in range(31) if k != 15]
            for m in range(KC):
                dd = dwd[di % 2]
                di += 1
                for k in range(31):
                    S.I("dve", "tensor_single_scalar", out=dd[:, k, :], in_=self.ident, scalar=dwT[:, m, k:k + 1], op=ALU.mult)
                for g in range(ng):
                    g0 = g * GS
                    pc = P[1 + ci2 % 2]
                    ci2 += 1
                    for n_, k in enumerate(order):
                        d = k - 15
                        if lat:
                            dt_ = d * 64
                            lo, hi = max(g0, -dt_), min(g0 + GS, SEQ - dt_)
                            if lo >= hi:
                                continue
                            S.I("pe", "matmul", out=pc[:, lo - g0:hi - g0], lhsT=dd[:, k, :], rhs=zbuf[:, m, lo + dt_:hi + dt_],
                                start=(n_ == 0), stop=(n_ == 30), skip_group_check=True)
                        else:
                            lo, hi = max(0, -d), min(CTX, CTX - d)
                            o = pc[:, 0:GS].re("p (s t) -> p s t", t=CTX)[:, :, lo:hi]
                            r = zbuf[:, m, g0:g0 + GS].re("p (s t) -> p s t", t=CTX)[:, :, lo + d:hi + d]
                            S.I("pe", "matmul", out=o, lhsT=dd[:, k, :], rhs=r, start=(n_ == 0), stop=(n_ == 30), skip_group_check=True)
                    S.I("act", "activation", out=vall[:, m, g0:g0 + GS], in_=pc[:, 0:GS], func=AF.Identity, bias=dwb[:, m:m + 1])
            for g in range(ng):
                g0 = g * GS
                v16 = vall[:, :, g0:g0 + GS]
                for m in range(KC):
                    vq = vsq[m % 2]
                    S.I("act", "activation", out=vq[:, 0:GS], in_=v16[:, m, :], func=AF.Square)
                    S.I("pe", "matmul", out=P[5][:, 0:GS], lhsT=self.onesm, rhs=v16[:, m, :], start=(m == 0), stop=(m == KC - 1))
                    S.I("pe", "matmul", out=P[6][:, 0:GS], lhsT=self.onesm, rhs=vq[:, 0:GS], start=(m == 0), stop=(m == KC - 1))
                S.I("act", "activation", out=mean[:, 0:GS], in_=P[5][:, 0:GS], func=AF.Identity)
                S.I("act", "activation", out=msq[:, 0:GS], in_=P[5][:, 0:GS], func=AF.Square)
                S.I("dve", "tensor_tensor", out=rstd[:, 0:GS], in0=P[6][:, 0:GS], in1=msq[:, 0:GS], op=ALU.subtract)
                S.I("act", "activation", out=rstd[:, 0:GS], in_=rstd[:, 0:GS], func=AF.Sqrt, bias=self.eps_ln)
                S.I("dve", "reciprocal", out=rstd[:, 0:GS], in_=rstd[:, 0:GS])
                S.I("dve", "scalar_tensor_tensor", out=nmr[:, 0:GS], in0=mean[:, 0:GS], scalar=-1.0, in1=rstd[:, 0:GS], op0=ALU.mult, op1=ALU.mult)
                for m in range(KC):
                    tt = t1[m % 2]
                    S.I("dve", "tensor_tensor", out=tt[:, 0:GS], in0=v16[:, m, :], in1=rstd[:, 0:GS], op=ALU.mult)
                    S.I("pool", "tensor_tensor", out=tt[:, 0:GS], in0=tt[:, 0:GS], in1=nmr[:, 0:GS], op=ALU.add)
                    S.I("act", "activation", out=s16[:, m, 0:GS], in_=tt[:, 0:GS], func=AF.Silu, scale=lng[:, m:m + 1], bias=lnb[:, m:m + 1])
                for j in range(GS // 128):
                    r0 = base + g0 + j * 128
                    for h_ in range(2):
                        for k in range(KC):
                            S.I("pe", "matmul", out=P[3 + h_], lhsT=s16[:, k, j * 128:(j + 1) * 128], rhs=w_out[:, k, h_ * 512:(h_ + 1) * 512],
                                start=(k == 0), stop=(k == KC - 1))
                    ob = obuf[j % 2]
                    self.residual_out([P[3], P[4]], gb, self.dv(src[r0:r0 + 128, :], (skey, r0)), self.dv(dst[r0:r0 + 128, :], (dkey, r0)),
                                      ob, nbuf["xt"][j % 2])

    def shortconv(self, idx, li, first, upd):
        S, nb, w = self.S, self.nb, self.W[li]
        self.arena_reset()
        sT, bT = self.load_modT(idx)
        w_in = self.alloc([128, KC, 3 * D], BF16)
        w_out = self.alloc([128, KC, D], BF16)
        self.load_w_bf16(w_in, w["w_in"], "sw_in%d" % li, 3 * D)
        self.load_w_bf16(w_out, w["w_out"], "sw_out%d" % li, D)
        cvT = self.alloc([128, KC, 3])
        for k in range(KC):
            S.dma("sp", cvT[:, k, :], self.dv(w["cv"][:, k * 128:(k + 1) * 128].rearrange("t p -> p t"), "cv%d" % li), allow_slow_non_contiguous=True)
        nbuf = self.norm_bufs()
        g1 = [self.alloc([128, D]) for _ in range(2)]
        hT = [self.alloc([128, KC, 512], BF16) for _ in range(2)]
        gcs = [self.alloc([128, 512]) for _ in range(2)]
        q = [self.alloc([128, 512]) for _ in range(2)]
        cc = [self.alloc([128, 512]) for _ in range(2)]
        p16 = self.alloc([128, KC, 512], BF16)
        obuf = [self.alloc([128, D]) for _ in range(2)]
        P = self.psum
        gi = 0
        for si, (sk, s) in enumerate(self.seqs(upd)):
            lat = sk == "lat"
            ntok = SEQ if lat else nb * CTX
            src, skey = self.xsrc(first, lat)
            dst, dkey = self.xdst(lat)
            base = s * SEQ if lat else 0
            GS = min(512, ntok)
            ng = ntok // GS
            RL = 64 if lat else CTX
            gb = g1[si % 2]
            self.load_rows_bc(gb, self.MOD[idx, 2, s:s + 1, :], "mod%d" % idx)
            for g in range(ng):
                g0 = g * GS
                h = hT[gi % 2]
                gi += 1
                for j in range(GS // 128):
                    r0 = base + g0 + j * 128
                    self.norm_transpose(nbuf, self.dv(src[r0:r0 + 128, :], (skey, r0)), sT, bT, s, h[:, :, j * 128:(j + 1) * 128])
                for m in range(KC):
                    pb, pc_, pv = P[1], P[2 + m % 2], P[4 + m % 2]
                    for (pp, off) in ((pc_, D), (pv, 2 * D)):
                        for k in range(KC):
                            S.I("pe", "matmul", out=pp[:, 0:GS], lhsT=w_in[:, k, off + m * 128:off + (m + 1) * 128], rhs=h[:, k, 0:GS],
                                start=(k == 0), stop=(k == KC - 1))
                    gct, qt, ct = gcs[m % 2], q[m % 2], cc[m % 2]
                    S.I("act", "activation", out=gct[:, 0:GS], in_=pc_[:, 0:GS], func=AF.Identity)
                    S.I("dve", "tensor_tensor", out=qt[:, 0:GS], in0=pv[:, 0:GS], in1=gct[:, 0:GS], op=ALU.mult)
                    S.I("act", "activation", out=ct[:, 0:GS], in_=qt[:, 0:GS], func=AF.Identity, scale=cvT[:, m, 1:2])
                    q3 = qt[:, 0:GS].re("p (r c) -> p r c", c=RL)
                    c3 = ct[:, 0:GS].re("p (r c) -> p r c", c=RL)
                    S.I("dve", "scalar_tensor_tensor", out=c3[:, :, 1:RL], in0=q3[:, :, 0:RL - 1], scalar=cvT[:, m, 0:1], in1=c3[:, :, 1:RL],
                        op0=ALU.mult, op1=ALU.add)
                    S.I("dve", "scalar_tensor_tensor", out=c3[:, :, 0:RL - 1], in0=q3[:, :, 1:RL], scalar=cvT[:, m, 2:3], in1=c3[:, :, 0:RL - 1],
                        op0=ALU.mult, op1=ALU.add)
                    for k in range(KC):
                        S.I("pe", "matmul", out=pb[:, 0:GS], lhsT=w_in[:, k, m * 128:(m + 1) * 128], rhs=h[:, k, 0:GS], start=(k == 0), stop=(k == KC - 1))
                    S.I("dve", "tensor_tensor", out=p16[:, m, 0:GS], in0=pb[:, 0:GS], in1=ct[:, 0:GS], op=ALU.mult)
                for j in range(GS // 128):
                    r0 = base + g0 + j * 128
                    for h_ in range(2):
                        for k in range(KC):
                            S.I("pe", "matmul", out=P[6 + h_], lhsT=p16[:, k, j * 128:(j + 1) * 128], rhs=w_out[:, k, h_ * 512:(h_ + 1) * 512],
                                start=(k == 0), stop=(k == KC - 1))
                    self.residual_out([P[6], P[7]], gb, self.dv(src[r0:r0 + 128, :], (skey, r0)), self.dv(dst[r0:r0 + 128, :], (dkey, r0)),
                                      obuf[j % 2], nbuf["xt"][j % 2])

    def s5(self, idx, li, first, upd):
        import math
        S, nb, w, P = self.S, self.nb, self.W[li], self.psum
        NTK = CTX + SEQ
        HTv = self.HT.rearrange("(k p) t -> p k t", p=128)
        YTv = self.YT.rearrange("(k p) t -> p k t", p=128)
        self.arena_reset()
        sT, bT = self.load_modT(idx)
        nbuf = self.norm_bufs()
        ht = [self.alloc([128, KC, 128], BF16) for _ in range(3)]
        i = 0
        for b in range(nb):
            for lat, n_t in ((False, CTX // 128), (True, SEQ // 128)):
                src, skey = self.xsrc(first, lat)
                for j in range(n_t):
                    r0 = (b * SEQ if lat else b * CTX) + j * 128
                    col = b * NTK + (CTX if lat else 0) + j * 128
                    t = ht[i % 3]
                    i += 1
                    self.norm_transpose(nbuf, self.dv(src[r0:r0 + 128, :], (skey, r0)), sT, bT, (b if lat else nb), t)
                    S.dma("sp", self.dv(HTv[:, :, col:col + 128], "ht"), t, disjoint=True)
        self.arena_reset()
        ND = 64
        f2 = lambda v: v.re("p d g -> p (d g)")
        are, aim, ldt = (self.alloc([128, 2, 32]) for _ in range(3))
        for d in range(2):
            S.dma("sp", are[:, d, :], self.dv(w["a_re"][d].rearrange("(G g) p -> (g p) G", g=2), "s5a"), allow_slow_non_contiguous=True)
            S.dma("sp", aim[:, d, :], self.dv(w["a_im"][d].rearrange("(G g) p -> (g p) G", g=2), "s5a"), allow_slow_non_contiguous=True)
            for g2 in range(2):
                srcv = w["ldt"][d:d + 1, :].rearrange("o (G g) -> o g G", g=2)[:, g2, :]
                S.dma("sp", ldt[g2 * 64:(g2 + 1) * 64, d, :], self.dv(srcv.to_broadcast([64, 32]), "s5a"), allow_slow_non_contiguous=True)
        names = ("dt", "mag", "ang", "c", "s", "ta", "tb", "den", "nre", "kre", "kim", "abr", "abi")
        T_ = {n: self.alloc([128, ND]) for n in names}
        hpi = self.alloc([128, 1])
        self._memset(hpi, math.pi / 2)
        A, B_ = f2(are), f2(aim)
        S.I("dve", "tensor_single_scalar", out=A, in_=A, scalar=-1e-4, op=ALU.min)
        S.I("act", "activation", out=T_["dt"], in_=f2(ldt), func=AF.Exp)
        S.I("dve", "tensor_tensor", out=T_["ta"], in0=T_["dt"], in1=A, op=ALU.mult)
        S.I("act", "activation", out=T_["mag"], in_=T_["ta"], func=AF.Exp)
        S.I("dve", "tensor_tensor", out=T_["ang"], in0=T_["dt"], in1=B_, op=ALU.mult)
        S.I("act", "activation", out=T_["s"], in_=T_["ang"], func=AF.Sin, scale=1.0 / 16)
        S.I("act", "activation", out=T_["c"], in_=T_["ang"], func=AF.Sin, scale=1.0 / 16, bias=hpi)

        def csquare(c, s, ta, tb):
            S.I("dve", "tensor_tensor", out=ta, in0=c, in1=c, op=ALU.mult)
            S.I("dve", "tensor_tensor", out=tb, in0=s, in1=s, op=ALU.mult)
            S.I("dve", "scalar_tensor_tensor", out=s, in0=c, scalar=2.0, in1=s, op0=ALU.mult, op1=ALU.mult)
            S.I("dve", "tensor_tensor", out=c, in0=ta, in1=tb, op=ALU.subtract)
        for _ in range(4):
            csquare(T_["c"], T_["s"], T_["ta"], T_["tb"])
        NP2 = 12
        Er = self.alloc([128, NP2, ND])
        Ei = self.alloc([128, NP2, ND])
        S.I("dve", "tensor_copy", out=Er[:, 0, :], in_=T_["c"])
        S.I("dve", "tensor_copy", out=Ei[:, 0, :], in_=T_["s"])
        for j in range(1, NP2):
            S.I("dve", "tensor_copy", out=Er[:, j, :], in_=Er[:, j - 1, :])
            S.I("dve", "tensor_copy", out=Ei[:, j, :], in_=Ei[:, j - 1, :])
            csquare(Er[:, j, :], Ei[:, j, :], T_["ta"], T_["tb"])
        S.I("dve", "tensor_tensor", out=T_["abr"], in0=T_["mag"], in1=T_["c"], op=ALU.mult)
        S.I("dve", "tensor_tensor", out=T_["abi"], in0=T_["mag"], in1=T_["s"], op=ALU.mult)
        S.I("dve", "tensor_tensor", out=T_["ta"], in0=A, in1=A, op=ALU.mult)
        S.I("dve", "tensor_tensor", out=T_["tb"], in0=B_, in1=B_, op=ALU.mult)
        S.I("dve", "tensor_tensor", out=T_["den"], in0=T_["ta"], in1=T_["tb"], op=ALU.add)
        S.I("dve", "reciprocal", out=T_["den"], in_=T_["den"])
        S.I("dve", "tensor_single_scalar", out=T_["nre"], in_=T_["abr"], scalar=-1.0, op=ALU.add)
        S.I("dve", "tensor_tensor", out=T_["ta"], in0=T_["nre"], in1=A, op=ALU.mult)
        S.I("dve", "tensor_tensor", out=T_["tb"], in0=T_["abi"], in1=B_, op=ALU.mult)
        S.I("dve", "tensor_tensor", out=T_["kre"], in0=T_["ta"], in1=T_["tb"], op=ALU.add)
        S.I("dve", "tensor_tensor", out=T_["kre"], in0=T_["kre"], in1=T_["den"], op=ALU.mult)
        S.I("dve", "tensor_tensor", out=T_["ta"], in0=T_["abi"], in1=A, op=ALU.mult)
        S.I("dve", "tensor_tensor", out=T_["tb"], in0=T_["nre"], in1=B_, op=ALU.mult)
        S.I("dve", "tensor_tensor", out=T_["kim"], in0=T_["ta"], in1=T_["tb"], op=ALU.subtract)
        S.I("dve", "tensor_tensor", out=T_["kim"], in0=T_["kim"], in1=T_["den"], op=ALU.mult)
        mag = T_["mag"]
        lB = self.alloc([32, ND, 2, 128], BF16)
        lC = self.alloc([128, ND, 2, 32], BF16)
        dvec = self.alloc([128, KC])
        self.load_featT(dvec, w["d"], "s5d")
        keep = self.aoff
        bre = self.alloc([128, 2, 32, 16])
        bim = self.alloc([128, 2, 32, 16])
        for d in range(2):
            S.dma("sp", bre[:, d], self.dv(w["b_re"][d].rearrange("(G g) p c -> (g p) G c", g=2), "s5b"))
            S.dma("sp", bim[:, d], self.dv(w["b_im"][d].rearrange("(G g) p c -> (g p) G c", g=2), "s5b"))
        bbr = self.alloc([128, ND, 16])
        bbi = self.alloc([128, ND, 16])
        tq = self.alloc([128, ND, 16])
        brf, bif = bre.re("p d g c -> p (d g) c"), bim.re("p d g c -> p (d g) c")
        kr3 = T_["kre"].un(2).bc([128, ND, 16])
        ki3 = T_["kim"].un(2).bc([128, ND, 16])
        S.I("dve", "tensor_tensor", out=bbr, in0=brf, in1=kr3, op=ALU.mult)
        S.I("dve", "tensor_tensor", out=tq, in0=bif, in1=ki3, op=ALU.mult)
        S.I("dve", "tensor_tensor", out=bbr, in0=bbr, in1=tq, op=ALU.subtract)
        S.I("dve", "tensor_tensor", out=bbi, in0=bif, in1=kr3, op=ALU.mult)
        S.I("dve", "tensor_tensor", out=tq, in0=brf, in1=ki3, op=ALU.mult)
        S.I("dve", "tensor_tensor", out=bbi, in0=bbi, in1=tq, op=ALU.add)
        bblk = [self.alloc([128, 2, 32], BF16) for _ in range(2)]
        for t in bblk:
            self._memset(t, 0.0)
        for dg in range(ND):
            t = bblk[dg % 2]
            for c_, bb in ((0, bbr), (1, bbi)):
                S.I("dve", "tensor_copy", out=t[0:64, c_, 0:16], in_=bb[0:64, dg, :])
                S.I("dve", "tensor_copy", out=t[64:128, c_, 16:32], in_=bb[64:128, dg, :])
            pt = P[dg % 2].bitcast(BF16)
            for c_ in range(2):
                S.I("pe", "transpose", out=pt[0:32, c_ * 128:(c_ + 1) * 128], in_=t[:, c_, :], identity=self.ident)
            S.I("act", "activation", out=lB[:, dg, :, :], in_=pt[0:32, 0:256].re("p (c q) -> p c q", c=2), func=AF.Identity)
        cnat = [self.alloc([32, 32, 128]) for _ in range(2)]
        ci = 0
        for d in range(2):
            for c_, nm in ((0, "c_re"), (1, "c_im")):
                t = cnat[ci % 2]
                ci += 1
                self._memset(t, 0.0)
                for g2 in range(2):
                    srcv = w[nm][d].rearrange("(G g) c p -> g c G p", g=2)[g2]
                    S.dma("sp", t[16 * g2:16 * g2 + 16, :, 64 * g2:64 * g2 + 64], self.dv(srcv, "s5c"), disjoint=True)
                for G in range(32):
                    pp = P[2 + G % 2]
                    S.I("pe", "transpose", out=pp[:, 0:32], in_=t[:, G, :], identity=self.identf[0:32, 0:32])
                    S.I("act", "activation", out=lC[:, d * 32 + G, c_, :], in_=pp[:, 0:32], func=AF.Identity, scale=(1.0 if c_ == 0 else -1.0))
        S.barrier()
        self.aoff = keep
        cosT = self.alloc([128, NTK])
        sinT = self.alloc([128, NTK])
        ttmp = [self.alloc([128, 1024]) for _ in range(2)]
        lCp = [self.alloc([128, 2, 128], BF16) for _ in range(2)]
        for t in lCp:
            self._memset(t, 0.0)
        u = [self.alloc([32, NTK], BF16) for _ in range(2)]
        bus = [[self.alloc([128, 512]) for _ in range(2)] for _ in range(2)]
        tm = [[self.alloc([128, 512]) for _ in range(4)] for _ in range(2)]
        Wr = [self.alloc([128, 512]) for _ in range(2)]
        Wi = [self.alloc([128, 512]) for _ in range(2)]
        Gr = [self.alloc([128, 512]) for _ in range(2)]
        Gi = [self.alloc([128, 512]) for _ in range(2)]
        to = tm
        Hr = [self.alloc([128, 512], BF16) for _ in range(2)]
        Hi = [self.alloc([128, 512], BF16) for _ in range(2)]
        yacc = [self.alloc([128, NTK]) for _ in range(nb)]
        inir = [self.alloc([128, 1]) for _ in range(2)]
        inii = [self.alloc([128, 1]) for _ in range(2)]
        hch = [self.alloc([128, NTK], BF16) for _ in range(2)]
        gt = [tm[0][0:3], tm[1][0:3]]
        y16 = [self.alloc([128, 512], BF16) for _ in range(2)]
        fw = [(0, CTX)] + [(CTX + 512 * i_, 512) for i_ in range(SEQ // 512)]
        rv = [(0, CTX)] + [(CTX + 512 * i_, 512) for i_ in reversed(range(SEQ // 512))]
        pcount = 0
        ui = 0
        for m in range(KC):
            for jj in range(4):
                G = 4 * m + jj
                for d in range(2):
                    dg = d * 32 + G
                    first_acc = (jj == 0 and d == 0)
                    self._memset(cosT[:, 0:1], 1.0)
                    self._memset(sinT[:, 0:1], 0.0)
                    n_have = 1
                    j = 0
                    while n_have < NTK:
                        n_new = min(n_have, NTK - n_have)
                        er, ei = Er[:, j, dg:dg + 1], Ei[:, j, dg:dg + 1]
                        ta, tb = ttmp[0][:, 0:n_new], ttmp[1][:, 0:n_new]
                        S.I("dve", "tensor_single_scalar", out=ta, in_=sinT[:, 0:n_new], scalar=ei, op=ALU.mult)
                        S.I("dve", "tensor_single_scalar", out=tb, in_=sinT[:, 0:n_new], scalar=er, op=ALU.mult)
                        S.I("dve", "scalar_tensor_tensor", out=sinT[:, n_have:n_have + n_new], in0=cosT[:, 0:n_new], scalar=ei, in1=tb, op0=ALU.mult, op1=ALU.add)
                        S.I("dve", "scalar_tensor_tensor", out=cosT[:, n_have:n_have + n_new], in0=cosT[:, 0:n_new], scalar=er, in1=ta, op0=ALU.mult, op1=ALU.subtract)
                        n_have += n_new
                        j += 1
                    lc = lCp[dg % 2]
                    S.I("dve", "tensor_copy", out=lc[:, :, 32 * jj:32 * jj + 32], in_=lC[:, dg, :, :])
                    if jj > 0:
                        pass
                    mg = mag[:, dg:dg + 1]
                    for b0 in range(0, nb, 2):
                        chains = list(range(b0, min(nb, b0 + 2)))
                        uts = {}
                        for c in chains:
                            uts[c] = u[c % 2]
                            S.dma("sp", uts[c], self.dv(self.HT[32 * G:32 * G + 32, c * NTK:(c + 1) * NTK], "ht"))
                        n0 = 0
                        first_piece = True
                        for (c0, L) in (fw if d == 0 else rv):
                            cs_, sn_ = cosT[:, n0:n0 + L], sinT[:, n0:n0 + L]
                            if d == 1:
                                cs_, sn_ = cs_.rev(), sn_.rev()
                            mb = mg.bc([128, L])
                            st = {}
                            for c in chains:
                                pi_ = c % 2
                                st[c] = dict(pr=P[0 + pi_], pim=P[2 + pi_], py=P[4 + pi_], br=bus[pi_][0][:, 0:L], bi=bus[pi_][1][:, 0:L],
                                             t=[x_[:, 0:L] for x_ in tm[pi_]], wr=Wr[pi_][:, 0:L], wi=Wi[pi_][:, 0:L],
                                             gr=Gr[pi_][:, 0:L], gi=Gi[pi_][:, 0:L], hr=Hr[pi_][:, 0:L], hi=Hi[pi_][:, 0:L],
                                             ir=inir[pi_], ii=inii[pi_], ut=uts[c], ya=yacc[c][:, c0:c0 + L])
                            fp_ = first_piece

                            def scan_op(q, which):
                                g_, w_, i_ = (q["gr"], q["wr"], q["ir"]) if which == 0 else (q["gi"], q["wi"], q["ii"])
                                ini = 0.0 if fp_ else i_
                                if d == 0:
                                    S.I("dve", "tensor_tensor_scan", out=g_, data0=mb, data1=w_, initial=ini, op0=ALU.mult, op1=ALU.add)
                                else:
                                    S.I("dve", "tensor_tensor_scan", out=g_.rev(), data0=mb, data1=w_.rev(), initial=ini, op0=ALU.mult, op1=ALU.add)

                            def save_init(q, which):
                                g_, i_ = (q["gr"], q["ir"]) if which == 0 else (q["gi"], q["ii"])
                                lastc = g_[:, L - 1:L] if d == 0 else g_[:, 0:1]
                                S.I("act", "activation", out=i_, in_=lastc, func=AF.Identity)

                            def acc_op(q):
                                if first_acc:
                                    S.I("act", "activation", out=q["ya"], in_=q["py"][:, 0:L], func=AF.Identity)
                                else:
                                    S.I("dve", "tensor_tensor", out=q["ya"], in0=q["py"][:, 0:L], in1=q["ya"], op=ALU.add)
                            steps = [
                                lambda q: S.I("pe", "matmul", out=q["pr"][:, 0:L], lhsT=lB[:, dg, 0, :], rhs=q["ut"][:, c0:c0 + L], start=True, stop=True),
                                lambda q: S.I("pe", "matmul", out=q["pim"][:, 0:L], lhsT=lB[:, dg, 1, :], rhs=q["ut"][:, c0:c0 + L], start=True, stop=True),
                                lambda q: S.I("act", "activation", out=q["br"], in_=q["pr"][:, 0:L], func=AF.Identity),
                                lambda q: S.I("act", "activation", out=q["bi"], in_=q["pim"][:, 0:L], func=AF.Identity),
                                lambda q: S.I("dve", "tensor_tensor", out=q["t"][0], in0=q["br"], in1=cs_, op=ALU.mult),
                                lambda q: S.I("pool", "tensor_tensor", out=q["t"][2], in0=q["bi"], in1=cs_, op=ALU.mult),
                                lambda q: S.I("dve", "tensor_tensor", out=q["t"][1], in0=q["bi"], in1=sn_, op=ALU.mult),
                                lambda q: S.I("pool", "tensor_tensor", out=q["t"][3], in0=q["br"], in1=sn_, op=ALU.mult),
                                lambda q: S.I("dve", "tensor_tensor", out=q["wr"], in0=q["t"][0], in1=q["t"][1], op=ALU.add),
                                lambda q: S.I("pool", "tensor_tensor", out=q["wi"], in0=q["t"][2], in1=q["t"][3], op=ALU.subtract),
                                lambda q: scan_op(q, 0),
                                lambda q: scan_op(q, 1),
                                lambda q: save_init(q, 0),
                                lambda q: save_init(q, 1),
                                lambda q: S.I("dve", "tensor_tensor", out=q["t"][0], in0=q["gr"], in1=cs_, op=ALU.mult),
                                lambda q: S.I("pool", "tensor_tensor", out=q["t"][2], in0=q["gr"], in1=sn_, op=ALU.mult),
                                lambda q: S.I("dve", "tensor_tensor", out=q["t"][1], in0=q["gi"], in1=sn_, op=ALU.mult),
                                lambda q: S.I("pool", "tensor_tensor", out=q["t"][3], in0=q["gi"], in1=cs_, op=ALU.mult),
                                lambda q: S.I("dve", "tensor_tensor", out=q["hr"], in0=q["t"][0], in1=q["t"][1], op=ALU.subtract),
                                lambda q: S.I("pool", "tensor_tensor", out=q["hi"], in0=q["t"][2], in1=q["t"][3], op=ALU.add),
                                lambda q: S.I("pe", "matmul", out=q["py"][:, 0:L], lhsT=lc[:, 0, :], rhs=q["hr"], start=True, stop=False),
                                lambda q: S.I("pe", "matmul", out=q["py"][:, 0:L], lhsT=lc[:, 1, :], rhs=q["hi"], start=False, stop=True),
                                lambda q: acc_op(q),
                            ]
                            for stp in steps:
                                for c in chains:
                                    stp(st[c])
                            n0 += L
                            first_piece = False
                    self.S._emit("dve", (lambda apx: (lambda e: e.memset(apx, 0.0)))(lc[:, :, 32 * jj:32 * jj + 32].ap), [], [lc.buf])
            for b in range(nb):
                hc = hch[b % 2]
                S.dma("sp", hc, self.dv(self.HT[128 * m:128 * m + 128, b * NTK:(b + 1) * NTK], "ht"))
                for pi2, (c0, L) in enumerate(fw):
                    tt, sq, sg_ = (x_[:, 0:L] for x_ in gt[pi2 % 2])
                    yo_ = y16[pi2 % 2][:, 0:L]
                    S.I("dve", "scalar_tensor_tensor", out=tt, in0=hc[:, c0:c0 + L], scalar=dvec[:, m:m + 1], in1=yacc[b][:, c0:c0 + L], op0=ALU.mult, op1=ALU.add)
                    S.I("act", "activation", out=sq, in_=tt, func=AF.Square)
                    S.I("act", "activation", out=sq, in_=sq, func=AF.Identity, scale=0.044715, bias=self.one_c)
                    S.I("pool", "tensor_tensor", out=sq, in0=sq, in1=tt, op=ALU.mult)
                    S.I("act", "activation", out=sg_, in_=sq, func=AF.Sigmoid, scale=1.5957691216057308)
                    S.I("pool", "tensor_tensor", out=yo_, in0=tt, in1=sg_, op=ALU.mult)
                    col = b * NTK + c0
                    S.dma("sp", self.dv(YTv[:, m, col:col + L], "yt"), yo_, disjoint=True)
        self.arena_reset()
        wg = self.alloc([128, KC, 2 * D], BF16)
        self.load_w_bf16(wg, w["w_glu"], "s5wg%d" % li, 2 * D)
        g1 = [self.alloc([128, D]) for _ in range(2)]
        yt = [self.alloc([128, KC, 128], BF16) for _ in range(2)]
        sgb = [self.alloc([128, D]) for _ in range(2)]
        obuf = [self.alloc([128, D]) for _ in range(2)]
        xb = [self.alloc([128, D]) for _ in range(2)]
        ti = 0
        for b in range(nb):
            self.load_rows_bc(g1[0], self.MOD[idx, 2, b:b + 1, :], "mod%d" % idx)
            if upd:
                self.load_rows_bc(g1[1], self.MOD[idx, 2, nb:nb + 1, :], "mod%d" % idx)
            for lat, n_t in (((False, CTX // 128),) if upd else ()) + ((True, SEQ // 128),):
                src, skey = self.xsrc(first, lat)
                dst, dkey = self.xdst(lat)
                gb = g1[0] if lat else g1[1]
                for j in range(n_t):
                    r0 = (b * SEQ if lat else b * CTX) + j * 128
                    col = b * NTK + (CTX if lat else 0) + j * 128
                    y_ = yt[ti % 2]
                    S.dma("sp", y_, self.dv(YTv[:, :, col:col + 128], "yt"))
                    for cb in range(4):
                        pp = P[cb]
                        for k in range(KC):
                            S.I("pe", "matmul", out=pp, lhsT=y_[:, k, :], rhs=wg[:, k, cb * 512:(cb + 1) * 512], start=(k == 0), stop=(k == KC - 1))
                    sg_, ob, xx = sgb[ti % 2], obuf[ti % 2], xb[ti % 2]
                    ti += 1
                    S.dma("sp", xx, self.dv(src[r0:r0 + 128, :], (skey, r0)))
                    for h_ in range(2):
                        S.I("act", "activation", out=sg_[:, h_ * 512:(h_ + 1) * 512], in_=P[2 + h_], func=AF.Sigmoid)
                        S.I("dve", "tensor_tensor", out=ob[:, h_ * 512:(h_ + 1) * 512], in0=P[h_], in1=sg_[:, h_ * 512:(h_ + 1) * 512], op=ALU.mult)
                    S.I("pool", "tensor_tensor", out=ob, in0=ob, in1=gb, op=ALU.mult)
                    S.I("dve", "tensor_tensor", out=ob, in0=ob, in1=xx, op=ALU.add)
                    S.dma("sp", self.dv(dst[r0:r0 + 128, :], (dkey, r0)), ob)

    def moe(self, idx, li, upd):
        S, nb, w, nc = self.S, self.nb, self.W[li], self.nc
        self.arena_reset()
        P = self.psum
        tiles = []
        for b in range(nb):
            for j in range(SEQ // 128):
                r0 = b * SEQ + j * 128
                tiles.append((self.xs, "xs", r0, b))
        if upd:
            for j in range(nb * CTX // 128):
                tiles.append((self.cs, "cs", j * 128, nb))
        NT = len(tiles)
        ntok = NT * 128
        nslot = ((2 * ntok + NE * (TS - 1)) + TS - 1) // TS * TS
        NST = nslot // TS

        oh1 = self.alloc([128, NT, NE])
        oh2 = self.alloc([128, NT, NE])
        L1 = self.alloc([128, NT])
        L2 = self.alloc([128, NT])
        W1 = self.alloc([128, NT])
        W2 = self.alloc([128, NT])
        run = self.alloc([128, NE])
        sl1 = self.alloc([128, NT], I32)
        sl2 = self.alloc([128, NT], I32)
        widx = self.alloc([128, NST], I32)
        keep = self.aoff

        wr = self.alloc([128, KC, 36])
        S.dma("sp", wr, self.dv(w["wr"].rearrange("(k p) n -> p k n", p=128), "wr%d" % li))
        brb = self.alloc([128, 36])
        self.load_rows_bc(brb, w["br"], "br%d" % li)
        sc2 = [self.alloc([128, D]) for _ in range(2)]
        sh2 = [self.alloc([128, D]) for _ in range(2)]
        nbuf = self.norm_bufs(4)
        h2 = [self.alloc([128, D]) for _ in range(4)]
        h16 = [self.alloc([128, D], BF16) for _ in range(4)]
        h2T = [self.alloc([128, KC, 128]) for _ in range(4)]
        GB = 4
        lg = self.alloc([128, GB, 36])
        gmx, gsum, gw, m1, m2, dm, den = (self.alloc([128, GB]) for _ in range(7))
        ohg = self.alloc([128, GB, 4])
        ex = self.alloc([128, GB, 4])
        t48 = self.alloc([128, GB, 4, 8])
        le, le2, o1, o2 = (self.alloc([128, GB, 8]) for _ in range(4))
        sel = self.alloc([128, GB, NE])
        sel16 = self.alloc([128, GB, NE], BF16)
        pf = self.alloc([128, GB, NE])
        tmp32 = self.alloc([128, GB, NE])
        self._memset(run, 0.0)
        cur_s = None
        for t0 in range(0, NT, GB):
            n = min(GB, NT - t0)
            for j in range(n):
                ti = t0 + j
                src, skey, r0, s = tiles[ti]
                if s != cur_s:
                    cur_s = s
                    cb = s % 2
                    self.load_rows_bc(sc2[cb], self.MOD[idx, 3, s:s + 1, :], "mod%d" % idx)
                    self.load_rows_bc(sh2[cb], self.MOD[idx, 4, s:s + 1, :], "mod%d" % idx, eng="act")
                xt, rs, i = self.rms_tile(nbuf, self.dv(src[r0:r0 + 128, :], (skey, r0)))
                hh, hb, hT_ = h2[ti % 4], h16[ti % 4], h2T[ti % 4]
                S.I("dve", "scalar_tensor_tensor", out=hh, in0=xt, scalar=rs, in1=sc2[cb], op0=ALU.mult, op1=ALU.mult)
                S.I("pool", "tensor_tensor", out=hh, in0=hh, in1=sh2[cb], op=ALU.add)
                S.I("act", "activation", out=hb, in_=hh, func=AF.Identity)
                S.dma("sp", self.dv(self.H2[ti * 128:(ti + 1) * 128, :], ("h2", ti)), hb)
                for k in range(KC):
                    pt = P[1 + (k // 4) % 2]
                    S.I("pe", "transpose", out=pt[:, (k % 4) * 128:(k % 4 + 1) * 128], in_=hh[:, k * 128:(k + 1) * 128], identity=self.identf)
                    if k % 4 == 3:
                        S.I("act", "activation", out=hT_[:, k - 3:k + 1, :], in_=pt.re("p (k t) -> p k t", k=4), func=AF.Identity)
                for k in range(KC):
                    S.I("pe", "matmul", out=P[3][:, j * 36:(j + 1) * 36], lhsT=hT_[:, k, :], rhs=wr[:, k, :], start=(k == 0), stop=(k == KC - 1),
                        skip_group_check=True)
            N_ = slice(0, n)
            S.I("dve", "tensor_tensor", out=lg[:, N_, :], in0=P[3][:, 0:n * 36].re("p (t c) -> p t c", c=36), in1=brb.un(1).bc([128, n, 36]), op=ALU.add)
            lgg = lg[:, N_, 0:4]
            S.I("dve", "reduce_max", out=gmx[:, N_], in_=lgg, axis=AX.X)
            S.I("dve", "tensor_tensor", out=ohg[:, N_, :], in0=lgg, in1=gmx[:, N_].un(2).bc([128, n, 4]), op=ALU.is_equal)
            S.I("dve", "tensor_tensor", out=ex[:, N_, :], in0=lgg, in1=gmx[:, N_].un(2).bc([128, n, 4]), op=ALU.subtract)
            S.I("act", "activation", out=ex[:, N_, :], in_=ex[:, N_, :], func=AF.Exp)
            S.I("dve", "reduce_sum", out=gsum[:, N_], in_=ex[:, N_, :], axis=AX.X)
            S.I("dve", "reciprocal", out=gw[:, N_], in_=gsum[:, N_])
            S.I("dve", "tensor_tensor", out=t48[:, N_], in0=lg[:, N_, 4:36].re("p t (g e) -> p t g e", g=4),
                in1=ohg[:, N_, :].un(3).bc([128, n, 4, 8]), op=ALU.mult)
            S.I("dve", "reduce_sum", out=le[:, N_, :], in_=t48[:, N_].re("p t g e -> p t e g"), axis=AX.X)
            S.I("dve", "reduce_max", out=m1[:, N_], in_=le[:, N_, :], axis=AX.X)
            S.I("dve", "tensor_tensor", out=o1[:, N_, :], in0=le[:, N_, :], in1=m1[:, N_].un(2).bc([128, n, 8]), op=ALU.is_equal)
            S.I("dve", "scalar_tensor_tensor", out=le2[:, N_, :], in0=o1[:, N_, :], scalar=-1e30, in1=le[:, N_, :], op0=ALU.mult, op1=ALU.add)
            S.I("dve", "reduce_max", out=m2[:, N_], in_=le2[:, N_, :], axis=AX.X)
            S.I("dve", "tensor_tensor", out=o2[:, N_, :], in0=le2[:, N_, :], in1=m2[:, N_].un(2).bc([128, n, 8]), op=ALU.is_equal)
            S.I("dve", "tensor_tensor", out=dm[:, N_], in0=m2[:, N_], in1=m1[:, N_], op=ALU.subtract)
            S.I("act", "activation", out=dm[:, N_], in_=dm[:, N_], func=AF.Exp)
            S.I("dve", "tensor_single_scalar", out=den[:, N_], in_=dm[:, N_], scalar=1.0, op=ALU.add)
            S.I("dve", "reciprocal", out=den[:, N_], in_=den[:, N_])
            S.I("dve", "tensor_tensor", out=W1[:, t0:t0 + n], in0=gw[:, N_], in1=den[:, N_], op=ALU.mult)
            S.I("dve", "tensor_tensor", out=W2[:, t0:t0 + n], in0=gw[:, N_], in1=W1[:, t0:t0 + n], op=ALU.subtract)
            o1g = oh1[:, t0:t0 + n, :].re("p t (g e) -> p t g e", g=4)
            o2g = oh2[:, t0:t0 + n, :].re("p t (g e) -> p t g e", g=4)
            S.I("dve", "tensor_tensor", out=o1g, in0=ohg[:, N_, :].un(3).bc([128, n, 4, 8]), in1=o1[:, N_, :].un(2).bc([128, n, 4, 8]), op=ALU.mult)
            S.I("dve", "tensor_tensor", out=o2g, in0=ohg[:, N_, :].un(3).bc([128, n, 4, 8]), in1=o2[:, N_, :].un(2).bc([128, n, 4, 8]), op=ALU.mult)
            S.I("dve", "tensor_tensor", out=sel[:, N_, :], in0=oh1[:, t0:t0 + n, :], in1=oh2[:, t0:t0 + n, :], op=ALU.add)
            S.I("dve", "tensor_copy", out=sel16[:, N_, :], in_=sel[:, N_, :])
            for j in range(n):
                S.I("pe", "matmul", out=P[4][:, j * NE:(j + 1) * NE], lhsT=self.ltri, rhs=sel16[:, j, :], start=True, stop=True, skip_group_check=True)
                S.I("pe", "matmul", out=P[5][:, j * NE:(j + 1) * NE], lhsT=self.ones, rhs=sel16[:, j, :], start=True, stop=True, skip_group_check=True)
            for j in range(n):
                S.I("dve", "tensor_tensor", out=pf[:, j, :], in0=P[4][:, j * NE:(j + 1) * NE], in1=run, op=ALU.add)
                S.I("dve", "tensor_tensor", out=run, in0=P[5][:, j * NE:(j + 1) * NE], in1=run, op=ALU.add)
            S.I("dve", "tensor_tensor", out=tmp32[:, N_, :], in0=pf[:, N_, :], in1=oh1[:, t0:t0 + n, :], op=ALU.mult)
            S.I("dve", "reduce_sum", out=L1[:, t0:t0 + n], in_=tmp32[:, N_, :], axis=AX.X)
            S.I("dve", "tensor_tensor", out=tmp32[:, N_, :], in0=pf[:, N_, :], in1=oh2[:, t0:t0 + n, :], op=ALU.mult)
            S.I("dve", "reduce_sum", out=L2[:, t0:t0 + n], in_=tmp32[:, N_, :], axis=AX.X)

        if 'moeB' in SKIP:
            return
        self.S.barrier()
        self.aoff = keep
        cnti = self.alloc([128, NE], I32)
        pad = self.alloc([128, NE])
        incl = self.alloc([128, NE])
        basee = self.alloc([128, NE])
        onesf = self.alloc([128, NE])
        big3 = self.alloc([128, NT, NE])
        sf = self.alloc([128, NT])
        sgrid = self.alloc([128, NST])
        cmp3 = self.alloc([128, NST, NE])
        ef = self.alloc([128, NST])
        S.I("dve", "tensor_copy", out=cnti, in_=run)
        S.I("dve", "tensor_single_scalar", out=cnti, in_=cnti, scalar=TS - 1, op=ALU.add)
        sh = TS.bit_length() - 1
        S.I("dve", "tensor_scalar", out=cnti, in0=cnti, scalar1=sh, scalar2=sh, op0=ALU.arith_shift_right, op1=ALU.logical_shift_left)
        S.I("dve", "tensor_copy", out=pad, in_=cnti)
        self._memset(onesf, 1.0)
        S.I("dve", "tensor_tensor_scan", out=incl, data0=onesf, data1=pad, initial=0.0, op0=ALU.mult, op1=ALU.add)
        S.I("dve", "tensor_tensor", out=basee, in0=incl, in1=pad, op=ALU.subtract)
        for (oh, L, sl) in ((oh1, L1, sl1), (oh2, L2, sl2)):
            S.I("dve", "tensor_tensor", out=big3, in0=oh, in1=basee.un(1).bc([128, NT, NE]), op=ALU.mult)
            S.I("dve", "reduce_sum", out=sf, in_=big3, axis=AX.X)
            S.I("dve", "tensor_tensor", out=sf, in0=sf, in1=L, op=ALU.add)
            S.I("dve", "tensor_copy", out=sl, in_=sf)
        S.I("pool", "iota", out=sgrid, pattern=[[TS, NST]], base=0, channel_multiplier=0, allow_small_or_imprecise_dtypes=True)
        S.I("dve", "tensor_tensor", out=cmp3, in0=incl.un(1).bc([128, NST, NE]), in1=sgrid.un(2).bc([128, NST, NE]), op=ALU.is_le)
        S.I("dve", "reduce_sum", out=ef, in_=cmp3, axis=AX.X)
        S.I("dve", "tensor_single_scalar", out=ef, in_=ef, scalar=float(NE - 1), op=ALU.min)
        same = self.alloc([128, NST])
        self._memset(same, 0.0)
        S.I("dve", "tensor_tensor", out=same[:, 2:NST], in0=ef[:, 2:NST], in1=ef[:, 0:NST - 2], op=ALU.is_equal)
        S.I("dve", "tensor_single_scalar", out=same, in_=same, scalar=float(1 << 20), op=ALU.mult)
        S.I("dve", "tensor_single_scalar", out=ef, in_=ef, scalar=128.0, op=ALU.mult)
        S.I("dve", "tensor_single_scalar", out=ef, in_=ef, scalar=self.iota_p, op=ALU.add)
        S.I("dve", "tensor_tensor", out=ef, in0=ef, in1=same, op=ALU.add)
        S.I("dve", "tensor_copy", out=widx, in_=ef)
        self.dbg("W1", W1); self.dbg("W2", W2); self.dbg("L1", L1); self.dbg("L2", L2); self.dbg("run", run)
        self.dbg("sl1", sl1, I32); self.dbg("sl2", sl2, I32); self.dbg("widx", widx, I32); self.dbg("oh1", oh1); self.dbg("oh2", oh2)
        self.dbg("incl", incl); self.dbg("basee", basee)
        hl = [self.alloc([128, D], BF16) for _ in range(4)]
        h2s_v = self.dv(self.H2S[0:nslot, :], "h2s")
        for ti in range(NT):
            t = hl[ti % 4]
            S.dma("sp", t, self.dv(self.H2[ti * 128:(ti + 1) * 128, :], ("h2", ti)))
            S.scatter(h2s_v, t, sl1[:, ti:ti + 1])
            S.scatter(h2s_v, t, sl2[:, ti:ti + 1])

        if 'moeC' in SKIP:
            return
        self.S.barrier()
        self.aoff = keep
        w13t = [self.alloc([128, KC * D], BF16) for _ in range(2)]
        w2t = [self.alloc([128, 4 * D], BF16) for _ in range(2)]
        hs = [self.alloc([128, 2, D], BF16) for _ in range(2)]
        hT = [self.alloc([128, KC, TS], BF16) for _ in range(2)]
        sa = [self.alloc([128, TS]) for _ in range(2)]
        u16 = [self.alloc([128, 4, TS], BF16) for _ in range(2)]
        yo = [self.alloc([128, D]) for _ in range(2)]
        w13v = self.dv(self.W13B.rearrange("(r a) n -> r (a n)", a=4), "w13b")
        w2v = self.dv(self.W2B.rearrange("(r a) n -> r (a n)", a=2), "w2b")
        yi = 0
        for s_ in range(NST):
            wa, wb, hsl, hTt, ut = w13t[s_ % 2], w2t[s_ % 2], hs[s_ % 2], hT[s_ % 2], u16[s_ % 2]
            S.gather(wa, w13v, widx[:, s_:s_ + 1], bound=NE * 128 - 1)
            S.gather(wb, w2v, widx[:, s_:s_ + 1], bound=NE * 128 - 1)
            S.dma("sp", hsl, self.dv(self.H2S[s_ * TS:(s_ + 1) * TS, :].rearrange("(a p) d -> p a d", p=128), "h2s"))
            for a in range(2):
                pt = P[a].bitcast(BF16)
                for k in range(KC):
                    S.I("pe", "transpose", out=pt[:, k * 128:(k + 1) * 128], in_=hsl[:, a, k * 128:(k + 1) * 128], identity=self.ident)
                S.I("act" if a == 0 else "dve", "activation" if a == 0 else "tensor_copy", out=hTt[:, :, a * 128:(a + 1) * 128],
                    in_=pt.re("p (k t) -> p k t", k=KC), **({"func": AF.Copy} if a == 0 else {}))
            for m in range(4):
                pa, pg = P[2 + m % 2], P[4 + m % 2]
                for (pp, off) in ((pa, 0), (pg, DE)):
                    for k in range(KC):
                        c0 = k * D + off + m * 128
                        S.I("pe", "matmul", out=pp[:, 0:TS], lhsT=wa[:, c0:c0 + 128], rhs=hTt[:, k, :], start=(k == 0), stop=(k == KC - 1))
                sat = sa[m % 2]
                S.I("act", "activation", out=sat, in_=pa[:, 0:TS], func=AF.Silu)
                S.I("dve", "tensor_tensor", out=ut[:, m, :], in0=pg[:, 0:TS], in1=sat, op=ALU.mult)
            for a in range(2):
                yt = yo[yi % 2]
                yi += 1
                for h_ in range(2):
                    pp = P[6 + h_]
                    for k in range(4):
                        S.I("pe", "matmul", out=pp, lhsT=ut[:, k, a * 128:(a + 1) * 128], rhs=wb[:, k * D + h_ * 512:k * D + (h_ + 1) * 512],
                            start=(k == 0), stop=(k == 3))
                    S.I("act" if h_ == 0 else "dve", "activation" if h_ == 0 else "tensor_copy", out=yt[:, h_ * 512:(h_ + 1) * 512], in_=pp,
                        **({"func": AF.Copy} if h_ == 0 else {}))
                r0 = s_ * TS + a * 128
                S.dma("sp", self.dv(self.YS[r0:r0 + 128, :], "ys"), yt, disjoint=True)

        if 'moeD' in SKIP:
            return
        self.S.barrier()
        self.aoff = keep
        g2 = [self.alloc([128, D]) for _ in range(2)]
        y1 = [self.alloc([128, D]) for _ in range(4)]
        y2 = [self.alloc([128, D]) for _ in range(4)]
        xt2 = [self.alloc([128, D]) for _ in range(4)]
        acc = [self.alloc([128, D]) for _ in range(4)]
        ysv = self.dv(self.YS[0:nslot, :], "ys")
        cur_s = None
        for ti, (src, skey, r0, s) in enumerate(tiles):
            if s != cur_s:
                cur_s = s
                cb = s % 2
                self.load_rows_bc(g2[cb], self.MOD[idx, 5, s:s + 1, :], "mod%d" % idx)
            a1, a2, xx, ac = y1[ti % 4], y2[ti % 4], xt2[ti % 4], acc[ti % 4]
            S.gather(a1, ysv, sl1[:, ti:ti + 1])
            S.gather(a2, ysv, sl2[:, ti:ti + 1])
            S.dma("sp", xx, self.dv(src[r0:r0 + 128, :], (skey, r0)))
            S.I("act", "activation", out=ac, in_=a1, func=AF.Identity, scale=W1[:, ti:ti + 1])
            S.I("dve", "scalar_tensor_tensor", out=ac, in0=a2, scalar=W2[:, ti:ti + 1], in1=ac, op0=ALU.mult, op1=ALU.add)
            S.I("pool", "tensor_tensor", out=ac, in0=ac, in1=g2[cb], op=ALU.mult)
            S.I("dve", "tensor_tensor", out=ac, in0=ac, in1=xx, op=ALU.add)
            S.dma("sp", self.dv(src[r0:r0 + 128, :], (skey, r0)), ac)

    def final_norm(self):
        S, nb = self.S, self.nb
        self.arena_reset()
        nbuf = self.norm_bufs(4)
        fg = self.alloc([128, D])
        self.load_rows_bc(fg, self.final_g, "final_g")
        ob = [self.alloc([128, D]) for _ in range(4)]
        for ti in range(nb * SEQ // 128):
            r0 = ti * 128
            xt, rs, i = self.rms_tile(nbuf, self.dv(self.xs[r0:r0 + 128, :], ("xs", r0)))
            o = ob[ti % 4]
            S.I("dve", "scalar_tensor_tensor", out=o, in0=xt, scalar=rs, in1=fg, op0=ALU.mult, op1=ALU.mult)
            S.dma("sp", self.dv(self.y_out[r0:r0 + 128, :], ("y", r0)), o)


def prep_weights(inp, layers):
    out = {}
    f = lambda a: np.ascontiguousarray(a, dtype=np.float32)
    out["c_ctx"] = f(inp["c_ctx"]).reshape(1, D)
    out["final_g"] = f(inp["final_g"]).reshape(1, D)
    for li in layers:
        kind, j = li % 3, li // 3
        out["ada_w_%d" % li] = f(inp["ada_w"][li])
        out["ada_b_%d" % li] = f(inp["ada_b"][li]).reshape(1, -1)
        out["norm1_g_%d" % li] = f(inp["norm1_g"][li]).reshape(1, D)
        out["norm2_g_%d" % li] = f(inp["norm2_g"][li]).reshape(1, D)
        out["moe_wr_%d" % li] = f(np.concatenate([inp["moe_wg"][li], inp["moe_we"][li]], axis=1))
        out["moe_br_%d" % li] = f(np.concatenate([inp["moe_bg"][li], inp["moe_be"][li]], axis=0)).reshape(1, 36)
        w13 = np.asarray(inp["moe_w13"][li]).reshape(NE, KC, 128, D).transpose(0, 2, 1, 3).reshape(NE * 128 * 4, 2048)
        out["moe_w13_%d" % li] = f(w13)
        w2 = np.asarray(inp["moe_w2"][li]).reshape(NE, 4, 128, D).transpose(0, 2, 1, 3).reshape(NE * 128 * 2, 2048)
        out["moe_w2_%d" % li] = f(w2)
        if kind == 0:
            out["conf_w_in_%d" % li] = f(inp["conf_w_in"][j])
            out["conf_dw_%d" % li] = f(inp["conf_dw"][j])
            out["conf_dw_b_%d" % li] = f(inp["conf_dw_b"][j]).reshape(1, D)
            out["conf_ln_g_%d" % li] = f(inp["conf_ln_g"][j]).reshape(1, D)
            out["conf_ln_b_%d" % li] = f(inp["conf_ln_b"][j]).reshape(1, D)
            out["conf_w_out_%d" % li] = f(inp["conf_w_out"][j])
        elif kind == 1:
            out["sc_w_in_%d" % li] = f(inp["sc_w_in"][j])
            out["sc_conv_%d" % li] = f(inp["sc_conv"][j])
            out["sc_w_out_%d" % li] = f(inp["sc_w_out"][j])
        else:
            for n in ("a_re", "a_im", "b_re", "b_im", "c_re", "c_im"):
                out["s5_%s_%d" % (n, li)] = f(inp["s5_" + n][j])
            out["s5_log_dt_%d" % li] = f(inp["s5_log_dt"][j])
            out["s5_d_%d" % li] = f(inp["s5_d"][j]).reshape(1, D)
            out["s5_w_glu_%d" % li] = f(inp["s5_w_glu"][j])
    return out


def run(inp, nb, ncores, layers, final=True, trace=False):
    prog = Prog(nb, layers, final)
    nc = prog.build()
    shared = prep_weights(inp, layers)
    x = np.asarray(inp["x"], dtype=np.float32)
    c = np.asarray(inp["c"], dtype=np.float32)
    ctx = np.asarray(inp["ctx"], dtype=np.float32)
    in_maps = []
    for k in range(ncores):
        m = dict(shared)
        m["x"] = np.ascontiguousarray(x[k * nb:(k + 1) * nb]).reshape(nb * SEQ, D)
        m["c"] = np.ascontiguousarray(c[k * nb:(k + 1) * nb])
        m["ctx"] = np.ascontiguousarray(ctx[k * nb:(k + 1) * nb]).reshape(nb * CTX, D)
        in_maps.append({n: m[n] for n in prog.in_names})
    res = run_bass_kernel_spmd(nc, in_maps, core_ids=list(range(ncores)), **({"trace": True} if trace else {}))
    y = np.concatenate([r["y"].reshape(nb, SEQ, D) for r in res.results], axis=0)
    return y, res


def kernel(**inputs):
    y, _ = run(inputs, nb=4, ncores=NCORES, layers=list(range(DEPTH)), final=True)
    return y.astype(np.float32)
```

```python
import contextlib
import numpy as np
import concourse.bass as bass
import concourse.mybir as mybir
from concourse.bass_utils import run_bass_kernel_spmd

F32 = mybir.dt.float32
BF16 = mybir.dt.bfloat16
I32 = mybir.dt.int32
AF = mybir.ActivationFunctionType
ALU = mybir.AluOpType
AX = mybir.AxisListType

D = 1024
KC = 8
SEQ = 2048
CTX = 256
NE = 32
DE = 512
TS = 256
RMS_EPS = 1e-6
LN_EPS = 1e-5
DEPTH = 4
NCORES = 8
SKIP = set()
DEBUG = False


class Buf:
    __slots__ = ("w", "wx", "r")

    def __init__(self):
        self.w = {}
        self.wx = {}
        self.r = {}


class V:
    __slots__ = ("ap", "buf")

    def __init__(self, ap, buf=None):
        self.ap = ap
        self.buf = buf if buf is not None else Buf()

    def __getitem__(self, k):
        return V(self.ap[k], self.buf)

    def re(self, pat, **kw):
        return V(self.ap.rearrange(pat, **kw), self.buf)

    def bc(self, shape):
        return V(self.ap.to_broadcast(list(shape)), self.buf)

    def un(self, axis):
        return V(self.ap.unsqueeze(axis), self.buf)

    def bitcast(self, dt):
        return V(self.ap.bitcast(dt), self.buf)

    def rev(self):
        a = list(self.ap.ap)
        s, c = a[-1]
        a[-1] = [-s, c]
        return V(bass.AP(self.ap.tensor, self.ap.offset + s * (c - 1), [list(x) for x in a]), self.buf)


def _merge(dst, src):
    for k, v in src.items():
        if dst.get(k, 0) < v:
            dst[k] = v


class Sched:
    ENG = ("pe", "act", "dve", "pool", "sp")
    NDS = {"sp": 16, "act": 16, "pool": 16}

    def __init__(self):
        self.ops = {e: [] for e in self.ENG}
        self.cnt = {e: 0 for e in self.ENG}
        self.waited = {e: {} for e in self.ENG}
        self.dnext = {e: 0 for e in self.NDS}
        self.dval = {e: [0] * n for e, n in self.NDS.items()}
        self.n_ops = 0
        self.pool_consts = set()
        self.regvals = {}

    def _emit(self, eng, fn, reads, writes, dma=False, disjoint=False, sreads=()):
        need = {}
        own = 0
        for b in reads:
            _merge(need, b.w)
        if eng != "pe":
            own = need.get(("c", eng), 0)
        for b in writes:
            _merge(need, b.r)
            _merge(need, b.wx if disjoint else b.w)
        if dma:
            j = self.dnext[eng]
            self.dnext[eng] = (j + 1) % self.NDS[eng]
            key = ("d", eng, j)
            if self.dval[eng][j] > 0:
                need[key] = max(need.get(key, 0), self.dval[eng][j])
            self.dval[eng][j] += 16
            ev = (key, self.dval[eng][j])
            inc = 16
        else:
            need.pop(("c", eng), None)
            if own > 0:
                need[("c", eng)] = own
            self.cnt[eng] += 1
            key = ("c", eng)
            ev = (key, self.cnt[eng])
            inc = 1
        wl = []
        wd = self.waited[eng]
        for k, v in need.items():
            if wd.get(k, 0) < v:
                wd[k] = v
                wl.append((k, v))
        self.ops[eng].append((wl, fn, key, inc))
        self.n_ops += 1
        for b in reads:
            if b.r.get(ev[0], 0) < ev[1]:
                b.r[ev[0]] = ev[1]
        for b in writes:
            if disjoint:
                b.w[ev[0]] = ev[1]
            else:
                b.w = {ev[0]: ev[1]}
                b.wx = {ev[0]: ev[1]}
                b.r = {}

    def I(self, eng, meth, disjoint=False, **kw):
        reads, writes, sreads, res = [], [], [], {}
        for k, v in kw.items():
            if isinstance(v, V):
                (writes if k in ("out", "accum_out") else reads).append(v.buf)
                if k in ("scalar", "scalar1", "scalar2", "scale", "bias", "initial"):
                    sreads.append(v.buf)
                res[k] = v.ap
            else:
                res[k] = v
        self._emit(eng, lambda e: getattr(e, meth)(**res), reads, writes, disjoint=disjoint, sreads=sreads)

    def dma(self, eng, out, in_, disjoint=False, **kw):
        o, i = out.ap, in_.ap
        if eng == "sp" and type(o.tensor).__name__ != "DRamTensorHandle":
            eng = "act"
        self._emit(eng, lambda e: e.dma_start(out=o, in_=i, **kw), [in_.buf], [out.buf], dma=True, disjoint=disjoint)

    def gather(self, out, src, idx, bound=None):
        o, i, x = out.ap, src.ap, idx.ap
        if bound is None:
            fn = lambda e: e.indirect_dma_start(out=o, out_offset=None, in_=i, in_offset=bass.IndirectOffsetOnAxis(ap=x, axis=0))
        else:
            self.pool_consts.add(bound)
            fn = lambda e: e.indirect_dma_start(out=o, out_offset=None, in_=i, in_offset=bass.IndirectOffsetOnAxis(ap=x, axis=0),
                                                bounds_check=self.regvals[bound], oob_is_err=False)
        self._emit("pool", fn, [src.buf, idx.buf], [out.buf], dma=True)

    def scatter(self, out, src, idx, **kw):
        o, i, x = out.ap, src.ap, idx.ap
        self._emit("pool", lambda e: e.indirect_dma_start(out=o, out_offset=bass.IndirectOffsetOnAxis(ap=x, axis=0),
                                                          in_=i, in_offset=None, **kw),
                   [src.buf, idx.buf], [out.buf], dma=True, disjoint=True)

    def barrier(self):
        allv = {("c", e): self.cnt[e] for e in self.ENG if self.cnt[e] > 0}
        for e, vals in self.dval.items():
            for j, v in enumerate(vals):
                if v > 0:
                    allv[("d", e, j)] = v
        for eng in self.ENG:
            wl = []
            wd = self.waited[eng]
            for k, v in allv.items():
                if k == ("c", eng):
                    continue
                if wd.get(k, 0) < v:
                    wd[k] = v
                    wl.append((k, v))
            if wl:
                self.ops[eng].append((wl, None, None, 0))

    def replay(self, nc, stack):
        sems = {}
        for e in self.ENG:
            sems[("c", e)] = stack.enter_context(nc.semaphore("c_" + e))
        for e, n in self.NDS.items():
            for j in range(n):
                sems[("d", e, j)] = stack.enter_context(nc.semaphore("d_%s_%d" % (e, j)))
        block = stack.enter_context(nc.Block())
        ops = self.ops

        def run(name, e):
            for wl, fn, key, inc in ops[name]:
                for k, v in wl:
                    e.wait_ge(sems[k], v)
                if fn is not None:
                    fn(e).then_inc(sems[key], inc)

        @block.tensor
        def _(e):
            run("pe", e)

        @block.scalar
        def _(e):
            run("act", e)

        @block.vector
        def _(e):
            run("dve", e)

        @block.gpsimd
        def _(e):
            for val in sorted(self.pool_consts):
                r = e.alloc_register("bc%d" % val)
                e.reg_mov(r, val)
                self.regvals[val] = e.snap(r)
            run("pool", e)

        @block.sync
        def _(e):
            run("sp", e)


class Prog:
    def __init__(self, nb, layers, final=True):
        self.nb = nb
        self.layers = list(layers)
        self.final = final
        self.S = Sched()
        self.nc = bass.Bass("TRN2", target_bir_lowering=False)
        self.stack = contextlib.ExitStack()
        self.dbufs = {}
        self.in_names = []

    def dram_in(self, name, shape, dt=F32):
        self.in_names.append(name)
        return self.nc.dram_tensor(name, list(shape), dt, kind="ExternalInput").ap()

    def dram_tmp(self, name, shape, dt=F32):
        return self.nc.dram_tensor(name, list(shape), dt, kind="Internal").ap()

    def dv(self, ap, key):
        b = self.dbufs.get(key)
        if b is None:
            b = self.dbufs[key] = Buf()
        return V(ap, b)

    def arena_reset(self):
        self.S.barrier()
        self.aoff = self.abase

    def alloc(self, shape, dt=F32):
        n = 1
        for s in shape[1:]:
            n *= s
        words = n if dt in (F32, I32) else (n + 1) // 2
        words = (words + 7) // 8 * 8
        assert self.aoff + words <= self.awords, ("SBUF arena overflow", self.aoff, words, self.awords)
        ap = self.big[:, self.aoff:self.aoff + words]
        self.aoff += words
        if dt != F32:
            ap = ap.bitcast(dt)
        ap = ap[0:shape[0], 0:n]
        if len(shape) > 2:
            names = " ".join("d%d" % i for i in range(len(shape) - 1))
            kw = {"d%d" % i: shape[i + 1] for i in range(len(shape) - 1)}
            ap = ap.rearrange("p (%s) -> p %s" % (names, names), **kw)
        return V(ap)

    def perm(self, shape, dt=F32):
        v = self.alloc(shape, dt)
        self.abase = self.aoff
        return v

    def build(self):
        nc, S, nb = self.nc, self.S, self.nb
        st = self.stack
        self.awords = 53000
        self.big = st.enter_context(nc.sbuf_tensor("big", [128, self.awords], F32))
        self.aoff = 0
        self.abase = 0
        self.psum = [V(st.enter_context(nc.psum_tensor("ps%d" % i, [128, 512], F32))[:, :]) for i in range(8)]
        nl = len(self.layers)
        nlat, nctx = nb * SEQ, nb * CTX

        self.x_in = self.dram_in("x", [nlat, D])
        self.c_in = self.dram_in("c", [nb, D])
        self.ctx_in = self.dram_in("ctx", [nctx, D])
        self.cctx_in = self.dram_in("c_ctx", [1, D])
        self.final_g = self.dram_in("final_g", [1, D])
        self.W = {}
        for li in self.layers:
            kind = li % 3
            w = {}
            w["ada_w"] = self.dram_in("ada_w_%d" % li, [D, 6 * D])
            w["ada_b"] = self.dram_in("ada_b_%d" % li, [1, 6 * D])
            w["n1"] = self.dram_in("norm1_g_%d" % li, [1, D])
            w["n2"] = self.dram_in("norm2_g_%d" % li, [1, D])
            w["wr"] = self.dram_in("moe_wr_%d" % li, [D, 36])
            w["br"] = self.dram_in("moe_br_%d" % li, [1, 36])
            w["w13"] = self.dram_in("moe_w13_%d" % li, [NE * 128 * 4, 2048])
            w["w2"] = self.dram_in("moe_w2_%d" % li, [NE * 128 * 2, 2048])
            if kind == 0:
                w["w_in"] = self.dram_in("conf_w_in_%d" % li, [D, 2 * D])
                w["dw"] = self.dram_in("conf_dw_%d" % li, [31, D])
                w["dw_b"] = self.dram_in("conf_dw_b_%d" % li, [1, D])
                w["ln_g"] = self.dram_in("conf_ln_g_%d" % li, [1, D])
                w["ln_b"] = self.dram_in("conf_ln_b_%d" % li, [1, D])
                w["w_out"] = self.dram_in("conf_w_out_%d" % li, [D, D])
            elif kind == 1:
                w["w_in"] = self.dram_in("sc_w_in_%d" % li, [D, 3 * D])
                w["cv"] = self.dram_in("sc_conv_%d" % li, [3, D])
                w["w_out"] = self.dram_in("sc_w_out_%d" % li, [D, D])
            else:
                w["a_re"] = self.dram_in("s5_a_re_%d" % li, [2, 64, 64])
                w["a_im"] = self.dram_in("s5_a_im_%d" % li, [2, 64, 64])
                w["ldt"] = self.dram_in("s5_log_dt_%d" % li, [2, 64])
                w["b_re"] = self.dram_in("s5_b_re_%d" % li, [2, 64, 64, 16])
                w["b_im"] = self.dram_in("s5_b_im_%d" % li, [2, 64, 64, 16])
                w["c_re"] = self.dram_in("s5_c_re_%d" % li, [2, 64, 16, 64])
                w["c_im"] = self.dram_in("s5_c_im_%d" % li, [2, 64, 16, 64])
                w["d"] = self.dram_in("s5_d_%d" % li, [1, D])
                w["w_glu"] = self.dram_in("s5_w_glu_%d" % li, [D, 2 * D])
            self.W[li] = w
        self.y_out = nc.dram_tensor("y", [nlat, D], F32, kind="ExternalOutput").ap()

        self.xs = self.dram_tmp("xs", [nlat, D])
        self.cs = self.dram_tmp("cs", [nctx, D])
        self.MOD = self.dram_tmp("modv", [nl, 6, nb + 1, D])
        ntok_max = nlat + nctx
        self.nslot_max = ((2 * ntok_max + NE * (TS - 1)) + TS - 1) // TS * TS
        self.H2 = self.dram_tmp("h2", [ntok_max, D], BF16)
        self.H2S = self.dram_tmp("h2s", [self.nslot_max, D], BF16)
        self.YS = self.dram_tmp("ys", [self.nslot_max, D])
        self.HT = self.dram_tmp("ht", [D, ntok_max], BF16)
        self.W13B = self.dram_tmp("w13b", [NE * 128 * 4, 2048], BF16)
        self.W2B = self.dram_tmp("w2b", [NE * 128 * 2, 2048], BF16)
        self.YT = self.dram_tmp("yt", [D, ntok_max], BF16)

        self.identf = self.perm([128, 128])
        self.ident = self.perm([128, 128], BF16)
        self.ltri = self.perm([128, 128], BF16)
        self.ones = self.perm([128, 128], BF16)
        self.onesm = self.perm([128, 128], BF16)
        self.eps_rms = self.perm([128, 1])
        self.eps_ln = self.perm([128, 1])
        self.iota_p = self.perm([128, 1])
        self.one_c = self.perm([128, 1])
        tmpf = self.alloc([128, 128])
        S.I("pool", "iota", out=self.identf, pattern=[[1, 128]], base=0, channel_multiplier=-1,
            allow_small_or_imprecise_dtypes=True)
        S.I("dve", "tensor_single_scalar", out=tmpf, in_=self.identf, scalar=0.0, op=ALU.is_gt)
        S.I("dve", "tensor_copy", out=self.ltri, in_=tmpf)
        S.I("dve", "tensor_single_scalar", out=self.identf, in_=self.identf, scalar=0.0, op=ALU.is_equal)
        S.I("dve", "tensor_copy", out=self.ident, in_=self.identf)
        self._memset(self.ones, 1.0)
        self._memset(self.onesm, 1.0 / 1024.0)
        self._memset(self.eps_rms, RMS_EPS)
        self._memset(self.eps_ln, LN_EPS)
        self._memset(self.one_c, 1.0)
        S.I("pool", "iota", out=self.iota_p, pattern=[[0, 1]], base=0, channel_multiplier=1,
            allow_small_or_imprecise_dtypes=True)
        self.aoff = self.abase

        self.arena_reset()
        z = self.alloc([128, 8 * D], BF16)
        self._memset(z, 0.0)
        rows = self.nslot_max
        r0 = 0
        while r0 < rows:
            n = min(1024, rows - r0)
            assert n % 128 == 0
            S.dma("sp", self.dv(self.H2S[r0:r0 + n, :].rearrange("(p a) d -> p (a d)", p=128), "h2s"),
                  z[:, 0:(n // 128) * D], disjoint=True)
            r0 += n

        self.prologue()
        first = True
        for idx, li in enumerate(self.layers):
            kind = li % 3
            upd = li < DEPTH - 1
            need_ctx = upd or kind == 2
            if "moe" not in SKIP:
                self.precast(li)
            if "mixer" in SKIP:
                self.copy_x(first, upd)
            elif kind == 0:
                self.conformer(idx, li, first, upd)
            elif kind == 1:
                self.shortconv(idx, li, first, upd)
            else:
                self.s5(idx, li, first, upd)
            first = False
            if "moe" not in SKIP:
                self.moe(idx, li, upd)
        if self.final and not getattr(self, "final_fused", False):
            self.final_norm()
        S.barrier()
        S.replay(nc, st)
        st.close()
        return nc

    def precast(self, li):
        w = self.W[li]
        for (src, dst, key, nrows) in ((w["w13"], self.W13B, "w13b", NE * 128 * 4), (w["w2"], self.W2B, "w2b", NE * 128 * 2)):
            for r0 in range(0, nrows, 1024):
                sv = src[r0:r0 + 1024, :].rearrange("(p a) n -> p a n", p=128)
                dv_ = dst[r0:r0 + 1024, :].rearrange("(p a) n -> p a n", p=128)
                self.S.dma("pool", self.dv(dv_, key), self.dv(sv, "w_in_%s_%d" % (key, li)), disjoint=True)

    def copy_x(self, first, upd):
        self.arena_reset()
        t = [self.alloc([128, D]) for _ in range(4)]
        i = 0
        for lat in ((True, False) if upd else (True,)):
            src, skey = self.xsrc(first, lat)
            dst, dkey = self.xdst(lat)
            n = self.nb * (SEQ if lat else CTX)
            for r0 in range(0, n, 128):
                self.S.dma("sp", t[i % 4], self.dv(src[r0:r0 + 128, :], (skey, r0)))
                self.S.dma("sp", self.dv(dst[r0:r0 + 128, :], (dkey, r0)), t[i % 4])
                i += 1

    def dbg(self, name, v, dt=F32):
        if not DEBUG:
            return
        shape = list(v.ap.shape)
        o = self.nc.dram_tensor("dbg_" + name, shape, dt, kind="ExternalOutput").ap()
        self.S.dma("sp", self.dv(o, "dbg_" + name), v)

    def _memset(self, v, val):
        ap = v.ap
        self.S._emit("dve", lambda e: e.memset(ap, val), [], [v.buf])

    def xsrc(self, first, lat):
        if lat:
            return (self.x_in if first else self.xs), ("xin" if first else "xs")
        return (self.ctx_in if first else self.cs), ("cin" if first else "cs")

    def xdst(self, lat):
        return (self.xs, "xs") if lat else (self.cs, "cs")

    def load_rows_bc(self, dst, src_row_ap, key, nparts=128, eng="sp"):
        n = src_row_ap.shape[-1]
        self.S.dma(eng, dst, self.dv(src_row_ap.to_broadcast([nparts, n]), key))

    def load_featT(self, dst, row_ap, key):
        self.S.dma("sp", dst, self.dv(row_ap.rearrange("o (k p) -> p (o k)", p=128), key), allow_slow_non_contiguous=True)

    def prologue(self):
        S, nb, nc = self.S, self.nb, self.nc
        self.arena_reset()
        ns = nb + 1
        cT = self.alloc([128, KC, ns])
        for b in range(nb):
            self.load_featT(cT[:, :, b], self.c_in[b:b + 1, :], "c_in")
        self.load_featT(cT[:, :, nb], self.cctx_in, "cctx_in")
        S.I("act", "activation", out=cT, in_=cT, func=AF.Silu)
        mrow = self.alloc([ns, 6 * D])
        abrow = self.alloc([ns, 6 * D])
        n1 = self.alloc([ns, D])
        n2 = self.alloc([ns, D])
        orow = self.alloc([ns, 6, D])
        awt = [self.alloc([128, KC, 512]) for _ in range(2)]
        for idx, li in enumerate(self.layers):
            w = self.W[li]
            self.load_rows_bc(abrow, w["ada_b"], "ada_b%d" % li, nparts=ns)
            self.load_rows_bc(n1, w["n1"], "n1_%d" % li, nparts=ns)
            self.load_rows_bc(n2, w["n2"], "n2_%d" % li, nparts=ns)
            awv = w["ada_w"].rearrange("(k p) n -> p k n", p=128)
            for cg in range(12):
                t = awt[cg % 2]
                S.dma("sp" if cg % 2 == 0 else "act", t, self.dv(awv[:, :, cg * 512:(cg + 1) * 512], "ada_w%d" % li))
                ps = self.psum[cg % 2]
                for k in range(KC):
                    S.I("pe", "matmul", out=ps[0:ns, :], lhsT=cT[:, k, :], rhs=t[:, k, :], start=(k == 0), stop=(k == KC - 1))
                S.I("dve", "tensor_tensor", out=mrow[:, cg * 512:(cg + 1) * 512], in0=ps[0:ns, :],
                    in1=abrow[:, cg * 512:(cg + 1) * 512], op=ALU.add)
            S.I("dve", "scalar_tensor_tensor", out=orow[:, 0, :], in0=mrow[:, D:2 * D], scalar=1.0, in1=n1, op0=ALU.add, op1=ALU.mult)
            S.I("dve", "tensor_copy", out=orow[:, 1, :], in_=mrow[:, 0:D])
            S.I("dve", "tensor_copy", out=orow[:, 2, :], in_=mrow[:, 2 * D:3 * D])
            S.I("dve", "scalar_tensor_tensor", out=orow[:, 3, :], in0=mrow[:, 4 * D:5 * D], scalar=1.0, in1=n2, op0=ALU.add, op1=ALU.mult)
            S.I("dve", "tensor_copy", out=orow[:, 4, :], in_=mrow[:, 3 * D:4 * D])
            S.I("dve", "tensor_copy", out=orow[:, 5, :], in_=mrow[:, 5 * D:6 * D])
            S.dma("sp", self.dv(self.MOD[idx].rearrange("k s d -> s k d"), "mod%d" % idx), orow)

    def mod_row(self, idx, kind, s):
        return self.dv(self.MOD[idx, kind, s:s + 1, :], "mod%d" % idx)

    def load_modT(self, idx):
        ns = self.nb + 1
        sT = self.alloc([128, KC, ns])
        bT = self.alloc([128, KC, ns])
        for s in range(ns):
            self.load_featT(sT[:, :, s], self.MOD[idx, 0, s:s + 1, :], "mod%d" % idx)
            self.load_featT(bT[:, :, s], self.MOD[idx, 1, s:s + 1, :], "mod%d" % idx)
        return sT, bT

    def norm_bufs(self, depth=2):
        nbuf = {"depth": depth}
        nbuf["xt"] = [self.alloc([128, D]) for _ in range(depth)]
        nbuf["sq"] = self.alloc([128, D])
        nbuf["x16"] = [self.alloc([128, D], BF16) for _ in range(2)]
        nbuf["ss"] = [self.alloc([128, 1]) for _ in range(depth)]
        nbuf["rs"] = [self.alloc([128, 1]) for _ in range(depth)]
        nbuf["tmp"] = self.alloc([128, KC, 128])
        nbuf["i"] = 0
        return nbuf

    def rms_tile(self, nbuf, src_v):
        S = self.S
        i = nbuf["i"] % nbuf["depth"]
        nbuf["i"] += 1
        xt, ss, rs = nbuf["xt"][i], nbuf["ss"][i], nbuf["rs"][i]
        S.dma("sp", xt, src_v)
        S.I("act", "activation", out=nbuf["sq"], in_=xt, func=AF.Square)
        S.I("dve", "reduce_sum", out=ss, in_=nbuf["sq"], axis=AX.X)
        S.I("act", "activation", out=rs, in_=ss, func=AF.Sqrt, scale=1.0 / D, bias=self.eps_rms)
        S.I("dve", "reciprocal", out=rs, in_=rs)
        return xt, rs, i

    def norm_transpose(self, nbuf, src_v, sT, bT, s, dst):
        S = self.S
        xt, rs, i = self.rms_tile(nbuf, src_v)
        x16 = nbuf["x16"][i % 2]
        S.I("act", "activation", out=x16, in_=xt, func=AF.Identity, scale=rs)
        pt = self.psum[0].bitcast(BF16)
        for k in range(KC):
            S.I("pe", "transpose", out=pt[:, k * 128:(k + 1) * 128], in_=x16[:, k * 128:(k + 1) * 128], identity=self.ident)
        tmp = nbuf["tmp"]
        S.I("dve", "tensor_tensor", out=tmp, in0=pt.re("p (k t) -> p k t", k=KC), in1=sT[:, :, s:s + 1].bc([128, KC, 128]), op=ALU.mult)
        S.I("dve", "tensor_tensor", out=dst, in0=tmp, in1=bT[:, :, s:s + 1].bc([128, KC, 128]), op=ALU.add)

    def seqs(self, with_ctx):
        out = [("lat", b) for b in range(self.nb)]
        if with_ctx:
            out.append(("ctx", self.nb))
        return out

    def load_w_bf16(self, dst, w_ap, key, ncols):
        wv = w_ap.rearrange("(k p) n -> p k n", p=128)
        for k in range(KC):
            for c0 in range(0, ncols, 2048):
                c1 = min(ncols, c0 + 2048)
                self.S.dma("pool", dst[:, k, c0:c1], self.dv(wv[:, k, c0:c1], key), disjoint=True)

    def residual_out(self, ps_halves, g_bc, src_v, dst_v, obuf, xbuf):
        S = self.S
        S.dma("sp", xbuf, src_v)
        for h in range(2):
            S.I("dve", "tensor_tensor", out=obuf[:, h * 512:(h + 1) * 512], in0=ps_halves[h], in1=g_bc[:, h * 512:(h + 1) * 512], op=ALU.mult)
        S.I("pool", "tensor_tensor", out=obuf, in0=obuf, in1=xbuf, op=ALU.add)
        S.dma("sp", dst_v, obuf)

    def conformer(self, idx, li, first, upd):
        S, nb, w = self.S, self.nb, self.W[li]
        self.arena_reset()
        sT, bT = self.load_modT(idx)
        w_in = self.alloc([128, KC, 2 * D], BF16)
        w_out = self.alloc([128, KC, D], BF16)
        self.load_w_bf16(w_in, w["w_in"], "cw_in%d" % li, 2 * D)
        self.load_w_bf16(w_out, w["w_out"], "cw_out%d" % li, D)
        dwT = self.alloc([128, KC, 31])
        for k in range(KC):
            S.dma("sp", dwT[:, k, :], self.dv(w["dw"][:, k * 128:(k + 1) * 128].rearrange("t p -> p t"), "dw%d" % li), allow_slow_non_contiguous=True)
        dwb = self.alloc([128, KC])
        lng = self.alloc([128, KC])
        lnb = self.alloc([128, KC])
        self.load_featT(dwb, w["dw_b"], "dwb%d" % li)
        self.load_featT(lng, w["ln_g"], "lng%d" % li)
        self.load_featT(lnb, w["ln_b"], "lnb%d" % li)
        nbuf = self.norm_bufs()
        g1 = [self.alloc([128, D]) for _ in range(2)]
        hT = [self.alloc([128, KC, 512], BF16) for _ in range(2)]
        zbuf = self.alloc([128, KC, SEQ], BF16)
        sg = [self.alloc([128, 512]) for _ in range(2)]
        dwd = [self.alloc([128, 31, 128], BF16) for _ in range(2)]
        vall = self.alloc([128, KC, SEQ], BF16)
        vsq = [self.alloc([128, 512], BF16) for _ in range(2)]
        s16 = self.alloc([128, KC, 512], BF16)
        msq = self.alloc([128, 512])
        mean = self.alloc([128, 512])
        rstd = self.alloc([128, 512])
        nmr = self.alloc([128, 512])
        t1 = [self.alloc([128, 512]) for _ in range(2)]
        obuf = [self.alloc([128, D])] * 2
        P = self.psum
        gi = 0
        di = 0
        ci2 = 0
        for si, (sk, s) in enumerate(self.seqs(upd)):
            lat = sk == "lat"
            ntok = SEQ if lat else nb * CTX
            src, skey = self.xsrc(first, lat)
            dst, dkey = self.xdst(lat)
            base = s * SEQ if lat else 0
            GS = min(512, ntok)
            ng = ntok // GS
            gb = g1[si % 2]
            self.load_rows_bc(gb, self.MOD[idx, 2, s:s + 1, :], "mod%d" % idx)
            for g in range(ng):
                h = hT[gi % 2]
                gi += 1
                for j in range(GS // 128):
                    r0 = base + g * GS + j * 128
                    self.norm_transpose(nbuf, self.dv(src[r0:r0 + 128, :], (skey, r0)), sT, bT, s, h[:, :, j * 128:(j + 1) * 128])
                for m in range(KC):
                    pv, pg = P[1 + m % 2], P[3 + m % 2]
                    for k in range(KC):
                        S.I("pe", "matmul", out=pv[:, 0:GS], lhsT=w_in[:, k, m * 128:(m + 1) * 128], rhs=h[:, k, 0:GS], start=(k == 0), stop=(k == KC - 1))
                    for k in range(KC):
                        S.I("pe", "matmul", out=pg[:, 0:GS], lhsT=w_in[:, k, D + m * 128:D + (m + 1) * 128], rhs=h[:, k, 0:GS], start=(k == 0), stop=(k == KC - 1))
                    sgt = sg[m % 2]
                    S.I("act", "activation", out=sgt[:, 0:GS], in_=pg[:, 0:GS], func=AF.Sigmoid)
                    S.I("dve", "tensor_tensor", out=zbuf[:, m, g * GS:(g + 1) * GS], in0=pv[:, 0:GS], in1=sgt[:, 0:GS], op=ALU.mult)
            order = [15] + [k for k in range(31) if k != 15]
            for m in range(KC):
                dd = dwd[di % 2]
                di += 1
                for k in range(31):
                    S.I("dve", "tensor_single_scalar", out=dd[:, k, :], in_=self.ident, scalar=dwT[:, m, k:k + 1], op=ALU.mult)
                for g in range(ng):
                    g0 = g * GS
                    pc = P[1 + ci2 % 2]
                    ci2 += 1
                    for n_, k in enumerate(order):
                        d = k - 15
                        if lat:
                            dt_ = d * 64
                            lo, hi = max(g0, -dt_), min(g0 + GS, SEQ - dt_)
                            if lo >= hi:
                                continue
                            S.I("pe", "matmul", out=pc[:, lo - g0:hi - g0], lhsT=dd[:, k, :], rhs=zbuf[:, m, lo + dt_:hi + dt_],
                                start=(n_ == 0), stop=(n_ == 30), skip_group_check=True)
                        else:
                            lo, hi = max(0, -d), min(CTX, CTX - d)
                            o = pc[:, 0:GS].re("p (s t) -> p s t", t=CTX)[:, :, lo:hi]
                            r = zbuf[:, m, g0:g0 + GS].re("p (s t) -> p s t", t=CTX)[:, :, lo + d:hi + d]
                            S.I("pe", "matmul", out=o, lhsT=dd[:, k, :], rhs=r, start=(n_ == 0), stop=(n_ == 30), skip_group_check=True)
                    S.I("act", "activation", out=vall[:, m, g0:g0 + GS], in_=pc[:, 0:GS], func=AF.Identity, bias=dwb[:, m:m + 1])
            for g in range(ng):
                g0 = g * GS
                v16 = vall[:, :, g0:g0 + GS]
                for m in range(KC):
                    vq = vsq[m % 2]
                    S.I("act", "activation", out=vq[:, 0:GS], in_=v16[:, m, :], func=AF.Square)
                    S.I("pe", "matmul", out=P[5][:, 0:GS], lhsT=self.onesm, rhs=v16[:, m, :], start=(m == 0), stop=(m == KC - 1))
                    S.I("pe", "matmul", out=P[6][:, 0:GS], lhsT=self.onesm, rhs=vq[:, 0:GS], start=(m == 0), stop=(m == KC - 1))
                S.I("act", "activation", out=mean[:, 0:GS], in_=P[5][:, 0:GS], func=AF.Identity)
                S.I("act", "activation", out=msq[:, 0:GS], in_=P[5][:, 0:GS], func=AF.Square)
                S.I("dve", "tensor_tensor", out=rstd[:, 0:GS], in0=P[6][:, 0:GS], in1=msq[:, 0:GS], op=ALU.subtract)
                S.I("act", "activation", out=rstd[:, 0:GS], in_=rstd[:, 0:GS], func=AF.Sqrt, bias=self.eps_ln)
                S.I("dve", "reciprocal", out=rstd[:, 0:GS], in_=rstd[:, 0:GS])
                S.I("dve", "scalar_tensor_tensor", out=nmr[:, 0:GS], in0=mean[:, 0:GS], scalar=-1.0, in1=rstd[:, 0:GS], op0=ALU.mult, op1=ALU.mult)
                for m in range(KC):
                    tt = t1[m % 2]
                    S.I("dve", "tensor_tensor", out=tt[:, 0:GS], in0=v16[:, m, :], in1=rstd[:, 0:GS], op=ALU.mult)
                    S.I("pool", "tensor_tensor", out=tt[:, 0:GS], in0=tt[:, 0:GS], in1=nmr[:, 0:GS], op=ALU.add)
                    S.I("act", "activation", out=s16[:, m, 0:GS], in_=tt[:, 0:GS], func=AF.Silu, scale=lng[:, m:m + 1], bias=lnb[:, m:m + 1])
                for j in range(GS // 128):
                    r0 = base + g0 + j * 128
                    for h_ in range(2):
                        for k in range(KC):
                            S.I("pe", "matmul", out=P[3 + h_], lhsT=s16[:, k, j * 128:(j + 1) * 128], rhs=w_out[:, k, h_ * 512:(h_ + 1) * 512],
                                start=(k == 0), stop=(k == KC - 1))
                    ob = obuf[j % 2]
                    self.residual_out([P[3], P[4]], gb, self.dv(src[r0:r0 + 128, :], (skey, r0)), self.dv(dst[r0:r0 + 128, :], (dkey, r0)),
                                      ob, nbuf["xt"][j % 2])

    def shortconv(self, idx, li, first, upd):
        S, nb, w = self.S, self.nb, self.W[li]
        self.arena_reset()
        sT, bT = self.load_modT(idx)
        w_in = self.alloc([128, KC, 3 * D], BF16)
        w_out = self.alloc([128, KC, D], BF16)
        self.load_w_bf16(w_in, w["w_in"], "sw_in%d" % li, 3 * D)
        self.load_w_bf16(w_out, w["w_out"], "sw_out%d" % li, D)
        cvT = self.alloc([128, KC, 3])
        for k in range(KC):
            S.dma("sp", cvT[:, k, :], self.dv(w["cv"][:, k * 128:(k + 1) * 128].rearrange("t p -> p t"), "cv%d" % li), allow_slow_non_contiguous=True)
        nbuf = self.norm_bufs()
        g1 = [self.alloc([128, D]) for _ in range(2)]
        hT = [self.alloc([128, KC, 512], BF16) for _ in range(2)]
        gcs = [self.alloc([128, 512]) for _ in range(2)]
        q = [self.alloc([128, 512]) for _ in range(2)]
        cc = [self.alloc([128, 512]) for _ in range(2)]
        p16 = self.alloc([128, KC, 512], BF16)
        obuf = [self.alloc([128, D]) for _ in range(2)]
        P = self.psum
        gi = 0
        for si, (sk, s) in enumerate(self.seqs(upd)):
            lat = sk == "lat"
            ntok = SEQ if lat else nb * CTX
            src, skey = self.xsrc(first, lat)
            dst, dkey = self.xdst(lat)
            base = s * SEQ if lat else 0
            GS = min(512, ntok)
            ng = ntok // GS
            RL = 64 if lat else CTX
            gb = g1[si % 2]
            self.load_rows_bc(gb, self.MOD[idx, 2, s:s + 1, :], "mod%d" % idx)
            for g in range(ng):
                g0 = g * GS
                h = hT[gi % 2]
                gi += 1
                for j in range(GS // 128):
                    r0 = base + g0 + j * 128
                    self.norm_transpose(nbuf, self.dv(src[r0:r0 + 128, :], (skey, r0)), sT, bT, s, h[:, :, j * 128:(j + 1) * 128])
                for m in range(KC):
                    pb, pc_, pv = P[1], P[2 + m % 2], P[4 + m % 2]
                    for (pp, off) in ((pc_, D), (pv, 2 * D)):
                        for k in range(KC):
                            S.I("pe", "matmul", out=pp[:, 0:GS], lhsT=w_in[:, k, off + m * 128:off + (m + 1) * 128], rhs=h[:, k, 0:GS],
                                start=(k == 0), stop=(k == KC - 1))
                    gct, qt, ct = gcs[m % 2], q[m % 2], cc[m % 2]
                    S.I("act", "activation", out=gct[:, 0:GS], in_=pc_[:, 0:GS], func=AF.Identity)
                    S.I("dve", "tensor_tensor", out=qt[:, 0:GS], in0=pv[:, 0:GS], in1=gct[:, 0:GS], op=ALU.mult)
                    S.I("act", "activation", out=ct[:, 0:GS], in_=qt[:, 0:GS], func=AF.Identity, scale=cvT[:, m, 1:2])
                    q3 = qt[:, 0:GS].re("p (r c) -> p r c", c=RL)
                    c3 = ct[:, 0:GS].re("p (r c) -> p r c", c=RL)
                    S.I("dve", "scalar_tensor_tensor", out=c3[:, :, 1:RL], in0=q3[:, :, 0:RL - 1], scalar=cvT[:, m, 0:1], in1=c3[:, :, 1:RL],
                        op0=ALU.mult, op1=ALU.add)
                    S.I("dve", "scalar_tensor_tensor", out=c3[:, :, 0:RL - 1], in0=q3[:, :, 1:RL], scalar=cvT[:, m, 2:3], in1=c3[:, :, 0:RL - 1],
                        op0=ALU.mult, op1=ALU.add)
                    for k in range(KC):
                        S.I("pe", "matmul", out=pb[:, 0:GS], lhsT=w_in[:, k, m * 128:(m + 1) * 128], rhs=h[:, k, 0:GS], start=(k == 0), stop=(k == KC - 1))
                    S.I("dve", "tensor_tensor", out=p16[:, m, 0:GS], in0=pb[:, 0:GS], in1=ct[:, 0:GS], op=ALU.mult)
                for j in range(GS // 128):
                    r0 = base + g0 + j * 128
                    for h_ in range(2):
                        for k in range(KC):
                            S.I("pe", "matmul", out=P[6 + h_], lhsT=p16[:, k, j * 128:(j + 1) * 128], rhs=w_out[:, k, h_ * 512:(h_ + 1) * 512],
                                start=(k == 0), stop=(k == KC - 1))
                    self.residual_out([P[6], P[7]], gb, self.dv(src[r0:r0 + 128, :], (skey, r0)), self.dv(dst[r0:r0 + 128, :], (dkey, r0)),
                                      obuf[j % 2], nbuf["xt"][j % 2])

    def s5(self, idx, li, first, upd):
        import math
        S, nb, w, P = self.S, self.nb, self.W[li], self.psum
        NTK = CTX + SEQ
        HTv = self.HT.rearrange("(k p) t -> p k t", p=128)
        YTv = self.YT.rearrange("(k p) t -> p k t", p=128)
        self.arena_reset()
        sT, bT = self.load_modT(idx)
        nbuf = self.norm_bufs()
        ht = [self.alloc([128, KC, 128], BF16) for _ in range(3)]
        i = 0
        for b in range(nb):
            for lat, n_t in ((False, CTX // 128), (True, SEQ // 128)):
                src, skey = self.xsrc(first, lat)
                for j in range(n_t):
                    r0 = (b * SEQ if lat else b * CTX) + j * 128
                    col = b * NTK + (CTX if lat else 0) + j * 128
                    t = ht[i % 3]
                    i += 1
                    self.norm_transpose(nbuf, self.dv(src[r0:r0 + 128, :], (skey, r0)), sT, bT, (b if lat else nb), t)
                    S.dma("sp", self.dv(HTv[:, :, col:col + 128], "ht"), t, disjoint=True)
        self.arena_reset()
        ND = 64
        f2 = lambda v: v.re("p d g -> p (d g)")
        are, aim, ldt = (self.alloc([128, 2, 32]) for _ in range(3))
        for d in range(2):
            S.dma("sp", are[:, d, :], self.dv(w["a_re"][d].rearrange("(G g) p -> (g p) G", g=2), "s5a"), allow_slow_non_contiguous=True)
            S.dma("sp", aim[:, d, :], self.dv(w["a_im"][d].rearrange("(G g) p -> (g p) G", g=2), "s5a"), allow_slow_non_contiguous=True)
            for g2 in range(2):
                srcv = w["ldt"][d:d + 1, :].rearrange("o (G g) -> o g G", g=2)[:, g2, :]
                S.dma("sp", ldt[g2 * 64:(g2 + 1) * 64, d, :], self.dv(srcv.to_broadcast([64, 32]), "s5a"), allow_slow_non_contiguous=True)
        names = ("dt", "mag", "ang", "c", "s", "ta", "tb", "den", "nre", "kre", "kim", "abr", "abi")
        T_ = {n: self.alloc([128, ND]) for n in names}
        hpi = self.alloc([128, 1])
        self._memset(hpi, math.pi / 2)
        A, B_ = f2(are), f2(aim)
        S.I("dve", "tensor_single_scalar", out=A, in_=A, scalar=-1e-4, op=ALU.min)
        S.I("act", "activation", out=T_["dt"], in_=f2(ldt), func=AF.Exp)
        S.I("dve", "tensor_tensor", out=T_["ta"], in0=T_["dt"], in1=A, op=ALU.mult)
        S.I("act", "activation", out=T_["mag"], in_=T_["ta"], func=AF.Exp)
        S.I("dve", "tensor_tensor", out=T_["ang"], in0=T_["dt"], in1=B_, op=ALU.mult)
        S.I("act", "activation", out=T_["s"], in_=T_["ang"], func=AF.Sin, scale=1.0 / 16)
        S.I("act", "activation", out=T_["c"], in_=T_["ang"], func=AF.Sin, scale=1.0 / 16, bias=hpi)

        def csquare(c, s, ta, tb):
            S.I("dve", "tensor_tensor", out=ta, in0=c, in1=c, op=ALU.mult)
            S.I("dve", "tensor_tensor", out=tb, in0=s, in1=s, op=ALU.mult)
            S.I("dve", "scalar_tensor_tensor", out=s, in0=c, scalar=2.0, in1=s, op0=ALU.mult, op1=ALU.mult)
            S.I("dve", "tensor_tensor", out=c, in0=ta, in1=tb, op=ALU.subtract)
        for _ in range(4):
            csquare(T_["c"], T_["s"], T_["ta"], T_["tb"])
        NP2 = 12
        Er = self.alloc([128, NP2, ND])
        Ei = self.alloc([128, NP2, ND])
        S.I("dve", "tensor_copy", out=Er[:, 0, :], in_=T_["c"])
        S.I("dve", "tensor_copy", out=Ei[:, 0, :], in_=T_["s"])
        for j in range(1, NP2):
            S.I("dve", "tensor_copy", out=Er[:, j, :], in_=Er[:, j - 1, :])
            S.I("dve", "tensor_copy", out=Ei[:, j, :], in_=Ei[:, j - 1, :])
            csquare(Er[:, j, :], Ei[:, j, :], T_["ta"], T_["tb"])
        S.I("dve", "tensor_tensor", out=T_["abr"], in0=T_["mag"], in1=T_["c"], op=ALU.mult)
        S.I("dve", "tensor_tensor", out=T_["abi"], in0=T_["mag"], in1=T_["s"], op=ALU.mult)
        S.I("dve", "tensor_tensor", out=T_["ta"], in0=A, in1=A, op=ALU.mult)
        S.I("dve", "tensor_tensor", out=T_["tb"], in0=B_, in1=B_, op=ALU.mult)
        S.I("dve", "tensor_tensor", out=T_["den"], in0=T_["ta"], in1=T_["tb"], op=ALU.add)
        S.I("dve", "reciprocal", out=T_["den"], in_=T_["den"])
        S.I("dve", "tensor_single_scalar", out=T_["nre"], in_=T_["abr"], scalar=-1.0, op=ALU.add)
        S.I("dve", "tensor_tensor", out=T_["ta"], in0=T_["nre"], in1=A, op=ALU.mult)
        S.I("dve", "tensor_tensor", out=T_["tb"], in0=T_["abi"], in1=B_, op=ALU.mult)
        S.I("dve", "tensor_tensor", out=T_["kre"], in0=T_["ta"], in1=T_["tb"], op=ALU.add)
        S.I("dve", "tensor_tensor", out=T_["kre"], in0=T_["kre"], in1=T_["den"], op=ALU.mult)
        S.I("dve", "tensor_tensor", out=T_["ta"], in0=T_["abi"], in1=A, op=ALU.mult)
        S.I("dve", "tensor_tensor", out=T_["tb"], in0=T_["nre"], in1=B_, op=ALU.mult)
        S.I("dve", "tensor_tensor", out=T_["kim"], in0=T_["ta"], in1=T_["tb"], op=ALU.subtract)
        S.I("dve", "tensor_tensor", out=T_["kim"], in0=T_["kim"], in1=T_["den"], op=ALU.mult)
        mag = T_["mag"]
        lB = self.alloc([32, ND, 2, 128], BF16)
        lC = self.alloc([128, ND, 2, 32], BF16)
        dvec = self.alloc([128, KC])
        self.load_featT(dvec, w["d"], "s5d")
        keep = self.aoff
        bre = self.alloc([128, 2, 32, 16])
        bim = self.alloc([128, 2, 32, 16])
        for d in range(2):
            S.dma("sp", bre[:, d], self.dv(w["b_re"][d].rearrange("(G g) p c -> (g p) G c", g=2), "s5b"))
            S.dma("sp", bim[:, d], self.dv(w["b_im"][d].rearrange("(G g) p c -> (g p) G c", g=2), "s5b"))
        bbr = self.alloc([128, ND, 16])
        bbi = self.alloc([128, ND, 16])
        tq = self.alloc([128, ND, 16])
        brf, bif = bre.re("p d g c -> p (d g) c"), bim.re("p d g c -> p (d g) c")
        kr3 = T_["kre"].un(2).bc([128, ND, 16])
        ki3 = T_["kim"].un(2).bc([128, ND, 16])
        S.I("dve", "tensor_tensor", out=bbr, in0=brf, in1=kr3, op=ALU.mult)
        S.I("dve", "tensor_tensor", out=tq, in0=bif, in1=ki3, op=ALU.mult)
        S.I("dve", "tensor_tensor", out=bbr, in0=bbr, in1=tq, op=ALU.subtract)
        S.I("dve", "tensor_tensor", out=bbi, in0=bif, in1=kr3, op=ALU.mult)
        S.I("dve", "tensor_tensor", out=tq, in0=brf, in1=ki3, op=ALU.mult)
        S.I("dve", "tensor_tensor", out=bbi, in0=bbi, in1=tq, op=ALU.add)
        bblk = [self.alloc([128, 2, 32], BF16) for _ in range(2)]
        for t in bblk:
            self._memset(t, 0.0)
        for dg in range(ND):
            t = bblk[dg % 2]
            for c_, bb in ((0, bbr), (1, bbi)):
                S.I("dve", "tensor_copy", out=t[0:64, c_, 0:16], in_=bb[0:64, dg, :])
                S.I("dve", "tensor_copy", out=t[64:128, c_, 16:32], in_=bb[64:128, dg, :])
            pt = P[dg % 2].bitcast(BF16)
            for c_ in range(2):
                S.I("pe", "transpose", out=pt[0:32, c_ * 128:(c_ + 1) * 128], in_=t[:, c_, :], identity=self.ident)
            S.I("act", "activation", out=lB[:, dg, :, :], in_=pt[0:32, 0:256].re("p (c q) -> p c q", c=2), func=AF.Identity)
        cnat = [self.alloc([32, 32, 128]) for _ in range(2)]
        ci = 0
        for d in range(2):
            for c_, nm in ((0, "c_re"), (1, "c_im")):
                t = cnat[ci % 2]
                ci += 1
                self._memset(t, 0.0)
                for g2 in range(2):
                    srcv = w[nm][d].rearrange("(G g) c p -> g c G p", g=2)[g2]
                    S.dma("sp", t[16 * g2:16 * g2 + 16, :, 64 * g2:64 * g2 + 64], self.dv(srcv, "s5c"), disjoint=True)
                for G in range(32):
                    pp = P[2 + G % 2]
                    S.I("pe", "transpose", out=pp[:, 0:32], in_=t[:, G, :], identity=self.identf[0:32, 0:32])
                    S.I("act", "activation", out=lC[:, d * 32 + G, c_, :], in_=pp[:, 0:32], func=AF.Identity, scale=(1.0 if c_ == 0 else -1.0))
        S.barrier()
        self.aoff = keep
        cosT = self.alloc([128, NTK])
        sinT = self.alloc([128, NTK])
        ttmp = [self.alloc([128, 1024]) for _ in range(2)]
        lCp = [self.alloc([128, 2, 128], BF16) for _ in range(2)]
        for t in lCp:
            self._memset(t, 0.0)
        u = [self.alloc([32, NTK], BF16) for _ in range(2)]
        bus = [[self.alloc([128, 512]) for _ in range(2)] for _ in range(2)]
        tm = [[self.alloc([128, 512]) for _ in range(4)] for _ in range(2)]
        Wr = [self.alloc([128, 512]) for _ in range(2)]
        Wi = [self.alloc([128, 512]) for _ in range(2)]
        Gr = [self.alloc([128, 512]) for _ in range(2)]
        Gi = [self.alloc([128, 512]) for _ in range(2)]
        to = tm
        Hr = [self.alloc([128, 512], BF16) for _ in range(2)]
        Hi = [self.alloc([128, 512], BF16) for _ in range(2)]
        yacc = [self.alloc([128, NTK]) for _ in range(nb)]
        inir = [self.alloc([128, 1]) for _ in range(2)]
        inii = [self.alloc([128, 1]) for _ in range(2)]
        hch = [self.alloc([128, NTK], BF16) for _ in range(2)]
        gt = [tm[0][0:3], tm[1][0:3]]
        y16 = [self.alloc([128, 512], BF16) for _ in range(2)]
        fw = [(0, CTX)] + [(CTX + 512 * i_, 512) for i_ in range(SEQ // 512)]
        rv = [(0, CTX)] + [(CTX + 512 * i_, 512) for i_ in reversed(range(SEQ // 512))]
        pcount = 0
        ui = 0
        for m in range(KC):
            for jj in range(4):
                G = 4 * m + jj
                for d in range(2):
                    dg = d * 32 + G
                    first_acc = (jj == 0 and d == 0)
                    self._memset(cosT[:, 0:1], 1.0)
                    self._memset(sinT[:, 0:1], 0.0)
                    n_have = 1
                    j = 0
                    while n_have < NTK:
                        n_new = min(n_have, NTK - n_have)
                        er, ei = Er[:, j, dg:dg + 1], Ei[:, j, dg:dg + 1]
                        ta, tb = ttmp[0][:, 0:n_new], ttmp[1][:, 0:n_new]
                        S.I("dve", "tensor_single_scalar", out=ta, in_=sinT[:, 0:n_new], scalar=ei, op=ALU.mult)
                        S.I("dve", "tensor_single_scalar", out=tb, in_=sinT[:, 0:n_new], scalar=er, op=ALU.mult)
                        S.I("dve", "scalar_tensor_tensor", out=sinT[:, n_have:n_have + n_new], in0=cosT[:, 0:n_new], scalar=ei, in1=tb, op0=ALU.mult, op1=ALU.add)
                        S.I("dve", "scalar_tensor_tensor", out=cosT[:, n_have:n_have + n_new], in0=cosT[:, 0:n_new], scalar=er, in1=ta, op0=ALU.mult, op1=ALU.subtract)
                        n_have += n_new
                        j += 1
                    lc = lCp[dg % 2]
                    S.I("dve", "tensor_copy", out=lc[:, :, 32 * jj:32 * jj + 32], in_=lC[:, dg, :, :])
                    if jj > 0:
                        pass
                    mg = mag[:, dg:dg + 1]
                    for b0 in range(0, nb, 2):
                        chains = list(range(b0, min(nb, b0 + 2)))
                        uts = {}
                        for c in chains:
                            uts[c] = u[c % 2]
                            S.dma("sp", uts[c], self.dv(self.HT[32 * G:32 * G + 32, c * NTK:(c + 1) * NTK], "ht"))
                        n0 = 0
                        first_piece = True
                        for (c0, L) in (fw if d == 0 else rv):
                            cs_, sn_ = cosT[:, n0:n0 + L], sinT[:, n0:n0 + L]
                            if d == 1:
                                cs_, sn_ = cs_.rev(), sn_.rev()
                            mb = mg.bc([128, L])
                            st = {}
                            for c in chains:
                                pi_ = c % 2
                                st[c] = dict(pr=P[0 + pi_], pim=P[2 + pi_], py=P[4 + pi_], br=bus[pi_][0][:, 0:L], bi=bus[pi_][1][:, 0:L],
                                             t=[x_[:, 0:L] for x_ in tm[pi_]], wr=Wr[pi_][:, 0:L], wi=Wi[pi_][:, 0:L],
                                             gr=Gr[pi_][:, 0:L], gi=Gi[pi_][:, 0:L], hr=Hr[pi_][:, 0:L], hi=Hi[pi_][:, 0:L],
                                             ir=inir[pi_], ii=inii[pi_], ut=uts[c], ya=yacc[c][:, c0:c0 + L])
                            fp_ = first_piece

                            def scan_op(q, which):
                                g_, w_, i_ = (q["gr"], q["wr"], q["ir"]) if which == 0 else (q["gi"], q["wi"], q["ii"])
                                ini = 0.0 if fp_ else i_
                                if d == 0:
                                    S.I("dve", "tensor_tensor_scan", out=g_, data0=mb, data1=w_, initial=ini, op0=ALU.mult, op1=ALU.add)
                                else:
                                    S.I("dve", "tensor_tensor_scan", out=g_.rev(), data0=mb, data1=w_.rev(), initial=ini, op0=ALU.mult, op1=ALU.add)

                            def save_init(q, which):
                                g_, i_ = (q["gr"], q["ir"]) if which == 0 else (q["gi"], q["ii"])
                                lastc = g_[:, L - 1:L] if d == 0 else g_[:, 0:1]
                                S.I("act", "activation", out=i_, in_=lastc, func=AF.Identity)

                            def acc_op(q):
                                if first_acc:
                                    S.I("act", "activation", out=q["ya"], in_=q["py"][:, 0:L], func=AF.Identity)
                                else:
                                    S.I("dve", "tensor_tensor", out=q["ya"], in0=q["py"][:, 0:L], in1=q["ya"], op=ALU.add)
                            steps = [
                                lambda q: S.I("pe", "matmul", out=q["pr"][:, 0:L], lhsT=lB[:, dg, 0, :], rhs=q["ut"][:, c0:c0 + L], start=True, stop=True),
                                lambda q: S.I("pe", "matmul", out=q["pim"][:, 0:L], lhsT=lB[:, dg, 1, :], rhs=q["ut"][:, c0:c0 + L], start=True, stop=True),
                                lambda q: S.I("act", "activation", out=q["br"], in_=q["pr"][:, 0:L], func=AF.Identity),
                                lambda q: S.I("act", "activation", out=q["bi"], in_=q["pim"][:, 0:L], func=AF.Identity),
                                lambda q: S.I("dve", "tensor_tensor", out=q["t"][0], in0=q["br"], in1=cs_, op=ALU.mult),
                                lambda q: S.I("pool", "tensor_tensor", out=q["t"][2], in0=q["bi"], in1=cs_, op=ALU.mult),
                                lambda q: S.I("dve", "tensor_tensor", out=q["t"][1], in0=q["bi"], in1=sn_, op=ALU.mult),
                                lambda q: S.I("pool", "tensor_tensor", out=q["t"][3], in0=q["br"], in1=sn_, op=ALU.mult),
                                lambda q: S.I("dve", "tensor_tensor", out=q["wr"], in0=q["t"][0], in1=q["t"][1], op=ALU.add),
                                lambda q: S.I("pool", "tensor_tensor", out=q["wi"], in0=q["t"][2], in1=q["t"][3], op=ALU.subtract),
                                lambda q: scan_op(q, 0),
                                lambda q: scan_op(q, 1),
                                lambda q: save_init(q, 0),
                                lambda q: save_init(q, 1),
                                lambda q: S.I("dve", "tensor_tensor", out=q["t"][0], in0=q["gr"], in1=cs_, op=ALU.mult),
                                lambda q: S.I("pool", "tensor_tensor", out=q["t"][2], in0=q["gr"], in1=sn_, op=ALU.mult),
                                lambda q: S.I("dve", "tensor_tensor", out=q["t"][1], in0=q["gi"], in1=sn_, op=ALU.mult),
                                lambda q: S.I("pool", "tensor_tensor", out=q["t"][3], in0=q["gi"], in1=cs_, op=ALU.mult),
                                lambda q: S.I("dve", "tensor_tensor", out=q["hr"], in0=q["t"][0], in1=q["t"][1], op=ALU.subtract),
                                lambda q: S.I("pool", "tensor_tensor", out=q["hi"], in0=q["t"][2], in1=q["t"][3], op=ALU.add),
                                lambda q: S.I("pe", "matmul", out=q["py"][:, 0:L], lhsT=lc[:, 0, :], rhs=q["hr"], start=True, stop=False),
                                lambda q: S.I("pe", "matmul", out=q["py"][:, 0:L], lhsT=lc[:, 1, :], rhs=q["hi"], start=False, stop=True),
                                lambda q: acc_op(q),
                            ]
                            for stp in steps:
                                for c in chains:
                                    stp(st[c])
                            n0 += L
                            first_piece = False
                    self.S._emit("dve", (lambda apx: (lambda e: e.memset(apx, 0.0)))(lc[:, :, 32 * jj:32 * jj + 32].ap), [], [lc.buf])
            for b in range(nb):
                hc = hch[b % 2]
                S.dma("sp", hc, self.dv(self.HT[128 * m:128 * m + 128, b * NTK:(b + 1) * NTK], "ht"))
                for pi2, (c0, L) in enumerate(fw):
                    tt, sq, sg_ = (x_[:, 0:L] for x_ in gt[pi2 % 2])
                    yo_ = y16[pi2 % 2][:, 0:L]
                    S.I("dve", "scalar_tensor_tensor", out=tt, in0=hc[:, c0:c0 + L], scalar=dvec[:, m:m + 1], in1=yacc[b][:, c0:c0 + L], op0=ALU.mult, op1=ALU.add)
                    S.I("act", "activation", out=sq, in_=tt, func=AF.Square)
                    S.I("act", "activation", out=sq, in_=sq, func=AF.Identity, scale=0.044715, bias=self.one_c)
                    S.I("pool", "tensor_tensor", out=sq, in0=sq, in1=tt, op=ALU.mult)
                    S.I("act", "activation", out=sg_, in_=sq, func=AF.Sigmoid, scale=1.5957691216057308)
                    S.I("pool", "tensor_tensor", out=yo_, in0=tt, in1=sg_, op=ALU.mult)
                    col = b * NTK + c0
                    S.dma("sp", self.dv(YTv[:, m, col:col + L], "yt"), yo_, disjoint=True)
        self.arena_reset()
        wg = self.alloc([128, KC, 2 * D], BF16)
        self.load_w_bf16(wg, w["w_glu"], "s5wg%d" % li, 2 * D)
        g1 = [self.alloc([128, D]) for _ in range(2)]
        yt = [self.alloc([128, KC, 128], BF16) for _ in range(2)]
        sgb = [self.alloc([128, D]) for _ in range(2)]
        obuf = [self.alloc([128, D]) for _ in range(2)]
        xb = [self.alloc([128, D]) for _ in range(2)]
        ti = 0
        for b in range(nb):
            self.load_rows_bc(g1[0], self.MOD[idx, 2, b:b + 1, :], "mod%d" % idx)
            if upd:
                self.load_rows_bc(g1[1], self.MOD[idx, 2, nb:nb + 1, :], "mod%d" % idx)
            for lat, n_t in (((False, CTX // 128),) if upd else ()) + ((True, SEQ // 128),):
                src, skey = self.xsrc(first, lat)
                dst, dkey = self.xdst(lat)
                gb = g1[0] if lat else g1[1]
                for j in range(n_t):
                    r0 = (b * SEQ if lat else b * CTX) + j * 128
                    col = b * NTK + (CTX if lat else 0) + j * 128
                    y_ = yt[ti % 2]
                    S.dma("sp", y_, self.dv(YTv[:, :, col:col + 128], "yt"))
                    for cb in range(4):
                        pp = P[cb]
                        for k in range(KC):
                            S.I("pe", "matmul", out=pp, lhsT=y_[:, k, :], rhs=wg[:, k, cb * 512:(cb + 1) * 512], start=(k == 0), stop=(k == KC - 1))
                    sg_, ob, xx = sgb[ti % 2], obuf[ti % 2], xb[ti % 2]
                    ti += 1
                    S.dma("sp", xx, self.dv(src[r0:r0 + 128, :], (skey, r0)))
                    for h_ in range(2):
                        S.I("act", "activation", out=sg_[:, h_ * 512:(h_ + 1) * 512], in_=P[2 + h_], func=AF.Sigmoid)
                        S.I("dve", "tensor_tensor", out=ob[:, h_ * 512:(h_ + 1) * 512], in0=P[h_], in1=sg_[:, h_ * 512:(h_ + 1) * 512], op=ALU.mult)
                    S.I("pool", "tensor_tensor", out=ob, in0=ob, in1=gb, op=ALU.mult)
                    S.I("dve", "tensor_tensor", out=ob, in0=ob, in1=xx, op=ALU.add)
                    S.dma("sp", self.dv(dst[r0:r0 + 128, :], (dkey, r0)), ob)

    def moe(self, idx, li, upd):
        S, nb, w, nc = self.S, self.nb, self.W[li], self.nc
        self.arena_reset()
        P = self.psum
        tiles = []
        for b in range(nb):
            for j in range(SEQ // 128):
                r0 = b * SEQ + j * 128
                tiles.append((self.xs, "xs", r0, b))
        if upd:
            for j in range(nb * CTX // 128):
                tiles.append((self.cs, "cs", j * 128, nb))
        NT = len(tiles)
        ntok = NT * 128
        nslot = ((2 * ntok + NE * (TS - 1)) + TS - 1) // TS * TS
        NST = nslot // TS

        oh1 = self.alloc([128, NT, NE])
        oh2 = self.alloc([128, NT, NE])
        L1 = self.alloc([128, NT])
        L2 = self.alloc([128, NT])
        W1 = self.alloc([128, NT])
        W2 = self.alloc([128, NT])
        run = self.alloc([128, NE])
        sl1 = self.alloc([128, NT], I32)
        sl2 = self.alloc([128, NT], I32)
        widx = self.alloc([128, NST], I32)
        keep = self.aoff

        wr = self.alloc([128, KC, 36])
        S.dma("sp", wr, self.dv(w["wr"].rearrange("(k p) n -> p k n", p=128), "wr%d" % li))
        brb = self.alloc([128, 36])
        self.load_rows_bc(brb, w["br"], "br%d" % li)
        sc2 = [self.alloc([128, D]) for _ in range(2)]
        sh2 = [self.alloc([128, D]) for _ in range(2)]
        nbuf = self.norm_bufs(4)
        h2 = [self.alloc([128, D]) for _ in range(4)]
        h16 = [self.alloc([128, D], BF16) for _ in range(4)]
        h2T = [self.alloc([128, KC, 128]) for _ in range(4)]
        GB = 4
        lg = self.alloc([128, GB, 36])
        gmx, gsum, gw, m1, m2, dm, den = (self.alloc([128, GB]) for _ in range(7))
        ohg = self.alloc([128, GB, 4])
        ex = self.alloc([128, GB, 4])
        t48 = self.alloc([128, GB, 4, 8])
        le, le2, o1, o2 = (self.alloc([128, GB, 8]) for _ in range(4))
        sel = self.alloc([128, GB, NE])
        sel16 = self.alloc([128, GB, NE], BF16)
        pf = self.alloc([128, GB, NE])
        tmp32 = self.alloc([128, GB, NE])
        self._memset(run, 0.0)
        cur_s = None
        for t0 in range(0, NT, GB):
            n = min(GB, NT - t0)
            for j in range(n):
                ti = t0 + j
                src, skey, r0, s = tiles[ti]
                if s != cur_s:
                    cur_s = s
                    cb = s % 2
                    self.load_rows_bc(sc2[cb], self.MOD[idx, 3, s:s + 1, :], "mod%d" % idx)
                    self.load_rows_bc(sh2[cb], self.MOD[idx, 4, s:s + 1, :], "mod%d" % idx, eng="act")
                xt, rs, i = self.rms_tile(nbuf, self.dv(src[r0:r0 + 128, :], (skey, r0)))
                hh, hb, hT_ = h2[ti % 4], h16[ti % 4], h2T[ti % 4]
                S.I("dve", "scalar_tensor_tensor", out=hh, in0=xt, scalar=rs, in1=sc2[cb], op0=ALU.mult, op1=ALU.mult)
                S.I("pool", "tensor_tensor", out=hh, in0=hh, in1=sh2[cb], op=ALU.add)
                S.I("act", "activation", out=hb, in_=hh, func=AF.Identity)
                S.dma("sp", self.dv(self.H2[ti * 128:(ti + 1) * 128, :], ("h2", ti)), hb)
                for k in range(KC):
                    pt = P[1 + (k // 4) % 2]
                    S.I("pe", "transpose", out=pt[:, (k % 4) * 128:(k % 4 + 1) * 128], in_=hh[:, k * 128:(k + 1) * 128], identity=self.identf)
                    if k % 4 == 3:
                        S.I("act", "activation", out=hT_[:, k - 3:k + 1, :], in_=pt.re("p (k t) -> p k t", k=4), func=AF.Identity)
                for k in range(KC):
                    S.I("pe", "matmul", out=P[3][:, j * 36:(j + 1) * 36], lhsT=hT_[:, k, :], rhs=wr[:, k, :], start=(k == 0), stop=(k == KC - 1),
                        skip_group_check=True)
            N_ = slice(0, n)
            S.I("dve", "tensor_tensor", out=lg[:, N_, :], in0=P[3][:, 0:n * 36].re("p (t c) -> p t c", c=36), in1=brb.un(1).bc([128, n, 36]), op=ALU.add)
            lgg = lg[:, N_, 0:4]
            S.I("dve", "reduce_max", out=gmx[:, N_], in_=lgg, axis=AX.X)
            S.I("dve", "tensor_tensor", out=ohg[:, N_, :], in0=lgg, in1=gmx[:, N_].un(2).bc([128, n, 4]), op=ALU.is_equal)
            S.I("dve", "tensor_tensor", out=ex[:, N_, :], in0=lgg, in1=gmx[:, N_].un(2).bc([128, n, 4]), op=ALU.subtract)
            S.I("act", "activation", out=ex[:, N_, :], in_=ex[:, N_, :], func=AF.Exp)
            S.I("dve", "reduce_sum", out=gsum[:, N_], in_=ex[:, N_, :], axis=AX.X)
            S.I("dve", "reciprocal", out=gw[:, N_], in_=gsum[:, N_])
            S.I("dve", "tensor_tensor", out=t48[:, N_], in0=lg[:, N_, 4:36].re("p t (g e) -> p t g e", g=4),
                in1=ohg[:, N_, :].un(3).bc([128, n, 4, 8]), op=ALU.mult)
            S.I("dve", "reduce_sum", out=le[:, N_, :], in_=t48[:, N_].re("p t g e -> p t e g"), axis=AX.X)
            S.I("dve", "reduce_max", out=m1[:, N_], in_=le[:, N_, :], axis=AX.X)
            S.I("dve", "tensor_tensor", out=o1[:, N_, :], in0=le[:, N_, :], in1=m1[:, N_].un(2).bc([128, n, 8]), op=ALU.is_equal)
            S.I("dve", "scalar_tensor_tensor", out=le2[:, N_, :], in0=o1[:, N_, :], scalar=-1e30, in1=le[:, N_, :], op0=ALU.mult, op1=ALU.add)
            S.I("dve", "reduce_max", out=m2[:, N_], in_=le2[:, N_, :], axis=AX.X)
            S.I("dve", "tensor_tensor", out=o2[:, N_, :], in0=le2[:, N_, :], in1=m2[:, N_].un(2).bc([128, n, 8]), op=ALU.is_equal)
            S.I("dve", "tensor_tensor", out=dm[:, N_], in0=m2[:, N_], in1=m1[:, N_], op=ALU.subtract)
            S.I("act", "activation", out=dm[:, N_], in_=dm[:, N_], func=AF.Exp)
            S.I("dve", "tensor_single_scalar", out=den[:, N_], in_=dm[:, N_], scalar=1.0, op=ALU.add)
            S.I("dve", "reciprocal", out=den[:, N_], in_=den[:, N_])
            S.I("dve", "tensor_tensor", out=W1[:, t0:t0 + n], in0=gw[:, N_], in1=den[:, N_], op=ALU.mult)
            S.I("dve", "tensor_tensor", out=W2[:, t0:t0 + n], in0=gw[:, N_], in1=W1[:, t0:t0 + n], op=ALU.subtract)
            o1g = oh1[:, t0:t0 + n, :].re("p t (g e) -> p t g e", g=4)
            o2g = oh2[:, t0:t0 + n, :].re("p t (g e) -> p t g e", g=4)
            S.I("dve", "tensor_tensor", out=o1g, in0=ohg[:, N_, :].un(3).bc([128, n, 4, 8]), in1=o1[:, N_, :].un(2).bc([128, n, 4, 8]), op=ALU.mult)
            S.I("dve", "tensor_tensor", out=o2g, in0=ohg[:, N_, :].un(3).bc([128, n, 4, 8]), in1=o2[:, N_, :].un(2).bc([128, n, 4, 8]), op=ALU.mult)
            S.I("dve", "tensor_tensor", out=sel[:, N_, :], in0=oh1[:, t0:t0 + n, :], in1=oh2[:, t0:t0 + n, :], op=ALU.add)
            S.I("dve", "tensor_copy", out=sel16[:, N_, :], in_=sel[:, N_, :])
            for j in range(n):
                S.I("pe", "matmul", out=P[4][:, j * NE:(j + 1) * NE], lhsT=self.ltri, rhs=sel16[:, j, :], start=True, stop=True, skip_group_check=True)
                S.I("pe", "matmul", out=P[5][:, j * NE:(j + 1) * NE], lhsT=self.ones, rhs=sel16[:, j, :], start=True, stop=True, skip_group_check=True)
            for j in range(n):
                S.I("dve", "tensor_tensor", out=pf[:, j, :], in0=P[4][:, j * NE:(j + 1) * NE], in1=run, op=ALU.add)
                S.I("dve", "tensor_tensor", out=run, in0=P[5][:, j * NE:(j + 1) * NE], in1=run, op=ALU.add)
            S.I("dve", "tensor_tensor", out=tmp32[:, N_, :], in0=pf[:, N_, :], in1=oh1[:, t0:t0 + n, :], op=ALU.mult)
            S.I("dve", "reduce_sum", out=L1[:, t0:t0 + n], in_=tmp32[:, N_, :], axis=AX.X)
            S.I("dve", "tensor_tensor", out=tmp32[:, N_, :], in0=pf[:, N_, :], in1=oh2[:, t0:t0 + n, :], op=ALU.mult)
            S.I("dve", "reduce_sum", out=L2[:, t0:t0 + n], in_=tmp32[:, N_, :], axis=AX.X)

        if 'moeB' in SKIP:
            return
        self.S.barrier()
        self.aoff = keep
        cnti = self.alloc([128, NE], I32)
        pad = self.alloc([128, NE])
        incl = self.alloc([128, NE])
        basee = self.alloc([128, NE])
        onesf = self.alloc([128, NE])
        big3 = self.alloc([128, NT, NE])
        sf = self.alloc([128, NT])
        sgrid = self.alloc([128, NST])
        cmp3 = self.alloc([128, NST, NE])
        ef = self.alloc([128, NST])
        S.I("dve", "tensor_copy", out=cnti, in_=run)
        S.I("dve", "tensor_single_scalar", out=cnti, in_=cnti, scalar=TS - 1, op=ALU.add)
        sh = TS.bit_length() - 1
        S.I("dve", "tensor_scalar", out=cnti, in0=cnti, scalar1=sh, scalar2=sh, op0=ALU.arith_shift_right, op1=ALU.logical_shift_left)
        S.I("dve", "tensor_copy", out=pad, in_=cnti)
        self._memset(onesf, 1.0)
        S.I("dve", "tensor_tensor_scan", out=incl, data0=onesf, data1=pad, initial=0.0, op0=ALU.mult, op1=ALU.add)
        S.I("dve", "tensor_tensor", out=basee, in0=incl, in1=pad, op=ALU.subtract)
        for (oh, L, sl) in ((oh1, L1, sl1), (oh2, L2, sl2)):
            S.I("dve", "tensor_tensor", out=big3, in0=oh, in1=basee.un(1).bc([128, NT, NE]), op=ALU.mult)
            S.I("dve", "reduce_sum", out=sf, in_=big3, axis=AX.X)
            S.I("dve", "tensor_tensor", out=sf, in0=sf, in1=L, op=ALU.add)
            S.I("dve", "tensor_copy", out=sl, in_=sf)
        S.I("pool", "iota", out=sgrid, pattern=[[TS, NST]], base=0, channel_multiplier=0, allow_small_or_imprecise_dtypes=True)
        S.I("dve", "tensor_tensor", out=cmp3, in0=incl.un(1).bc([128, NST, NE]), in1=sgrid.un(2).bc([128, NST, NE]), op=ALU.is_le)
        S.I("dve", "reduce_sum", out=ef, in_=cmp3, axis=AX.X)
        S.I("dve", "tensor_single_scalar", out=ef, in_=ef, scalar=float(NE - 1), op=ALU.min)
        same = self.alloc([128, NST])
        self._memset(same, 0.0)
        S.I("dve", "tensor_tensor", out=same[:, 2:NST], in0=ef[:, 2:NST], in1=ef[:, 0:NST - 2], op=ALU.is_equal)
        S.I("dve", "tensor_single_scalar", out=same, in_=same, scalar=float(1 << 20), op=ALU.mult)
        S.I("dve", "tensor_single_scalar", out=ef, in_=ef, scalar=128.0, op=ALU.mult)
        S.I("dve", "tensor_single_scalar", out=ef, in_=ef, scalar=self.iota_p, op=ALU.add)
        S.I("dve", "tensor_tensor", out=ef, in0=ef, in1=same, op=ALU.add)
        S.I("dve", "tensor_copy", out=widx, in_=ef)
        self.dbg("W1", W1); self.dbg("W2", W2); self.dbg("L1", L1); self.dbg("L2", L2); self.dbg("run", run)
        self.dbg("sl1", sl1, I32); self.dbg("sl2", sl2, I32); self.dbg("widx", widx, I32); self.dbg("oh1", oh1); self.dbg("oh2", oh2)
        self.dbg("incl", incl); self.dbg("basee", basee)
        hl = [self.alloc([128, D], BF16) for _ in range(4)]
        h2s_v = self.dv(self.H2S[0:nslot, :], "h2s")
        for ti in range(NT):
            t = hl[ti % 4]
            S.dma("sp", t, self.dv(self.H2[ti * 128:(ti + 1) * 128, :], ("h2", ti)))
            S.scatter(h2s_v, t, sl1[:, ti:ti + 1])
            S.scatter(h2s_v, t, sl2[:, ti:ti + 1])

        if 'moeC' in SKIP:
            return
        self.S.barrier()
        self.aoff = keep
        w13t = [self.alloc([128, KC * D], BF16) for _ in range(2)]
        w2t = [self.alloc([128, 4 * D], BF16) for _ in range(2)]
        hs = [self.alloc([128, 2, D], BF16) for _ in range(2)]
        hT = [self.alloc([128, KC, TS], BF16) for _ in range(2)]
        sa = [self.alloc([128, TS]) for _ in range(2)]
        u16 = [self.alloc([128, 4, TS], BF16) for _ in range(2)]
        yo = [self.alloc([128, D]) for _ in range(2)]
        w13v = self.dv(self.W13B.rearrange("(r a) n -> r (a n)", a=4), "w13b")
        w2v = self.dv(self.W2B.rearrange("(r a) n -> r (a n)", a=2), "w2b")
        yi = 0
        for s_ in range(NST):
            wa, wb, hsl, hTt, ut = w13t[s_ % 2], w2t[s_ % 2], hs[s_ % 2], hT[s_ % 2], u16[s_ % 2]
            S.gather(wa, w13v, widx[:, s_:s_ + 1], bound=NE * 128 - 1)
            S.gather(wb, w2v, widx[:, s_:s_ + 1], bound=NE * 128 - 1)
            S.dma("sp", hsl, self.dv(self.H2S[s_ * TS:(s_ + 1) * TS, :].rearrange("(a p) d -> p a d", p=128), "h2s"))
            for a in range(2):
                pt = P[a].bitcast(BF16)
                for k in range(KC):
                    S.I("pe", "transpose", out=pt[:, k * 128:(k + 1) * 128], in_=hsl[:, a, k * 128:(k + 1) * 128], identity=self.ident)
                S.I("act" if a == 0 else "dve", "activation" if a == 0 else "tensor_copy", out=hTt[:, :, a * 128:(a + 1) * 128],
                    in_=pt.re("p (k t) -> p k t", k=KC), **({"func": AF.Copy} if a == 0 else {}))
            for m in range(4):
                pa, pg = P[2 + m % 2], P[4 + m % 2]
                for (pp, off) in ((pa, 0), (pg, DE)):
                    for k in range(KC):
                        c0 = k * D + off + m * 128
                        S.I("pe", "matmul", out=pp[:, 0:TS], lhsT=wa[:, c0:c0 + 128], rhs=hTt[:, k, :], start=(k == 0), stop=(k == KC - 1))
                sat = sa[m % 2]
                S.I("act", "activation", out=sat, in_=pa[:, 0:TS], func=AF.Silu)
                S.I("dve", "tensor_tensor", out=ut[:, m, :], in0=pg[:, 0:TS], in1=sat, op=ALU.mult)
            for a in range(2):
                yt = yo[yi % 2]
                yi += 1
                for h_ in range(2):
                    pp = P[6 + h_]
                    for k in range(4):
                        S.I("pe", "matmul", out=pp, lhsT=ut[:, k, a * 128:(a + 1) * 128], rhs=wb[:, k * D + h_ * 512:k * D + (h_ + 1) * 512],
                            start=(k == 0), stop=(k == 3))
                    S.I("act" if h_ == 0 else "dve", "activation" if h_ == 0 else "tensor_copy", out=yt[:, h_ * 512:(h_ + 1) * 512], in_=pp,
                        **({"func": AF.Copy} if h_ == 0 else {}))
                r0 = s_ * TS + a * 128
                S.dma("sp", self.dv(self.YS[r0:r0 + 128, :], "ys"), yt, disjoint=True)

        if 'moeD' in SKIP:
            return
        self.S.barrier()
        self.aoff = keep
        g2 = [self.alloc([128, D]) for _ in range(2)]
        y1 = [self.alloc([128, D]) for _ in range(4)]
        y2 = [self.alloc([128, D]) for _ in range(4)]
        xt2 = [self.alloc([128, D]) for _ in range(4)]
        acc = [self.alloc([128, D]) for _ in range(4)]
        ysv = self.dv(self.YS[0:nslot, :], "ys")
        fuse_final = self.final and (li == self.layers[-1]) and not upd
        if fuse_final:
            ffg = self.alloc([128, D])
            self.load_rows_bc(ffg, self.final_g, "final_g")
            fsq = self.alloc([128, D])
            fss = [self.alloc([128, 1]) for _ in range(4)]
            frs = [self.alloc([128, 1]) for _ in range(4)]
            fob = [self.alloc([128, D]) for _ in range(4)]
            self.final_fused = True
        cur_s = None
        for ti, (src, skey, r0, s) in enumerate(tiles):
            if s != cur_s:
                cur_s = s
                cb = s % 2
                self.load_rows_bc(g2[cb], self.MOD[idx, 5, s:s + 1, :], "mod%d" % idx)
            a1, a2, xx, ac = y1[ti % 4], y2[ti % 4], xt2[ti % 4], acc[ti % 4]
            S.gather(a1, ysv, sl1[:, ti:ti + 1])
            S.gather(a2, ysv, sl2[:, ti:ti + 1])
            S.dma("sp", xx, self.dv(src[r0:r0 + 128, :], (skey, r0)))
            S.I("act", "activation", out=ac, in_=a1, func=AF.Identity, scale=W1[:, ti:ti + 1])
            S.I("dve", "scalar_tensor_tensor", out=ac, in0=a2, scalar=W2[:, ti:ti + 1], in1=ac, op0=ALU.mult, op1=ALU.add)
            S.I("pool", "tensor_tensor", out=ac, in0=ac, in1=g2[cb], op=ALU.mult)
            S.I("dve", "tensor_tensor", out=ac, in0=ac, in1=xx, op=ALU.add)
            if fuse_final:
                ss_, rs_, ob_ = fss[ti % 4], frs[ti % 4], fob[ti % 4]
                S.I("act", "activation", out=fsq, in_=ac, func=AF.Square)
                S.I("dve", "reduce_sum", out=ss_, in_=fsq, axis=AX.X)
                S.I("act", "activation", out=rs_, in_=ss_, func=AF.Sqrt, scale=1.0 / D, bias=self.eps_rms)
                S.I("dve", "reciprocal", out=rs_, in_=rs_)
                S.I("dve", "scalar_tensor_tensor", out=ob_, in0=ac, scalar=rs_, in1=ffg, op0=ALU.mult, op1=ALU.mult)
                S.dma("sp", self.dv(self.y_out[r0:r0 + 128, :], ("y", r0)), ob_)
            else:
                S.dma("sp", self.dv(src[r0:r0 + 128, :], (skey, r0)), ac)

    def final_norm(self):
        S, nb = self.S, self.nb
        self.arena_reset()
        nbuf = self.norm_bufs(4)
        fg = self.alloc([128, D])
        self.load_rows_bc(fg, self.final_g, "final_g")
        ob = [self.alloc([128, D]) for _ in range(4)]
        for ti in range(nb * SEQ // 128):
            r0 = ti * 128
            xt, rs, i = self.rms_tile(nbuf, self.dv(self.xs[r0:r0 + 128, :], ("xs", r0)))
            o = ob[ti % 4]
            S.I("dve", "scalar_tensor_tensor", out=o, in0=xt, scalar=rs, in1=fg, op0=ALU.mult, op1=ALU.mult)
            S.dma("sp", self.dv(self.y_out[r0:r0 + 128, :], ("y", r0)), o)


def prep_weights(inp, layers):
    out = {}
    f = lambda a: np.ascontiguousarray(a, dtype=np.float32)
    out["c_ctx"] = f(inp["c_ctx"]).reshape(1, D)
    out["final_g"] = f(inp["final_g"]).reshape(1, D)
    for li in layers:
        kind, j = li % 3, li // 3
        out["ada_w_%d" % li] = f(inp["ada_w"][li])
        out["ada_b_%d" % li] = f(inp["ada_b"][li]).reshape(1, -1)
        out["norm1_g_%d" % li] = f(inp["norm1_g"][li]).reshape(1, D)
        out["norm2_g_%d" % li] = f(inp["norm2_g"][li]).reshape(1, D)
        out["moe_wr_%d" % li] = f(np.concatenate([inp["moe_wg"][li], inp["moe_we"][li]], axis=1))
        out["moe_br_%d" % li] = f(np.concatenate([inp["moe_bg"][li], inp["moe_be"][li]], axis=0)).reshape(1, 36)
        w13 = np.asarray(inp["moe_w13"][li]).reshape(NE, KC, 128, D).transpose(0, 2, 1, 3).reshape(NE * 128 * 4, 2048)
        out["moe_w13_%d" % li] = f(w13)
        w2 = np.asarray(inp["moe_w2"][li]).reshape(NE, 4, 128, D).transpose(0, 2, 1, 3).reshape(NE * 128 * 2, 2048)
        out["moe_w2_%d" % li] = f(w2)
        if kind == 0:
            out["conf_w_in_%d" % li] = f(inp["conf_w_in"][j])
            out["conf_dw_%d" % li] = f(inp["conf_dw"][j])
            out["conf_dw_b_%d" % li] = f(inp["conf_dw_b"][j]).reshape(1, D)
            out["conf_ln_g_%d" % li] = f(inp["conf_ln_g"][j]).reshape(1, D)
            out["conf_ln_b_%d" % li] = f(inp["conf_ln_b"][j]).reshape(1, D)
            out["conf_w_out_%d" % li] = f(inp["conf_w_out"][j])
        elif kind == 1:
            out["sc_w_in_%d" % li] = f(inp["sc_w_in"][j])
            out["sc_conv_%d" % li] = f(inp["sc_conv"][j])
            out["sc_w_out_%d" % li] = f(inp["sc_w_out"][j])
        else:
            for n in ("a_re", "a_im", "b_re", "b_im", "c_re", "c_im"):
                out["s5_%s_%d" % (n, li)] = f(inp["s5_" + n][j])
            out["s5_log_dt_%d" % li] = f(inp["s5_log_dt"][j])
            out["s5_d_%d" % li] = f(inp["s5_d"][j]).reshape(1, D)
            out["s5_w_glu_%d" % li] = f(inp["s5_w_glu"][j])
    return out


def run(inp, nb, ncores, layers, final=True, trace=False):
    prog = Prog(nb, layers, final)
    nc = prog.build()
    shared = prep_weights(inp, layers)
    x = np.asarray(inp["x"], dtype=np.float32)
    c = np.asarray(inp["c"], dtype=np.float32)
    ctx = np.asarray(inp["ctx"], dtype=np.float32)
    in_maps = []
    for k in range(ncores):
        m = dict(shared)
        m["x"] = np.ascontiguousarray(x[k * nb:(k + 1) * nb]).reshape(nb * SEQ, D)
        m["c"] = np.ascontiguousarray(c[k * nb:(k + 1) * nb])
        m["ctx"] = np.ascontiguousarray(ctx[k * nb:(k + 1) * nb]).reshape(nb * CTX, D)
        in_maps.append({n: m[n] for n in prog.in_names})
    res = run_bass_kernel_spmd(nc, in_maps, core_ids=list(range(ncores)), **({"trace": True} if trace else {}))
    y = np.concatenate([r["y"].reshape(nb, SEQ, D) for r in res.results], axis=0)
    return y, res


def kernel(**inputs):
    y, _ = run(inputs, nb=4, ncores=NCORES, layers=list(range(DEPTH)), final=True)
    return y.astype(np.float32)
```
